# Optimizing a Trainium2 kernel written in Bass

```python
import jax
import jax.numpy as jnp
from jax import lax
import numpy as np


D_MODEL = 1024
BATCH = 8
SEQ = 4096
DEPTH = 2

GRID_W = 64
CTX_LEN = 256
MIX_W = D_MODEL
FOURIER_W = MIX_W // 2
FOURIER_GROUPS = 4
DN_W = MIX_W - FOURIER_W
DN_HEADS = 4
DN_HEAD_DIM = DN_W // DN_HEADS
CONV_K = 5
CHUNK = 64
N_EXPERTS = 32
TOP_K = 4
D_FF_EXPERT = D_MODEL
SWIGLU_LIMIT = 7.0
SWIGLU_ALPHA = 1.702
MOE_BLOCK = 256
IN_W = FOURIER_W + 4 * DN_W + 4 * DN_HEADS
EPS = 1e-6

kernel_name = "hybrid_fourier_gdn_moe_prefix_dit"


def _rms_norm(x, g):
    xf = x.astype(jnp.float32)
    y = xf * lax.rsqrt(jnp.mean(xf * xf, axis=-1, keepdims=True) + EPS) * g.astype(jnp.float32)
    return y.astype(x.dtype)


def _split_mod(m):
    return jnp.split(m[..., None, :], 6, axis=-1)


def _short_conv(u, w):
    ch = u.shape[-1]
    return lax.conv_general_dilated(
        u, w[:, None, :].astype(u.dtype), window_strides=(1,),
        padding=[(CONV_K // 2, CONV_K // 2)],
        dimension_numbers=('NWC', 'WIO', 'NWC'), feature_group_count=ch)


def _l2norm(t):
    return t * lax.rsqrt(jnp.sum(t * t, axis=-1, keepdims=True) + EPS)


def _fourier(f):
    b, l, _ = f.shape
    fg = f.astype(jnp.float32).reshape(b, l, FOURIER_GROUPS, FOURIER_W // FOURIER_GROUPS)
    return jnp.fft.fftn(fg, axes=(1, 3), norm='ortho').real.reshape(b, l, FOURIER_W)


def _dn_inputs(u, conv_w):
    b, l, _ = u.shape
    u = u.astype(jnp.float32)
    qkv = jax.nn.silu(_short_conv(u[..., :3 * DN_W], conv_w.astype(jnp.float32)))
    q, k, v = jnp.split(qkv, 3, axis=-1)
    heads = lambda t: t.reshape(b, l, DN_HEADS, DN_HEAD_DIM).transpose(0, 2, 1, 3)
    q = _l2norm(heads(q)) * (DN_HEAD_DIM ** -0.5)
    k = _l2norm(heads(k))
    v = heads(v)
    z = u[..., 3 * DN_W:4 * DN_W]
    ab = u[..., 4 * DN_W:].reshape(b, l, 2, 2, DN_HEADS)
    return q, k, v, z, ab


def _gated_delta_chunked(q, k, v, g, beta, s0):
    b, h, l, dk = q.shape
    dv = v.shape[-1]
    n = l // CHUNK
    q = q.reshape(b, h, n, CHUNK, dk)
    k = k.reshape(b, h, n, CHUNK, dk)
    v = v.reshape(b, h, n, CHUNK, dv)
    beta = beta.reshape(b, h, n, CHUNK)
    gam = jnp.cumsum(g.reshape(b, h, n, CHUNK), axis=-1)
    idx = jnp.arange(CHUNK)
    incl = idx[:, None] >= idx[None, :]
    strict = idx[:, None] > idx[None, :]
    decay = jnp.exp(jnp.where(incl, gam[..., :, None] - gam[..., None, :], -jnp.inf))
    kb = k * beta[..., None]
    lmat = jnp.where(strict, jnp.einsum('bhnid,bhnjd->bhnij', kb, k) * decay, 0.0)
    eye = jnp.eye(CHUNK, dtype=q.dtype)
    tmat = lax.linalg.triangular_solve(lmat + eye, jnp.broadcast_to(eye, lmat.shape),
                                       left_side=True, lower=True, unit_diagonal=True)
    u_val = tmat @ (v * beta[..., None])
    w_k = tmat @ (kb * jnp.exp(gam)[..., None])
    attn = jnp.einsum('bhnid,bhnjd->bhnij', q, k) * decay
    qg = q * jnp.exp(gam)[..., None]
    kend = k * jnp.exp(gam[..., -1:] - gam)[..., None]
    gend = jnp.exp(gam[..., -1])

    def step(s, xs):
        u_i, w_i, a_i, qg_i, ke_i, ge_i = xs
        v_new = u_i - w_i @ s
        o_i = qg_i @ s + a_i @ v_new
        s = s * ge_i[..., None, None] + jnp.einsum('bhck,bhcv->bhkv', ke_i, v_new)
        return s, o_i

    xs = tuple(jnp.moveaxis(t, 2, 0) for t in (u_val, w_k, attn, qg, kend, gend))
    s_fin, o = lax.scan(step, s0, xs)
    o = jnp.moveaxis(o, 0, 2).reshape(b, h, l, dv)
    return o, s_fin


def _bidir_delta(q, k, v, ab, a_log, dt_bias, s0_fwd, s0_bwd):
    a_log = a_log.astype(jnp.float32)
    dt_bias = dt_bias.astype(jnp.float32)
    outs, states = [], []
    for d, s0 in ((0, s0_fwd), (1, s0_bwd)):
        g = -jnp.exp(a_log[d])[None, :, None] * jax.nn.softplus(
            ab[:, :, d, 0, :].transpose(0, 2, 1) + dt_bias[d][None, :, None])
        beta = jax.nn.sigmoid(ab[:, :, d, 1, :].transpose(0, 2, 1))
        args = (q, k, v, g, beta)
        if d == 1:
            args = tuple(jnp.flip(t, axis=2) for t in args)
        o, s = _gated_delta_chunked(*args, s0)
        if d == 1:
            o = jnp.flip(o, axis=2)
        outs.append(o)
        states.append(s)
    return outs[0] + outs[1], states[0], states[1]


def _gated_out(o, z, g_out):
    b, h, l, dv = o.shape
    o = o.transpose(0, 2, 1, 3)
    o = o * lax.rsqrt(jnp.mean(o * o, axis=-1, keepdims=True) + EPS) * g_out.astype(jnp.float32)
    o = o * jax.nn.silu(z.reshape(b, l, h, dv))
    return o.reshape(b, l, h * dv)


def _moe(h, w_router, b_router, w_gate, b_gate, w_up, b_up, w_down, b_down):
    t, d = h.shape
    logits = (h @ w_router + b_router).astype(jnp.float32)
    top_v, top_i = lax.top_k(logits, TOP_K)
    gates = jax.nn.softmax(top_v, axis=-1)
    tk = t * TOP_K
    flat_e = top_i.reshape(tk)
    flat_tok = jnp.repeat(jnp.arange(t, dtype=jnp.int32), TOP_K)
    flat_g = gates.reshape(tk)
    order = jnp.argsort(flat_e)
    se = flat_e[order]
    counts = jnp.bincount(flat_e, length=N_EXPERTS)
    group_start = jnp.cumsum(counts) - counts
    padded = (counts + MOE_BLOCK - 1) // MOE_BLOCK * MOE_BLOCK
    padded_end = jnp.cumsum(padded)
    padded_start = padded_end - padded
    dest = padded_start[se] + jnp.arange(tk) - group_start[se]
    n_blocks = -(-tk // MOE_BLOCK) + N_EXPERTS
    p = n_blocks * MOE_BLOCK
    slot_tok = jnp.full((p,), t, jnp.int32).at[dest].set(flat_tok[order])
    slot_gate = jnp.zeros((p,), jnp.float32).at[dest].set(flat_g[order])
    block_e = jnp.minimum(
        jnp.searchsorted(padded_end, jnp.arange(n_blocks) * MOE_BLOCK, side='right'), N_EXPERTS - 1)
    h_pad = jnp.concatenate([h, jnp.zeros((1, d), h.dtype)], axis=0)

    def expert_block(args):
        tok, gate, e = args
        xb = h_pad[tok]
        a = jnp.minimum(xb @ w_gate[e] + b_gate[e], SWIGLU_LIMIT)
        u = jnp.clip(xb @ w_up[e] + b_up[e], -SWIGLU_LIMIT, SWIGLU_LIMIT)
        y = (a * jax.nn.sigmoid(SWIGLU_ALPHA * a) * (u + 1.0)) @ w_down[e] + b_down[e]
        return y * gate[:, None].astype(y.dtype)

    y = lax.map(expert_block, (slot_tok.reshape(n_blocks, MOE_BLOCK),
                               slot_gate.reshape(n_blocks, MOE_BLOCK), block_e))
    return jnp.zeros((t + 1, d), h.dtype).at[slot_tok].add(y.reshape(p, d))[:t]


def _layer(x, xc, c, c_ctx, w_mod, b_mod, g_norm1, w_in, conv_w, a_log, dt_bias, g_out_norm,
           w_out, g_norm2, w_router, b_router, w_gate, b_gate, w_up, b_up, w_down, b_down,
           update_ctx):
    dt = x.dtype
    bsz, seq, d = x.shape
    sh1, sc1, gt1, sh2, sc2, gt2 = _split_mod(jax.nn.silu(c) @ w_mod + b_mod)
    sh1c, sc1c, gt1c, sh2c, sc2c, gt2c = _split_mod(jax.nn.silu(c_ctx) @ w_mod + b_mod)

    hx = _rms_norm(x, g_norm1) * (1.0 + sc1) + sh1
    hc = _rms_norm(xc, g_norm1) * (1.0 + sc1c) + sh1c
    ux = hx @ w_in
    uc = hc @ w_in

    qc, kc, vc, zc, abc = _dn_inputs(uc[..., FOURIER_W:], conv_w)
    s_zero = jnp.zeros((xc.shape[0], DN_HEADS, DN_HEAD_DIM, DN_HEAD_DIM), jnp.float32)
    oc, s_ctx_f, s_ctx_b = _bidir_delta(qc, kc, vc, abc, a_log, dt_bias, s_zero, s_zero)

    qx, kx, vx, zx, abx = _dn_inputs(ux[..., FOURIER_W:], conv_w)
    ox, _, _ = _bidir_delta(qx, kx, vx, abx, a_log, dt_bias, s_ctx_f, s_ctx_b)

    mix_x = jnp.concatenate([_fourier(ux[..., :FOURIER_W]), _gated_out(ox, zx, g_out_norm)],
                            axis=-1).astype(dt) @ w_out
    x = x + gt1 * mix_x
    h2x = _rms_norm(x, g_norm2) * (1.0 + sc2) + sh2

    if update_ctx:
        mix_c = jnp.concatenate([_fourier(uc[..., :FOURIER_W]), _gated_out(oc, zc, g_out_norm)],
                                axis=-1).astype(dt) @ w_out
        xc = xc + gt1c * mix_c
        h2c = _rms_norm(xc, g_norm2) * (1.0 + sc2c) + sh2c
        n_lat = bsz * seq
        tokens = jnp.concatenate([h2x.reshape(n_lat, d), h2c.reshape(-1, d)], axis=0)
        y = _moe(tokens, w_router, b_router, w_gate, b_gate, w_up, b_up, w_down, b_down)
        x = x + gt2 * y[:n_lat].reshape(bsz, seq, d)
        xc = xc + gt2c * y[n_lat:].reshape(xc.shape)
    else:
        y = _moe(h2x.reshape(bsz * seq, d), w_router, b_router, w_gate, b_gate, w_up, b_up,
                 w_down, b_down)
        x = x + gt2 * y.reshape(bsz, seq, d)
    return x, xc


def setup_inputs(seed: int = 0) -> dict:
    key = jax.random.key(seed)
    ks = jax.random.split(key, 26)
    d, nl, e, f = D_MODEL, DEPTH, N_EXPERTS, D_FF_EXPERT
    f32 = jnp.float32

    def nrm(k, shape, fan_in, s=1.0):
        return s * jax.random.normal(k, shape, f32) * (fan_in ** -0.5)

    def small(k, shape, s=0.02):
        return s * jax.random.normal(k, shape, f32)

    dt_init = jnp.exp(jax.random.uniform(ks[10], (nl, 2, DN_HEADS), f32,
                                         jnp.log(1e-3), jnp.log(1e-1)))
    return {
        'x': jax.random.normal(ks[0], (BATCH, SEQ, d), f32),
        'c': jax.random.normal(ks[1], (BATCH, d), f32),
        'ctx': jax.random.normal(ks[2], (BATCH, CTX_LEN, d), f32),
        'c_ctx': jax.random.normal(ks[3], (d,), f32),
        'w_mod': nrm(ks[4], (nl, d, 6 * d), d, 0.5),
        'b_mod': small(ks[5], (nl, 6 * d)),
        'g_norm1': 1.0 + small(ks[6], (nl, d)),
        'w_in': nrm(ks[7], (nl, d, IN_W), d),
        'conv_w': nrm(ks[8], (nl, CONV_K, 3 * DN_W), CONV_K),
        'a_log': jnp.log(jax.random.uniform(ks[9], (nl, 2, DN_HEADS), f32, 1.0, 16.0)),
        'dt_bias': dt_init + jnp.log(-jnp.expm1(-dt_init)),
        'g_out_norm': 1.0 + small(ks[11], (nl, DN_HEAD_DIM)),
        'w_out': nrm(ks[12], (nl, MIX_W, d), MIX_W),
        'g_norm2': 1.0 + small(ks[13], (nl, d)),
        'w_router': nrm(ks[14], (nl, d, e), d),
        'b_router': small(ks[15], (nl, e), 0.01),
        'w_gate': nrm(ks[16], (nl, e, d, f), d),
        'b_gate': small(ks[17], (nl, e, f)),
        'w_up': nrm(ks[18], (nl, e, d, f), d),
        'b_up': small(ks[19], (nl, e, f)),
        'w_down': nrm(ks[20], (nl, e, f, d), f),
        'b_down': small(ks[21], (nl, e, d)),
        'g_final': 1.0 + small(ks[22], (d,)),
    }


def reference(x, c, ctx, c_ctx, w_mod, b_mod, g_norm1, w_in, conv_w, a_log, dt_bias,
              g_out_norm, w_out, g_norm2, w_router, b_router, w_gate, b_gate, w_up, b_up,
              w_down, b_down, g_final):
    xc = ctx
    for l in range(DEPTH):
        x, xc = _layer(x, xc, c, c_ctx, w_mod[l], b_mod[l], g_norm1[l], w_in[l], conv_w[l],
                       a_log[l], dt_bias[l], g_out_norm[l], w_out[l], g_norm2[l], w_router[l],
                       b_router[l], w_gate[l], b_gate[l], w_up[l], b_up[l], w_down[l], b_down[l],
                       l < DEPTH - 1)
    return _rms_norm(x, g_final)
```

```python
import numpy as np
import concourse.bass as bass
import concourse.mybir as mybir
from concourse.alu_op_type import AluOpType as ALU
from contextlib import ExitStack
from concourse.bass_utils import run_bass_kernel_spmd

F32 = mybir.dt.float32
BF16 = mybir.dt.bfloat16
I32 = mybir.dt.int32
U32 = mybir.dt.uint32
AF = mybir.ActivationFunctionType
AX = mybir.AxisListType


class Res:
    __slots__ = ("name", "w", "rs", "excl")

    def __init__(self, name="", excl=False):
        self.name = name
        self.w = None
        self.rs = {}
        self.excl = excl


class Op:
    __slots__ = ("eng", "fn", "reads", "writes", "stream", "deps", "sig", "sigidx", "waits")

    def __init__(self, eng, fn, reads, writes, stream):
        self.eng = eng
        self.fn = fn
        self.reads = reads
        self.writes = writes
        self.stream = stream
        self.deps = None
        self.sig = False
        self.sigidx = -1
        self.waits = None


class Sched:
    CH = 16000
    CHD = 1000
    COMPUTE = ("pe", "act", "dve", "pool")

    def __init__(self, nc):
        self.nc = nc
        self.ops = []
        self._dcnt = {}

    def op(self, eng, fn, reads=(), writes=()):
        self.ops.append(Op(eng, fn, tuple(reads), tuple(writes), None))

    NSLOT = {"ld": 12, "scr": 12, "wc": 6, "ld2": 6, "st": 4, "ig": 8, "igs": 4, "cv": 8}

    def dma(self, queue, fn, reads=(), writes=(), stream="ld"):
        n = self._dcnt.get(stream, 0)
        self._dcnt[stream] = n + 1
        self.ops.append(Op(queue, fn, tuple(reads), tuple(writes), f"{stream}#{n % self.NSLOT.get(stream, 8)}"))

    def barrier(self):
        self.ops.append(None)

    def finalize(self, es, final_streams=()):
        nc = self.nc
        raw = self.ops
        ops = []
        bar_after = {}
        lastkey = {}
        pend = None
        for o in raw:
            if o is None:
                pend = dict(lastkey)
                continue
            i = len(ops)
            ops.append(o)
            key = o.stream if o.stream is not None else o.eng
            if pend is not None:
                bar_after[i] = pend
                pend = None
            if not key.startswith("cv#"):
                lastkey[key] = i
        self.ops = ops
        cur_bar = set()
        prev_dma = {}
        for i, o in enumerate(ops):
            if i in bar_after:
                cur_bar = set(bar_after[i].values())
                pend = None
            deps = set(cur_bar)
            if any(r.excl for r in o.reads):
                o.writes = tuple(o.writes) + tuple(r for r in o.reads if r.excl)
                o.reads = tuple(r for r in o.reads if not r.excl)
            for r in o.reads:
                if r.w is not None:
                    deps.add(r.w)
            for w in o.writes:
                if w.w is not None:
                    deps.add(w.w)
                for k, j in w.rs.items():
                    deps.add(j)
            key = o.stream if o.stream is not None else o.eng
            for r in o.reads:
                r.rs[key] = i
            for w in o.writes:
                w.w = i
                w.rs = {}
            if o.stream is not None:
                if o.stream in prev_dma:
                    deps.add(prev_dma[o.stream])
                prev_dma[o.stream] = i
            deps.discard(i)
            o.deps = deps
            for j in deps:
                ops[j].sig = True
        cnt = {}
        for o in ops:
            key = o.stream if o.stream is not None else o.eng
            if o.stream is not None:
                o.sig = True
            if o.sig:
                o.sigidx = cnt.get(key, 0)
                cnt[key] = o.sigidx + 1
        self.cnt = cnt
        known = {e: {} for e in ("pe", "act", "dve", "pool", "sp")}
        clocks = [None] * len(ops)
        for i, o in enumerate(ops):
            kn = known[o.eng]
            waits = {}
            for j in sorted(o.deps):
                p = ops[j]
                pkey = p.stream if p.stream is not None else p.eng
                if p.stream is None and p.eng == o.eng:
                    if o.eng == "pe":
                        continue
                if kn.get(pkey, -1) >= p.sigidx:
                    continue
                if waits.get(pkey, -1) < p.sigidx:
                    waits[pkey] = p.sigidx
                pc = clocks[j]
                for k, v in pc.items():
                    if kn.get(k, -1) < v:
                        kn[k] = v
            for k, v in waits.items():
                if kn.get(k, -1) < v:
                    kn[k] = v
            o.waits = waits
            ck = dict(kn)
            if o.sig:
                key = o.stream if o.stream is not None else o.eng
                ck[key] = max(ck.get(key, -1), o.sigidx)
            clocks[i] = ck
        self.sems = {}
        for key, n in cnt.items():
            ch = self.CH if key in self.COMPUTE else self.CHD
            nch = (n + ch - 1) // ch
            self.sems[key] = [es.enter_context(nc.semaphore(f"s_{key}_{c}")) for c in range(nch)]
        per_eng = {e: [] for e in ("pe", "act", "dve", "pool", "sp")}
        for o in ops:
            per_eng[o.eng].append(o)
        block = es.enter_context(nc.Block())
        sems = self.sems
        CH = self.CH
        CHD = self.CHD

        def wait(eng, k, v):
            if k in self.COMPUTE:
                eng.wait_ge(sems[k][v // CH], v % CH + 1)
            else:
                c = v // CHD
                if c > 0:
                    eng.wait_ge(sems[k][c - 1], CHD * 16)
                eng.wait_ge(sems[k][c], (v % CHD + 1) * 16)

        def emit(eng, lst, finals):
            for o in lst:
                for k, v in o.waits.items():
                    wait(eng, k, v)
                inst = o.fn(eng)
                if o.sig:
                    key = o.stream if o.stream is not None else o.eng
                    if o.stream is not None:
                        inst.then_inc(sems[key][o.sigidx // CHD], 16)
                    else:
                        inst.then_inc(sems[key][o.sigidx // CH], 1)
            for k in cnt:
                if k not in self.COMPUTE and k.split("#")[0] in finals:
                    wait(eng, k, cnt[k] - 1)

        @block.sync
        def _(e):
            emit(e, per_eng["sp"], final_streams)

        @block.tensor
        def _(e):
            emit(e, per_eng["pe"], ())

        @block.scalar
        def _(e):
            emit(e, per_eng["act"], ())

        @block.vector
        def _(e):
            emit(e, per_eng["dve"], ())

        @block.gpsimd
        def _(e):
            emit(e, per_eng["pool"], ())
        return {k: len(v) for k, v in per_eng.items()}
D = 1024
SEQ = 4096
CTX = 256
T = SEQ + CTX
NT = T // 128
NTL = SEQ // 128
INW = 2576
NE = 32
EPS = 1e-6
NEG = -30000.0
NBMAX = (4 * T) // 128 + NE


def host_consts():
    import ml_dtypes
    c = {}
    p = np.arange(128)
    c["ident"] = np.eye(128, dtype=np.float32)
    c["ones"] = np.ones((128, 128), np.float32)
    same = (p[:, None] // 64) == (p[None, :] // 64)
    c["m_ls"] = np.where(same & (p[:, None] > p[None, :]), 0.0, NEG).astype(np.float32)
    c["m_li"] = np.where(same & (p[:, None] >= p[None, :]), 0.0, NEG).astype(np.float32)
    c["m_us"] = np.where(same & (p[:, None] < p[None, :]), 0.0, NEG).astype(np.float32)
    c["m_ui"] = np.where(same & (p[:, None] <= p[None, :]), 0.0, NEG).astype(np.float32)
    c["tri_f"] = (same & (p[:, None] <= p[None, :])).astype(np.float32)
    c["tri_b"] = (same & (p[:, None] >= p[None, :])).astype(np.float32)
    c["blk"] = same.astype(np.float32)
    c["tri_s"] = (p[:, None] < p[None, :]).astype(np.float32)
    tok = (np.arange(NT)[None, :] * 128 + p[:, None]).astype(np.int32)
    c["tokidf"] = tok.view(np.float32)
    c["widxbase"] = (np.arange(8)[None, :] * 128 + p[:, None]).astype(np.float32)
    c["blockval"] = (128.0 * (p[:, None] + 128 * np.arange(2)[None, :])).astype(np.float32)
    c["eidx"] = p[:, None].astype(np.float32)
    pr = np.zeros((128, NBMAX, 2), np.int32); pr[:, :, 0] = T
    c["padrec"] = pr.view(np.float32)
    ang = 2 * np.pi * np.outer(p, p) / 128.0
    c["cs128"] = np.concatenate([np.cos(ang), np.sin(ang)], axis=1).astype(np.float32)
    for nm, L in (("L", SEQ), ("C", CTX)):
        t = np.arange(L, dtype=np.int64)
        a = 2 * np.pi * ((np.outer(t, t) % L).astype(np.float64)) / L
        nrm = 1.0 / np.sqrt(L * 128.0)
        c["cos" + nm] = (np.cos(a) * nrm).astype(ml_dtypes.bfloat16)
        c["nsin" + nm] = (-np.sin(a) * nrm).astype(ml_dtypes.bfloat16)
    return c


CONST_SPECS = [("ident", [128, 128], "f"), ("ones", [128, 128], "f"), ("m_ls", [128, 128], "f"),
               ("m_li", [128, 128], "f"), ("m_us", [128, 128], "f"), ("m_ui", [128, 128], "f"),
               ("tri_f", [128, 128], "f"), ("tri_b", [128, 128], "f"), ("blk", [128, 128], "f"),
               ("cs128", [128, 256], "f"), ("tri_s", [128, 128], "f"), ("tokidf", [128, NT], "f"), ("widxbase", [128, 8], "f"),
               ("blockval", [128, 2], "f"), ("eidx", [128, 1], "f"), ("padrec", [128, NBMAX, 2], "f"), ("cosL", [SEQ, SEQ], "b"), ("nsinL", [SEQ, SEQ], "b"),
               ("cosC", [CTX, CTX], "b"), ("nsinC", [CTX, CTX], "b")]

W_SPECS = [("w_mod", [2, D, 6 * D]), ("b_mod", [2, 6 * D]), ("g_norm1", [2, D]), ("w_in", [2, D, INW]),
           ("conv_w", [2, 5, 1536]), ("a_log", [2, 8]), ("dt_bias", [2, 8]), ("g_out_norm", [2, 128]),
           ("w_out", [2, D, D]), ("g_norm2", [2, D]), ("w_router", [2, D, NE]), ("b_router", [2, NE]),
           ("w_gate", [2, NE, D, D]), ("b_gate", [2, NE, D]), ("w_up", [2, NE, D, D]), ("b_up", [2, NE, D]),
           ("w_down", [2, NE, D, D]), ("b_down", [2, NE, D]), ("g_final", [1, D])]


_UC = [0]


def usb(nc, name, shape, dt):
    _UC[0] += 1
    return nc.sbuf_tensor(f"{name}_{_UC[0]}", shape, dt)


def ups(nc, name, shape, dt):
    _UC[0] += 1
    return nc.psum_tensor(f"{name}_{_UC[0]}", shape, dt)


class Rot:
    def __init__(self, items):
        self.items = items
        self.i = 0

    def next(self):
        it = self.items[self.i % len(self.items)]
        self.i += 1
        return it


def build(stage=99, dbg=()):
    nc = bass.Bass("TRN2", target_bir_lowering=False)
    IN = {}

    def din(name, shape, dt=F32):
        IN[name] = nc.dram_tensor(name, shape, dt, kind="ExternalInput").ap()
        return IN[name]

    xin = din("xin", [T, D])
    cc = din("cc", [2, D])
    for nm, shp in W_SPECS:
        din(nm, shp)
    for nm, shp, k in CONST_SPECS:
        din(nm, shp, F32 if k == "f" else BF16)
    out = nc.dram_tensor("out", [SEQ, D], F32, kind="ExternalOutput").ap()

    def dscr(name, shape, dt=F32):
        kind = "ExternalOutput" if name in dbg else "Internal"
        return nc.dram_tensor(name, shape, dt, kind=kind).ap()

    xres = dscr("xres", [T, D])
    modv = dscr("modv", [2, 2, 6 * D])
    uT = dscr("uT", [INW, T])
    abd = dscr("abd", [T, 16])
    mixT = dscr("mixT", [D, T], BF16)
    h2rows = dscr("h2rows", [T + 1, D], BF16)
    yacc = dscr("yacc", [T + 1, D])
    slotrec = dscr("slotrec", [NBMAX * 128, 2])
    wbf = [[dscr(f"wbf{i}_{m}", [NE * 128, 8 * D], BF16) for m in range(3)] for i in range(2)]
    r_wbf = [Res(), Res()]

    def conv_thunks(l):
        th = []
        for ex in range(NE):
            for m, nm in enumerate(("w_gate", "w_up", "w_down")):
                def fn(l=l, ex=ex, m=m, nm=nm):
                    r0 = ex * 128
                    S.dma("pool", lambda e: e.dma_start(out=wbf[l][m][r0:r0 + 128, :], in_=IN[nm][l, ex].rearrange("(p j) f -> p (j f)", j=8), max_dma_last_dim=4096),
                          writes=[r_wbf[l]], stream="cv")
                th.append(fn)
        return th
    dbg_g = dscr("dbg_g", [T, NE]) if "dbg_g" in dbg else None
    dbg_oT = dscr("dbg_oT", [4, 128, T]) if "dbg_oT" in dbg else None

    es = ExitStack()
    with es:
        S = Sched(nc)
        gsb = lambda n, s, d: es.enter_context(usb(nc, n, s, d))
        ident = gsb("identS", [128, 128], F32); r_const = Res("const")
        ones = gsb("onesS", [128, 128], F32)
        identb = gsb("identb", [128, 128], BF16)
        S.dma("sp", lambda e: e.dma_start(out=ident[:], in_=IN["ident"]), writes=[r_const], stream="ld")
        S.dma("sp", lambda e: e.dma_start(out=ones[:], in_=IN["ones"]), writes=[r_const], stream="ld")
        S.op("dve", lambda e: e.tensor_copy(out=identb[:], in_=ident[:]), reads=[r_const], writes=[r_const])
        gates = gsb("gates", [128, NT, NE], F32); r_gates = [Res() for _ in range(NT)]

        phase0(nc, S, IN, modv, ident, ones, r_const)
        for l in range(2):
            if stage < 1:
                break
            nt_act = NT if l == 0 else NTL
            phase1(nc, S, IN, l, xin if l == 0 else xres, modv, uT, abd, ident, identb, ones, r_const)
            if stage < 2:
                break
            phase2(nc, S, IN, l, uT, mixT, r_const)
            if stage < 3:
                break
            phase3(nc, S, IN, l, uT, abd, mixT, ident, ones, r_const, dbg_oT if l == 0 else None, conv_thunks(l))
            if stage < 4:
                break
            phase4(nc, S, IN, l, xin if l == 0 else xres, xres, modv, mixT, h2rows, gates, r_gates, ident, ones, r_const, nt_act, dbg_g if l == 0 else None)
            if stage < 5:
                break
            phase5(nc, S, IN, l, xres, modv, h2rows, yacc, slotrec, gates, r_gates, ident, ones, r_const, nt_act, out, wbf[l], r_wbf[l])
            if stage < 6:
                break
        if stage < 6:
            with ExitStack() as ph:
                z = ph.enter_context(usb(nc, "zz", [128, D], F32)); rz = Res()
                S.barrier()
                S.op("dve", lambda e: e.memset(z[:], 0.0), writes=[rz])
                S.dma("sp", lambda e: e.dma_start(out=out[0:128, :], in_=z[:]), reads=[rz], stream="st")
        S.barrier()
        fin = ("st", "ld", "wc", "scr", "ig", "igs", "cv")
        stats = S.finalize(es, final_streams=fin)
    return nc, stats
def phase0(nc, S, IN, modv, ident, ones, r_const):
    S.barrier()
    with ExitStack() as ph:
        sb = lambda n, s, d: ph.enter_context(usb(nc, n, s, d))
        ccr = sb("p0_ccr", [2, D], F32); r_ccr = Res()
        scT = sb("p0_scT", [128, 8, 2], F32); r_scT = Res()
        bm = sb("p0_bm", [1, 2, 6 * D], F32); r_bm = Res()
        modsb = sb("p0_mod", [2, 2, 6 * D], F32); r_mod = Res()
        wbufs = Rot([(sb(f"p0_w{i}", [128, 8, 512], F32), Res()) for i in range(2)])
        pT = ph.enter_context(ups(nc, "p0_pT", [128, 8, 2], F32)); r_pT = Res()
        pss = Rot([(ph.enter_context(ups(nc, f"p0_ps{i}", [2, 512], F32)), Res()) for i in range(2)])
        S.dma("sp", lambda e: e.dma_start(out=ccr[:], in_=IN["cc"]), writes=[r_ccr], stream="ld")
        S.dma("sp", lambda e: e.dma_start(out=bm[:], in_=IN["b_mod"].rearrange("(o l) n -> o l n", o=1)), writes=[r_bm], stream="ld")
        S.op("act", lambda e: e.activation(out=ccr[:], in_=ccr[:], func=AF.Silu), reads=[r_ccr], writes=[r_ccr])
        for j in range(8):
            S.op("pe", lambda e, j=j: e.transpose(out=pT[:, j, :], in_=ccr[:, j * 128:(j + 1) * 128], identity=ident[0:2, 0:2]),
                 reads=[r_ccr, r_const], writes=[r_pT])
        S.op("dve", lambda e: e.tensor_copy(out=scT[:], in_=pT[:]), reads=[r_pT], writes=[r_scT])
        for l in range(2):
            wv = IN["w_mod"][l].rearrange("(j p) n -> p j n", p=128)
            for n in range(12):
                wt, r_w = wbufs.next()
                S.dma("sp", lambda e, wt=wt, n=n, wv=wv: e.dma_start(out=wt[:], in_=wv[:, :, n * 512:(n + 1) * 512]), writes=[r_w], stream="ld")
                pst, r_ps = pss.next()
                for j in range(8):
                    S.op("pe", lambda e, j=j, wt=wt, pst=pst: e.matmul(pst[:], lhsT=scT[:, j, :], rhs=wt[:, j, :], start=(j == 0), stop=False),
                         reads=[r_scT, r_w], writes=[r_ps])
                S.op("pe", lambda e, pst=pst, l=l, n=n: e.matmul(pst[:], lhsT=ones[0:1, 0:2], rhs=bm[0:1, l, n * 512:(n + 1) * 512], start=False, stop=True),
                     reads=[r_bm, r_const], writes=[r_ps])
                S.op("dve", lambda e, pst=pst, l=l, n=n: e.tensor_copy(out=modsb[:, l, n * 512:(n + 1) * 512], in_=pst[:]), reads=[r_ps], writes=[r_mod])
        S.dma("sp", lambda e: e.dma_start(out=modv.rearrange("l r n -> r l n"), in_=modsb[:]), reads=[r_mod], stream="scr")


def load_mod_bc(nc, S, ph, modv, l, r, k, name, extra_g=None, plus1=False, stream="ld"):
    t = ph.enter_context(usb(nc, name, [128, D], F32)); res = Res()
    S.dma("sp", lambda e: e.dma_start(out=t[:], in_=modv[l, r:r + 1, k * D:(k + 1) * D].to_broadcast([128, D])), writes=[res], stream=stream)
    if plus1:
        g = ph.enter_context(usb(nc, name + "_g", [128, D], F32)); rg = Res()
        S.dma("sp", lambda e: e.dma_start(out=g[:], in_=extra_g.to_broadcast([128, D])), writes=[rg], stream=stream)
        S.op("dve", lambda e: e.scalar_tensor_tensor(out=t[:], in0=t[:], scalar=1.0, in1=g[:], op0=ALU.add, op1=ALU.mult), reads=[res, rg], writes=[res])
    return t, res


def rstd_ops(S, xt, r_x, junk, r_junk, st, r_st):
    S.op("act", lambda e: e.activation(out=junk[:], in_=xt[:], func=AF.Square, accum_out=st[:, 0:1]), reads=[r_x], writes=[r_junk, r_st])
    S.op("dve", lambda e: e.tensor_scalar(out=st[:, 1:2], in0=st[:, 0:1], scalar1=1.0 / D, scalar2=EPS, op0=ALU.mult, op1=ALU.add), reads=[r_st], writes=[r_st])
    S.op("act", lambda e: e.sqrt(out=st[:, 2:3], in_=st[:, 1:2]), reads=[r_st], writes=[r_st])
    S.op("dve", lambda e: e.reciprocal(out=st[:, 3:4], in_=st[:, 2:3]), reads=[r_st], writes=[r_st])


def phase1(nc, S, IN, l, xsrc, modv, uT, abd, ident, identb, ones, r_const):
    S.barrier()
    with ExitStack() as ph:
        sb = lambda n, s, d: ph.enter_context(usb(nc, n, s, d))
        G1 = [None, None]; SH1 = [None, None]
        for r in range(2):
            G1[r] = load_mod_bc(nc, S, ph, modv, l, r, 1, f"p1_G{r}", extra_g=IN["g_norm1"][l:l + 1, :], plus1=True)
            SH1[r] = load_mod_bc(nc, S, ph, modv, l, r, 0, f"p1_SH{r}")
        winb = sb("p1_winb", [128, 8, INW], BF16); r_win = Res()
        wv = IN["w_in"][l].rearrange("(j p) n -> p j n", p=128)
        for j in range(8):
            for h in range(2):
                S.dma("pool", lambda e, j=j, h=h: e.dma_start(out=winb[:, j, h * 1288:(h + 1) * 1288], in_=wv[:, j, h * 1288:(h + 1) * 1288]),
                      writes=[r_win], stream="wc")
        xts = Rot([(sb(f"p1_x{i}", [128, D], F32), Res()) for i in range(3)])
        junk = sb("p1_junk", [128, D], BF16); r_junk = Res()
        sts = Rot([(sb(f"p1_st{i}", [128, 4], F32), Res()) for i in range(3)])
        t1s = Rot([(sb(f"p1_t1{i}", [128, D], F32), Res()) for i in range(2)])
        hxbs = Rot([(sb(f"p1_hxb{i}", [128, D], BF16), Res()) for i in range(2)])
        hxTs = Rot([(sb(f"p1_hxT{i}", [128, 8, 512], BF16), Res()) for i in range(2)])
        stg = Rot([(sb(f"p1_stg{i}", [128, 512], F32), Res()) for i in range(3)])
        abs_ = Rot([(sb(f"p1_ab{i}", [128, 16], F32), Res()) for i in range(2)])
        ptr = Rot([(ph.enter_context(ups(nc, f"p1_ptr{i}", [128, 8, 128], BF16)), Res()) for i in range(2)])
        pmm = Rot([(ph.enter_context(ups(nc, f"p1_pmm{i}", [128, 512], F32)), Res()) for i in range(4)])
        pab = Rot([(ph.enter_context(ups(nc, f"p1_pab{i}", [128, 16], F32)), Res()) for i in range(2)])
        blocks = [(b * 4, 4) for b in range(8)] + [(32, 2)]
        for (t0, ntl) in blocks:
            hxT, r_hxT = hxTs.next()
            ntok = ntl * 128
            for ti in range(ntl):
                t = t0 + ti
                r = 0 if t < NTL else 1
                xt, r_x = xts.next()
                S.dma("sp", lambda e, xt=xt, t=t: e.dma_start(out=xt[:], in_=xsrc[t * 128:(t + 1) * 128, :]), writes=[r_x], stream="ld")
                st, r_st = sts.next()
                rstd_ops(S, xt, r_x, junk, r_junk, st, r_st)
                t1, r_t1 = t1s.next()
                S.op("dve", lambda e, t1=t1, xt=xt, st=st, r=r: e.scalar_tensor_tensor(out=t1[:], in0=xt[:], scalar=st[:, 3:4], in1=G1[r][0][:], op0=ALU.mult, op1=ALU.mult),
                     reads=[r_x, r_st, G1[r][1]], writes=[r_t1])
                hxb, r_hxb = hxbs.next()
                S.op("pool", lambda e, hxb=hxb, t1=t1, r=r: e.tensor_tensor(out=hxb[:], in0=t1[:], in1=SH1[r][0][:], op=ALU.add),
                     reads=[r_t1, SH1[r][1]], writes=[r_hxb])
                pt, r_pt = ptr.next()
                for j in range(8):
                    S.op("pe", lambda e, pt=pt, hxb=hxb, j=j: e.transpose(out=pt[:, j, :], in_=hxb[:, j * 128:(j + 1) * 128], identity=identb[:]),
                         reads=[r_hxb, r_const], writes=[r_pt])
                S.op("act", lambda e, pt=pt, hxT=hxT, ti=ti: e.copy(out=hxT[:, :, ti * 128:(ti + 1) * 128], in_=pt[:]), reads=[r_pt], writes=[r_hxT])
                pa, r_pa = pab.next()
                for j in range(8):
                    S.op("pe", lambda e, pa=pa, hxT=hxT, ti=ti, j=j: e.matmul(pa[:], lhsT=hxT[:, j, ti * 128:(ti + 1) * 128], rhs=winb[:, j, 2560:2576], start=(j == 0), stop=(j == 7)),
                         reads=[r_hxT, r_win], writes=[r_pa])
                ab, r_ab = abs_.next()
                S.op("dve", lambda e, ab=ab, pa=pa: e.tensor_copy(out=ab[:], in_=pa[:]), reads=[r_pa], writes=[r_ab])
                S.dma("sp", lambda e, ab=ab, t=t: e.dma_start(out=abd[t * 128:(t + 1) * 128, :], in_=ab[:]), reads=[r_ab], stream="scr")
            for c in range(20):
                pm, r_pm = pmm.next()
                for j in range(8):
                    S.op("pe", lambda e, pm=pm, hxT=hxT, c=c, j=j, ntok=ntok: e.matmul(pm[:, 0:ntok], lhsT=winb[:, j, c * 128:(c + 1) * 128], rhs=hxT[:, j, 0:ntok], start=(j == 0), stop=(j == 7)),
                         reads=[r_hxT, r_win], writes=[r_pm])
                sg, r_sg = stg.next()
                eng = "act" if c % 2 == 0 else "dve"
                if eng == "act":
                    S.op("act", lambda e, sg=sg, pm=pm, ntok=ntok: e.copy(out=sg[:, 0:ntok], in_=pm[:, 0:ntok]), reads=[r_pm], writes=[r_sg])
                else:
                    S.op("dve", lambda e, sg=sg, pm=pm, ntok=ntok: e.tensor_copy(out=sg[:, 0:ntok], in_=pm[:, 0:ntok]), reads=[r_pm], writes=[r_sg])
                S.dma("sp", lambda e, sg=sg, c=c, t0=t0, ntok=ntok: e.dma_start(out=uT[c * 128:(c + 1) * 128, t0 * 128:t0 * 128 + ntok], in_=sg[:, 0:ntok]), reads=[r_sg], stream="scr")


def phase2(nc, S, IN, l, uT, mixT, r_const):
    S.barrier()
    with ExitStack() as ph:
        sb = lambda n, s, d: ph.enter_context(usb(nc, n, s, d))
        cs = sb("p2_cs", [128, 256], F32); r_cs = Res()
        S.dma("sp", lambda e: e.dma_start(out=cs[:], in_=IN["cs128"]), writes=[r_cs], stream="ld")
        ntiles = NT if l == 0 else NTL
        FCS = sb("p2_fcs", [128, NT, 4, 256], BF16); r_fcs = [Res() for _ in range(NT)]
        fts = Rot([(sb(f"p2_ft{i}", [128, 4, 128], F32), Res()) for i in range(3)])
        pas = Rot([(ph.enter_context(ups(nc, f"p2_pa{i}", [128, 4, 256], F32)), Res()) for i in range(2)])
        pos = Rot([(ph.enter_context(ups(nc, f"p2_po{i}", [128, 512], F32)), Res()) for i in range(4)])
        for t in range(ntiles):
            ft, r_ft = fts.next()
            S.dma("sp", lambda e, ft=ft, t=t: e.dma_start(out=ft[:], in_=uT[0:512, t * 128:(t + 1) * 128].rearrange("(g p) t -> p g t", p=128)), writes=[r_ft], stream="ld")
            pa, r_pa = pas.next()
            for g in range(4):
                S.op("pe", lambda e, pa=pa, ft=ft, g=g: e.matmul(pa[:, g, :], lhsT=ft[:, g, :], rhs=cs[:], start=True, stop=True), reads=[r_ft, r_cs], writes=[r_pa])
            if t % 2 == 0:
                S.op("act", lambda e, pa=pa, t=t: e.copy(out=FCS[:, t, :, :], in_=pa[:]), reads=[r_pa], writes=[r_fcs[t]])
            else:
                S.op("dve", lambda e, pa=pa, t=t: e.tensor_copy(out=FCS[:, t, :, :], in_=pa[:]), reads=[r_pa], writes=[r_fcs[t]])
        cosb = Rot([(sb(f"p2_cos{i}", [128, NTL, 256], BF16), Res()) for i in range(2)])
        sinb = Rot([(sb(f"p2_sin{i}", [128, NTL, 256], BF16), Res()) for i in range(2)])
        ostg = Rot([(sb(f"p2_os{i}", [128, 256], BF16), Res()) for i in range(3)])
        segs = [(0, NTL, "L")] + ([(NTL, 2, "C")] if l == 0 else [])
        for (t0, ntl, nm) in segs:
            cv = IN["cos" + nm].rearrange("(j p) k -> p j k", p=128)
            sv = IN["nsin" + nm].rearrange("(j p) k -> p j k", p=128)
            for kb in range(ntl * 128 // 256):
                cb, r_cb = cosb.next(); sn, r_sn = sinb.next()
                S.dma("sp", lambda e, cb=cb, kb=kb, cv=cv, ntl=ntl: e.dma_start(out=cb[:, 0:ntl, :], in_=cv[:, :, kb * 256:(kb + 1) * 256]), writes=[r_cb], stream="ld")
                S.dma("act", lambda e, sn=sn, kb=kb, sv=sv, ntl=ntl: e.dma_start(out=sn[:, 0:ntl, :], in_=sv[:, :, kb * 256:(kb + 1) * 256]), writes=[r_sn], stream="ld2")
                for g in range(4):
                    po, r_po = pos.next()
                    for j in range(ntl):
                        S.op("pe", lambda e, po=po, j=j, g=g, cb=cb, t0=t0: e.matmul(po[:, 0:256], lhsT=FCS[:, t0 + j, g, 0:128], rhs=cb[:, j, :], start=(j == 0), stop=False),
                             reads=[r_fcs[t0 + j], r_cb], writes=[r_po])
                        S.op("pe", lambda e, po=po, j=j, g=g, sn=sn, t0=t0, ntl=ntl: e.matmul(po[:, 0:256], lhsT=FCS[:, t0 + j, g, 128:256], rhs=sn[:, j, :], start=False, stop=(j == ntl - 1)),
                             reads=[r_fcs[t0 + j], r_sn], writes=[r_po])
                    og, r_og = ostg.next()
                    if g % 2 == 0:
                        S.op("act", lambda e, og=og, po=po: e.copy(out=og[:], in_=po[:, 0:256]), reads=[r_po], writes=[r_og])
                    else:
                        S.op("dve", lambda e, og=og, po=po: e.tensor_copy(out=og[:], in_=po[:, 0:256]), reads=[r_po], writes=[r_og])
                    S.dma("sp", lambda e, og=og, g=g, t0=t0, kb=kb: e.dma_start(out=mixT[g * 128:(g + 1) * 128, t0 * 128 + kb * 256:t0 * 128 + (kb + 1) * 256], in_=og[:]), reads=[r_og], stream="scr")


WC = T + 4


def tcol(t):
    return t * 128 + (4 if t >= NTL else 0)


def phase3(nc, S, IN, l, uT, abd, mixT, ident, ones, r_const, dbg_oT, bg=()):
    S.barrier()
    with ExitStack() as ph:
        sb = lambda n, s, d: ph.enter_context(usb(nc, n, s, d))
        pst = lambda n, s, d: ph.enter_context(ups(nc, n, s, d))
        r_c3 = Res()
        cm = {}
        for nm in ("m_ls", "m_li", "m_us", "m_ui", "tri_f", "tri_b", "blk"):
            cm[nm] = sb("p3_" + nm, [128, 128], F32)
            S.dma("sp", lambda e, nm=nm: e.dma_start(out=cm[nm][:], in_=IN[nm]), writes=[r_c3], stream="ld")
        cwr = sb("p3_cwr", [5, 1536], F32)
        gor = sb("p3_gor", [1, 128], F32)
        S.dma("sp", lambda e: e.dma_start(out=cwr[:], in_=IN["conv_w"][l]), writes=[r_c3], stream="ld")
        S.dma("sp", lambda e: e.dma_start(out=gor[:], in_=IN["g_out_norm"][l:l + 1, :]), writes=[r_c3], stream="ld")
        alb = sb("p3_alb", [128, 8], F32); dtb = sb("p3_dtb", [128, 8], F32)
        S.dma("sp", lambda e: e.dma_start(out=alb[:], in_=IN["a_log"][l:l + 1, :].to_broadcast([128, 8])), writes=[r_c3], stream="ld")
        S.dma("sp", lambda e: e.dma_start(out=dtb[:], in_=IN["dt_bias"][l:l + 1, :].to_broadcast([128, 8])), writes=[r_c3], stream="ld")
        banks = [pst(f"p3_bank{i}", [128, 512], F32) for i in range(8)]
        qtile = lambda b, q: banks[b][:, q * 128:(q + 1) * 128]
        rbank = [Res(excl=True) for _ in range(8)]
        pcw = banks[0][:, 0:104].rearrange("p (m k) -> p m k", k=8); r_pcw = rbank[0]
        cw = sb("p3_cw", [128, 13, 8], F32)
        for m in range(12):
            S.op("pe", lambda e, m=m: e.transpose(out=pcw[:, m, 0:5], in_=cwr[:, m * 128:(m + 1) * 128], identity=ident[0:5, 0:5]), reads=[r_c3, r_const], writes=[r_pcw])
        S.op("pe", lambda e: e.transpose(out=pcw[:, 12, 0:1], in_=gor[:, :], identity=ident[0:1, 0:1]), reads=[r_c3, r_const], writes=[r_pcw])
        r_cw = Res()
        S.op("dve", lambda e: e.memset(cw[:], 0.0), writes=[r_cw])
        for m in range(12):
            S.op("dve", lambda e, m=m: e.tensor_copy(out=cw[:, m, 0:5], in_=pcw[:, m, 0:5]), reads=[r_pcw], writes=[r_cw])
        S.op("dve", lambda e: e.tensor_copy(out=cw[:, 12, 0:1], in_=pcw[:, 12, 0:1]), reads=[r_pcw], writes=[r_cw])
        nea = sb("p3_nea", [128, 8], F32)
        S.op("act", lambda e: e.activation(out=nea[:], in_=alb[:], func=AF.Exp), reads=[r_c3], writes=[r_c3])
        S.op("dve", lambda e: e.tensor_scalar(out=nea[:], in0=nea[:], scalar1=-1.0, scalar2=None, op0=ALU.mult), reads=[r_c3], writes=[r_c3])
        names = ("BT", "NBT", "GAM", "EG", "BEG", "EK0", "EK1")
        GA = {nm: sb("p3_" + nm, [128, NT, 8], F32) for nm in names}
        r_ga = [Res() for _ in range(NT)]
        abt = Rot([(sb(f"p3_abt{i}", [128, 16], F32), Res()) for i in range(2)])
        tmp = Rot([(sb(f"p3_gt{i}", [128, 4, 8], F32), Res()) for i in range(2)])
        pgs = Rot([(banks[0][:, 128:144], r_pcw)])
        rowm = sb("p3_rowm", [128, 2], F32)
        S.op("dve", lambda e: e.tensor_copy(out=rowm[:, 0:1], in_=cm["blk"][:, 0:1]), reads=[r_c3], writes=[r_c3])
        S.op("dve", lambda e: e.tensor_copy(out=rowm[:, 1:2], in_=cm["blk"][:, 127:128]), reads=[r_c3], writes=[r_c3])
        for t in range(NT):
            ab, r_ab = abt.next()
            S.dma("sp", lambda e, ab=ab, t=t: e.dma_start(out=ab[:], in_=abd[t * 128:(t + 1) * 128, :]), writes=[r_ab], stream="ld")
            tm, r_tm = tmp.next()
            abv = ab[:].rearrange("p (d k h) -> p d k h", d=2, k=2)
            X = tm[:, 0, :].rearrange("p (d h) -> p d h", d=2)
            S.op("dve", lambda e, X=X, abv=abv: e.tensor_tensor(out=X, in0=abv[:, :, 0, :], in1=dtb[:].rearrange("p (d h) -> p d h", d=2), op=ALU.add), reads=[r_ab, r_c3], writes=[r_tm])
            S.op("act", lambda e, tm=tm: e.activation(out=tm[:, 0, :], in_=tm[:, 0, :], func=AF.Exp), reads=[r_tm], writes=[r_tm])
            S.op("act", lambda e, tm=tm: e.activation(out=tm[:, 0, :], in_=tm[:, 0, :], func=AF.Ln, bias=1.0), reads=[r_tm], writes=[r_tm])
            S.op("dve", lambda e, tm=tm: e.tensor_tensor(out=tm[:, 1, :], in0=tm[:, 0, :], in1=nea[:], op=ALU.mult), reads=[r_tm, r_c3], writes=[r_tm])
            B = tm[:, 2, :].rearrange("p (d h) -> p d h", d=2)
            S.op("act", lambda e, B=B, abv=abv: e.activation(out=B, in_=abv[:, :, 1, :], func=AF.Exp, scale=-1.0), reads=[r_ab], writes=[r_tm])
            S.op("dve", lambda e, tm=tm: e.tensor_scalar(out=tm[:, 2, :], in0=tm[:, 2, :], scalar1=1.0, scalar2=None, op0=ALU.add), reads=[r_tm], writes=[r_tm])
            S.op("dve", lambda e, tm=tm, t=t: e.reciprocal(out=GA["BT"][:, t, :], in_=tm[:, 2, :]), reads=[r_tm], writes=[r_ga[t]])
            S.op("dve", lambda e, t=t: e.tensor_scalar(out=GA["NBT"][:, t, :], in0=GA["BT"][:, t, :], scalar1=-1.0, scalar2=None, op0=ALU.mult), reads=[r_ga[t]], writes=[r_ga[t]])
            pg, r_pg = pgs.next()
            S.op("pe", lambda e, pg=pg, tm=tm: e.matmul(pg[:, 0:4], lhsT=cm["tri_f"][:], rhs=tm[:, 1, 0:4], start=True, stop=True), reads=[r_tm, r_c3], writes=[r_pg])
            S.op("pe", lambda e, pg=pg, tm=tm: e.matmul(pg[:, 4:8], lhsT=cm["tri_b"][:], rhs=tm[:, 1, 4:8], start=True, stop=True), reads=[r_tm, r_c3], writes=[r_pg])
            S.op("pe", lambda e, pg=pg, tm=tm: e.matmul(pg[:, 8:16], lhsT=cm["blk"][:], rhs=tm[:, 1, :], start=True, stop=True), reads=[r_tm, r_c3], writes=[r_pg])
            S.op("dve", lambda e, pg=pg, t=t: e.tensor_copy(out=GA["GAM"][:, t, :], in_=pg[:, 0:8]), reads=[r_pg], writes=[r_ga[t]])
            S.op("act", lambda e, pg=pg, t=t: e.activation(out=GA["EG"][:, t, :], in_=pg[:, 0:8], func=AF.Exp), reads=[r_pg], writes=[r_ga[t]])
            S.op("dve", lambda e, t=t: e.tensor_tensor(out=GA["BEG"][:, t, :], in0=GA["EG"][:, t, :], in1=GA["BT"][:, t, :], op=ALU.mult), reads=[r_ga[t]], writes=[r_ga[t]])
            S.op("dve", lambda e, pg=pg, tm=tm, t=t: e.tensor_tensor(out=tm[:, 3, :], in0=pg[:, 8:16], in1=GA["GAM"][:, t, :], op=ALU.subtract), reads=[r_pg, r_ga[t]], writes=[r_tm])
            S.op("act", lambda e, tm=tm: e.activation(out=tm[:, 3, :], in_=tm[:, 3, :], func=AF.Exp), reads=[r_tm], writes=[r_tm])
            S.op("dve", lambda e, tm=tm, t=t: e.tensor_scalar(out=GA["EK0"][:, t, :], in0=tm[:, 3, :], scalar1=rowm[:, 0:1], scalar2=None, op0=ALU.mult), reads=[r_tm, r_c3], writes=[r_ga[t]])
            S.op("dve", lambda e, tm=tm, t=t: e.tensor_scalar(out=GA["EK1"][:, t, :], in0=tm[:, 3, :], scalar1=rowm[:, 1:2], scalar2=None, op0=ALU.mult), reads=[r_tm, r_c3], writes=[r_ga[t]])
        raws = Rot([(sb(f"p3_raw{i}", [128, WC + 4], F32), Res()) for i in range(2)])
        QKV = [(sb(f"p3_qkv{i}", [128, WC], F32), Res()) for i in range(3)]
        oT = sb("p3_oT", [128, WC], F32); r_oT = [Res() for _ in range(NT)]
        r_oTall = Res()
        pn = banks[1]; r_pn = rbank[1]
        rns = Rot([(sb(f"p3_rn{i}", [128, 512], F32), Res()) for i in range(2)])
        zts = Rot([(sb(f"p3_z{i}", [128, 512], F32), Res()) for i in range(2)])
        obs = Rot([(sb(f"p3_ob{i}", [128, 512], BF16), Res()) for i in range(2)])

        KSLOT = 4; DEPTH = 2
        INTER = ("dg", "Dm", "E1", "E2", "N", "NTs", "TT", "Pa", "PTa", "Pb", "PTb", "Rv", "Rw")
        OUTS = ("EGr", "at", "u", "wT", "qg", "ke0", "ke1")
        BF_NAMES = ("Nb", "NTs", "TT", "Pa", "PTa", "Pb", "PTb", "Rv", "Rw")
        BI = [{n: (sb(f"p3_{n}_s{s}", [128, 128], BF16 if n in BF_NAMES else F32), Res()) for n in INTER + ("Nb",)} for s in range(KSLOT)]
        BO = [{n: Rot([(sb(f"p3_{n}_d{d}_{i}", [128, 128], F32), Res()) for i in range(DEPTH)]) for n in OUTS} for d in range(2)]
        VN = [Rot([(sb(f"p3_vn{d}_{i}", [128, 128], F32), Res()) for i in range(2)]) for d in range(2)]
        rbank_ = rbank
        slot_bank = (2, 3, 4, 7)
        PQ = [Rot([(qtile(slot_bank[s], qi), rbank_[slot_bank[s]]) for qi in range(4)]) for s in range(KSLOT)]
        PSC = {d: {n: (qtile(5 + d, qi), rbank_[5 + d]) for qi, n in enumerate(("ps1", "po", "pS"))} for d in range(2)}
        Sst = [(sb(f"p3_S{d}", [128, 128], F32), Res()) for d in range(2)]
        bg = list(bg)
        chunks9 = [(i * 512, 512) for i in range(8)] + [(4100, 256)]

        import os as _os
        LVL = int(_os.environ.get("P3_LEVEL", "9")); NTI = int(_os.environ.get("P3_NT", str(NT)))
        for h in range(4 if LVL >= 9 else (1 if LVL >= 1 else 0)):
            for which in range(3):
                raw, r_raw = raws.next()
                row0 = 512 + which * 512 + h * 128
                S.op("pool", lambda e, raw=raw: e.memset(raw[:], 0.0), writes=[r_raw])
                S.dma("sp", lambda e, raw=raw, row0=row0: e.dma_start(out=raw[:, 2:2 + SEQ], in_=uT[row0:row0 + 128, 0:SEQ]), writes=[r_raw], stream="ld")
                S.dma("sp", lambda e, raw=raw, row0=row0: e.dma_start(out=raw[:, SEQ + 6:SEQ + 6 + CTX], in_=uT[row0:row0 + 128, SEQ:T]), writes=[r_raw], stream="ld")
                dst, r_dst = QKV[which]
                m = which * 4 + h
                S.op("dve", lambda e, dst=dst, raw=raw, m=m: e.tensor_scalar(out=dst[:], in0=raw[:, 0:WC], scalar1=cw[:, m, 0:1], scalar2=None, op0=ALU.mult), reads=[r_raw, r_cw], writes=[r_dst])
                for k in range(1, 5):
                    S.op("dve", lambda e, dst=dst, raw=raw, m=m, k=k: e.scalar_tensor_tensor(out=dst[:], in0=raw[:, k:k + WC], scalar=cw[:, m, k:k + 1], in1=dst[:], op0=ALU.mult, op1=ALU.add),
                         reads=[r_raw, r_cw, r_dst], writes=[r_dst])
                S.op("act", lambda e, dst=dst: e.activation(out=dst[:], in_=dst[:], func=AF.Silu), reads=[r_dst], writes=[r_dst])
                if which < 2:
                    sq, r_sq = raws.items[(raws.i) % 2]
                    S.op("pool", lambda e, sq=sq, dst=dst: e.tensor_tensor(out=sq[:, 0:WC], in0=dst[:], in1=dst[:], op=ALU.mult), reads=[r_dst], writes=[r_sq])
                    for (c0, cn) in chunks9:
                        S.op("pe", lambda e, sq=sq, c0=c0, cn=cn: e.matmul(pn[:, 0:cn], lhsT=ones[:], rhs=sq[:, c0:c0 + cn], start=True, stop=True), reads=[r_sq, r_const], writes=[r_pn])
                        rn, r_rn = rns.next()
                        S.op("dve", lambda e, rn=rn, cn=cn: e.tensor_scalar(out=rn[:, 0:cn], in0=pn[:, 0:cn], scalar1=EPS, scalar2=None, op0=ALU.add), reads=[r_pn], writes=[r_rn])
                        S.op("act", lambda e, rn=rn, cn=cn: e.sqrt(out=rn[:, 0:cn], in_=rn[:, 0:cn]), reads=[r_rn], writes=[r_rn])
                        S.op("dve", lambda e, rn=rn, cn=cn: e.reciprocal(out=rn[:, 0:cn], in_=rn[:, 0:cn]), reads=[r_rn], writes=[r_rn])
                        sc = (128.0 ** -0.5) if which == 0 else 1.0
                        S.op("dve", lambda e, rn=rn, dst=dst, c0=c0, cn=cn, sc=sc: e.scalar_tensor_tensor(out=dst[:, c0:c0 + cn], in0=dst[:, c0:c0 + cn], scalar=sc, in1=rn[:, 0:cn], op0=ALU.mult, op1=ALU.mult),
                             reads=[r_rn, r_dst], writes=[r_dst])
            qT, r_q = QKV[0]; kT, r_k = QKV[1]; vT, r_v = QKV[2]
            for d in range(2):
                S.op("dve", lambda e, d=d: e.memset(Sst[d][0][:], 0.0), writes=[Sst[d][1]])
            seqs = [[32, 33] + list(range(32)), [33, 32] + list(range(31, -1, -1))]
            if LVL < 2:
                seqs = [[], []]
            else:
                seqs = [s_[:NTI] for s_ in seqs]
            written = set()
            PREP = {}
            scanned = [0, 0]

            def prep_gen(t, d, s):
                c0 = tcol(t); col = d * 4 + h
                bi = BI[s]; pq = PQ[s]
                ksl = kT[:, c0:c0 + 128]; qsl = qT[:, c0:c0 + 128]; vsl = vT[:, c0:c0 + 128]
                gam = GA["GAM"][:, t, col:col + 1]
                dg, r_dg = bi["dg"]; Dm, r_Dm = bi["Dm"]; E1, r_E1 = bi["E1"]; E2, r_E2 = bi["E2"]
                EGr, r_EGr = BO[d]["EGr"].next()
                gr, r_gr = pq.next()
                S.op("dve", lambda e: e.tensor_scalar(out=dg[:], in0=ident[:], scalar1=gam, scalar2=None, op0=ALU.mult), reads=[r_const, r_ga[t]], writes=[r_dg])
                S.op("pe", lambda e: e.matmul(gr[:], lhsT=ones[:], rhs=dg[:], start=True, stop=True), reads=[r_dg, r_const], writes=[r_gr])
                S.op("dve", lambda e: e.tensor_scalar(out=Dm[:], in0=gr[:], scalar1=-1.0, scalar2=gam, op0=ALU.mult, op1=ALU.add), reads=[r_gr, r_ga[t]], writes=[r_Dm])
                S.op("act", lambda e: e.activation(out=EGr[:], in_=gr[:], func=AF.Exp), reads=[r_gr], writes=[r_EGr])
                yield
                m1 = cm["m_ls"] if d == 0 else cm["m_us"]
                m2 = cm["m_ui"] if d == 0 else cm["m_li"]
                S.op("pool", lambda e: e.tensor_tensor(out=E1[:], in0=Dm[:], in1=m1[:], op=ALU.add), reads=[r_Dm, r_c3], writes=[r_E1])
                S.op("pool", lambda e: e.tensor_tensor(out=E2[:], in0=m2[:], in1=Dm[:], op=ALU.subtract), reads=[r_Dm, r_c3], writes=[r_E2])
                S.op("act", lambda e: e.activation(out=E1[:], in_=E1[:], func=AF.Exp), reads=[r_E1], writes=[r_E1])
                S.op("act", lambda e: e.activation(out=E2[:], in_=E2[:], func=AF.Exp), reads=[r_E2], writes=[r_E2])
                yield
                N, r_N = bi["N"]; at, r_at = BO[d]["at"].next()
                nbt = GA["NBT"][:, t, col:col + 1]
                kk, r_kk = pq.next()
                S.op("pe", lambda e: e.matmul(kk[:], lhsT=ksl, rhs=ksl, start=True, stop=True), reads=[r_k], writes=[r_kk])
                S.op("dve", lambda e: e.scalar_tensor_tensor(out=N[:], in0=kk[:], scalar=nbt, in1=E1[:], op0=ALU.mult, op1=ALU.mult), reads=[r_kk, r_ga[t], r_E1], writes=[r_N])
                Nb, r_Nb = bi["Nb"]
                S.op("act", lambda e: e.copy(out=Nb[:], in_=N[:]), reads=[r_N], writes=[r_Nb])
                kq, r_kq = pq.next()
                S.op("pe", lambda e: e.matmul(kq[:], lhsT=ksl, rhs=qsl, start=True, stop=True), reads=[r_k, r_q], writes=[r_kq])
                S.op("dve", lambda e: e.tensor_tensor(out=at[:], in0=kq[:], in1=E2[:], op=ALU.mult), reads=[r_kq, r_E2], writes=[r_at])
                yield
                Rv, r_Rv = bi["Rv"]; Rw, r_Rw = bi["Rw"]
                ke0, r_ke0 = BO[d]["ke0"].next(); ke1, r_ke1 = BO[d]["ke1"].next(); qg, r_qg = BO[d]["qg"].next()
                bt = GA["BT"][:, t, col:col + 1]; beg = GA["BEG"][:, t, col:col + 1]
                kt, r_kt = pq.next()
                S.op("pe", lambda e: e.transpose(out=kt[:], in_=ksl, identity=ident[:]), reads=[r_k, r_const], writes=[r_kt])
                S.op("act", lambda e: e.activation(out=Rw[:], in_=kt[:], func=AF.Copy, scale=beg), reads=[r_kt, r_ga[t]], writes=[r_Rw])
                S.op("dve", lambda e: e.tensor_scalar(out=ke0[:], in0=kt[:], scalar1=GA["EK0"][:, t, col:col + 1], scalar2=None, op0=ALU.mult), reads=[r_kt, r_ga[t]], writes=[r_ke0])
                S.op("dve", lambda e: e.tensor_scalar(out=ke1[:], in0=kt[:], scalar1=GA["EK1"][:, t, col:col + 1], scalar2=None, op0=ALU.mult), reads=[r_kt, r_ga[t]], writes=[r_ke1])
                vt, r_vt = pq.next()
                S.op("pe", lambda e: e.transpose(out=vt[:], in_=vsl, identity=ident[:]), reads=[r_v, r_const], writes=[r_vt])
                S.op("act", lambda e: e.activation(out=Rv[:], in_=vt[:], func=AF.Copy, scale=bt), reads=[r_vt, r_ga[t]], writes=[r_Rv])
                S.op("pool", lambda e: e.tensor_tensor(out=qg[:], in0=qsl, in1=EGr[:], op=ALU.mult), reads=[r_q, r_EGr], writes=[r_qg])
                yield
                NTs, r_NTs = bi["NTs"]; TT, r_TT = bi["TT"]
                ntp, r_ntp = pq.next()
                S.op("pe", lambda e: e.transpose(out=ntp[:], in_=N[:], identity=ident[:]), reads=[r_N, r_const], writes=[r_ntp])
                S.op("act", lambda e: e.copy(out=NTs[:], in_=ntp[:]), reads=[r_ntp], writes=[r_NTs])
                S.op("dve", lambda e: e.tensor_tensor(out=TT[:], in0=ntp[:], in1=ident[:], op=ALU.add), reads=[r_ntp, r_const], writes=[r_TT])
                yield
                P_, r_P = Nb, r_Nb
                PT_, r_PT = NTs, r_NTs
                for lev in range(1, 6):
                    p2, r_p2 = pq.next()
                    S.op("pe", lambda e, p2=p2, PT_=PT_, P_=P_: e.matmul(p2[:], lhsT=PT_[:], rhs=P_[:], start=True, stop=True), reads=[r_P, r_PT], writes=[r_p2])
                    nP, r_nP = bi["Pa" if lev % 2 else "Pb"]
                    S.op("act", lambda e, nP=nP, p2=p2: e.copy(out=nP[:], in_=p2[:]), reads=[r_p2], writes=[r_nP])
                    if lev < 5:
                        pt2, r_pt2 = pq.next()
                        S.op("pe", lambda e, pt2=pt2, PT_=PT_, P_=P_: e.matmul(pt2[:], lhsT=P_[:], rhs=PT_[:], start=True, stop=True), reads=[r_P, r_PT], writes=[r_pt2])
                        nPT, r_nPT = bi["PTa" if lev % 2 else "PTb"]
                        S.op("dve", lambda e, nPT=nPT, pt2=pt2: e.tensor_copy(out=nPT[:], in_=pt2[:]), reads=[r_pt2], writes=[r_nPT])
                    yield
                    up, r_up = pq.next()
                    S.op("pe", lambda e, up=up, nP=nP: e.matmul(up[:], lhsT=nP[:], rhs=TT[:], start=True, stop=True), reads=[r_nP, r_TT], writes=[r_up])
                    S.op("dve", lambda e, up=up: e.tensor_tensor(out=TT[:], in0=up[:], in1=TT[:], op=ALU.add), reads=[r_up, r_TT], writes=[r_TT])
                    P_, r_P = nP, r_nP
                    if lev < 5:
                        PT_, r_PT = nPT, r_nPT
                    yield
                u, r_u = BO[d]["u"].next(); wT, r_wT = BO[d]["wT"].next()
                pu, r_pu = pq.next()
                S.op("pe", lambda e: e.matmul(pu[:], lhsT=TT[:], rhs=Rv[:], start=True, stop=True), reads=[r_TT, r_Rv], writes=[r_pu])
                S.op("act", lambda e: e.copy(out=u[:], in_=pu[:]), reads=[r_pu], writes=[r_u])
                pw, r_pw = pq.next()
                S.op("pe", lambda e: e.matmul(pw[:], lhsT=Rw[:], rhs=TT[:], start=True, stop=True), reads=[r_TT, r_Rw], writes=[r_pw])
                S.op("dve", lambda e: e.tensor_copy(out=wT[:], in_=pw[:]), reads=[r_pw], writes=[r_wT])
                PREP[(t, d)] = dict(EGr=(EGr, r_EGr), at=(at, r_at), u=(u, r_u), wT=(wT, r_wT), qg=(qg, r_qg), ke0=(ke0, r_ke0), ke1=(ke1, r_ke1))

            def scan_gen(d):
                Sd, r_S = Sst[d]
                for t in seqs[d]:
                    while (t, d) not in PREP:
                        yield "wait"
                    pr = PREP[(t, d)]
                    c0 = tcol(t)
                    EGr, r_EGr = pr["EGr"]; at, r_at = pr["at"]; u, r_u = pr["u"]; wT, r_wT = pr["wT"]; qg, r_qg = pr["qg"]
                    for c in ((0, 1) if d == 0 else (1, 0)):
                        cs_ = slice(c * 64, (c + 1) * 64)
                        gcol = c * 64 + (63 if d == 0 else 0)
                        ps1, r_ps1 = PSC[d]["ps1"]; po, r_po = PSC[d]["po"]; pS, r_pS = PSC[d]["pS"]
                        vn, r_vn = VN[d].next()
                        ke, r_ke = pr["ke0"] if c == 0 else pr["ke1"]
                        S.op("pe", lambda e, wT=wT: e.matmul(ps1[:], lhsT=wT[:], rhs=Sd[:], start=True, stop=True), reads=[r_wT, r_S], writes=[r_ps1])
                        yield
                        S.op("dve", lambda e, vn=vn, u=u: e.tensor_tensor(out=vn[:], in0=u[:], in1=ps1[:], op=ALU.subtract), reads=[r_u, r_ps1], writes=[r_vn])
                        yield
                        S.op("pe", lambda e, qg=qg, cs_=cs_: e.matmul(po[:, 0:64], lhsT=Sd[:], rhs=qg[:, cs_], start=True, stop=False), reads=[r_S, r_qg], writes=[r_po])
                        S.op("pe", lambda e, vn=vn, at=at, cs_=cs_: e.matmul(po[:, 0:64], lhsT=vn[:], rhs=at[:, cs_], start=False, stop=True), reads=[r_vn, r_at], writes=[r_po])
                        S.op("pe", lambda e, ke=ke, vn=vn: e.matmul(pS[:], lhsT=ke[:], rhs=vn[:], start=True, stop=True), reads=[r_ke, r_vn], writes=[r_pS])
                        yield
                        osl = oT[:, c0 + c * 64:c0 + (c + 1) * 64]
                        if (t, c) not in written:
                            written.add((t, c))
                            S.op("act", lambda e, osl=osl: e.copy(out=osl, in_=po[:, 0:64]), reads=[r_po], writes=[r_oT[t]])
                        else:
                            S.op("dve", lambda e, osl=osl: e.tensor_tensor(out=osl, in0=po[:, 0:64], in1=osl, op=ALU.add), reads=[r_po, r_oT[t]], writes=[r_oT[t]])
                        S.op("dve", lambda e, EGr=EGr, gcol=gcol: e.scalar_tensor_tensor(out=Sd[:], in0=Sd[:], scalar=EGr[:, gcol:gcol + 1], in1=pS[:], op0=ALU.mult, op1=ALU.add),
                             reads=[r_S, r_EGr, r_pS], writes=[r_S])
                        yield
                    scanned[d] += 1

            queue = []
            for i in range(len(seqs[0])):
                for d in range(2):
                    queue.append((seqs[d][i], d, i))
            active = {}
            scans = [scan_gen(0), scan_gen(1)]
            scan_done = [len(seqs[0]) == 0, len(seqs[1]) == 0]
            nbg = 0
            while not all(scan_done):
                progressed = False
                while queue and len(active) < KSLOT and (queue[0][2] - scanned[queue[0][1]] < DEPTH):
                    t_, d_, i_ = queue.pop(0)
                    s_ = [x for x in range(KSLOT) if x not in active][0]
                    active[s_] = prep_gen(t_, d_, s_)
                    progressed = True
                    nbg += 1
                    if bg and nbg % 2 == 0:
                        bg.pop(0)()
                for s_ in list(active.keys()):
                    try:
                        next(active[s_]); progressed = True
                    except StopIteration:
                        del active[s_]; progressed = True
                for d in range(2):
                    if not scan_done[d]:
                        try:
                            r_ = next(scans[d])
                            if r_ != "wait":
                                progressed = True
                        except StopIteration:
                            scan_done[d] = True; progressed = True
                assert progressed, "phase3 scheduler stuck"
            if dbg_oT is not None:
                S.dma("sp", lambda e, h=h: e.dma_start(out=dbg_oT[h, :, 0:SEQ], in_=oT[:, 0:SEQ]), reads=r_oT, stream="scr")
                S.dma("sp", lambda e, h=h: e.dma_start(out=dbg_oT[h, :, SEQ:T], in_=oT[:, SEQ + 4:SEQ + 4 + CTX]), reads=r_oT, stream="scr")
            sq, r_sq = raws.next()
            for ci, (c0, cn) in enumerate(chunks9):
                tiles = list(range(ci * 4, ci * 4 + 4)) if ci < 8 else [32, 33]
                rds = [r_oT[t] for t in tiles]
                S.op("pool", lambda e, sq=sq, c0=c0, cn=cn: e.tensor_tensor(out=sq[:, c0:c0 + cn], in0=oT[:, c0:c0 + cn], in1=oT[:, c0:c0 + cn], op=ALU.mult), reads=rds, writes=[r_sq])
                S.op("pe", lambda e, sq=sq, c0=c0, cn=cn: e.matmul(pn[:, 0:cn], lhsT=ones[:], rhs=sq[:, c0:c0 + cn], start=True, stop=True), reads=[r_sq, r_const], writes=[r_pn])
                rn, r_rn = rns.next()
                S.op("dve", lambda e, rn=rn, cn=cn: e.tensor_scalar(out=rn[:, 0:cn], in0=pn[:, 0:cn], scalar1=1.0 / 128, scalar2=EPS, op0=ALU.mult, op1=ALU.add), reads=[r_pn], writes=[r_rn])
                S.op("act", lambda e, rn=rn, cn=cn: e.sqrt(out=rn[:, 0:cn], in_=rn[:, 0:cn]), reads=[r_rn], writes=[r_rn])
                S.op("dve", lambda e, rn=rn, cn=cn: e.reciprocal(out=rn[:, 0:cn], in_=rn[:, 0:cn]), reads=[r_rn], writes=[r_rn])
                S.op("dve", lambda e, rn=rn, c0=c0, cn=cn: e.scalar_tensor_tensor(out=rn[:, 0:cn], in0=oT[:, c0:c0 + cn], scalar=cw[:, 12, 0:1], in1=rn[:, 0:cn], op0=ALU.mult, op1=ALU.mult),
                     reads=rds + [r_rn, r_cw], writes=[r_rn])
                zt, r_zt = zts.next()
                tok0 = ci * 512 if ci < 8 else SEQ
                zrow = 2048 + h * 128
                S.dma("sp", lambda e, zt=zt, tok0=tok0, cn=cn, zrow=zrow: e.dma_start(out=zt[:, 0:cn], in_=uT[zrow:zrow + 128, tok0:tok0 + cn]), writes=[r_zt], stream="ld")
                S.op("act", lambda e, zt=zt, cn=cn: e.activation(out=zt[:, 0:cn], in_=zt[:, 0:cn], func=AF.Silu), reads=[r_zt], writes=[r_zt])
                ob, r_ob = obs.next()
                S.op("pool", lambda e, ob=ob, rn=rn, zt=zt, cn=cn: e.tensor_tensor(out=ob[:, 0:cn], in0=rn[:, 0:cn], in1=zt[:, 0:cn], op=ALU.mult), reads=[r_rn, r_zt], writes=[r_ob])
                S.dma("sp", lambda e, ob=ob, tok0=tok0, cn=cn, h=h: e.dma_start(out=mixT[512 + h * 128:512 + (h + 1) * 128, tok0:tok0 + cn], in_=ob[:, 0:cn]), reads=[r_ob], stream="scr")
        while bg:
            bg.pop(0)()


def phase4(nc, S, IN, l, xsrc, xres, modv, mixT, h2rows, gates, r_gates, ident, ones, r_const, nt_act, dbg_g):
    S.barrier()
    with ExitStack() as ph:
        sb = lambda n, s, d: ph.enter_context(usb(nc, n, s, d))
        pst = lambda n, s, d: ph.enter_context(ups(nc, n, s, d))
        nr = 2 if nt_act > NTL else 1
        GT1 = [load_mod_bc(nc, S, ph, modv, l, r, 2, f"p4_GT{r}") for r in range(nr)]
        G2 = [load_mod_bc(nc, S, ph, modv, l, r, 4, f"p4_G{r}", extra_g=IN["g_norm2"][l:l + 1, :], plus1=True) for r in range(nr)]
        SH2 = [load_mod_bc(nc, S, ph, modv, l, r, 3, f"p4_SH{r}") for r in range(nr)]
        woutb = sb("p4_wout", [128, 8, D], BF16); r_wo = Res()
        wv = IN["w_out"][l].rearrange("(j p) n -> p j n", p=128)
        for j in range(8):
            S.dma("pool", lambda e, j=j: e.dma_start(out=woutb[:, j, :], in_=wv[:, j, :]), writes=[r_wo], stream="wc")
        wrf = sb("p4_wr", [128, 8, NE], F32); r_wr = Res()
        brr = sb("p4_br", [1, NE], F32)
        S.dma("sp", lambda e: e.dma_start(out=wrf[:], in_=IN["w_router"][l].rearrange("(j p) n -> p j n", p=128)), writes=[r_wr], stream="ld")
        S.dma("sp", lambda e: e.dma_start(out=brr[:], in_=IN["b_router"][l:l + 1, :]), writes=[r_wr], stream="ld")
        mixs = Rot([(sb(f"p4_mx{i}", [128, 8, 128], BF16), Res()) for i in range(2)])
        xts = Rot([(sb(f"p4_x{i}", [128, D], F32), Res()) for i in range(2)])
        tmps = Rot([(sb(f"p4_t{i}", [128, D], F32), Res()) for i in range(2)])
        xns = Rot([(sb(f"p4_xn{i}", [128, D], F32), Res()) for i in range(2)])
        h2s = Rot([(sb(f"p4_h2{i}", [128, D], F32), Res()) for i in range(2)])
        junk = sb("p4_junk", [128, D], BF16); r_junk = Res()
        sts = Rot([(sb(f"p4_st{i}", [128, 4], F32), Res()) for i in range(2)])
        h2bs = Rot([(sb(f"p4_hb{i}", [128, D], BF16), Res()) for i in range(2)])
        h2fs = Rot([(sb(f"p4_hf{i}", [128, 8, 128], F32), Res()) for i in range(2)])
        lgs = Rot([(sb(f"p4_lg{i}", [128, 4, NE], F32), Res()) for i in range(2)])
        t8s = Rot([(sb(f"p4_t8{i}", [128, 16], F32), Res()) for i in range(2)])
        pys = Rot([(pst(f"p4_py{i}", [128, D], F32), Res()) for i in range(2)])
        ptr = Rot([(pst("p4_ptr", [128, 8, 128], F32), Res(excl=True))])
        pls = Rot([(pst(f"p4_pl{i}", [128, NE], F32), Res()) for i in range(2)])
        for t in range(nt_act):
            r = 0 if t < NTL else 1
            mx, r_mx = mixs.next()
            S.dma("sp", lambda e, mx=mx, t=t: e.dma_start(out=mx[:], in_=mixT[:, t * 128:(t + 1) * 128].rearrange("(j p) t -> p j t", p=128)), writes=[r_mx], stream="ld")
            xt, r_x = xts.next()
            S.dma("act", lambda e, xt=xt, t=t: e.dma_start(out=xt[:], in_=xsrc[t * 128:(t + 1) * 128, :]), writes=[r_x], stream="ld2")
            py, r_py = pys.next()
            for half in range(2):
                for j in range(8):
                    S.op("pe", lambda e, py=py, mx=mx, half=half, j=j: e.matmul(py[:, half * 512:(half + 1) * 512], lhsT=mx[:, j, :], rhs=woutb[:, j, half * 512:(half + 1) * 512], start=(j == 0), stop=(j == 7)),
                         reads=[r_mx, r_wo], writes=[r_py])
            tp, r_tp = tmps.next()
            S.op("dve", lambda e, tp=tp, py=py, r=r: e.tensor_tensor(out=tp[:], in0=py[:], in1=GT1[r][0][:], op=ALU.mult), reads=[r_py, GT1[r][1]], writes=[r_tp])
            xn, r_xn = xns.next()
            S.op("pool", lambda e, xn=xn, tp=tp, xt=xt: e.tensor_tensor(out=xn[:], in0=tp[:], in1=xt[:], op=ALU.add), reads=[r_tp, r_x], writes=[r_xn])
            S.dma("sp", lambda e, xn=xn, t=t: e.dma_start(out=xres[t * 128:(t + 1) * 128, :], in_=xn[:]), reads=[r_xn], stream="scr")
            st, r_st = sts.next()
            rstd_ops(S, xn, r_xn, junk, r_junk, st, r_st)
            h2, r_h2 = h2s.next()
            S.op("dve", lambda e, h2=h2, xn=xn, st=st, r=r: e.scalar_tensor_tensor(out=h2[:], in0=xn[:], scalar=st[:, 3:4], in1=G2[r][0][:], op0=ALU.mult, op1=ALU.mult), reads=[r_xn, r_st, G2[r][1]], writes=[r_h2])
            S.op("pool", lambda e, h2=h2, r=r: e.tensor_tensor(out=h2[:], in0=h2[:], in1=SH2[r][0][:], op=ALU.add), reads=[r_h2, SH2[r][1]], writes=[r_h2])
            pt, r_pt = ptr.next()
            for j in range(8):
                S.op("pe", lambda e, pt=pt, h2=h2, j=j: e.transpose(out=pt[:, j, :], in_=h2[:, j * 128:(j + 1) * 128], identity=ident[:]), reads=[r_h2, r_const], writes=[r_pt])
            hb, r_hb = h2bs.next(); hf, r_hf = h2fs.next()
            S.op("act", lambda e, hb=hb, h2=h2: e.copy(out=hb[:], in_=h2[:]), reads=[r_h2], writes=[r_hb])
            S.op("dve", lambda e, hf=hf, pt=pt: e.tensor_copy(out=hf[:], in_=pt[:]), reads=[r_pt], writes=[r_hf])
            S.dma("sp", lambda e, hb=hb, t=t: e.dma_start(out=h2rows[t * 128:(t + 1) * 128, :], in_=hb[:]), reads=[r_hb], stream="scr")
            pl, r_pl = pls.next()
            for j in range(8):
                S.op("pe", lambda e, pl=pl, hf=hf, j=j: e.matmul(pl[:], lhsT=hf[:, j, :], rhs=wrf[:, j, :], start=(j == 0), stop=False), reads=[r_hf, r_wr], writes=[r_pl])
            S.op("pe", lambda e, pl=pl: e.matmul(pl[:], lhsT=ones[0:1, :], rhs=brr[0:1, :], start=False, stop=True), reads=[r_wr, r_const], writes=[r_pl])
            lg, r_lg = lgs.next(); t8, r_t8 = t8s.next()
            S.op("dve", lambda e, lg=lg, pl=pl: e.tensor_copy(out=lg[:, 0, :], in_=pl[:]), reads=[r_pl], writes=[r_lg])
            S.op("dve", lambda e, lg=lg, t8=t8: e.max(out=t8[:, 0:8], in_=lg[:, 0, :]), reads=[r_lg], writes=[r_t8])
            S.op("dve", lambda e, lg=lg, t8=t8: e.tensor_scalar(out=lg[:, 1, :], in0=lg[:, 0, :], scalar1=t8[:, 3:4], scalar2=None, op0=ALU.is_ge), reads=[r_lg, r_t8], writes=[r_lg])
            S.op("dve", lambda e, t8=t8: e.tensor_scalar(out=t8[:, 8:9], in0=t8[:, 0:1], scalar1=-1.0, scalar2=None, op0=ALU.mult), reads=[r_t8], writes=[r_t8])
            S.op("act", lambda e, lg=lg, t8=t8: e.activation(out=lg[:, 2, :], in_=lg[:, 0, :], func=AF.Exp, bias=t8[:, 8:9], scale=1.0), reads=[r_lg, r_t8], writes=[r_lg])
            S.op("dve", lambda e, lg=lg: e.tensor_tensor(out=lg[:, 3, :], in0=lg[:, 2, :], in1=lg[:, 1, :], op=ALU.mult), reads=[r_lg], writes=[r_lg])
            S.op("dve", lambda e, lg=lg, t8=t8: e.reduce_sum(out=t8[:, 9:10], in_=lg[:, 3, :], axis=AX.X), reads=[r_lg], writes=[r_t8])
            S.op("dve", lambda e, t8=t8: e.reciprocal(out=t8[:, 10:11], in_=t8[:, 9:10]), reads=[r_t8], writes=[r_t8])
            S.op("dve", lambda e, lg=lg, t8=t8, t=t: e.tensor_scalar(out=gates[:, t, :], in0=lg[:, 3, :], scalar1=t8[:, 10:11], scalar2=None, op0=ALU.mult), reads=[r_lg, r_t8], writes=[r_gates[t]])
            if dbg_g is not None:
                S.dma("sp", lambda e, t=t: e.dma_start(out=dbg_g[t * 128:(t + 1) * 128, :], in_=gates[:, t, :]), reads=[r_gates[t]], stream="scr")


def phase5(nc, S, IN, l, xres, modv, h2rows, yacc, slotrec, gates, r_gates, ident, ones, r_const, nt_act, out, wbf, r_wbfl):
    S.barrier()
    last = (l == 1)
    NB = (4 * nt_act * 128) // 128 + NE
    with ExitStack() as ph:
        sb = lambda n, s, d: ph.enter_context(usb(nc, n, s, d))
        pst = lambda n, s, d: ph.enter_context(ups(nc, n, s, d))
        nr = 2 if nt_act > NTL else 1
        GT2 = [load_mod_bc(nc, S, ph, modv, l, r, 5, f"p5_GT{r}") for r in range(nr)]
        if last:
            gfb = sb("p5_gf", [128, D], F32); r_gf = Res()
            S.dma("sp", lambda e: e.dma_start(out=gfb[:], in_=IN["g_final"].to_broadcast([128, D])), writes=[r_gf], stream="ld")
        r_k = Res()
        cst = {}
        for nm, shp in (("tri_s", [128, 128]), ("tokidf", [128, NT]), ("widxbase", [128, 8]), ("blockval", [128, 2]), ("eidx", [128, 1])):
            cst[nm] = sb("p5_" + nm, shp, F32)
            S.dma("sp", lambda e, nm=nm: e.dma_start(out=cst[nm][:], in_=IN[nm]), writes=[r_k], stream="ld")
        bnat = sb("p5_bnat", [NE, 3, D], F32); bb = sb("p5_bb", [NE, 3, D], BF16); r_bb = Res()
        for k, nm in enumerate(("b_gate", "b_up", "b_down")):
            S.dma("sp", lambda e, k=k, nm=nm: e.dma_start(out=bnat[:, k, :], in_=IN[nm][l]), writes=[r_bb], stream="ld")
        S.op("dve", lambda e: e.tensor_copy(out=bb[:], in_=bnat[:]), reads=[r_bb], writes=[r_bb])
        zt = sb("p5_zt", [128, D], F32); r_zt = Res(); r_yacc = Res(); r_h2r = Res(); r_slot = Res()
        zb = sb("p5_zb", [1, D], BF16)
        S.op("dve", lambda e: e.memset(zt[:], 0.0), writes=[r_zt])
        S.op("dve", lambda e: e.memset(zb[:], 0.0), writes=[r_zt])
        for t in range(NT):
            S.dma("sp", lambda e, t=t: e.dma_start(out=yacc[t * 128:(t + 1) * 128, :], in_=zt[:]), reads=[r_zt], writes=[r_yacc], stream="scr")
        S.dma("sp", lambda e: e.dma_start(out=yacc[T:T + 1, :], in_=zt[0:1, :]), reads=[r_zt], writes=[r_yacc], stream="scr")
        S.dma("sp", lambda e: e.dma_start(out=h2rows[T:T + 1, :], in_=zb[:]), reads=[r_zt], writes=[r_h2r], stream="scr")
        prt = sb("p5_prt", [128, NBMAX, 2], F32)
        S.dma("sp", lambda e: e.dma_start(out=prt[:], in_=IN["padrec"]), writes=[r_zt], stream="ld")
        S.dma("sp", lambda e: e.dma_start(out=slotrec.rearrange("(p a) b -> p a b", a=NBMAX), in_=prt[:]), reads=[r_zt], writes=[r_slot], stream="scr")
        pm = pst("p5_pm", [128, 512], F32); r_pm = Res(excl=True)
        M = sb("p5_M", [128, NT, NE], F32); r_M = Res()
        POS = sb("p5_POS", [128, NT, NE], F32); r_POS = Res()
        cum = sb("p5_cum", [128, NE], F32); r_cum = Res()
        rg = list(r_gates[:nt_act])
        S.op("dve", lambda e: e.tensor_single_scalar(out=M[:, 0:nt_act, :], in_=gates[:, 0:nt_act, :], scalar=0.0, op=ALU.is_gt), reads=rg, writes=[r_M])
        S.op("dve", lambda e: e.memset(cum[:], 0.0), writes=[r_cum])
        for t in range(nt_act):
            S.op("pe", lambda e, t=t: e.matmul(pm[:, 0:NE], lhsT=cst["tri_s"][:], rhs=M[:, t, :], start=True, stop=False), reads=[r_M, r_k], writes=[r_pm])
            S.op("pe", lambda e, t=t: e.matmul(pm[:, 0:NE], lhsT=ones[:], rhs=cum[:], start=False, stop=True), reads=[r_cum, r_const], writes=[r_pm])
            S.op("act", lambda e, t=t: e.copy(out=POS[:, t, :], in_=pm[:, 0:NE]), reads=[r_pm], writes=[r_POS])
            S.op("dve", lambda e, t=t: e.tensor_tensor(out=cum[:], in0=cum[:], in1=M[:, t, :], op=ALU.add), reads=[r_M, r_cum], writes=[r_cum])
        mt = sb("p5_mt", [128, 8, NE], F32); r_mt = Res()
        mti = sb("p5_mti", [128, 2, NE], I32)
        S.op("pe", lambda e: e.matmul(pm[:, 0:NE], lhsT=ones[:], rhs=cum[:], start=True, stop=True), reads=[r_cum, r_const], writes=[r_pm])
        S.op("dve", lambda e: e.tensor_scalar(out=mti[:, 0, :], in0=pm[:, 0:NE], scalar1=127.0, scalar2=None, op0=ALU.add), reads=[r_pm], writes=[r_mt])
        S.op("dve", lambda e: e.tensor_single_scalar(out=mti[:, 1, :], in_=mti[:, 0, :], scalar=7, op=ALU.arith_shift_right), reads=[r_mt], writes=[r_mt])
        S.op("dve", lambda e: e.tensor_single_scalar(out=mti[:, 0, :], in_=mti[:, 1, :], scalar=7, op=ALU.logical_shift_left), reads=[r_mt], writes=[r_mt])
        S.op("dve", lambda e: e.tensor_copy(out=mt[:, 0, :], in_=mti[:, 0, :]), reads=[r_mt], writes=[r_mt])
        S.op("dve", lambda e: e.memset(mt[:, 7, :], 1.0), writes=[r_mt])
        S.op("dve", lambda e: e.tensor_tensor_scan(out=mt[:, 1, :], data0=mt[:, 7, :], data1=mt[:, 0, :], initial=0.0, op0=ALU.mult, op1=ALU.add), reads=[r_mt], writes=[r_mt])
        S.op("dve", lambda e: e.tensor_tensor(out=mt[:, 2, :], in0=mt[:, 1, :], in1=mt[:, 0, :], op=ALU.subtract), reads=[r_mt], writes=[r_mt])
        S.op("dve", lambda e: e.tensor_single_scalar(out=mt[:, 3, :], in_=mt[:, 0, :], scalar=0.0, op=ALU.is_gt), reads=[r_mt], writes=[r_mt])
        for t in range(nt_act):
            S.op("dve", lambda e, t=t: e.tensor_tensor(out=POS[:, t, :], in0=POS[:, t, :], in1=mt[:, 2, :], op=ALU.add), reads=[r_POS, r_mt], writes=[r_POS])
        recs = sb("p5_recs", [128, NT * 4, 2], F32); r_recs = Res()
        idxf = sb("p5_idxf", [128, NT * 4], F32); idxi = sb("p5_idxi", [128, NT * 4], I32); r_idx = Res()
        v8s = Rot([(sb(f"p5_v8{i}", [128, 8], F32), Res()) for i in range(2)])
        ohs = Rot([(sb(f"p5_oh{i}", [128, NE], F32), Res()) for i in range(2)])
        for t in range(nt_act):
            v8, r_v8 = v8s.next()
            S.op("dve", lambda e, v8=v8, t=t: e.max(out=v8[:], in_=gates[:, t, :]), reads=[r_gates[t]], writes=[r_v8])
            for k in range(4):
                q = t * 4 + k
                oh, r_oh = ohs.next()
                S.op("dve", lambda e, oh=oh, v8=v8, t=t, k=k: e.tensor_scalar(out=oh[:], in0=gates[:, t, :], scalar1=v8[:, k:k + 1], scalar2=None, op0=ALU.is_equal), reads=[r_gates[t], r_v8], writes=[r_oh])
                S.op("dve", lambda e, oh=oh, t=t: e.tensor_tensor(out=oh[:], in0=oh[:], in1=POS[:, t, :], op=ALU.mult), reads=[r_oh, r_POS], writes=[r_oh])
                S.op("dve", lambda e, oh=oh, q=q: e.reduce_sum(out=idxf[:, q:q + 1], in_=oh[:], axis=AX.X), reads=[r_oh], writes=[r_idx])
                S.op("act", lambda e, q=q, t=t: e.copy(out=recs[:, q, 0:1], in_=cst["tokidf"][:, t:t + 1]), reads=[r_k], writes=[r_recs])
                S.op("act", lambda e, q=q, v8=v8, k=k: e.copy(out=recs[:, q, 1:2], in_=v8[:, k:k + 1]), reads=[r_v8], writes=[r_recs])
        S.op("dve", lambda e: e.tensor_copy(out=idxi[:, 0:nt_act * 4], in_=idxf[:, 0:nt_act * 4]), reads=[r_idx], writes=[r_idx])
        for q in range(nt_act * 4):
            S.dma("pool", lambda e, q=q: e.indirect_dma_start(out=slotrec, out_offset=bass.IndirectOffsetOnAxis(ap=idxi[:, q:q + 1], axis=0), in_=recs[:, q, :], in_offset=None),
                  reads=[r_idx, r_recs], writes=[r_slot], stream="igs")
        EO = sb("p5_EO", [128, 256], F32); OH = sb("p5_OH", [NE, 256], F32); r_bm = Res()
        dgt = sb("p5_dgt", [128, 128], F32); r_dgt = Res()
        colv = sb("p5_colv", [128, 8], F32); r_colv = Res()
        cmpt = sb("p5_cmp", [128, NE], F32); r_cmp = Res()
        EBt = sb("p5_EB", [128, 256], F32); CHt = sb("p5_CH", [128, 256], F32)
        for c in range(2):
            bv = cst["blockval"][:, c:c + 1]
            S.op("dve", lambda e, bv=bv: e.tensor_scalar(out=cmpt[:], in0=mt[:, 1, :], scalar1=bv, scalar2=None, op0=ALU.is_le), reads=[r_mt, r_k], writes=[r_cmp])
            S.op("dve", lambda e, c=c: e.reduce_sum(out=colv[:, c:c + 1], in_=cmpt[:], axis=AX.X), reads=[r_cmp], writes=[r_colv])
            S.op("dve", lambda e, c=c: e.tensor_scalar(out=colv[:, c:c + 1], in0=colv[:, c:c + 1], scalar1=float(NE - 1), scalar2=None, op0=ALU.min), reads=[r_colv], writes=[r_colv])
            S.op("dve", lambda e, bv=bv: e.tensor_scalar(out=cmpt[:], in0=mt[:, 2, :], scalar1=bv, scalar2=None, op0=ALU.is_equal), reads=[r_mt, r_k], writes=[r_cmp])
            S.op("dve", lambda e: e.tensor_tensor(out=cmpt[:], in0=cmpt[:], in1=mt[:, 3, :], op=ALU.mult), reads=[r_cmp, r_mt], writes=[r_cmp])
            S.op("dve", lambda e, c=c: e.tensor_reduce(out=colv[:, 2 + c:3 + c], in_=cmpt[:], axis=AX.X, op=ALU.max), reads=[r_cmp], writes=[r_colv])
            for kk, dst in ((c, EBt), (2 + c, CHt)):
                S.op("dve", lambda e, kk=kk: e.tensor_scalar(out=dgt[:], in0=ident[:], scalar1=colv[:, kk:kk + 1], scalar2=None, op0=ALU.mult), reads=[r_colv, r_const], writes=[r_dgt])
                S.op("pe", lambda e: e.matmul(pm[:, 0:128], lhsT=ones[:], rhs=dgt[:], start=True, stop=True), reads=[r_dgt, r_const], writes=[r_pm])
                S.op("act", lambda e, dst=dst, c=c: e.copy(out=dst[:, c * 128:(c + 1) * 128], in_=pm[:, 0:128]), reads=[r_pm], writes=[r_bm])
        S.op("dve", lambda e: e.tensor_scalar(out=CHt[:], in0=CHt[:], scalar1=-1.0e7, scalar2=1.0e7, op0=ALU.mult, op1=ALU.add), reads=[r_bm], writes=[r_bm])
        S.op("dve", lambda e: e.scalar_tensor_tensor(out=EO[:], in0=EBt[:], scalar=128.0, in1=CHt[:], op0=ALU.mult, op1=ALU.add), reads=[r_bm], writes=[r_bm])
        S.op("dve", lambda e: e.tensor_scalar(out=OH[:], in0=EBt[0:NE, :], scalar1=cst["eidx"][0:NE, 0:1], scalar2=None, op0=ALU.is_equal), reads=[r_bm, r_k], writes=[r_bm])
        wg = sb("p5_wg", [128, 8, D], BF16); wu = sb("p5_wu", [128, 8, D], BF16); wd = sb("p5_wd", [128, 8, D], BF16)
        r_wg = Res(); r_wu = Res(); r_wd = Res()
        recb = Rot([(sb(f"p5_rb{i}", [128, 2], F32), Res()) for i in range(3)])
        xgs = Rot([(sb(f"p5_xg{i}", [128, D], BF16), Res()) for i in range(2)])
        xTs = Rot([(sb(f"p5_xT{i}", [128, 8, 128], BF16), Res()) for i in range(2)])
        wix = Rot([(sb(f"p5_wi{i}", [128, 1], I32), Res()) for i in range(2)])
        ohb = Rot([(sb(f"p5_ohb{i}", [NE, 128], BF16), Res()) for i in range(2)])
        a_s = Rot([(sb(f"p5_a{i}", [128, 512], F32), Res()) for i in range(2)])
        sg_s = Rot([(sb(f"p5_sg{i}", [128, 512], F32), Res()) for i in range(2)])
        u_s = Rot([(sb(f"p5_u{i}", [128, 512], F32), Res()) for i in range(2)])
        acts = Rot([(sb(f"p5_act{i}", [128, 8, 128], BF16), Res()) for i in range(2)])
        atms = Rot([(sb(f"p5_atm{i}", [128, D], BF16), Res()) for i in range(2)])
        ygs = Rot([(sb(f"p5_yg{i}", [128, D], F32), Res()) for i in range(2)])
        ptr = Rot([(pst("p5_ptr", [128, 8, 128], BF16), Res(excl=True))])
        pAs = Rot([(pst(f"p5_pA{i}", [128, 512], F32), Res()) for i in range(2)])
        pUs = Rot([(pst(f"p5_pU{i}", [128, 512], F32), Res()) for i in range(2)])
        pYs = Rot([(pst(f"p5_pY{i}", [128, 512], F32), Res()) for i in range(2)])
        identb = sb("p5_idb", [128, 128], BF16)
        S.op("dve", lambda e: e.tensor_copy(out=identb[:], in_=ident[:]), reads=[r_const], writes=[r_k])

        BC = {}

        def bcreg(e):
            if "r" not in BC:
                BC["r"] = e.alloc_register(f"bc{l}")
                e.reg_mov(BC["r"], NE * 128 - 1)
            return BC["r"]

        def stage_in(b):
            rb, r_rb = recb.next()
            S.dma("sp", lambda e, rb=rb, b=b: e.dma_start(out=rb[:], in_=slotrec[b * 128:(b + 1) * 128, :]), reads=[r_slot], writes=[r_rb], stream="ld")
            xg, r_xg = xgs.next()
            S.dma("pool", lambda e, xg=xg, rb=rb: e.indirect_dma_start(out=xg[:], out_offset=None, in_=h2rows, in_offset=bass.IndirectOffsetOnAxis(ap=rb[:, 0:1].bitcast(I32), axis=0)),
                  reads=[r_rb, r_h2r], writes=[r_xg], stream="ig")
            wi, r_wi = wix.next()
            S.op("dve", lambda e, wi=wi, b=b: e.tensor_scalar(out=wi[:], in0=cst["eidx"][:], scalar1=EO[:, b:b + 1], scalar2=None, op0=ALU.add), reads=[r_bm, r_k], writes=[r_wi])
            for m, (wt, r_w) in enumerate(((wg, r_wg), (wu, r_wu), (wd, r_wd))):
                S.dma("pool", lambda e, wi=wi, m=m, wt=wt: e.indirect_dma_start(out=wt[:].rearrange("p j f -> p (j f)"), out_offset=None, in_=wbf[m], in_offset=bass.IndirectOffsetOnAxis(ap=wi[:, 0:1], axis=0),
                                                                             bounds_check=bcreg(e), oob_is_err=False), reads=[r_wi, r_wbfl], writes=[r_w], stream="wc")
            ob, r_ob = ohb.next()
            S.op("act", lambda e, ob=ob, b=b: e.activation(out=ob[:], in_=ones[0:NE, :], func=AF.Copy, scale=OH[0:NE, b:b + 1]), reads=[r_bm, r_const], writes=[r_ob])
            return (rb, r_rb, xg, r_xg, ob, r_ob)

        def compute(b, st):
            rb, r_rb, xg, r_xg, ob, r_ob = st
            pt, r_pt = ptr.next()
            for j in range(8):
                S.op("pe", lambda e, pt=pt, xg=xg, j=j: e.transpose(out=pt[:, j, :], in_=xg[:, j:D:8], identity=identb[:]), reads=[r_xg, r_k], writes=[r_pt])
            xT, r_xT = xTs.next()
            S.op("act", lambda e, xT=xT, pt=pt: e.copy(out=xT[:], in_=pt[:]), reads=[r_pt], writes=[r_xT])
            atm, r_atm = atms.next()
            for hf in range(2):
                pA, r_pA = pAs.next(); pU, r_pU = pUs.next()
                for (pp, r_pp, wt, r_w, bk) in ((pA, r_pA, wg, r_wg, 0), (pU, r_pU, wu, r_wu, 1)):
                    for j in range(8):
                        S.op("pe", lambda e, pp=pp, wt=wt, j=j, xT=xT, hf=hf: e.matmul(pp[:], lhsT=xT[:, j, :], rhs=wt[:, j, hf * 512:(hf + 1) * 512], start=(j == 0), stop=False),
                             reads=[r_w, r_xT], writes=[r_pp])
                    S.op("pe", lambda e, pp=pp, bk=bk, ob=ob, hf=hf: e.matmul(pp[:], lhsT=ob[:], rhs=bb[:, bk, hf * 512:(hf + 1) * 512], start=False, stop=True),
                         reads=[r_bb, r_ob], writes=[r_pp])
                a, r_a = a_s.next(); sg, r_sg = sg_s.next(); u1, r_u1 = u_s.next()
                S.op("dve", lambda e, a=a, pA=pA: e.tensor_scalar(out=a[:], in0=pA[:], scalar1=7.0, scalar2=None, op0=ALU.min), reads=[r_pA], writes=[r_a])
                S.op("act", lambda e, sg=sg, a=a: e.activation(out=sg[:], in_=a[:], func=AF.Sigmoid, scale=1.702), reads=[r_a], writes=[r_sg])
                S.op("dve", lambda e, u1=u1, pU=pU: e.tensor_scalar(out=u1[:], in0=pU[:], scalar1=7.0, scalar2=-7.0, op0=ALU.min, op1=ALU.max), reads=[r_pU], writes=[r_u1])
                S.op("dve", lambda e, sg=sg, a=a: e.tensor_tensor(out=sg[:], in0=sg[:], in1=a[:], op=ALU.mult), reads=[r_a, r_sg], writes=[r_sg])
                S.op("dve", lambda e, atm=atm, sg=sg, u1=u1, hf=hf: e.scalar_tensor_tensor(out=atm[:, hf * 512:(hf + 1) * 512], in0=u1[:], scalar=1.0, in1=sg[:], op0=ALU.add, op1=ALU.mult),
                     reads=[r_sg, r_u1], writes=[r_atm])
            pt2, r_pt2 = ptr.next()
            for j in range(8):
                S.op("pe", lambda e, pt2=pt2, atm=atm, j=j: e.transpose(out=pt2[:, j, :], in_=atm[:, j:D:8], identity=identb[:]), reads=[r_atm, r_k], writes=[r_pt2])
            actT, r_act = acts.next()
            S.op("act", lambda e, actT=actT, pt2=pt2: e.copy(out=actT[:], in_=pt2[:]), reads=[r_pt2], writes=[r_act])
            yg, r_yg = ygs.next()
            for half in range(2):
                pY, r_pY = pYs.next()
                for f in range(8):
                    S.op("pe", lambda e, pY=pY, actT=actT, f=f, half=half: e.matmul(pY[:], lhsT=actT[:, f, :], rhs=wd[:, f, half * 512:(half + 1) * 512], start=(f == 0), stop=False),
                         reads=[r_act, r_wd], writes=[r_pY])
                S.op("pe", lambda e, pY=pY, ob=ob, half=half: e.matmul(pY[:], lhsT=ob[:], rhs=bb[:, 2, half * 512:(half + 1) * 512], start=False, stop=True), reads=[r_ob, r_bb], writes=[r_pY])
                S.op("act", lambda e, yg=yg, pY=pY, rb=rb, half=half: e.activation(out=yg[:, half * 512:(half + 1) * 512], in_=pY[:], func=AF.Copy, scale=rb[:, 1:2]), reads=[r_pY, r_rb], writes=[r_yg])
            return (yg, r_yg, rb, r_rb)

        def stage_out(res):
            yg, r_yg, rb, r_rb = res
            S.dma("pool", lambda e, yg=yg, rb=rb: e.indirect_dma_start(out=yacc, out_offset=bass.IndirectOffsetOnAxis(ap=rb[:, 0:1].bitcast(I32), axis=0), in_=yg[:], in_offset=None, compute_op=ALU.add),
                  reads=[r_yg, r_rb], writes=[r_yacc], stream="igs")

        st = stage_in(0)
        for b in range(NB):
            res = compute(b, st)
            if b + 1 < NB:
                st = stage_in(b + 1)
            stage_out(res)
        xts = Rot([(sb(f"p5_xt{i}", [128, D], F32), Res()) for i in range(2)])
        yts = Rot([(sb(f"p5_yt{i}", [128, D], F32), Res()) for i in range(2)])
        junk = sb("p5_junk", [128, D], BF16); r_junk = Res()
        sts = Rot([(sb(f"p5_st{i}", [128, 4], F32), Res()) for i in range(2)])
        for t in range(nt_act):
            r = 0 if t < NTL else 1
            xt, r_xt = xts.next(); yt, r_yt = yts.next()
            S.dma("sp", lambda e, xt=xt, t=t: e.dma_start(out=xt[:], in_=xres[t * 128:(t + 1) * 128, :]), writes=[r_xt], stream="ld")
            S.dma("act", lambda e, yt=yt, t=t: e.dma_start(out=yt[:], in_=yacc[t * 128:(t + 1) * 128, :]), reads=[r_yacc], writes=[r_yt], stream="ld2")
            S.op("dve", lambda e, yt=yt, r=r: e.tensor_tensor(out=yt[:], in0=yt[:], in1=GT2[r][0][:], op=ALU.mult), reads=[r_yt, GT2[r][1]], writes=[r_yt])
            S.op("pool", lambda e, yt=yt, xt=xt: e.tensor_tensor(out=yt[:], in0=yt[:], in1=xt[:], op=ALU.add), reads=[r_yt, r_xt], writes=[r_yt])
            if not last:
                S.dma("sp", lambda e, yt=yt, t=t: e.dma_start(out=xres[t * 128:(t + 1) * 128, :], in_=yt[:]), reads=[r_yt], stream="scr")
            else:
                st_, r_st = sts.next()
                rstd_ops(S, yt, r_yt, junk, r_junk, st_, r_st)
                S.op("dve", lambda e, yt=yt, st_=st_: e.scalar_tensor_tensor(out=yt[:], in0=yt[:], scalar=st_[:, 3:4], in1=gfb[:], op0=ALU.mult, op1=ALU.mult), reads=[r_yt, r_st, r_gf], writes=[r_yt])
                S.dma("sp", lambda e, yt=yt, t=t: e.dma_start(out=out[t * 128:(t + 1) * 128, :], in_=yt[:]), reads=[r_yt], stream="st")


_CACHE = {}


def make_in_maps(inputs):
    consts = host_consts()
    shared = {}
    for nm, shp in W_SPECS:
        a = np.ascontiguousarray(np.asarray(inputs[nm], dtype=np.float32)).reshape(shp)
        shared[nm] = a
    shared.update(consts)
    x = np.asarray(inputs["x"], dtype=np.float32)
    ctx = np.asarray(inputs["ctx"], dtype=np.float32)
    c = np.asarray(inputs["c"], dtype=np.float32)
    c_ctx = np.asarray(inputs["c_ctx"], dtype=np.float32)
    maps = []
    for b in range(8):
        m = dict(shared)
        m["xin"] = np.ascontiguousarray(np.concatenate([x[b], ctx[b]], axis=0))
        m["cc"] = np.ascontiguousarray(np.stack([c[b], c_ctx], axis=0))
        maps.append(m)
    return maps


def kernel(**inputs):
    if "nc" not in _CACHE:
        _CACHE["nc"] = build()[0]
    nc = _CACHE["nc"]
    maps = make_in_maps(inputs)
    res = run_bass_kernel_spmd(nc, maps, core_ids=list(range(8)))
    return np.stack([np.asarray(r["out"], dtype=np.float32) for r in res.results], axis=0)
```

```python
import numpy as np
import concourse.bass as bass
import concourse.mybir as mybir
from concourse.alu_op_type import AluOpType as ALU
from contextlib import ExitStack
from concourse.bass_utils import run_bass_kernel_spmd

F32 = mybir.dt.float32
BF16 = mybir.dt.bfloat16
I32 = mybir.dt.int32
U32 = mybir.dt.uint32
AF = mybir.ActivationFunctionType
AX = mybir.AxisListType


class Res:
    __slots__ = ("name", "w", "rs", "excl")

    def __init__(self, name="", excl=False):
        self.name = name
        self.w = None
        self.rs = {}
        self.excl = excl


class Op:
    __slots__ = ("eng", "fn", "reads", "writes", "stream", "deps", "sig", "sigidx", "waits")

    def __init__(self, eng, fn, reads, writes, stream):
        self.eng = eng
        self.fn = fn
        self.reads = reads
        self.writes = writes
        self.stream = stream
        self.deps = None
        self.sig = False
        self.sigidx = -1
        self.waits = None


class Sched:
    CH = 16000
    CHD = 1000
    COMPUTE = ("pe", "act", "dve", "pool")

    def __init__(self, nc):
        self.nc = nc
        self.ops = []
        self._dcnt = {}

    def op(self, eng, fn, reads=(), writes=()):
        self.ops.append(Op(eng, fn, tuple(reads), tuple(writes), None))

    NSLOT = {"ld": 12, "scr": 12, "wc": 6, "ld2": 6, "st": 4, "ig": 8, "igs": 4, "cv": 8}

    def dma(self, queue, fn, reads=(), writes=(), stream="ld"):
        n = self._dcnt.get(stream, 0)
        self._dcnt[stream] = n + 1
        self.ops.append(Op(queue, fn, tuple(reads), tuple(writes), f"{stream}#{n % self.NSLOT.get(stream, 8)}"))

    def barrier(self):
        self.ops.append(None)

    def finalize(self, es, final_streams=()):
        nc = self.nc
        raw = self.ops
        ops = []
        bar_after = {}
        lastkey = {}
        pend = None
        for o in raw:
            if o is None:
                pend = dict(lastkey)
                continue
            i = len(ops)
            ops.append(o)
            key = o.stream if o.stream is not None else o.eng
            if pend is not None:
                bar_after[i] = pend
                pend = None
            if not key.startswith("cv#"):
                lastkey[key] = i
        self.ops = ops
        cur_bar = set()
        prev_dma = {}
        for i, o in enumerate(ops):
            if i in bar_after:
                cur_bar = set(bar_after[i].values())
                pend = None
            deps = set(cur_bar)
            if any(r.excl for r in o.reads):
                o.writes = tuple(o.writes) + tuple(r for r in o.reads if r.excl)
                o.reads = tuple(r for r in o.reads if not r.excl)
            for r in o.reads:
                if r.w is not None:
                    deps.add(r.w)
            for w in o.writes:
                if w.w is not None:
                    deps.add(w.w)
                for k, j in w.rs.items():
                    deps.add(j)
            key = o.stream if o.stream is not None else o.eng
            for r in o.reads:
                r.rs[key] = i
            for w in o.writes:
                w.w = i
                w.rs = {}
            if o.stream is not None:
                if o.stream in prev_dma:
                    deps.add(prev_dma[o.stream])
                prev_dma[o.stream] = i
            deps.discard(i)
            o.deps = deps
            for j in deps:
                ops[j].sig = True
        cnt = {}
        for o in ops:
            key = o.stream if o.stream is not None else o.eng
            if o.stream is not None:
                o.sig = True
            if o.sig:
                o.sigidx = cnt.get(key, 0)
                cnt[key] = o.sigidx + 1
        self.cnt = cnt
        known = {e: {} for e in ("pe", "act", "dve", "pool", "sp")}
        clocks = [None] * len(ops)
        for i, o in enumerate(ops):
            kn = known[o.eng]
            waits = {}
            for j in sorted(o.deps):
                p = ops[j]
                pkey = p.stream if p.stream is not None else p.eng
                if p.stream is None and p.eng == o.eng:
                    if o.eng == "pe":
                        continue
                if kn.get(pkey, -1) >= p.sigidx:
                    continue
                if waits.get(pkey, -1) < p.sigidx:
                    waits[pkey] = p.sigidx
                pc = clocks[j]
                for k, v in pc.items():
                    if kn.get(k, -1) < v:
                        kn[k] = v
            for k, v in waits.items():
                if kn.get(k, -1) < v:
                    kn[k] = v
            o.waits = waits
            ck = dict(kn)
            if o.sig:
                key = o.stream if o.stream is not None else o.eng
                ck[key] = max(ck.get(key, -1), o.sigidx)
            clocks[i] = ck
        self.sems = {}
        for key, n in cnt.items():
            ch = self.CH if key in self.COMPUTE else self.CHD
            nch = (n + ch - 1) // ch
            self.sems[key] = [es.enter_context(nc.semaphore(f"s_{key}_{c}")) for c in range(nch)]
        per_eng = {e: [] for e in ("pe", "act", "dve", "pool", "sp")}
        for o in ops:
            per_eng[o.eng].append(o)
        block = es.enter_context(nc.Block())
        sems = self.sems
        CH = self.CH
        CHD = self.CHD

        def wait(eng, k, v):
            if k in self.COMPUTE:
                eng.wait_ge(sems[k][v // CH], v % CH + 1)
            else:
                c = v // CHD
                if c > 0:
                    eng.wait_ge(sems[k][c - 1], CHD * 16)
                eng.wait_ge(sems[k][c], (v % CHD + 1) * 16)

        def emit(eng, lst, finals):
            for o in lst:
                for k, v in o.waits.items():
                    wait(eng, k, v)
                inst = o.fn(eng)
                if o.sig:
                    key = o.stream if o.stream is not None else o.eng
                    if o.stream is not None:
                        inst.then_inc(sems[key][o.sigidx // CHD], 16)
                    else:
                        inst.then_inc(sems[key][o.sigidx // CH], 1)
            for k in cnt:
                if k not in self.COMPUTE and k.split("#")[0] in finals:
                    wait(eng, k, cnt[k] - 1)

        @block.sync
        def _(e):
            emit(e, per_eng["sp"], final_streams)

        @block.tensor
        def _(e):
            emit(e, per_eng["pe"], ())

        @block.scalar
        def _(e):
            emit(e, per_eng["act"], ())

        @block.vector
        def _(e):
            emit(e, per_eng["dve"], ())

        @block.gpsimd
        def _(e):
            emit(e, per_eng["pool"], ())
        return {k: len(v) for k, v in per_eng.items()}
D = 1024
SEQ = 4096
CTX = 256
T = SEQ + CTX
NT = T // 128
NTL = SEQ // 128
INW = 2576
NE = 32
EPS = 1e-6
NEG = -30000.0
NBMAX = (4 * T) // 128 + NE


def host_consts():
    import ml_dtypes
    c = {}
    p = np.arange(128)
    c["ident"] = np.eye(128, dtype=np.float32)
    c["ones"] = np.ones((128, 128), np.float32)
    same = (p[:, None] // 64) == (p[None, :] // 64)
    c["m_ls"] = np.where(same & (p[:, None] > p[None, :]), 0.0, NEG).astype(np.float32)
    c["m_li"] = np.where(same & (p[:, None] >= p[None, :]), 0.0, NEG).astype(np.float32)
    c["m_us"] = np.where(same & (p[:, None] < p[None, :]), 0.0, NEG).astype(np.float32)
    c["m_ui"] = np.where(same & (p[:, None] <= p[None, :]), 0.0, NEG).astype(np.float32)
    c["tri_f"] = (same & (p[:, None] <= p[None, :])).astype(np.float32)
    c["tri_b"] = (same & (p[:, None] >= p[None, :])).astype(np.float32)
    c["blk"] = same.astype(np.float32)
    c["tri_s"] = (p[:, None] < p[None, :]).astype(np.float32)
    tok = (np.arange(NT)[None, :] * 128 + p[:, None]).astype(np.int32)
    c["tokidf"] = tok.view(np.float32)
    c["widxbase"] = (np.arange(8)[None, :] * 128 + p[:, None]).astype(np.float32)
    c["blockval"] = (128.0 * (p[:, None] + 128 * np.arange(2)[None, :])).astype(np.float32)
    c["eidx"] = p[:, None].astype(np.float32)
    pr = np.zeros((128, NBMAX, 2), np.int32); pr[:, :, 0] = T
    c["padrec"] = pr.view(np.float32)
    ang = 2 * np.pi * np.outer(p, p) / 128.0
    c["cs128"] = np.concatenate([np.cos(ang), np.sin(ang)], axis=1).astype(np.float32)
    for nm, L in (("L", SEQ), ("C", CTX)):
        t = np.arange(L, dtype=np.int64)
        a = 2 * np.pi * ((np.outer(t, t) % L).astype(np.float64)) / L
        nrm = 1.0 / np.sqrt(L * 128.0)
        c["cos" + nm] = (np.cos(a) * nrm).astype(ml_dtypes.bfloat16)
        c["nsin" + nm] = (-np.sin(a) * nrm).astype(ml_dtypes.bfloat16)
    return c


CONST_SPECS = [("ident", [128, 128], "f"), ("ones", [128, 128], "f"), ("m_ls", [128, 128], "f"),
               ("m_li", [128, 128], "f"), ("m_us", [128, 128], "f"), ("m_ui", [128, 128], "f"),
               ("tri_f", [128, 128], "f"), ("tri_b", [128, 128], "f"), ("blk", [128, 128], "f"),
               ("cs128", [128, 256], "f"), ("tri_s", [128, 128], "f"), ("tokidf", [128, NT], "f"), ("widxbase", [128, 8], "f"),
               ("blockval", [128, 2], "f"), ("eidx", [128, 1], "f"), ("padrec", [128, NBMAX, 2], "f"), ("cosL", [SEQ, SEQ], "b"), ("nsinL", [SEQ, SEQ], "b"),
               ("cosC", [CTX, CTX], "b"), ("nsinC", [CTX, CTX], "b")]

W_SPECS = [("w_mod", [2, D, 6 * D]), ("b_mod", [2, 6 * D]), ("g_norm1", [2, D]), ("w_in", [2, D, INW]),
           ("conv_w", [2, 5, 1536]), ("a_log", [2, 8]), ("dt_bias", [2, 8]), ("g_out_norm", [2, 128]),
           ("w_out", [2, D, D]), ("g_norm2", [2, D]), ("w_router", [2, D, NE]), ("b_router", [2, NE]),
           ("w_gate", [2, NE, D, D]), ("b_gate", [2, NE, D]), ("w_up", [2, NE, D, D]), ("b_up", [2, NE, D]),
           ("w_down", [2, NE, D, D]), ("b_down", [2, NE, D]), ("g_final", [1, D])]


_UC = [0]


def usb(nc, name, shape, dt):
    _UC[0] += 1
    return nc.sbuf_tensor(f"{name}_{_UC[0]}", shape, dt)


def ups(nc, name, shape, dt):
    _UC[0] += 1
    return nc.psum_tensor(f"{name}_{_UC[0]}", shape, dt)


class Rot:
    def __init__(self, items):
        self.items = items
        self.i = 0

    def next(self):
        it = self.items[self.i % len(self.items)]
        self.i += 1
        return it


def build(stage=99, dbg=()):
    nc = bass.Bass("TRN2", target_bir_lowering=False)
    IN = {}

    def din(name, shape, dt=F32):
        IN[name] = nc.dram_tensor(name, shape, dt, kind="ExternalInput").ap()
        return IN[name]

    xin = din("xin", [T, D])
    cc = din("cc", [2, D])
    for nm, shp in W_SPECS:
        din(nm, shp)
    for nm, shp, k in CONST_SPECS:
        din(nm, shp, F32 if k == "f" else BF16)
    out = nc.dram_tensor("out", [SEQ, D], F32, kind="ExternalOutput").ap()

    def dscr(name, shape, dt=F32):
        kind = "ExternalOutput" if name in dbg else "Internal"
        return nc.dram_tensor(name, shape, dt, kind=kind).ap()

    xres = dscr("xres", [T, D])
    modv = dscr("modv", [2, 2, 6 * D])
    uT = dscr("uT", [INW, T])
    abd = dscr("abd", [T, 16])
    mixT = dscr("mixT", [D, T], BF16)
    h2rows = dscr("h2rows", [T + 1, D], BF16)
    yacc = dscr("yacc", [T + 1, D])
    slotrec = dscr("slotrec", [NBMAX * 128, 2])
    wbf = [[dscr(f"wbf{i}_{m}", [NE * 128, 8 * D], BF16) for m in range(3)] for i in range(2)]
    r_wbf = [Res(), Res()]

    def conv_thunks(l):
        th = []
        for ex in range(NE):
            for m, nm in enumerate(("w_gate", "w_up", "w_down")):
                def fn(l=l, ex=ex, m=m, nm=nm):
                    r0 = ex * 128
                    S.dma("pool", lambda e: e.dma_start(out=wbf[l][m][r0:r0 + 128, :], in_=IN[nm][l, ex].rearrange("(p j) f -> p (j f)", j=8), max_dma_last_dim=4096),
                          writes=[r_wbf[l]], stream="cv")
                th.append(fn)
        return th
    dbg_g = dscr("dbg_g", [T, NE]) if "dbg_g" in dbg else None
    dbg_oT = dscr("dbg_oT", [4, 128, T]) if "dbg_oT" in dbg else None

    es = ExitStack()
    with es:
        S = Sched(nc)
        gsb = lambda n, s, d: es.enter_context(usb(nc, n, s, d))
        ident = gsb("identS", [128, 128], F32); r_const = Res("const")
        ones = gsb("onesS", [128, 128], F32)
        identb = gsb("identb", [128, 128], BF16)
        S.dma("sp", lambda e: e.dma_start(out=ident[:], in_=IN["ident"]), writes=[r_const], stream="ld")
        S.dma("sp", lambda e: e.dma_start(out=ones[:], in_=IN["ones"]), writes=[r_const], stream="ld")
        S.op("dve", lambda e: e.tensor_copy(out=identb[:], in_=ident[:]), reads=[r_const], writes=[r_const])
        gates = gsb("gates", [128, NT, NE], F32); r_gates = [Res() for _ in range(NT)]

        phase0(nc, S, IN, modv, ident, ones, r_const)
        for l in range(2):
            if stage < 1:
                break
            nt_act = NT if l == 0 else NTL
            phase1(nc, S, IN, l, xin if l == 0 else xres, modv, uT, abd, ident, identb, ones, r_const)
            if stage < 2:
                break
            phase2(nc, S, IN, l, uT, mixT, r_const)
            if stage < 3:
                break
            phase3(nc, S, IN, l, uT, abd, mixT, ident, ones, r_const, dbg_oT if l == 0 else None, conv_thunks(l))
            if stage < 4:
                break
            phase4(nc, S, IN, l, xin if l == 0 else xres, xres, modv, mixT, h2rows, gates, r_gates, ident, ones, r_const, nt_act, dbg_g if l == 0 else None)
            if stage < 5:
                break
            phase5(nc, S, IN, l, xres, modv, h2rows, yacc, slotrec, gates, r_gates, ident, ones, r_const, nt_act, out, wbf[l], r_wbf[l])
            if stage < 6:
                break
        if stage < 6:
            with ExitStack() as ph:
                z = ph.enter_context(usb(nc, "zz", [128, D], F32)); rz = Res()
                S.barrier()
                S.op("dve", lambda e: e.memset(z[:], 0.0), writes=[rz])
                S.dma("sp", lambda e: e.dma_start(out=out[0:128, :], in_=z[:]), reads=[rz], stream="st")
        S.barrier()
        fin = ("st", "ld", "wc", "scr", "ig", "igs", "cv")
        stats = S.finalize(es, final_streams=fin)
    return nc, stats
def phase0(nc, S, IN, modv, ident, ones, r_const):
    S.barrier()
    with ExitStack() as ph:
        sb = lambda n, s, d: ph.enter_context(usb(nc, n, s, d))
        ccr = sb("p0_ccr", [2, D], F32); r_ccr = Res()
        scT = sb("p0_scT", [128, 8, 2], F32); r_scT = Res()
        bm = sb("p0_bm", [1, 2, 6 * D], F32); r_bm = Res()
        modsb = sb("p0_mod", [2, 2, 6 * D], F32); r_mod = Res()
        wbufs = Rot([(sb(f"p0_w{i}", [128, 8, 512], F32), Res()) for i in range(2)])
        pT = ph.enter_context(ups(nc, "p0_pT", [128, 8, 2], F32)); r_pT = Res()
        pss = Rot([(ph.enter_context(ups(nc, f"p0_ps{i}", [2, 512], F32)), Res()) for i in range(2)])
        S.dma("sp", lambda e: e.dma_start(out=ccr[:], in_=IN["cc"]), writes=[r_ccr], stream="ld")
        S.dma("sp", lambda e: e.dma_start(out=bm[:], in_=IN["b_mod"].rearrange("(o l) n -> o l n", o=1)), writes=[r_bm], stream="ld")
        S.op("act", lambda e: e.activation(out=ccr[:], in_=ccr[:], func=AF.Silu), reads=[r_ccr], writes=[r_ccr])
        for j in range(8):
            S.op("pe", lambda e, j=j: e.transpose(out=pT[:, j, :], in_=ccr[:, j * 128:(j + 1) * 128], identity=ident[0:2, 0:2]),
                 reads=[r_ccr, r_const], writes=[r_pT])
        S.op("dve", lambda e: e.tensor_copy(out=scT[:], in_=pT[:]), reads=[r_pT], writes=[r_scT])
        for l in range(2):
            wv = IN["w_mod"][l].rearrange("(j p) n -> p j n", p=128)
            for n in range(12):
                wt, r_w = wbufs.next()
                S.dma("sp", lambda e, wt=wt, n=n, wv=wv: e.dma_start(out=wt[:], in_=wv[:, :, n * 512:(n + 1) * 512]), writes=[r_w], stream="ld")
                pst, r_ps = pss.next()
                for j in range(8):
                    S.op("pe", lambda e, j=j, wt=wt, pst=pst: e.matmul(pst[:], lhsT=scT[:, j, :], rhs=wt[:, j, :], start=(j == 0), stop=False),
                         reads=[r_scT, r_w], writes=[r_ps])
                S.op("pe", lambda e, pst=pst, l=l, n=n: e.matmul(pst[:], lhsT=ones[0:1, 0:2], rhs=bm[0:1, l, n * 512:(n + 1) * 512], start=False, stop=True),
                     reads=[r_bm, r_const], writes=[r_ps])
                S.op("dve", lambda e, pst=pst, l=l, n=n: e.tensor_copy(out=modsb[:, l, n * 512:(n + 1) * 512], in_=pst[:]), reads=[r_ps], writes=[r_mod])
        S.dma("sp", lambda e: e.dma_start(out=modv.rearrange("l r n -> r l n"), in_=modsb[:]), reads=[r_mod], stream="scr")


def load_mod_bc(nc, S, ph, modv, l, r, k, name, extra_g=None, plus1=False, stream="ld"):
    t = ph.enter_context(usb(nc, name, [128, D], F32)); res = Res()
    S.dma("sp", lambda e: e.dma_start(out=t[:], in_=modv[l, r:r + 1, k * D:(k + 1) * D].to_broadcast([128, D])), writes=[res], stream=stream)
    if plus1:
        g = ph.enter_context(usb(nc, name + "_g", [128, D], F32)); rg = Res()
        S.dma("sp", lambda e: e.dma_start(out=g[:], in_=extra_g.to_broadcast([128, D])), writes=[rg], stream=stream)
        S.op("dve", lambda e: e.scalar_tensor_tensor(out=t[:], in0=t[:], scalar=1.0, in1=g[:], op0=ALU.add, op1=ALU.mult), reads=[res, rg], writes=[res])
    return t, res


def rstd_ops(S, xt, r_x, junk, r_junk, st, r_st):
    S.op("act", lambda e: e.activation(out=junk[:], in_=xt[:], func=AF.Square, accum_out=st[:, 0:1]), reads=[r_x], writes=[r_junk, r_st])
    S.op("dve", lambda e: e.tensor_scalar(out=st[:, 1:2], in0=st[:, 0:1], scalar1=1.0 / D, scalar2=EPS, op0=ALU.mult, op1=ALU.add), reads=[r_st], writes=[r_st])
    S.op("act", lambda e: e.sqrt(out=st[:, 2:3], in_=st[:, 1:2]), reads=[r_st], writes=[r_st])
    S.op("dve", lambda e: e.reciprocal(out=st[:, 3:4], in_=st[:, 2:3]), reads=[r_st], writes=[r_st])


def phase1(nc, S, IN, l, xsrc, modv, uT, abd, ident, identb, ones, r_const):
    S.barrier()
    with ExitStack() as ph:
        sb = lambda n, s, d: ph.enter_context(usb(nc, n, s, d))
        G1 = [None, None]; SH1 = [None, None]
        for r in range(2):
            G1[r] = load_mod_bc(nc, S, ph, modv, l, r, 1, f"p1_G{r}", extra_g=IN["g_norm1"][l:l + 1, :], plus1=True)
            SH1[r] = load_mod_bc(nc, S, ph, modv, l, r, 0, f"p1_SH{r}")
        winb = sb("p1_winb", [128, 8, INW], BF16); r_win = Res()
        wv = IN["w_in"][l].rearrange("(j p) n -> p j n", p=128)
        for j in range(8):
            for h in range(2):
                S.dma("pool", lambda e, j=j, h=h: e.dma_start(out=winb[:, j, h * 1288:(h + 1) * 1288], in_=wv[:, j, h * 1288:(h + 1) * 1288]),
                      writes=[r_win], stream="wc")
        xts = Rot([(sb(f"p1_x{i}", [128, D], F32), Res()) for i in range(3)])
        junk = sb("p1_junk", [128, D], BF16); r_junk = Res()
        sts = Rot([(sb(f"p1_st{i}", [128, 4], F32), Res()) for i in range(3)])
        t1s = Rot([(sb(f"p1_t1{i}", [128, D], F32), Res()) for i in range(2)])
        hxbs = Rot([(sb(f"p1_hxb{i}", [128, D], BF16), Res()) for i in range(2)])
        hxTs = Rot([(sb(f"p1_hxT{i}", [128, 8, 512], BF16), Res()) for i in range(2)])
        stg = Rot([(sb(f"p1_stg{i}", [128, 512], F32), Res()) for i in range(3)])
        abs_ = Rot([(sb(f"p1_ab{i}", [128, 16], F32), Res()) for i in range(2)])
        ptr = Rot([(ph.enter_context(ups(nc, f"p1_ptr{i}", [128, 8, 128], BF16)), Res()) for i in range(2)])
        pmm = Rot([(ph.enter_context(ups(nc, f"p1_pmm{i}", [128, 512], F32)), Res()) for i in range(4)])
        pab = Rot([(ph.enter_context(ups(nc, f"p1_pab{i}", [128, 16], F32)), Res()) for i in range(2)])
        blocks = [(b * 4, 4) for b in range(8)] + [(32, 2)]
        for (t0, ntl) in blocks:
            hxT, r_hxT = hxTs.next()
            ntok = ntl * 128
            for ti in range(ntl):
                t = t0 + ti
                r = 0 if t < NTL else 1
                xt, r_x = xts.next()
                S.dma("sp", lambda e, xt=xt, t=t: e.dma_start(out=xt[:], in_=xsrc[t * 128:(t + 1) * 128, :]), writes=[r_x], stream="ld")
                st, r_st = sts.next()
                rstd_ops(S, xt, r_x, junk, r_junk, st, r_st)
                t1, r_t1 = t1s.next()
                S.op("dve", lambda e, t1=t1, xt=xt, st=st, r=r: e.scalar_tensor_tensor(out=t1[:], in0=xt[:], scalar=st[:, 3:4], in1=G1[r][0][:], op0=ALU.mult, op1=ALU.mult),
                     reads=[r_x, r_st, G1[r][1]], writes=[r_t1])
                hxb, r_hxb = hxbs.next()
                S.op("pool", lambda e, hxb=hxb, t1=t1, r=r: e.tensor_tensor(out=hxb[:], in0=t1[:], in1=SH1[r][0][:], op=ALU.add),
                     reads=[r_t1, SH1[r][1]], writes=[r_hxb])
                pt, r_pt = ptr.next()
                for j in range(8):
                    S.op("pe", lambda e, pt=pt, hxb=hxb, j=j: e.transpose(out=pt[:, j, :], in_=hxb[:, j * 128:(j + 1) * 128], identity=identb[:]),
                         reads=[r_hxb, r_const], writes=[r_pt])
                S.op("act", lambda e, pt=pt, hxT=hxT, ti=ti: e.copy(out=hxT[:, :, ti * 128:(ti + 1) * 128], in_=pt[:]), reads=[r_pt], writes=[r_hxT])
                pa, r_pa = pab.next()
                for j in range(8):
                    S.op("pe", lambda e, pa=pa, hxT=hxT, ti=ti, j=j: e.matmul(pa[:], lhsT=hxT[:, j, ti * 128:(ti + 1) * 128], rhs=winb[:, j, 2560:2576], start=(j == 0), stop=(j == 7)),
                         reads=[r_hxT, r_win], writes=[r_pa])
                ab, r_ab = abs_.next()
                S.op("dve", lambda e, ab=ab, pa=pa: e.tensor_copy(out=ab[:], in_=pa[:]), reads=[r_pa], writes=[r_ab])
                S.dma("sp", lambda e, ab=ab, t=t: e.dma_start(out=abd[t * 128:(t + 1) * 128, :], in_=ab[:]), reads=[r_ab], stream="scr")
            for c in range(20):
                pm, r_pm = pmm.next()
                for j in range(8):
                    S.op("pe", lambda e, pm=pm, hxT=hxT, c=c, j=j, ntok=ntok: e.matmul(pm[:, 0:ntok], lhsT=winb[:, j, c * 128:(c + 1) * 128], rhs=hxT[:, j, 0:ntok], start=(j == 0), stop=(j == 7)),
                         reads=[r_hxT, r_win], writes=[r_pm])
                sg, r_sg = stg.next()
                eng = "act" if c % 2 == 0 else "dve"
                if eng == "act":
                    S.op("act", lambda e, sg=sg, pm=pm, ntok=ntok: e.copy(out=sg[:, 0:ntok], in_=pm[:, 0:ntok]), reads=[r_pm], writes=[r_sg])
                else:
                    S.op("dve", lambda e, sg=sg, pm=pm, ntok=ntok: e.tensor_copy(out=sg[:, 0:ntok], in_=pm[:, 0:ntok]), reads=[r_pm], writes=[r_sg])
                S.dma("sp", lambda e, sg=sg, c=c, t0=t0, ntok=ntok: e.dma_start(out=uT[c * 128:(c + 1) * 128, t0 * 128:t0 * 128 + ntok], in_=sg[:, 0:ntok]), reads=[r_sg], stream="scr")


def phase2(nc, S, IN, l, uT, mixT, r_const):
    S.barrier()
    with ExitStack() as ph:
        sb = lambda n, s, d: ph.enter_context(usb(nc, n, s, d))
        cs = sb("p2_cs", [128, 256], F32); r_cs = Res()
        S.dma("sp", lambda e: e.dma_start(out=cs[:], in_=IN["cs128"]), writes=[r_cs], stream="ld")
        ntiles = NT if l == 0 else NTL
        FCS = sb("p2_fcs", [128, NT, 4, 256], BF16); r_fcs = [Res() for _ in range(NT)]
        fts = Rot([(sb(f"p2_ft{i}", [128, 4, 128], F32), Res()) for i in range(3)])
        pas = Rot([(ph.enter_context(ups(nc, f"p2_pa{i}", [128, 4, 256], F32)), Res()) for i in range(2)])
        pos = Rot([(ph.enter_context(ups(nc, f"p2_po{i}", [128, 512], F32)), Res()) for i in range(4)])
        for t in range(ntiles):
            ft, r_ft = fts.next()
            S.dma("sp", lambda e, ft=ft, t=t: e.dma_start(out=ft[:], in_=uT[0:512, t * 128:(t + 1) * 128].rearrange("(g p) t -> p g t", p=128)), writes=[r_ft], stream="ld")
            pa, r_pa = pas.next()
            for g in range(4):
                S.op("pe", lambda e, pa=pa, ft=ft, g=g: e.matmul(pa[:, g, :], lhsT=ft[:, g, :], rhs=cs[:], start=True, stop=True), reads=[r_ft, r_cs], writes=[r_pa])
            if t % 2 == 0:
                S.op("act", lambda e, pa=pa, t=t: e.copy(out=FCS[:, t, :, :], in_=pa[:]), reads=[r_pa], writes=[r_fcs[t]])
            else:
                S.op("dve", lambda e, pa=pa, t=t: e.tensor_copy(out=FCS[:, t, :, :], in_=pa[:]), reads=[r_pa], writes=[r_fcs[t]])
        cosb = Rot([(sb(f"p2_cos{i}", [128, NTL, 256], BF16), Res()) for i in range(2)])
        sinb = Rot([(sb(f"p2_sin{i}", [128, NTL, 256], BF16), Res()) for i in range(2)])
        ostg = Rot([(sb(f"p2_os{i}", [128, 256], BF16), Res()) for i in range(3)])
        segs = [(0, NTL, "L")] + ([(NTL, 2, "C")] if l == 0 else [])
        for (t0, ntl, nm) in segs:
            cv = IN["cos" + nm].rearrange("(j p) k -> p j k", p=128)
            sv = IN["nsin" + nm].rearrange("(j p) k -> p j k", p=128)
            for kb in range(ntl * 128 // 256):
                cb, r_cb = cosb.next(); sn, r_sn = sinb.next()
                S.dma("sp", lambda e, cb=cb, kb=kb, cv=cv, ntl=ntl: e.dma_start(out=cb[:, 0:ntl, :], in_=cv[:, :, kb * 256:(kb + 1) * 256]), writes=[r_cb], stream="ld")
                S.dma("act", lambda e, sn=sn, kb=kb, sv=sv, ntl=ntl: e.dma_start(out=sn[:, 0:ntl, :], in_=sv[:, :, kb * 256:(kb + 1) * 256]), writes=[r_sn], stream="ld2")
                for g in range(4):
                    po, r_po = pos.next()
                    for j in range(ntl):
                        S.op("pe", lambda e, po=po, j=j, g=g, cb=cb, t0=t0: e.matmul(po[:, 0:256], lhsT=FCS[:, t0 + j, g, 0:128], rhs=cb[:, j, :], start=(j == 0), stop=False),
                             reads=[r_fcs[t0 + j], r_cb], writes=[r_po])
                        S.op("pe", lambda e, po=po, j=j, g=g, sn=sn, t0=t0, ntl=ntl: e.matmul(po[:, 0:256], lhsT=FCS[:, t0 + j, g, 128:256], rhs=sn[:, j, :], start=False, stop=(j == ntl - 1)),
                             reads=[r_fcs[t0 + j], r_sn], writes=[r_po])
                    og, r_og = ostg.next()
                    if g % 2 == 0:
                        S.op("act", lambda e, og=og, po=po: e.copy(out=og[:], in_=po[:, 0:256]), reads=[r_po], writes=[r_og])
                    else:
                        S.op("dve", lambda e, og=og, po=po: e.tensor_copy(out=og[:], in_=po[:, 0:256]), reads=[r_po], writes=[r_og])
                    S.dma("sp", lambda e, og=og, g=g, t0=t0, kb=kb: e.dma_start(out=mixT[g * 128:(g + 1) * 128, t0 * 128 + kb * 256:t0 * 128 + (kb + 1) * 256], in_=og[:]), reads=[r_og], stream="scr")


WC = T + 4


def tcol(t):
    return t * 128 + (4 if t >= NTL else 0)


def phase3(nc, S, IN, l, uT, abd, mixT, ident, ones, r_const, dbg_oT, bg=()):
    S.barrier()
    with ExitStack() as ph:
        sb = lambda n, s, d: ph.enter_context(usb(nc, n, s, d))
        pst = lambda n, s, d: ph.enter_context(ups(nc, n, s, d))
        r_c3 = Res()
        cm = {}
        for nm in ("m_ls", "m_li", "m_us", "m_ui", "tri_f", "tri_b", "blk"):
            cm[nm] = sb("p3_" + nm, [128, 128], F32)
            S.dma("sp", lambda e, nm=nm: e.dma_start(out=cm[nm][:], in_=IN[nm]), writes=[r_c3], stream="ld")
        cwr = sb("p3_cwr", [5, 1536], F32)
        gor = sb("p3_gor", [1, 128], F32)
        S.dma("sp", lambda e: e.dma_start(out=cwr[:], in_=IN["conv_w"][l]), writes=[r_c3], stream="ld")
        S.dma("sp", lambda e: e.dma_start(out=gor[:], in_=IN["g_out_norm"][l:l + 1, :]), writes=[r_c3], stream="ld")
        alb = sb("p3_alb", [128, 8], F32); dtb = sb("p3_dtb", [128, 8], F32)
        S.dma("sp", lambda e: e.dma_start(out=alb[:], in_=IN["a_log"][l:l + 1, :].to_broadcast([128, 8])), writes=[r_c3], stream="ld")
        S.dma("sp", lambda e: e.dma_start(out=dtb[:], in_=IN["dt_bias"][l:l + 1, :].to_broadcast([128, 8])), writes=[r_c3], stream="ld")
        banks = [pst(f"p3_bank{i}", [128, 512], F32) for i in range(8)]
        qtile = lambda b, q: banks[b][:, q * 128:(q + 1) * 128]
        rbank = [Res(excl=True) for _ in range(8)]
        pcw = banks[0][:, 0:104].rearrange("p (m k) -> p m k", k=8); r_pcw = rbank[0]
        cw = sb("p3_cw", [128, 13, 8], F32)
        for m in range(12):
            S.op("pe", lambda e, m=m: e.transpose(out=pcw[:, m, 0:5], in_=cwr[:, m * 128:(m + 1) * 128], identity=ident[0:5, 0:5]), reads=[r_c3, r_const], writes=[r_pcw])
        S.op("pe", lambda e: e.transpose(out=pcw[:, 12, 0:1], in_=gor[:, :], identity=ident[0:1, 0:1]), reads=[r_c3, r_const], writes=[r_pcw])
        r_cw = Res()
        S.op("dve", lambda e: e.memset(cw[:], 0.0), writes=[r_cw])
        for m in range(12):
            S.op("dve", lambda e, m=m: e.tensor_copy(out=cw[:, m, 0:5], in_=pcw[:, m, 0:5]), reads=[r_pcw], writes=[r_cw])
        S.op("dve", lambda e: e.tensor_copy(out=cw[:, 12, 0:1], in_=pcw[:, 12, 0:1]), reads=[r_pcw], writes=[r_cw])
        nea = sb("p3_nea", [128, 8], F32)
        S.op("act", lambda e: e.activation(out=nea[:], in_=alb[:], func=AF.Exp), reads=[r_c3], writes=[r_c3])
        S.op("dve", lambda e: e.tensor_scalar(out=nea[:], in0=nea[:], scalar1=-1.0, scalar2=None, op0=ALU.mult), reads=[r_c3], writes=[r_c3])
        names = ("BT", "NBT", "GAM", "EG", "BEG", "EK0", "EK1")
        GA = {nm: sb("p3_" + nm, [128, NT, 8], F32) for nm in names}
        r_ga = [Res() for _ in range(NT)]
        abt = Rot([(sb(f"p3_abt{i}", [128, 16], F32), Res()) for i in range(2)])
        tmp = Rot([(sb(f"p3_gt{i}", [128, 4, 8], F32), Res()) for i in range(2)])
        pgs = Rot([(banks[0][:, 128:144], r_pcw)])
        rowm = sb("p3_rowm", [128, 2], F32)
        S.op("dve", lambda e: e.tensor_copy(out=rowm[:, 0:1], in_=cm["blk"][:, 0:1]), reads=[r_c3], writes=[r_c3])
        S.op("dve", lambda e: e.tensor_copy(out=rowm[:, 1:2], in_=cm["blk"][:, 127:128]), reads=[r_c3], writes=[r_c3])
        for t in range(NT):
            ab, r_ab = abt.next()
            S.dma("sp", lambda e, ab=ab, t=t: e.dma_start(out=ab[:], in_=abd[t * 128:(t + 1) * 128, :]), writes=[r_ab], stream="ld")
            tm, r_tm = tmp.next()
            abv = ab[:].rearrange("p (d k h) -> p d k h", d=2, k=2)
            X = tm[:, 0, :].rearrange("p (d h) -> p d h", d=2)
            S.op("dve", lambda e, X=X, abv=abv: e.tensor_tensor(out=X, in0=abv[:, :, 0, :], in1=dtb[:].rearrange("p (d h) -> p d h", d=2), op=ALU.add), reads=[r_ab, r_c3], writes=[r_tm])
            S.op("act", lambda e, tm=tm: e.activation(out=tm[:, 0, :], in_=tm[:, 0, :], func=AF.Exp), reads=[r_tm], writes=[r_tm])
            S.op("act", lambda e, tm=tm: e.activation(out=tm[:, 0, :], in_=tm[:, 0, :], func=AF.Ln, bias=1.0), reads=[r_tm], writes=[r_tm])
            S.op("dve", lambda e, tm=tm: e.tensor_tensor(out=tm[:, 1, :], in0=tm[:, 0, :], in1=nea[:], op=ALU.mult), reads=[r_tm, r_c3], writes=[r_tm])
            B = tm[:, 2, :].rearrange("p (d h) -> p d h", d=2)
            S.op("act", lambda e, B=B, abv=abv: e.activation(out=B, in_=abv[:, :, 1, :], func=AF.Exp, scale=-1.0), reads=[r_ab], writes=[r_tm])
            S.op("dve", lambda e, tm=tm: e.tensor_scalar(out=tm[:, 2, :], in0=tm[:, 2, :], scalar1=1.0, scalar2=None, op0=ALU.add), reads=[r_tm], writes=[r_tm])
            S.op("dve", lambda e, tm=tm, t=t: e.reciprocal(out=GA["BT"][:, t, :], in_=tm[:, 2, :]), reads=[r_tm], writes=[r_ga[t]])
            S.op("dve", lambda e, t=t: e.tensor_scalar(out=GA["NBT"][:, t, :], in0=GA["BT"][:, t, :], scalar1=-1.0, scalar2=None, op0=ALU.mult), reads=[r_ga[t]], writes=[r_ga[t]])
            pg, r_pg = pgs.next()
            S.op("pe", lambda e, pg=pg, tm=tm: e.matmul(pg[:, 0:4], lhsT=cm["tri_f"][:], rhs=tm[:, 1, 0:4], start=True, stop=True), reads=[r_tm, r_c3], writes=[r_pg])
            S.op("pe", lambda e, pg=pg, tm=tm: e.matmul(pg[:, 4:8], lhsT=cm["tri_b"][:], rhs=tm[:, 1, 4:8], start=True, stop=True), reads=[r_tm, r_c3], writes=[r_pg])
            S.op("pe", lambda e, pg=pg, tm=tm: e.matmul(pg[:, 8:16], lhsT=cm["blk"][:], rhs=tm[:, 1, :], start=True, stop=True), reads=[r_tm, r_c3], writes=[r_pg])
            S.op("dve", lambda e, pg=pg, t=t: e.tensor_copy(out=GA["GAM"][:, t, :], in_=pg[:, 0:8]), reads=[r_pg], writes=[r_ga[t]])
            S.op("act", lambda e, pg=pg, t=t: e.activation(out=GA["EG"][:, t, :], in_=pg[:, 0:8], func=AF.Exp), reads=[r_pg], writes=[r_ga[t]])
            S.op("dve", lambda e, t=t: e.tensor_tensor(out=GA["BEG"][:, t, :], in0=GA["EG"][:, t, :], in1=GA["BT"][:, t, :], op=ALU.mult), reads=[r_ga[t]], writes=[r_ga[t]])
            S.op("dve", lambda e, pg=pg, tm=tm, t=t: e.tensor_tensor(out=tm[:, 3, :], in0=pg[:, 8:16], in1=GA["GAM"][:, t, :], op=ALU.subtract), reads=[r_pg, r_ga[t]], writes=[r_tm])
            S.op("act", lambda e, tm=tm: e.activation(out=tm[:, 3, :], in_=tm[:, 3, :], func=AF.Exp), reads=[r_tm], writes=[r_tm])
            S.op("dve", lambda e, tm=tm, t=t: e.tensor_scalar(out=GA["EK0"][:, t, :], in0=tm[:, 3, :], scalar1=rowm[:, 0:1], scalar2=None, op0=ALU.mult), reads=[r_tm, r_c3], writes=[r_ga[t]])
            S.op("dve", lambda e, tm=tm, t=t: e.tensor_scalar(out=GA["EK1"][:, t, :], in0=tm[:, 3, :], scalar1=rowm[:, 1:2], scalar2=None, op0=ALU.mult), reads=[r_tm, r_c3], writes=[r_ga[t]])
        raws = Rot([(sb(f"p3_raw{i}", [128, WC + 4], F32), Res()) for i in range(2)])
        QKV = [(sb(f"p3_qkv{i}", [128, WC], F32), Res()) for i in range(3)]
        oT = sb("p3_oT", [128, WC], F32); r_oT = [Res() for _ in range(NT)]
        r_oTall = Res()
        pn = banks[1]; r_pn = rbank[1]
        rns = Rot([(sb(f"p3_rn{i}", [128, 512], F32), Res()) for i in range(2)])
        zts = Rot([(sb(f"p3_z{i}", [128, 512], F32), Res()) for i in range(2)])
        obs = Rot([(sb(f"p3_ob{i}", [128, 512], BF16), Res()) for i in range(2)])

        KSLOT = 4; DEPTH = 2
        INTER = ("dg", "Dm", "E1", "E2", "N", "NTs", "TT", "Pa", "PTa", "Pb", "PTb", "Rv", "Rw")
        OUTS = ("EGr", "at", "u", "wT", "qg", "ke0", "ke1")
        BF_NAMES = ("Nb", "NTs", "TT", "Pa", "PTa", "Pb", "PTb", "Rv", "Rw")
        BI = [{n: (sb(f"p3_{n}_s{s}", [128, 128], BF16 if n in BF_NAMES else F32), Res()) for n in INTER + ("Nb",)} for s in range(KSLOT)]
        BO = [{n: Rot([(sb(f"p3_{n}_d{d}_{i}", [128, 128], F32), Res()) for i in range(DEPTH)]) for n in OUTS} for d in range(2)]
        VN = [Rot([(sb(f"p3_vn{d}_{i}", [128, 128], F32), Res()) for i in range(2)]) for d in range(2)]
        rbank_ = rbank
        slot_bank = (2, 3, 4, 7)
        PQ = [Rot([(qtile(slot_bank[s], qi), rbank_[slot_bank[s]]) for qi in range(4)]) for s in range(KSLOT)]
        PSC = {d: {n: (qtile(5 + d, qi), rbank_[5 + d]) for qi, n in enumerate(("ps1", "po", "pS"))} for d in range(2)}
        Sst = [(sb(f"p3_S{d}", [128, 128], F32), Res()) for d in range(2)]
        bg = list(bg)
        chunks9 = [(i * 512, 512) for i in range(8)] + [(4100, 256)]

        import os as _os
        LVL = int(_os.environ.get("P3_LEVEL", "9")); NTI = int(_os.environ.get("P3_NT", str(NT)))
        for h in range(4 if LVL >= 9 else (1 if LVL >= 1 else 0)):
            for which in range(3):
                raw, r_raw = raws.next()
                row0 = 512 + which * 512 + h * 128
                S.op("pool", lambda e, raw=raw: e.memset(raw[:], 0.0), writes=[r_raw])
                S.dma("sp", lambda e, raw=raw, row0=row0: e.dma_start(out=raw[:, 2:2 + SEQ], in_=uT[row0:row0 + 128, 0:SEQ]), writes=[r_raw], stream="ld")
                S.dma("sp", lambda e, raw=raw, row0=row0: e.dma_start(out=raw[:, SEQ + 6:SEQ + 6 + CTX], in_=uT[row0:row0 + 128, SEQ:T]), writes=[r_raw], stream="ld")
                dst, r_dst = QKV[which]
                m = which * 4 + h
                S.op("dve", lambda e, dst=dst, raw=raw, m=m: e.tensor_scalar(out=dst[:], in0=raw[:, 0:WC], scalar1=cw[:, m, 0:1], scalar2=None, op0=ALU.mult), reads=[r_raw, r_cw], writes=[r_dst])
                for k in range(1, 5):
                    S.op("dve", lambda e, dst=dst, raw=raw, m=m, k=k: e.scalar_tensor_tensor(out=dst[:], in0=raw[:, k:k + WC], scalar=cw[:, m, k:k + 1], in1=dst[:], op0=ALU.mult, op1=ALU.add),
                         reads=[r_raw, r_cw, r_dst], writes=[r_dst])
                S.op("act", lambda e, dst=dst: e.activation(out=dst[:], in_=dst[:], func=AF.Silu), reads=[r_dst], writes=[r_dst])
                if which < 2:
                    sq, r_sq = raws.items[(raws.i) % 2]
                    S.op("pool", lambda e, sq=sq, dst=dst: e.tensor_tensor(out=sq[:, 0:WC], in0=dst[:], in1=dst[:], op=ALU.mult), reads=[r_dst], writes=[r_sq])
                    for (c0, cn) in chunks9:
                        S.op("pe", lambda e, sq=sq, c0=c0, cn=cn: e.matmul(pn[:, 0:cn], lhsT=ones[:], rhs=sq[:, c0:c0 + cn], start=True, stop=True), reads=[r_sq, r_const], writes=[r_pn])
                        rn, r_rn = rns.next()
                        S.op("dve", lambda e, rn=rn, cn=cn: e.tensor_scalar(out=rn[:, 0:cn], in0=pn[:, 0:cn], scalar1=EPS, scalar2=None, op0=ALU.add), reads=[r_pn], writes=[r_rn])
                        S.op("act", lambda e, rn=rn, cn=cn: e.sqrt(out=rn[:, 0:cn], in_=rn[:, 0:cn]), reads=[r_rn], writes=[r_rn])
                        S.op("dve", lambda e, rn=rn, cn=cn: e.reciprocal(out=rn[:, 0:cn], in_=rn[:, 0:cn]), reads=[r_rn], writes=[r_rn])
                        sc = (128.0 ** -0.5) if which == 0 else 1.0
                        S.op("dve", lambda e, rn=rn, dst=dst, c0=c0, cn=cn, sc=sc: e.scalar_tensor_tensor(out=dst[:, c0:c0 + cn], in0=dst[:, c0:c0 + cn], scalar=sc, in1=rn[:, 0:cn], op0=ALU.mult, op1=ALU.mult),
                             reads=[r_rn, r_dst], writes=[r_dst])
            qT, r_q = QKV[0]; kT, r_k = QKV[1]; vT, r_v = QKV[2]
            for d in range(2):
                S.op("dve", lambda e, d=d: e.memset(Sst[d][0][:], 0.0), writes=[Sst[d][1]])
            seqs = [[32, 33] + list(range(32)), [33, 32] + list(range(31, -1, -1))]
            if LVL < 2:
                seqs = [[], []]
            else:
                seqs = [s_[:NTI] for s_ in seqs]
            written = set()
            PREP = {}
            scanned = [0, 0]

            def prep_gen(t, d, s):
                c0 = tcol(t); col = d * 4 + h
                bi = BI[s]; pq = PQ[s]
                ksl = kT[:, c0:c0 + 128]; qsl = qT[:, c0:c0 + 128]; vsl = vT[:, c0:c0 + 128]
                gam = GA["GAM"][:, t, col:col + 1]
                dg, r_dg = bi["dg"]; Dm, r_Dm = bi["Dm"]; E1, r_E1 = bi["E1"]; E2, r_E2 = bi["E2"]
                EGr, r_EGr = BO[d]["EGr"].next()
                gr, r_gr = pq.next()
                S.op("dve", lambda e: e.tensor_scalar(out=dg[:], in0=ident[:], scalar1=gam, scalar2=None, op0=ALU.mult), reads=[r_const, r_ga[t]], writes=[r_dg])
                S.op("pe", lambda e: e.matmul(gr[:], lhsT=ones[:], rhs=dg[:], start=True, stop=True), reads=[r_dg, r_const], writes=[r_gr])
                S.op("dve", lambda e: e.tensor_scalar(out=Dm[:], in0=gr[:], scalar1=-1.0, scalar2=gam, op0=ALU.mult, op1=ALU.add), reads=[r_gr, r_ga[t]], writes=[r_Dm])
                S.op("act", lambda e: e.activation(out=EGr[:], in_=gr[:], func=AF.Exp), reads=[r_gr], writes=[r_EGr])
                yield
                m1 = cm["m_ls"] if d == 0 else cm["m_us"]
                m2 = cm["m_ui"] if d == 0 else cm["m_li"]
                S.op("pool", lambda e: e.tensor_tensor(out=E1[:], in0=Dm[:], in1=m1[:], op=ALU.add), reads=[r_Dm, r_c3], writes=[r_E1])
                S.op("pool", lambda e: e.tensor_tensor(out=E2[:], in0=m2[:], in1=Dm[:], op=ALU.subtract), reads=[r_Dm, r_c3], writes=[r_E2])
                S.op("act", lambda e: e.activation(out=E1[:], in_=E1[:], func=AF.Exp), reads=[r_E1], writes=[r_E1])
                S.op("act", lambda e: e.activation(out=E2[:], in_=E2[:], func=AF.Exp), reads=[r_E2], writes=[r_E2])
                yield
                N, r_N = bi["N"]; at, r_at = BO[d]["at"].next()
                nbt = GA["NBT"][:, t, col:col + 1]
                kk, r_kk = pq.next()
                S.op("pe", lambda e: e.matmul(kk[:], lhsT=ksl, rhs=ksl, start=True, stop=True), reads=[r_k], writes=[r_kk])
                S.op("dve", lambda e: e.scalar_tensor_tensor(out=N[:], in0=kk[:], scalar=nbt, in1=E1[:], op0=ALU.mult, op1=ALU.mult), reads=[r_kk, r_ga[t], r_E1], writes=[r_N])
                Nb, r_Nb = bi["Nb"]
                S.op("act", lambda e: e.copy(out=Nb[:], in_=N[:]), reads=[r_N], writes=[r_Nb])
                kq, r_kq = pq.next()
                S.op("pe", lambda e: e.matmul(kq[:], lhsT=ksl, rhs=qsl, start=True, stop=True), reads=[r_k, r_q], writes=[r_kq])
                S.op("dve", lambda e: e.tensor_tensor(out=at[:], in0=kq[:], in1=E2[:], op=ALU.mult), reads=[r_kq, r_E2], writes=[r_at])
                yield
                Rv, r_Rv = bi["Rv"]; Rw, r_Rw = bi["Rw"]
                ke0, r_ke0 = BO[d]["ke0"].next(); ke1, r_ke1 = BO[d]["ke1"].next(); qg, r_qg = BO[d]["qg"].next()
                bt = GA["BT"][:, t, col:col + 1]; beg = GA["BEG"][:, t, col:col + 1]
                kt, r_kt = pq.next()
                S.op("pe", lambda e: e.transpose(out=kt[:], in_=ksl, identity=ident[:]), reads=[r_k, r_const], writes=[r_kt])
                S.op("act", lambda e: e.activation(out=Rw[:], in_=kt[:], func=AF.Copy, scale=beg), reads=[r_kt, r_ga[t]], writes=[r_Rw])
                S.op("dve", lambda e: e.tensor_scalar(out=ke0[:], in0=kt[:], scalar1=GA["EK0"][:, t, col:col + 1], scalar2=None, op0=ALU.mult), reads=[r_kt, r_ga[t]], writes=[r_ke0])
                S.op("dve", lambda e: e.tensor_scalar(out=ke1[:], in0=kt[:], scalar1=GA["EK1"][:, t, col:col + 1], scalar2=None, op0=ALU.mult), reads=[r_kt, r_ga[t]], writes=[r_ke1])
                vt, r_vt = pq.next()
                S.op("pe", lambda e: e.transpose(out=vt[:], in_=vsl, identity=ident[:]), reads=[r_v, r_const], writes=[r_vt])
                S.op("act", lambda e: e.activation(out=Rv[:], in_=vt[:], func=AF.Copy, scale=bt), reads=[r_vt, r_ga[t]], writes=[r_Rv])
                S.op("pool", lambda e: e.tensor_tensor(out=qg[:], in0=qsl, in1=EGr[:], op=ALU.mult), reads=[r_q, r_EGr], writes=[r_qg])
                yield
                NTs, r_NTs = bi["NTs"]; TT, r_TT = bi["TT"]
                ntp, r_ntp = pq.next()
                S.op("pe", lambda e: e.transpose(out=ntp[:], in_=N[:], identity=ident[:]), reads=[r_N, r_const], writes=[r_ntp])
                S.op("act", lambda e: e.copy(out=NTs[:], in_=ntp[:]), reads=[r_ntp], writes=[r_NTs])
                S.op("dve", lambda e: e.tensor_tensor(out=TT[:], in0=ntp[:], in1=ident[:], op=ALU.add), reads=[r_ntp, r_const], writes=[r_TT])
                yield
                P_, r_P = Nb, r_Nb
                PT_, r_PT = NTs, r_NTs
                for lev in range(1, 6):
                    p2, r_p2 = pq.next()
                    S.op("pe", lambda e, p2=p2, PT_=PT_, P_=P_: e.matmul(p2[:], lhsT=PT_[:], rhs=P_[:], start=True, stop=True), reads=[r_P, r_PT], writes=[r_p2])
                    nP, r_nP = bi["Pa" if lev % 2 else "Pb"]
                    S.op("act", lambda e, nP=nP, p2=p2: e.copy(out=nP[:], in_=p2[:]), reads=[r_p2], writes=[r_nP])
                    if lev < 5:
                        pt2, r_pt2 = pq.next()
                        S.op("pe", lambda e, pt2=pt2, PT_=PT_, P_=P_: e.matmul(pt2[:], lhsT=P_[:], rhs=PT_[:], start=True, stop=True), reads=[r_P, r_PT], writes=[r_pt2])
                        nPT, r_nPT = bi["PTa" if lev % 2 else "PTb"]
                        S.op("dve", lambda e, nPT=nPT, pt2=pt2: e.tensor_copy(out=nPT[:], in_=pt2[:]), reads=[r_pt2], writes=[r_nPT])
                    yield
                    up, r_up = pq.next()
                    S.op("pe", lambda e, up=up, nP=nP: e.matmul(up[:], lhsT=nP[:], rhs=TT[:], start=True, stop=True), reads=[r_nP, r_TT], writes=[r_up])
                    S.op("dve", lambda e, up=up: e.tensor_tensor(out=TT[:], in0=up[:], in1=TT[:], op=ALU.add), reads=[r_up, r_TT], writes=[r_TT])
                    P_, r_P = nP, r_nP
                    if lev < 5:
                        PT_, r_PT = nPT, r_nPT
                    yield
                u, r_u = BO[d]["u"].next(); wT, r_wT = BO[d]["wT"].next()
                pu, r_pu = pq.next()
                S.op("pe", lambda e: e.matmul(pu[:], lhsT=TT[:], rhs=Rv[:], start=True, stop=True), reads=[r_TT, r_Rv], writes=[r_pu])
                S.op("act", lambda e: e.copy(out=u[:], in_=pu[:]), reads=[r_pu], writes=[r_u])
                pw, r_pw = pq.next()
                S.op("pe", lambda e: e.matmul(pw[:], lhsT=Rw[:], rhs=TT[:], start=True, stop=True), reads=[r_TT, r_Rw], writes=[r_pw])
                S.op("dve", lambda e: e.tensor_copy(out=wT[:], in_=pw[:]), reads=[r_pw], writes=[r_wT])
                PREP[(t, d)] = dict(EGr=(EGr, r_EGr), at=(at, r_at), u=(u, r_u), wT=(wT, r_wT), qg=(qg, r_qg), ke0=(ke0, r_ke0), ke1=(ke1, r_ke1))

            def scan_gen(d):
                Sd, r_S = Sst[d]
                for t in seqs[d]:
                    while (t, d) not in PREP:
                        yield "wait"
                    pr = PREP[(t, d)]
                    c0 = tcol(t)
                    EGr, r_EGr = pr["EGr"]; at, r_at = pr["at"]; u, r_u = pr["u"]; wT, r_wT = pr["wT"]; qg, r_qg = pr["qg"]
                    for c in ((0, 1) if d == 0 else (1, 0)):
                        cs_ = slice(c * 64, (c + 1) * 64)
                        gcol = c * 64 + (63 if d == 0 else 0)
                        ps1, r_ps1 = PSC[d]["ps1"]; po, r_po = PSC[d]["po"]; pS, r_pS = PSC[d]["pS"]
                        vn, r_vn = VN[d].next()
                        ke, r_ke = pr["ke0"] if c == 0 else pr["ke1"]
                        S.op("pe", lambda e, wT=wT: e.matmul(ps1[:], lhsT=wT[:], rhs=Sd[:], start=True, stop=True), reads=[r_wT, r_S], writes=[r_ps1])
                        yield
                        S.op("dve", lambda e, vn=vn, u=u: e.tensor_tensor(out=vn[:], in0=u[:], in1=ps1[:], op=ALU.subtract), reads=[r_u, r_ps1], writes=[r_vn])
                        yield
                        S.op("pe", lambda e, qg=qg, cs_=cs_: e.matmul(po[:, 0:64], lhsT=Sd[:], rhs=qg[:, cs_], start=True, stop=False), reads=[r_S, r_qg], writes=[r_po])
                        S.op("pe", lambda e, vn=vn, at=at, cs_=cs_: e.matmul(po[:, 0:64], lhsT=vn[:], rhs=at[:, cs_], start=False, stop=True), reads=[r_vn, r_at], writes=[r_po])
                        S.op("pe", lambda e, ke=ke, vn=vn: e.matmul(pS[:], lhsT=ke[:], rhs=vn[:], start=True, stop=True), reads=[r_ke, r_vn], writes=[r_pS])
                        yield
                        osl = oT[:, c0 + c * 64:c0 + (c + 1) * 64]
                        if (t, c) not in written:
                            written.add((t, c))
                            S.op("act", lambda e, osl=osl: e.copy(out=osl, in_=po[:, 0:64]), reads=[r_po], writes=[r_oT[t]])
                        else:
                            S.op("dve", lambda e, osl=osl: e.tensor_tensor(out=osl, in0=po[:, 0:64], in1=osl, op=ALU.add), reads=[r_po, r_oT[t]], writes=[r_oT[t]])
                        S.op("dve", lambda e, EGr=EGr, gcol=gcol: e.scalar_tensor_tensor(out=Sd[:], in0=Sd[:], scalar=EGr[:, gcol:gcol + 1], in1=pS[:], op0=ALU.mult, op1=ALU.add),
                             reads=[r_S, r_EGr, r_pS], writes=[r_S])
                        yield
                    scanned[d] += 1

            queue = []
            for i in range(len(seqs[0])):
                for d in range(2):
                    queue.append((seqs[d][i], d, i))
            active = {}
            scans = [scan_gen(0), scan_gen(1)]
            scan_done = [len(seqs[0]) == 0, len(seqs[1]) == 0]
            nbg = 0
            while not all(scan_done):
                progressed = False
                while queue and len(active) < KSLOT and (queue[0][2] - scanned[queue[0][1]] < DEPTH):
                    t_, d_, i_ = queue.pop(0)
                    s_ = [x for x in range(KSLOT) if x not in active][0]
                    active[s_] = prep_gen(t_, d_, s_)
                    progressed = True
                    nbg += 1
                    if bg and nbg % 2 == 0:
                        bg.pop(0)()
                for s_ in list(active.keys()):
                    try:
                        next(active[s_]); progressed = True
                    except StopIteration:
                        del active[s_]; progressed = True
                for d in range(2):
                    if not scan_done[d]:
                        try:
                            r_ = next(scans[d])
                            if r_ != "wait":
                                progressed = True
                        except StopIteration:
                            scan_done[d] = True; progressed = True
                assert progressed, "phase3 scheduler stuck"
            if dbg_oT is not None:
                S.dma("sp", lambda e, h=h: e.dma_start(out=dbg_oT[h, :, 0:SEQ], in_=oT[:, 0:SEQ]), reads=r_oT, stream="scr")
                S.dma("sp", lambda e, h=h: e.dma_start(out=dbg_oT[h, :, SEQ:T], in_=oT[:, SEQ + 4:SEQ + 4 + CTX]), reads=r_oT, stream="scr")
            sq, r_sq = raws.next()
            for ci, (c0, cn) in enumerate(chunks9):
                tiles = list(range(ci * 4, ci * 4 + 4)) if ci < 8 else [32, 33]
                rds = [r_oT[t] for t in tiles]
                S.op("pool", lambda e, sq=sq, c0=c0, cn=cn: e.tensor_tensor(out=sq[:, c0:c0 + cn], in0=oT[:, c0:c0 + cn], in1=oT[:, c0:c0 + cn], op=ALU.mult), reads=rds, writes=[r_sq])
                S.op("pe", lambda e, sq=sq, c0=c0, cn=cn: e.matmul(pn[:, 0:cn], lhsT=ones[:], rhs=sq[:, c0:c0 + cn], start=True, stop=True), reads=[r_sq, r_const], writes=[r_pn])
                rn, r_rn = rns.next()
                S.op("dve", lambda e, rn=rn, cn=cn: e.tensor_scalar(out=rn[:, 0:cn], in0=pn[:, 0:cn], scalar1=1.0 / 128, scalar2=EPS, op0=ALU.mult, op1=ALU.add), reads=[r_pn], writes=[r_rn])
                S.op("act", lambda e, rn=rn, cn=cn: e.sqrt(out=rn[:, 0:cn], in_=rn[:, 0:cn]), reads=[r_rn], writes=[r_rn])
                S.op("dve", lambda e, rn=rn, cn=cn: e.reciprocal(out=rn[:, 0:cn], in_=rn[:, 0:cn]), reads=[r_rn], writes=[r_rn])
                S.op("dve", lambda e, rn=rn, c0=c0, cn=cn: e.scalar_tensor_tensor(out=rn[:, 0:cn], in0=oT[:, c0:c0 + cn], scalar=cw[:, 12, 0:1], in1=rn[:, 0:cn], op0=ALU.mult, op1=ALU.mult),
                     reads=rds + [r_rn, r_cw], writes=[r_rn])
                zt, r_zt = zts.next()
                tok0 = ci * 512 if ci < 8 else SEQ
                zrow = 2048 + h * 128
                S.dma("sp", lambda e, zt=zt, tok0=tok0, cn=cn, zrow=zrow: e.dma_start(out=zt[:, 0:cn], in_=uT[zrow:zrow + 128, tok0:tok0 + cn]), writes=[r_zt], stream="ld")
                S.op("act", lambda e, zt=zt, cn=cn: e.activation(out=zt[:, 0:cn], in_=zt[:, 0:cn], func=AF.Silu), reads=[r_zt], writes=[r_zt])
                ob, r_ob = obs.next()
                S.op("pool", lambda e, ob=ob, rn=rn, zt=zt, cn=cn: e.tensor_tensor(out=ob[:, 0:cn], in0=rn[:, 0:cn], in1=zt[:, 0:cn], op=ALU.mult), reads=[r_rn, r_zt], writes=[r_ob])
                S.dma("sp", lambda e, ob=ob, tok0=tok0, cn=cn, h=h: e.dma_start(out=mixT[512 + h * 128:512 + (h + 1) * 128, tok0:tok0 + cn], in_=ob[:, 0:cn]), reads=[r_ob], stream="scr")
        while bg:
            bg.pop(0)()


def phase4(nc, S, IN, l, xsrc, xres, modv, mixT, h2rows, gates, r_gates, ident, ones, r_const, nt_act, dbg_g):
    S.barrier()
    with ExitStack() as ph:
        sb = lambda n, s, d: ph.enter_context(usb(nc, n, s, d))
        pst = lambda n, s, d: ph.enter_context(ups(nc, n, s, d))
        nr = 2 if nt_act > NTL else 1
        GT1 = [load_mod_bc(nc, S, ph, modv, l, r, 2, f"p4_GT{r}") for r in range(nr)]
        G2 = [load_mod_bc(nc, S, ph, modv, l, r, 4, f"p4_G{r}", extra_g=IN["g_norm2"][l:l + 1, :], plus1=True) for r in range(nr)]
        SH2 = [load_mod_bc(nc, S, ph, modv, l, r, 3, f"p4_SH{r}") for r in range(nr)]
        woutb = sb("p4_wout", [128, 8, D], BF16); r_wo = Res()
        wv = IN["w_out"][l].rearrange("(j p) n -> p j n", p=128)
        for j in range(8):
            S.dma("pool", lambda e, j=j: e.dma_start(out=woutb[:, j, :], in_=wv[:, j, :]), writes=[r_wo], stream="wc")
        wrf = sb("p4_wr", [128, 8, NE], F32); r_wr = Res()
        brr = sb("p4_br", [1, NE], F32)
        S.dma("sp", lambda e: e.dma_start(out=wrf[:], in_=IN["w_router"][l].rearrange("(j p) n -> p j n", p=128)), writes=[r_wr], stream="ld")
        S.dma("sp", lambda e: e.dma_start(out=brr[:], in_=IN["b_router"][l:l + 1, :]), writes=[r_wr], stream="ld")
        mixs = Rot([(sb(f"p4_mx{i}", [128, 8, 128], BF16), Res()) for i in range(2)])
        xts = Rot([(sb(f"p4_x{i}", [128, D], F32), Res()) for i in range(2)])
        tmps = Rot([(sb(f"p4_t{i}", [128, D], F32), Res()) for i in range(2)])
        xns = Rot([(sb(f"p4_xn{i}", [128, D], F32), Res()) for i in range(2)])
        h2s = Rot([(sb(f"p4_h2{i}", [128, D], F32), Res()) for i in range(2)])
        junk = sb("p4_junk", [128, D], BF16); r_junk = Res()
        sts = Rot([(sb(f"p4_st{i}", [128, 4], F32), Res()) for i in range(2)])
        h2bs = Rot([(sb(f"p4_hb{i}", [128, D], BF16), Res()) for i in range(2)])
        h2fs = Rot([(sb(f"p4_hf{i}", [128, 8, 128], F32), Res()) for i in range(2)])
        lgs = Rot([(sb(f"p4_lg{i}", [128, 4, NE], F32), Res()) for i in range(2)])
        t8s = Rot([(sb(f"p4_t8{i}", [128, 16], F32), Res()) for i in range(2)])
        pys = Rot([(pst(f"p4_py{i}", [128, D], F32), Res()) for i in range(2)])
        ptr = Rot([(pst("p4_ptr", [128, 8, 128], F32), Res(excl=True))])
        pls = Rot([(pst(f"p4_pl{i}", [128, NE], F32), Res()) for i in range(2)])
        for t in range(nt_act):
            r = 0 if t < NTL else 1
            mx, r_mx = mixs.next()
            S.dma("sp", lambda e, mx=mx, t=t: e.dma_start(out=mx[:], in_=mixT[:, t * 128:(t + 1) * 128].rearrange("(j p) t -> p j t", p=128)), writes=[r_mx], stream="ld")
            xt, r_x = xts.next()
            S.dma("act", lambda e, xt=xt, t=t: e.dma_start(out=xt[:], in_=xsrc[t * 128:(t + 1) * 128, :]), writes=[r_x], stream="ld2")
            py, r_py = pys.next()
            for half in range(2):
                for j in range(8):
                    S.op("pe", lambda e, py=py, mx=mx, half=half, j=j: e.matmul(py[:, half * 512:(half + 1) * 512], lhsT=mx[:, j, :], rhs=woutb[:, j, half * 512:(half + 1) * 512], start=(j == 0), stop=(j == 7)),
                         reads=[r_mx, r_wo], writes=[r_py])
            tp, r_tp = tmps.next()
            S.op("dve", lambda e, tp=tp, py=py, r=r: e.tensor_tensor(out=tp[:], in0=py[:], in1=GT1[r][0][:], op=ALU.mult), reads=[r_py, GT1[r][1]], writes=[r_tp])
            xn, r_xn = xns.next()
            S.op("pool", lambda e, xn=xn, tp=tp, xt=xt: e.tensor_tensor(out=xn[:], in0=tp[:], in1=xt[:], op=ALU.add), reads=[r_tp, r_x], writes=[r_xn])
            S.dma("sp", lambda e, xn=xn, t=t: e.dma_start(out=xres[t * 128:(t + 1) * 128, :], in_=xn[:]), reads=[r_xn], stream="scr")
            st, r_st = sts.next()
            rstd_ops(S, xn, r_xn, junk, r_junk, st, r_st)
            h2, r_h2 = h2s.next()
            S.op("dve", lambda e, h2=h2, xn=xn, st=st, r=r: e.scalar_tensor_tensor(out=h2[:], in0=xn[:], scalar=st[:, 3:4], in1=G2[r][0][:], op0=ALU.mult, op1=ALU.mult), reads=[r_xn, r_st, G2[r][1]], writes=[r_h2])
            S.op("pool", lambda e, h2=h2, r=r: e.tensor_tensor(out=h2[:], in0=h2[:], in1=SH2[r][0][:], op=ALU.add), reads=[r_h2, SH2[r][1]], writes=[r_h2])
            pt, r_pt = ptr.next()
            for j in range(8):
                S.op("pe", lambda e, pt=pt, h2=h2, j=j: e.transpose(out=pt[:, j, :], in_=h2[:, j * 128:(j + 1) * 128], identity=ident[:]), reads=[r_h2, r_const], writes=[r_pt])
            hb, r_hb = h2bs.next(); hf, r_hf = h2fs.next()
            S.op("act", lambda e, hb=hb, h2=h2: e.copy(out=hb[:], in_=h2[:]), reads=[r_h2], writes=[r_hb])
            S.op("dve", lambda e, hf=hf, pt=pt: e.tensor_copy(out=hf[:], in_=pt[:]), reads=[r_pt], writes=[r_hf])
            S.dma("sp", lambda e, hb=hb, t=t: e.dma_start(out=h2rows[t * 128:(t + 1) * 128, :], in_=hb[:]), reads=[r_hb], stream="scr")
            pl, r_pl = pls.next()
            for j in range(8):
                S.op("pe", lambda e, pl=pl, hf=hf, j=j: e.matmul(pl[:], lhsT=hf[:, j, :], rhs=wrf[:, j, :], start=(j == 0), stop=False), reads=[r_hf, r_wr], writes=[r_pl])
            S.op("pe", lambda e, pl=pl: e.matmul(pl[:], lhsT=ones[0:1, :], rhs=brr[0:1, :], start=False, stop=True), reads=[r_wr, r_const], writes=[r_pl])
            lg, r_lg = lgs.next(); t8, r_t8 = t8s.next()
            S.op("dve", lambda e, lg=lg, pl=pl: e.tensor_copy(out=lg[:, 0, :], in_=pl[:]), reads=[r_pl], writes=[r_lg])
            S.op("dve", lambda e, lg=lg, t8=t8: e.max(out=t8[:, 0:8], in_=lg[:, 0, :]), reads=[r_lg], writes=[r_t8])
            S.op("dve", lambda e, lg=lg, t8=t8: e.tensor_scalar(out=lg[:, 1, :], in0=lg[:, 0, :], scalar1=t8[:, 3:4], scalar2=None, op0=ALU.is_ge), reads=[r_lg, r_t8], writes=[r_lg])
            S.op("dve", lambda e, t8=t8: e.tensor_scalar(out=t8[:, 8:9], in0=t8[:, 0:1], scalar1=-1.0, scalar2=None, op0=ALU.mult), reads=[r_t8], writes=[r_t8])
            S.op("act", lambda e, lg=lg, t8=t8: e.activation(out=lg[:, 2, :], in_=lg[:, 0, :], func=AF.Exp, bias=t8[:, 8:9], scale=1.0), reads=[r_lg, r_t8], writes=[r_lg])
            S.op("dve", lambda e, lg=lg: e.tensor_tensor(out=lg[:, 3, :], in0=lg[:, 2, :], in1=lg[:, 1, :], op=ALU.mult), reads=[r_lg], writes=[r_lg])
            S.op("dve", lambda e, lg=lg, t8=t8: e.reduce_sum(out=t8[:, 9:10], in_=lg[:, 3, :], axis=AX.X), reads=[r_lg], writes=[r_t8])
            S.op("dve", lambda e, t8=t8: e.reciprocal(out=t8[:, 10:11], in_=t8[:, 9:10]), reads=[r_t8], writes=[r_t8])
            S.op("dve", lambda e, lg=lg, t8=t8, t=t: e.tensor_scalar(out=gates[:, t, :], in0=lg[:, 3, :], scalar1=t8[:, 10:11], scalar2=None, op0=ALU.mult), reads=[r_lg, r_t8], writes=[r_gates[t]])
            if dbg_g is not None:
                S.dma("sp", lambda e, t=t: e.dma_start(out=dbg_g[t * 128:(t + 1) * 128, :], in_=gates[:, t, :]), reads=[r_gates[t]], stream="scr")


def phase5(nc, S, IN, l, xres, modv, h2rows, yacc, slotrec, gates, r_gates, ident, ones, r_const, nt_act, out, wbf, r_wbfl):
    S.barrier()
    last = (l == 1)
    NB = (4 * nt_act * 128) // 128 + NE
    with ExitStack() as ph:
        sb = lambda n, s, d: ph.enter_context(usb(nc, n, s, d))
        pst = lambda n, s, d: ph.enter_context(ups(nc, n, s, d))
        nr = 2 if nt_act > NTL else 1
        GT2 = [load_mod_bc(nc, S, ph, modv, l, r, 5, f"p5_GT{r}") for r in range(nr)]
        if last:
            gfb = sb("p5_gf", [128, D], F32); r_gf = Res()
            S.dma("sp", lambda e: e.dma_start(out=gfb[:], in_=IN["g_final"].to_broadcast([128, D])), writes=[r_gf], stream="ld")
        r_k = Res()
        cst = {}
        for nm, shp in (("tri_s", [128, 128]), ("tokidf", [128, NT]), ("widxbase", [128, 8]), ("blockval", [128, 2]), ("eidx", [128, 1])):
            cst[nm] = sb("p5_" + nm, shp, F32)
            S.dma("sp", lambda e, nm=nm: e.dma_start(out=cst[nm][:], in_=IN[nm]), writes=[r_k], stream="ld")
        bnat = sb("p5_bnat", [NE, 3, D], F32); bb = sb("p5_bb", [NE, 3, D], BF16); r_bb = Res()
        for k, nm in enumerate(("b_gate", "b_up", "b_down")):
            S.dma("sp", lambda e, k=k, nm=nm: e.dma_start(out=bnat[:, k, :], in_=IN[nm][l]), writes=[r_bb], stream="ld")
        S.op("dve", lambda e: e.tensor_copy(out=bb[:], in_=bnat[:]), reads=[r_bb], writes=[r_bb])
        zt = sb("p5_zt", [128, D], F32); r_zt = Res(); r_yacc = Res(); r_h2r = Res(); r_slot = Res()
        zb = sb("p5_zb", [1, D], BF16)
        S.op("dve", lambda e: e.memset(zt[:], 0.0), writes=[r_zt])
        S.op("dve", lambda e: e.memset(zb[:], 0.0), writes=[r_zt])
        for t in range(NT):
            S.dma("sp", lambda e, t=t: e.dma_start(out=yacc[t * 128:(t + 1) * 128, :], in_=zt[:]), reads=[r_zt], writes=[r_yacc], stream="scr")
        S.dma("sp", lambda e: e.dma_start(out=yacc[T:T + 1, :], in_=zt[0:1, :]), reads=[r_zt], writes=[r_yacc], stream="scr")
        S.dma("sp", lambda e: e.dma_start(out=h2rows[T:T + 1, :], in_=zb[:]), reads=[r_zt], writes=[r_h2r], stream="scr")
        prt = sb("p5_prt", [128, NBMAX, 2], F32)
        S.dma("sp", lambda e: e.dma_start(out=prt[:], in_=IN["padrec"]), writes=[r_zt], stream="ld")
        r_slots = [Res() for _ in range(nt_act * 4)]
        S.dma("sp", lambda e: e.dma_start(out=slotrec.rearrange("(p a) b -> p a b", a=NBMAX), in_=prt[:]), reads=[r_zt], writes=[r_slot] + r_slots, stream="scr")
        pm = pst("p5_pm", [128, 512], F32); r_pm = Res(excl=True)
        M = sb("p5_M", [128, NT, NE], F32); r_M = Res()
        POS = sb("p5_POS", [128, NT, NE], F32); r_POS = Res()
        cum = sb("p5_cum", [128, NE], F32); r_cum = Res()
        rg = list(r_gates[:nt_act])
        S.op("dve", lambda e: e.tensor_single_scalar(out=M[:, 0:nt_act, :], in_=gates[:, 0:nt_act, :], scalar=0.0, op=ALU.is_gt), reads=rg, writes=[r_M])
        S.op("dve", lambda e: e.memset(cum[:], 0.0), writes=[r_cum])
        for t in range(nt_act):
            S.op("pe", lambda e, t=t: e.matmul(pm[:, 0:NE], lhsT=cst["tri_s"][:], rhs=M[:, t, :], start=True, stop=False), reads=[r_M, r_k], writes=[r_pm])
            S.op("pe", lambda e, t=t: e.matmul(pm[:, 0:NE], lhsT=ones[:], rhs=cum[:], start=False, stop=True), reads=[r_cum, r_const], writes=[r_pm])
            S.op("act", lambda e, t=t: e.copy(out=POS[:, t, :], in_=pm[:, 0:NE]), reads=[r_pm], writes=[r_POS])
            S.op("dve", lambda e, t=t: e.tensor_tensor(out=cum[:], in0=cum[:], in1=M[:, t, :], op=ALU.add), reads=[r_M, r_cum], writes=[r_cum])
        mt = sb("p5_mt", [128, 8, NE], F32); r_mt = Res()
        mti = sb("p5_mti", [128, 2, NE], I32)
        S.op("pe", lambda e: e.matmul(pm[:, 0:NE], lhsT=ones[:], rhs=cum[:], start=True, stop=True), reads=[r_cum, r_const], writes=[r_pm])
        S.op("dve", lambda e: e.tensor_scalar(out=mti[:, 0, :], in0=pm[:, 0:NE], scalar1=127.0, scalar2=None, op0=ALU.add), reads=[r_pm], writes=[r_mt])
        S.op("dve", lambda e: e.tensor_single_scalar(out=mti[:, 1, :], in_=mti[:, 0, :], scalar=7, op=ALU.arith_shift_right), reads=[r_mt], writes=[r_mt])
        S.op("dve", lambda e: e.tensor_single_scalar(out=mti[:, 0, :], in_=mti[:, 1, :], scalar=7, op=ALU.logical_shift_left), reads=[r_mt], writes=[r_mt])
        S.op("dve", lambda e: e.tensor_copy(out=mt[:, 0, :], in_=mti[:, 0, :]), reads=[r_mt], writes=[r_mt])
        S.op("dve", lambda e: e.memset(mt[:, 7, :], 1.0), writes=[r_mt])
        S.op("dve", lambda e: e.tensor_tensor_scan(out=mt[:, 1, :], data0=mt[:, 7, :], data1=mt[:, 0, :], initial=0.0, op0=ALU.mult, op1=ALU.add), reads=[r_mt], writes=[r_mt])
        S.op("dve", lambda e: e.tensor_tensor(out=mt[:, 2, :], in0=mt[:, 1, :], in1=mt[:, 0, :], op=ALU.subtract), reads=[r_mt], writes=[r_mt])
        S.op("dve", lambda e: e.tensor_single_scalar(out=mt[:, 3, :], in_=mt[:, 0, :], scalar=0.0, op=ALU.is_gt), reads=[r_mt], writes=[r_mt])
        for t in range(nt_act):
            S.op("dve", lambda e, t=t: e.tensor_tensor(out=POS[:, t, :], in0=POS[:, t, :], in1=mt[:, 2, :], op=ALU.add), reads=[r_POS, r_mt], writes=[r_POS])
        recs = sb("p5_recs", [128, NT * 4, 2], F32); r_recs = Res()
        idxf = sb("p5_idxf", [128, NT * 4], F32); idxi = sb("p5_idxi", [128, NT * 4], I32); r_idx = Res()
        v8s = Rot([(sb(f"p5_v8{i}", [128, 8], F32), Res()) for i in range(2)])
        ohs = Rot([(sb(f"p5_oh{i}", [128, NE], F32), Res()) for i in range(2)])
        for t in range(nt_act):
            v8, r_v8 = v8s.next()
            S.op("dve", lambda e, v8=v8, t=t: e.max(out=v8[:], in_=gates[:, t, :]), reads=[r_gates[t]], writes=[r_v8])
            for k in range(4):
                q = t * 4 + k
                oh, r_oh = ohs.next()
                S.op("dve", lambda e, oh=oh, v8=v8, t=t, k=k: e.tensor_scalar(out=oh[:], in0=gates[:, t, :], scalar1=v8[:, k:k + 1], scalar2=None, op0=ALU.is_equal), reads=[r_gates[t], r_v8], writes=[r_oh])
                S.op("dve", lambda e, oh=oh, t=t: e.tensor_tensor(out=oh[:], in0=oh[:], in1=POS[:, t, :], op=ALU.mult), reads=[r_oh, r_POS], writes=[r_oh])
                S.op("dve", lambda e, oh=oh, q=q: e.reduce_sum(out=idxf[:, q:q + 1], in_=oh[:], axis=AX.X), reads=[r_oh], writes=[r_idx])
                S.op("act", lambda e, q=q, t=t: e.copy(out=recs[:, q, 0:1], in_=cst["tokidf"][:, t:t + 1]), reads=[r_k], writes=[r_recs])
                S.op("act", lambda e, q=q, v8=v8, k=k: e.copy(out=recs[:, q, 1:2], in_=v8[:, k:k + 1]), reads=[r_v8], writes=[r_recs])
        S.op("dve", lambda e: e.tensor_copy(out=idxi[:, 0:nt_act * 4], in_=idxf[:, 0:nt_act * 4]), reads=[r_idx], writes=[r_idx])
        for q in range(nt_act * 4):
            S.dma("pool", lambda e, q=q: e.indirect_dma_start(out=slotrec, out_offset=bass.IndirectOffsetOnAxis(ap=idxi[:, q:q + 1], axis=0), in_=recs[:, q, :], in_offset=None),
                  reads=[r_idx, r_recs, r_slot], writes=[r_slots[q]], stream="igs")
        EO = sb("p5_EO", [128, 256], F32); OH = sb("p5_OH", [NE, 256], F32); r_bm = Res()
        dgt = sb("p5_dgt", [128, 128], F32); r_dgt = Res()
        colv = sb("p5_colv", [128, 8], F32); r_colv = Res()
        cmpt = sb("p5_cmp", [128, NE], F32); r_cmp = Res()
        EBt = sb("p5_EB", [128, 256], F32); CHt = sb("p5_CH", [128, 256], F32)
        for c in range(2):
            bv = cst["blockval"][:, c:c + 1]
            S.op("dve", lambda e, bv=bv: e.tensor_scalar(out=cmpt[:], in0=mt[:, 1, :], scalar1=bv, scalar2=None, op0=ALU.is_le), reads=[r_mt, r_k], writes=[r_cmp])
            S.op("dve", lambda e, c=c: e.reduce_sum(out=colv[:, c:c + 1], in_=cmpt[:], axis=AX.X), reads=[r_cmp], writes=[r_colv])
            S.op("dve", lambda e, c=c: e.tensor_scalar(out=colv[:, c:c + 1], in0=colv[:, c:c + 1], scalar1=float(NE - 1), scalar2=None, op0=ALU.min), reads=[r_colv], writes=[r_colv])
            S.op("dve", lambda e, bv=bv: e.tensor_scalar(out=cmpt[:], in0=mt[:, 2, :], scalar1=bv, scalar2=None, op0=ALU.is_equal), reads=[r_mt, r_k], writes=[r_cmp])
            S.op("dve", lambda e: e.tensor_tensor(out=cmpt[:], in0=cmpt[:], in1=mt[:, 3, :], op=ALU.mult), reads=[r_cmp, r_mt], writes=[r_cmp])
            S.op("dve", lambda e, c=c: e.tensor_reduce(out=colv[:, 2 + c:3 + c], in_=cmpt[:], axis=AX.X, op=ALU.max), reads=[r_cmp], writes=[r_colv])
            for kk, dst in ((c, EBt), (2 + c, CHt)):
                S.op("dve", lambda e, kk=kk: e.tensor_scalar(out=dgt[:], in0=ident[:], scalar1=colv[:, kk:kk + 1], scalar2=None, op0=ALU.mult), reads=[r_colv, r_const], writes=[r_dgt])
                S.op("pe", lambda e: e.matmul(pm[:, 0:128], lhsT=ones[:], rhs=dgt[:], start=True, stop=True), reads=[r_dgt, r_const], writes=[r_pm])
                S.op("act", lambda e, dst=dst, c=c: e.copy(out=dst[:, c * 128:(c + 1) * 128], in_=pm[:, 0:128]), reads=[r_pm], writes=[r_bm])
        S.op("dve", lambda e: e.tensor_scalar(out=CHt[:], in0=CHt[:], scalar1=-1.0e7, scalar2=1.0e7, op0=ALU.mult, op1=ALU.add), reads=[r_bm], writes=[r_bm])
        S.op("dve", lambda e: e.scalar_tensor_tensor(out=EO[:], in0=EBt[:], scalar=128.0, in1=CHt[:], op0=ALU.mult, op1=ALU.add), reads=[r_bm], writes=[r_bm])
        S.op("dve", lambda e: e.tensor_scalar(out=OH[:], in0=EBt[0:NE, :], scalar1=cst["eidx"][0:NE, 0:1], scalar2=None, op0=ALU.is_equal), reads=[r_bm, r_k], writes=[r_bm])
        wg = sb("p5_wg", [128, 8, D], BF16); wu = sb("p5_wu", [128, 8, D], BF16); wd = sb("p5_wd", [128, 8, D], BF16)
        r_wg = Res(); r_wu = Res(); r_wd = Res()
        recb = Rot([(sb(f"p5_rb{i}", [128, 2], F32), Res()) for i in range(3)])
        xgs = Rot([(sb(f"p5_xg{i}", [128, D], BF16), Res()) for i in range(2)])
        xTs = Rot([(sb(f"p5_xT{i}", [128, 8, 128], BF16), Res()) for i in range(2)])
        wix = Rot([(sb(f"p5_wi{i}", [128, 1], I32), Res()) for i in range(2)])
        ohb = Rot([(sb(f"p5_ohb{i}", [NE, 128], BF16), Res()) for i in range(2)])
        a_s = Rot([(sb(f"p5_a{i}", [128, 512], F32), Res()) for i in range(2)])
        sg_s = Rot([(sb(f"p5_sg{i}", [128, 512], F32), Res()) for i in range(2)])
        u_s = Rot([(sb(f"p5_u{i}", [128, 512], F32), Res()) for i in range(2)])
        acts = Rot([(sb(f"p5_act{i}", [128, 8, 128], BF16), Res()) for i in range(2)])
        atms = Rot([(sb(f"p5_atm{i}", [128, D], BF16), Res()) for i in range(2)])
        ygs = Rot([(sb(f"p5_yg{i}", [128, D], F32), Res()) for i in range(2)])
        ptr = Rot([(pst("p5_ptr", [128, 8, 128], BF16), Res(excl=True))])
        pAs = Rot([(pst(f"p5_pA{i}", [128, 512], F32), Res()) for i in range(2)])
        pUs = Rot([(pst(f"p5_pU{i}", [128, 512], F32), Res()) for i in range(2)])
        pYs = Rot([(pst(f"p5_pY{i}", [128, 512], F32), Res()) for i in range(2)])
        identb = sb("p5_idb", [128, 128], BF16)
        S.op("dve", lambda e: e.tensor_copy(out=identb[:], in_=ident[:]), reads=[r_const], writes=[r_k])

        BC = {}

        def bcreg(e):
            if "r" not in BC:
                BC["r"] = e.alloc_register(f"bc{l}")
                e.reg_mov(BC["r"], NE * 128 - 1)
            return BC["r"]

        def stage_in(b):
            rb, r_rb = recb.next()
            S.dma("sp", lambda e, rb=rb, b=b: e.dma_start(out=rb[:], in_=slotrec[b * 128:(b + 1) * 128, :]), reads=[r_slot] + r_slots, writes=[r_rb], stream="ld")
            xg, r_xg = xgs.next()
            S.dma("pool", lambda e, xg=xg, rb=rb: e.indirect_dma_start(out=xg[:], out_offset=None, in_=h2rows, in_offset=bass.IndirectOffsetOnAxis(ap=rb[:, 0:1].bitcast(I32), axis=0)),
                  reads=[r_rb, r_h2r], writes=[r_xg], stream="ig")
            wi, r_wi = wix.next()
            S.op("dve", lambda e, wi=wi, b=b: e.tensor_scalar(out=wi[:], in0=cst["eidx"][:], scalar1=EO[:, b:b + 1], scalar2=None, op0=ALU.add), reads=[r_bm, r_k], writes=[r_wi])
            for m, (wt, r_w) in enumerate(((wg, r_wg), (wu, r_wu), (wd, r_wd))):
                S.dma("pool", lambda e, wi=wi, m=m, wt=wt: e.indirect_dma_start(out=wt[:].rearrange("p j f -> p (j f)"), out_offset=None, in_=wbf[m], in_offset=bass.IndirectOffsetOnAxis(ap=wi[:, 0:1], axis=0),
                                                                             bounds_check=bcreg(e), oob_is_err=False), reads=[r_wi, r_wbfl], writes=[r_w], stream="wc")
            ob, r_ob = ohb.next()
            S.op("act", lambda e, ob=ob, b=b: e.activation(out=ob[:], in_=ones[0:NE, :], func=AF.Copy, scale=OH[0:NE, b:b + 1]), reads=[r_bm, r_const], writes=[r_ob])
            return (rb, r_rb, xg, r_xg, ob, r_ob)

        def compute(b, st):
            rb, r_rb, xg, r_xg, ob, r_ob = st
            pt, r_pt = ptr.next()
            for j in range(8):
                S.op("pe", lambda e, pt=pt, xg=xg, j=j: e.transpose(out=pt[:, j, :], in_=xg[:, j:D:8], identity=identb[:]), reads=[r_xg, r_k], writes=[r_pt])
            xT, r_xT = xTs.next()
            S.op("act", lambda e, xT=xT, pt=pt: e.copy(out=xT[:], in_=pt[:]), reads=[r_pt], writes=[r_xT])
            atm, r_atm = atms.next()
            for hf in range(2):
                pA, r_pA = pAs.next(); pU, r_pU = pUs.next()
                for (pp, r_pp, wt, r_w, bk) in ((pA, r_pA, wg, r_wg, 0), (pU, r_pU, wu, r_wu, 1)):
                    for j in range(8):
                        S.op("pe", lambda e, pp=pp, wt=wt, j=j, xT=xT, hf=hf: e.matmul(pp[:], lhsT=xT[:, j, :], rhs=wt[:, j, hf * 512:(hf + 1) * 512], start=(j == 0), stop=False),
                             reads=[r_w, r_xT], writes=[r_pp])
                    S.op("pe", lambda e, pp=pp, bk=bk, ob=ob, hf=hf: e.matmul(pp[:], lhsT=ob[:], rhs=bb[:, bk, hf * 512:(hf + 1) * 512], start=False, stop=True),
                         reads=[r_bb, r_ob], writes=[r_pp])
                a, r_a = a_s.next(); sg, r_sg = sg_s.next(); u1, r_u1 = u_s.next()
                S.op("dve", lambda e, a=a, pA=pA: e.tensor_scalar(out=a[:], in0=pA[:], scalar1=7.0, scalar2=None, op0=ALU.min), reads=[r_pA], writes=[r_a])
                S.op("act", lambda e, sg=sg, a=a: e.activation(out=sg[:], in_=a[:], func=AF.Sigmoid, scale=1.702), reads=[r_a], writes=[r_sg])
                S.op("dve", lambda e, u1=u1, pU=pU: e.tensor_scalar(out=u1[:], in0=pU[:], scalar1=7.0, scalar2=-7.0, op0=ALU.min, op1=ALU.max), reads=[r_pU], writes=[r_u1])
                S.op("dve", lambda e, sg=sg, a=a: e.tensor_tensor(out=sg[:], in0=sg[:], in1=a[:], op=ALU.mult), reads=[r_a, r_sg], writes=[r_sg])
                S.op("dve", lambda e, atm=atm, sg=sg, u1=u1, hf=hf: e.scalar_tensor_tensor(out=atm[:, hf * 512:(hf + 1) * 512], in0=u1[:], scalar=1.0, in1=sg[:], op0=ALU.add, op1=ALU.mult),
                     reads=[r_sg, r_u1], writes=[r_atm])
            pt2, r_pt2 = ptr.next()
            for j in range(8):
                S.op("pe", lambda e, pt2=pt2, atm=atm, j=j: e.transpose(out=pt2[:, j, :], in_=atm[:, j:D:8], identity=identb[:]), reads=[r_atm, r_k], writes=[r_pt2])
            actT, r_act = acts.next()
            S.op("act", lambda e, actT=actT, pt2=pt2: e.copy(out=actT[:], in_=pt2[:]), reads=[r_pt2], writes=[r_act])
            yg, r_yg = ygs.next()
            for half in range(2):
                pY, r_pY = pYs.next()
                for f in range(8):
                    S.op("pe", lambda e, pY=pY, actT=actT, f=f, half=half: e.matmul(pY[:], lhsT=actT[:, f, :], rhs=wd[:, f, half * 512:(half + 1) * 512], start=(f == 0), stop=False),
                         reads=[r_act, r_wd], writes=[r_pY])
                S.op("pe", lambda e, pY=pY, ob=ob, half=half: e.matmul(pY[:], lhsT=ob[:], rhs=bb[:, 2, half * 512:(half + 1) * 512], start=False, stop=True), reads=[r_ob, r_bb], writes=[r_pY])
                S.op("act", lambda e, yg=yg, pY=pY, rb=rb, half=half: e.activation(out=yg[:, half * 512:(half + 1) * 512], in_=pY[:], func=AF.Copy, scale=rb[:, 1:2]), reads=[r_pY, r_rb], writes=[r_yg])
            return (yg, r_yg, rb, r_rb)

        def stage_out(res):
            yg, r_yg, rb, r_rb = res
            S.dma("pool", lambda e, yg=yg, rb=rb: e.indirect_dma_start(out=yacc, out_offset=bass.IndirectOffsetOnAxis(ap=rb[:, 0:1].bitcast(I32), axis=0), in_=yg[:], in_offset=None, compute_op=ALU.add),
                  reads=[r_yg, r_rb], writes=[r_yacc], stream="igs")

        st = stage_in(0)
        for b in range(NB):
            res = compute(b, st)
            if b + 1 < NB:
                st = stage_in(b + 1)
            stage_out(res)
        xts = Rot([(sb(f"p5_xt{i}", [128, D], F32), Res()) for i in range(2)])
        yts = Rot([(sb(f"p5_yt{i}", [128, D], F32), Res()) for i in range(2)])
        junk = sb("p5_junk", [128, D], BF16); r_junk = Res()
        sts = Rot([(sb(f"p5_st{i}", [128, 4], F32), Res()) for i in range(2)])
        for t in range(nt_act):
            r = 0 if t < NTL else 1
            xt, r_xt = xts.next(); yt, r_yt = yts.next()
            S.dma("sp", lambda e, xt=xt, t=t: e.dma_start(out=xt[:], in_=xres[t * 128:(t + 1) * 128, :]), writes=[r_xt], stream="ld")
            S.dma("act", lambda e, yt=yt, t=t: e.dma_start(out=yt[:], in_=yacc[t * 128:(t + 1) * 128, :]), reads=[r_yacc], writes=[r_yt], stream="ld2")
            S.op("dve", lambda e, yt=yt, r=r: e.tensor_tensor(out=yt[:], in0=yt[:], in1=GT2[r][0][:], op=ALU.mult), reads=[r_yt, GT2[r][1]], writes=[r_yt])
            S.op("pool", lambda e, yt=yt, xt=xt: e.tensor_tensor(out=yt[:], in0=yt[:], in1=xt[:], op=ALU.add), reads=[r_yt, r_xt], writes=[r_yt])
            if not last:
                S.dma("sp", lambda e, yt=yt, t=t: e.dma_start(out=xres[t * 128:(t + 1) * 128, :], in_=yt[:]), reads=[r_yt], stream="scr")
            else:
                st_, r_st = sts.next()
                rstd_ops(S, yt, r_yt, junk, r_junk, st_, r_st)
                S.op("dve", lambda e, yt=yt, st_=st_: e.scalar_tensor_tensor(out=yt[:], in0=yt[:], scalar=st_[:, 3:4], in1=gfb[:], op0=ALU.mult, op1=ALU.mult), reads=[r_yt, r_st, r_gf], writes=[r_yt])
                S.dma("sp", lambda e, yt=yt, t=t: e.dma_start(out=out[t * 128:(t + 1) * 128, :], in_=yt[:]), reads=[r_yt], stream="st")


_CACHE = {}


def make_in_maps(inputs):
    consts = host_consts()
    shared = {}
    for nm, shp in W_SPECS:
        a = np.ascontiguousarray(np.asarray(inputs[nm], dtype=np.float32)).reshape(shp)
        shared[nm] = a
    shared.update(consts)
    x = np.asarray(inputs["x"], dtype=np.float32)
    ctx = np.asarray(inputs["ctx"], dtype=np.float32)
    c = np.asarray(inputs["c"], dtype=np.float32)
    c_ctx = np.asarray(inputs["c_ctx"], dtype=np.float32)
    maps = []
    for b in range(8):
        m = dict(shared)
        m["xin"] = np.ascontiguousarray(np.concatenate([x[b], ctx[b]], axis=0))
        m["cc"] = np.ascontiguousarray(np.stack([c[b], c_ctx], axis=0))
        maps.append(m)
    return maps


def kernel(**inputs):
    if "nc" not in _CACHE:
        _CACHE["nc"] = build()[0]
    nc = _CACHE["nc"]
    maps = make_in_maps(inputs)
    res = run_bass_kernel_spmd(nc, maps, core_ids=list(range(8)))
    return np.stack([np.asarray(r["out"], dtype=np.float32) for r in res.results], axis=0)
```

```python
import numpy as np
import concourse.bass as bass
import concourse.mybir as mybir
from concourse.alu_op_type import AluOpType as ALU
from contextlib import ExitStack
from concourse.bass_utils import run_bass_kernel_spmd

F32 = mybir.dt.float32
BF16 = mybir.dt.bfloat16
I32 = mybir.dt.int32
U32 = mybir.dt.uint32
AF = mybir.ActivationFunctionType
AX = mybir.AxisListType


class Res:
    __slots__ = ("name", "w", "rs", "excl")

    def __init__(self, name="", excl=False):
        self.name = name
        self.w = None
        self.rs = {}
        self.excl = excl


class Op:
    __slots__ = ("eng", "fn", "reads", "writes", "stream", "deps", "sig", "sigidx", "waits")

    def __init__(self, eng, fn, reads, writes, stream):
        self.eng = eng
        self.fn = fn
        self.reads = reads
        self.writes = writes
        self.stream = stream
        self.deps = None
        self.sig = False
        self.sigidx = -1
        self.waits = None


class Sched:
    CH = 16000
    CHD = 1000
    COMPUTE = ("pe", "act", "dve", "pool")

    def __init__(self, nc):
        self.nc = nc
        self.ops = []
        self._dcnt = {}

    def op(self, eng, fn, reads=(), writes=()):
        self.ops.append(Op(eng, fn, tuple(reads), tuple(writes), None))

    NSLOT = {"ld": 12, "scr": 12, "wc": 6, "ld2": 6, "st": 4, "ig": 8, "igs": 4, "cv": 8}

    def dma(self, queue, fn, reads=(), writes=(), stream="ld"):
        n = self._dcnt.get(stream, 0)
        self._dcnt[stream] = n + 1
        self.ops.append(Op(queue, fn, tuple(reads), tuple(writes), f"{stream}#{n % self.NSLOT.get(stream, 8)}"))

    def barrier(self):
        self.ops.append(None)

    def finalize(self, es, final_streams=()):
        nc = self.nc
        raw = self.ops
        ops = []
        bar_after = {}
        lastkey = {}
        pend = None
        for o in raw:
            if o is None:
                pend = dict(lastkey)
                continue
            i = len(ops)
            ops.append(o)
            key = o.stream if o.stream is not None else o.eng
            if pend is not None:
                bar_after[i] = pend
                pend = None
            if not key.startswith("cv#"):
                lastkey[key] = i
        self.ops = ops
        cur_bar = set()
        prev_dma = {}
        for i, o in enumerate(ops):
            if i in bar_after:
                cur_bar = set(bar_after[i].values())
                pend = None
            deps = set(cur_bar)
            if any(r.excl for r in o.reads):
                o.writes = tuple(o.writes) + tuple(r for r in o.reads if r.excl)
                o.reads = tuple(r for r in o.reads if not r.excl)
            for r in o.reads:
                if r.w is not None:
                    deps.add(r.w)
            for w in o.writes:
                if w.w is not None:
                    deps.add(w.w)
                for k, j in w.rs.items():
                    deps.add(j)
            key = o.stream if o.stream is not None else o.eng
            for r in o.reads:
                r.rs[key] = i
            for w in o.writes:
                w.w = i
                w.rs = {}
            if o.stream is not None:
                if o.stream in prev_dma:
                    deps.add(prev_dma[o.stream])
                prev_dma[o.stream] = i
            deps.discard(i)
            o.deps = deps
            for j in deps:
                ops[j].sig = True
        cnt = {}
        for o in ops:
            key = o.stream if o.stream is not None else o.eng
            if o.stream is not None:
                o.sig = True
            if o.sig:
                o.sigidx = cnt.get(key, 0)
                cnt[key] = o.sigidx + 1
        self.cnt = cnt
        known = {e: {} for e in ("pe", "act", "dve", "pool", "sp")}
        clocks = [None] * len(ops)
        for i, o in enumerate(ops):
            kn = known[o.eng]
            waits = {}
            for j in sorted(o.deps):
                p = ops[j]
                pkey = p.stream if p.stream is not None else p.eng
                if p.stream is None and p.eng == o.eng:
                    if o.eng == "pe":
                        continue
                if kn.get(pkey, -1) >= p.sigidx:
                    continue
                if waits.get(pkey, -1) < p.sigidx:
                    waits[pkey] = p.sigidx
                pc = clocks[j]
                for k, v in pc.items():
                    if kn.get(k, -1) < v:
                        kn[k] = v
            for k, v in waits.items():
                if kn.get(k, -1) < v:
                    kn[k] = v
            o.waits = waits
            ck = dict(kn)
            if o.sig:
                key = o.stream if o.stream is not None else o.eng
                ck[key] = max(ck.get(key, -1), o.sigidx)
            clocks[i] = ck
        self.sems = {}
        for key, n in cnt.items():
            ch = self.CH if key in self.COMPUTE else self.CHD
            nch = (n + ch - 1) // ch
            self.sems[key] = [es.enter_context(nc.semaphore(f"s_{key}_{c}")) for c in range(nch)]
        per_eng = {e: [] for e in ("pe", "act", "dve", "pool", "sp")}
        for o in ops:
            per_eng[o.eng].append(o)
        block = es.enter_context(nc.Block())
        sems = self.sems
        CH = self.CH
        CHD = self.CHD

        def wait(eng, k, v):
            if k in self.COMPUTE:
                eng.wait_ge(sems[k][v // CH], v % CH + 1)
            else:
                c = v // CHD
                if c > 0:
                    eng.wait_ge(sems[k][c - 1], CHD * 16)
                eng.wait_ge(sems[k][c], (v % CHD + 1) * 16)

        def emit(eng, lst, finals):
            for o in lst:
                for k, v in o.waits.items():
                    wait(eng, k, v)
                inst = o.fn(eng)
                if o.sig:
                    key = o.stream if o.stream is not None else o.eng
                    if o.stream is not None:
                        inst.then_inc(sems[key][o.sigidx // CHD], 16)
                    else:
                        inst.then_inc(sems[key][o.sigidx // CH], 1)
            for k in cnt:
                if k not in self.COMPUTE and k.split("#")[0] in finals:
                    wait(eng, k, cnt[k] - 1)

        @block.sync
        def _(e):
            emit(e, per_eng["sp"], final_streams)

        @block.tensor
        def _(e):
            emit(e, per_eng["pe"], ())

        @block.scalar
        def _(e):
            emit(e, per_eng["act"], ())

        @block.vector
        def _(e):
            emit(e, per_eng["dve"], ())

        @block.gpsimd
        def _(e):
            emit(e, per_eng["pool"], ())
        return {k: len(v) for k, v in per_eng.items()}
D = 1024
SEQ = 4096
CTX = 256
T = SEQ + CTX
NT = T // 128
NTL = SEQ // 128
INW = 2576
NE = 32
EPS = 1e-6
NEG = -30000.0
NBMAX = (4 * T) // 128 + NE


def host_consts():
    import ml_dtypes
    c = {}
    p = np.arange(128)
    c["ident"] = np.eye(128, dtype=np.float32)
    c["ones"] = np.ones((128, 128), np.float32)
    same = (p[:, None] // 64) == (p[None, :] // 64)
    c["m_ls"] = np.where(same & (p[:, None] > p[None, :]), 0.0, NEG).astype(np.float32)
    c["m_li"] = np.where(same & (p[:, None] >= p[None, :]), 0.0, NEG).astype(np.float32)
    c["m_us"] = np.where(same & (p[:, None] < p[None, :]), 0.0, NEG).astype(np.float32)
    c["m_ui"] = np.where(same & (p[:, None] <= p[None, :]), 0.0, NEG).astype(np.float32)
    c["tri_f"] = (same & (p[:, None] <= p[None, :])).astype(np.float32)
    c["tri_b"] = (same & (p[:, None] >= p[None, :])).astype(np.float32)
    c["blk"] = same.astype(np.float32)
    c["tri_s"] = (p[:, None] < p[None, :]).astype(np.float32)
    tok = (np.arange(NT)[None, :] * 128 + p[:, None]).astype(np.int32)
    c["tokidf"] = tok.view(np.float32)
    c["widxbase"] = (np.arange(8)[None, :] * 128 + p[:, None]).astype(np.float32)
    c["blockval"] = (128.0 * (p[:, None] + 128 * np.arange(2)[None, :])).astype(np.float32)
    c["eidx"] = p[:, None].astype(np.float32)
    pr = np.zeros((128, NBMAX, 2), np.int32); pr[:, :, 0] = T
    c["padrec"] = pr.view(np.float32)
    ang = 2 * np.pi * np.outer(p, p) / 128.0
    c["cs128"] = np.concatenate([np.cos(ang), np.sin(ang)], axis=1).astype(np.float32)
    for nm, L in (("L", SEQ), ("C", CTX)):
        t = np.arange(L, dtype=np.int64)
        a = 2 * np.pi * ((np.outer(t, t) % L).astype(np.float64)) / L
        nrm = 1.0 / np.sqrt(L * 128.0)
        c["cos" + nm] = (np.cos(a) * nrm).astype(ml_dtypes.bfloat16)
        c["nsin" + nm] = (-np.sin(a) * nrm).astype(ml_dtypes.bfloat16)
    return c


CONST_SPECS = [("ident", [128, 128], "f"), ("ones", [128, 128], "f"), ("m_ls", [128, 128], "f"),
               ("m_li", [128, 128], "f"), ("m_us", [128, 128], "f"), ("m_ui", [128, 128], "f"),
               ("tri_f", [128, 128], "f"), ("tri_b", [128, 128], "f"), ("blk", [128, 128], "f"),
               ("cs128", [128, 256], "f"), ("tri_s", [128, 128], "f"), ("tokidf", [128, NT], "f"), ("widxbase", [128, 8], "f"),
               ("blockval", [128, 2], "f"), ("eidx", [128, 1], "f"), ("padrec", [128, NBMAX, 2], "f"), ("cosL", [SEQ, SEQ], "b"), ("nsinL", [SEQ, SEQ], "b"),
               ("cosC", [CTX, CTX], "b"), ("nsinC", [CTX, CTX], "b")]

W_SPECS = [("w_mod", [2, D, 6 * D]), ("b_mod", [2, 6 * D]), ("g_norm1", [2, D]), ("w_in", [2, D, INW]),
           ("conv_w", [2, 5, 1536]), ("a_log", [2, 8]), ("dt_bias", [2, 8]), ("g_out_norm", [2, 128]),
           ("w_out", [2, D, D]), ("g_norm2", [2, D]), ("w_router", [2, D, NE]), ("b_router", [2, NE]),
           ("w_gate", [2, NE, D, D]), ("b_gate", [2, NE, D]), ("w_up", [2, NE, D, D]), ("b_up", [2, NE, D]),
           ("w_down", [2, NE, D, D]), ("b_down", [2, NE, D]), ("g_final", [1, D])]


_UC = [0]


def usb(nc, name, shape, dt):
    _UC[0] += 1
    return nc.sbuf_tensor(f"{name}_{_UC[0]}", shape, dt)


def ups(nc, name, shape, dt):
    _UC[0] += 1
    return nc.psum_tensor(f"{name}_{_UC[0]}", shape, dt)


class Rot:
    def __init__(self, items):
        self.items = items
        self.i = 0

    def next(self):
        it = self.items[self.i % len(self.items)]
        self.i += 1
        return it


def build(stage=99, dbg=()):
    nc = bass.Bass("TRN2", target_bir_lowering=False)
    IN = {}

    def din(name, shape, dt=F32):
        IN[name] = nc.dram_tensor(name, shape, dt, kind="ExternalInput").ap()
        return IN[name]

    xin = din("xin", [T, D])
    cc = din("cc", [2, D])
    for nm, shp in W_SPECS:
        din(nm, shp)
    for nm, shp, k in CONST_SPECS:
        din(nm, shp, F32 if k == "f" else BF16)
    out = nc.dram_tensor("out", [SEQ, D], F32, kind="ExternalOutput").ap()

    def dscr(name, shape, dt=F32):
        kind = "ExternalOutput" if name in dbg else "Internal"
        return nc.dram_tensor(name, shape, dt, kind=kind).ap()

    xres = dscr("xres", [T, D])
    modv = dscr("modv", [2, 2, 6 * D])
    uT = dscr("uT", [INW, T])
    abd = dscr("abd", [T, 16])
    mixT = dscr("mixT", [D, T], BF16)
    h2rows = dscr("h2rows", [T + 1, D], BF16)
    yacc = dscr("yacc", [T + 1, D])
    slotrec = dscr("slotrec", [NBMAX * 128, 2])
    wbf = [[dscr(f"wbf{i}_{m}", [NE * 128, 8 * D], BF16) for m in range(3)] for i in range(2)]
    r_wbf = [Res(), Res()]

    def conv_thunks(l):
        th = []
        for ex in range(NE):
            for m, nm in enumerate(("w_gate", "w_up", "w_down")):
                def fn(l=l, ex=ex, m=m, nm=nm):
                    r0 = ex * 128
                    S.dma("pool", lambda e: e.dma_start(out=wbf[l][m][r0:r0 + 128, :], in_=IN[nm][l, ex].rearrange("(p j) f -> p (j f)", j=8), max_dma_last_dim=4096),
                          writes=[r_wbf[l]], stream="cv")
                th.append(fn)
        return th
    dbg_g = dscr("dbg_g", [T, NE]) if "dbg_g" in dbg else None
    dbg_oT = dscr("dbg_oT", [4, 128, T]) if "dbg_oT" in dbg else None

    es = ExitStack()
    with es:
        S = Sched(nc)
        gsb = lambda n, s, d: es.enter_context(usb(nc, n, s, d))
        ident = gsb("identS", [128, 128], F32); r_const = Res("const")
        ones = gsb("onesS", [128, 128], F32)
        identb = gsb("identb", [128, 128], BF16)
        S.dma("sp", lambda e: e.dma_start(out=ident[:], in_=IN["ident"]), writes=[r_const], stream="ld")
        S.dma("sp", lambda e: e.dma_start(out=ones[:], in_=IN["ones"]), writes=[r_const], stream="ld")
        S.op("dve", lambda e: e.tensor_copy(out=identb[:], in_=ident[:]), reads=[r_const], writes=[r_const])
        gates = gsb("gates", [128, NT, NE], F32); r_gates = [Res() for _ in range(NT)]

        phase0(nc, S, IN, modv, ident, ones, r_const)
        for l in range(2):
            if stage < 1:
                break
            nt_act = NT if l == 0 else NTL
            phase1(nc, S, IN, l, xin if l == 0 else xres, modv, uT, abd, ident, identb, ones, r_const)
            if stage < 2:
                break
            phase2(nc, S, IN, l, uT, mixT, r_const)
            if stage < 3:
                break
            phase3(nc, S, IN, l, uT, abd, mixT, ident, ones, r_const, dbg_oT if l == 0 else None, conv_thunks(l))
            if stage < 4:
                break
            phase4(nc, S, IN, l, xin if l == 0 else xres, xres, modv, mixT, h2rows, gates, r_gates, ident, ones, r_const, nt_act, dbg_g if l == 0 else None)
            if stage < 5:
                break
            phase5(nc, S, IN, l, xres, modv, h2rows, yacc, slotrec, gates, r_gates, ident, ones, r_const, nt_act, out, wbf[l], r_wbf[l])
            if stage < 6:
                break
        if stage < 6:
            with ExitStack() as ph:
                z = ph.enter_context(usb(nc, "zz", [128, D], F32)); rz = Res()
                S.barrier()
                S.op("dve", lambda e: e.memset(z[:], 0.0), writes=[rz])
                S.dma("sp", lambda e: e.dma_start(out=out[0:128, :], in_=z[:]), reads=[rz], stream="st")
        S.barrier()
        fin = ("st", "ld", "wc", "scr", "ig", "igs", "cv")
        stats = S.finalize(es, final_streams=fin)
    return nc, stats
def phase0(nc, S, IN, modv, ident, ones, r_const):
    S.barrier()
    with ExitStack() as ph:
        sb = lambda n, s, d: ph.enter_context(usb(nc, n, s, d))
        ccr = sb("p0_ccr", [2, D], F32); r_ccr = Res()
        scT = sb("p0_scT", [128, 8, 2], F32); r_scT = Res()
        bm = sb("p0_bm", [1, 2, 6 * D], F32); r_bm = Res()
        modsb = sb("p0_mod", [2, 2, 6 * D], F32); r_mod = Res()
        wbufs = Rot([(sb(f"p0_w{i}", [128, 8, 512], F32), Res()) for i in range(2)])
        pT = ph.enter_context(ups(nc, "p0_pT", [128, 8, 2], F32)); r_pT = Res()
        pss = Rot([(ph.enter_context(ups(nc, f"p0_ps{i}", [2, 512], F32)), Res()) for i in range(2)])
        S.dma("sp", lambda e: e.dma_start(out=ccr[:], in_=IN["cc"]), writes=[r_ccr], stream="ld")
        S.dma("sp", lambda e: e.dma_start(out=bm[:], in_=IN["b_mod"].rearrange("(o l) n -> o l n", o=1)), writes=[r_bm], stream="ld")
        S.op("act", lambda e: e.activation(out=ccr[:], in_=ccr[:], func=AF.Silu), reads=[r_ccr], writes=[r_ccr])
        for j in range(8):
            S.op("pe", lambda e, j=j: e.transpose(out=pT[:, j, :], in_=ccr[:, j * 128:(j + 1) * 128], identity=ident[0:2, 0:2]),
                 reads=[r_ccr, r_const], writes=[r_pT])
        S.op("dve", lambda e: e.tensor_copy(out=scT[:], in_=pT[:]), reads=[r_pT], writes=[r_scT])
        for l in range(2):
            wv = IN["w_mod"][l].rearrange("(j p) n -> p j n", p=128)
            for n in range(12):
                wt, r_w = wbufs.next()
                S.dma("sp", lambda e, wt=wt, n=n, wv=wv: e.dma_start(out=wt[:], in_=wv[:, :, n * 512:(n + 1) * 512]), writes=[r_w], stream="ld")
                pst, r_ps = pss.next()
                for j in range(8):
                    S.op("pe", lambda e, j=j, wt=wt, pst=pst: e.matmul(pst[:], lhsT=scT[:, j, :], rhs=wt[:, j, :], start=(j == 0), stop=False),
                         reads=[r_scT, r_w], writes=[r_ps])
                S.op("pe", lambda e, pst=pst, l=l, n=n: e.matmul(pst[:], lhsT=ones[0:1, 0:2], rhs=bm[0:1, l, n * 512:(n + 1) * 512], start=False, stop=True),
                     reads=[r_bm, r_const], writes=[r_ps])
                S.op("dve", lambda e, pst=pst, l=l, n=n: e.tensor_copy(out=modsb[:, l, n * 512:(n + 1) * 512], in_=pst[:]), reads=[r_ps], writes=[r_mod])
        S.dma("sp", lambda e: e.dma_start(out=modv.rearrange("l r n -> r l n"), in_=modsb[:]), reads=[r_mod], stream="scr")


def load_mod_bc(nc, S, ph, modv, l, r, k, name, extra_g=None, plus1=False, stream="ld"):
    t = ph.enter_context(usb(nc, name, [128, D], F32)); res = Res()
    S.dma("sp", lambda e: e.dma_start(out=t[:], in_=modv[l, r:r + 1, k * D:(k + 1) * D].to_broadcast([128, D])), writes=[res], stream=stream)
    if plus1:
        g = ph.enter_context(usb(nc, name + "_g", [128, D], F32)); rg = Res()
        S.dma("sp", lambda e: e.dma_start(out=g[:], in_=extra_g.to_broadcast([128, D])), writes=[rg], stream=stream)
        S.op("dve", lambda e: e.scalar_tensor_tensor(out=t[:], in0=t[:], scalar=1.0, in1=g[:], op0=ALU.add, op1=ALU.mult), reads=[res, rg], writes=[res])
    return t, res


def rstd_ops(S, xt, r_x, junk, r_junk, st, r_st):
    S.op("act", lambda e: e.activation(out=junk[:], in_=xt[:], func=AF.Square, accum_out=st[:, 0:1]), reads=[r_x], writes=[r_junk, r_st])
    S.op("dve", lambda e: e.tensor_scalar(out=st[:, 1:2], in0=st[:, 0:1], scalar1=1.0 / D, scalar2=EPS, op0=ALU.mult, op1=ALU.add), reads=[r_st], writes=[r_st])
    S.op("act", lambda e: e.sqrt(out=st[:, 2:3], in_=st[:, 1:2]), reads=[r_st], writes=[r_st])
    S.op("dve", lambda e: e.reciprocal(out=st[:, 3:4], in_=st[:, 2:3]), reads=[r_st], writes=[r_st])


def phase1(nc, S, IN, l, xsrc, modv, uT, abd, ident, identb, ones, r_const):
    S.barrier()
    with ExitStack() as ph:
        sb = lambda n, s, d: ph.enter_context(usb(nc, n, s, d))
        G1 = [None, None]; SH1 = [None, None]
        for r in range(2):
            G1[r] = load_mod_bc(nc, S, ph, modv, l, r, 1, f"p1_G{r}", extra_g=IN["g_norm1"][l:l + 1, :], plus1=True)
            SH1[r] = load_mod_bc(nc, S, ph, modv, l, r, 0, f"p1_SH{r}")
        winb = sb("p1_winb", [128, 8, INW], BF16); r_win = Res()
        wv = IN["w_in"][l].rearrange("(j p) n -> p j n", p=128)
        for j in range(8):
            for h in range(2):
                S.dma("pool", lambda e, j=j, h=h: e.dma_start(out=winb[:, j, h * 1288:(h + 1) * 1288], in_=wv[:, j, h * 1288:(h + 1) * 1288]),
                      writes=[r_win], stream="wc")
        xts = Rot([(sb(f"p1_x{i}", [128, D], F32), Res()) for i in range(3)])
        junk = sb("p1_junk", [128, D], BF16); r_junk = Res()
        sts = Rot([(sb(f"p1_st{i}", [128, 4], F32), Res()) for i in range(3)])
        t1s = Rot([(sb(f"p1_t1{i}", [128, D], F32), Res()) for i in range(2)])
        hxbs = Rot([(sb(f"p1_hxb{i}", [128, D], BF16), Res()) for i in range(2)])
        hxTs = Rot([(sb(f"p1_hxT{i}", [128, 8, 512], BF16), Res()) for i in range(2)])
        stg = Rot([(sb(f"p1_stg{i}", [128, 512], F32), Res()) for i in range(3)])
        abs_ = Rot([(sb(f"p1_ab{i}", [128, 16], F32), Res()) for i in range(2)])
        ptr = Rot([(ph.enter_context(ups(nc, f"p1_ptr{i}", [128, 8, 128], BF16)), Res()) for i in range(2)])
        pmm = Rot([(ph.enter_context(ups(nc, f"p1_pmm{i}", [128, 512], F32)), Res()) for i in range(4)])
        pab = Rot([(ph.enter_context(ups(nc, f"p1_pab{i}", [128, 16], F32)), Res()) for i in range(2)])
        blocks = [(b * 4, 4) for b in range(8)] + [(32, 2)]
        for (t0, ntl) in blocks:
            hxT, r_hxT = hxTs.next()
            ntok = ntl * 128
            for ti in range(ntl):
                t = t0 + ti
                r = 0 if t < NTL else 1
                xt, r_x = xts.next()
                S.dma("sp", lambda e, xt=xt, t=t: e.dma_start(out=xt[:], in_=xsrc[t * 128:(t + 1) * 128, :]), writes=[r_x], stream="ld")
                st, r_st = sts.next()
                rstd_ops(S, xt, r_x, junk, r_junk, st, r_st)
                t1, r_t1 = t1s.next()
                S.op("dve", lambda e, t1=t1, xt=xt, st=st, r=r: e.scalar_tensor_tensor(out=t1[:], in0=xt[:], scalar=st[:, 3:4], in1=G1[r][0][:], op0=ALU.mult, op1=ALU.mult),
                     reads=[r_x, r_st, G1[r][1]], writes=[r_t1])
                hxb, r_hxb = hxbs.next()
                S.op("pool", lambda e, hxb=hxb, t1=t1, r=r: e.tensor_tensor(out=hxb[:], in0=t1[:], in1=SH1[r][0][:], op=ALU.add),
                     reads=[r_t1, SH1[r][1]], writes=[r_hxb])
                pt, r_pt = ptr.next()
                for j in range(8):
                    S.op("pe", lambda e, pt=pt, hxb=hxb, j=j: e.transpose(out=pt[:, j, :], in_=hxb[:, j * 128:(j + 1) * 128], identity=identb[:]),
                         reads=[r_hxb, r_const], writes=[r_pt])
                S.op("act", lambda e, pt=pt, hxT=hxT, ti=ti: e.copy(out=hxT[:, :, ti * 128:(ti + 1) * 128], in_=pt[:]), reads=[r_pt], writes=[r_hxT])
                pa, r_pa = pab.next()
                for j in range(8):
                    S.op("pe", lambda e, pa=pa, hxT=hxT, ti=ti, j=j: e.matmul(pa[:], lhsT=hxT[:, j, ti * 128:(ti + 1) * 128], rhs=winb[:, j, 2560:2576], start=(j == 0), stop=(j == 7)),
                         reads=[r_hxT, r_win], writes=[r_pa])
                ab, r_ab = abs_.next()
                S.op("dve", lambda e, ab=ab, pa=pa: e.tensor_copy(out=ab[:], in_=pa[:]), reads=[r_pa], writes=[r_ab])
                S.dma("sp", lambda e, ab=ab, t=t: e.dma_start(out=abd[t * 128:(t + 1) * 128, :], in_=ab[:]), reads=[r_ab], stream="scr")
            for c in range(20):
                pm, r_pm = pmm.next()
                for j in range(8):
                    S.op("pe", lambda e, pm=pm, hxT=hxT, c=c, j=j, ntok=ntok: e.matmul(pm[:, 0:ntok], lhsT=winb[:, j, c * 128:(c + 1) * 128], rhs=hxT[:, j, 0:ntok], start=(j == 0), stop=(j == 7)),
                         reads=[r_hxT, r_win], writes=[r_pm])
                sg, r_sg = stg.next()
                eng = "act" if c % 2 == 0 else "dve"
                if eng == "act":
                    S.op("act", lambda e, sg=sg, pm=pm, ntok=ntok: e.copy(out=sg[:, 0:ntok], in_=pm[:, 0:ntok]), reads=[r_pm], writes=[r_sg])
                else:
                    S.op("dve", lambda e, sg=sg, pm=pm, ntok=ntok: e.tensor_copy(out=sg[:, 0:ntok], in_=pm[:, 0:ntok]), reads=[r_pm], writes=[r_sg])
                S.dma("sp", lambda e, sg=sg, c=c, t0=t0, ntok=ntok: e.dma_start(out=uT[c * 128:(c + 1) * 128, t0 * 128:t0 * 128 + ntok], in_=sg[:, 0:ntok]), reads=[r_sg], stream="scr")


def phase2(nc, S, IN, l, uT, mixT, r_const):
    S.barrier()
    with ExitStack() as ph:
        sb = lambda n, s, d: ph.enter_context(usb(nc, n, s, d))
        cs = sb("p2_cs", [128, 256], F32); r_cs = Res()
        S.dma("sp", lambda e: e.dma_start(out=cs[:], in_=IN["cs128"]), writes=[r_cs], stream="ld")
        ntiles = NT if l == 0 else NTL
        FCS = sb("p2_fcs", [128, NT, 4, 256], BF16); r_fcs = [Res() for _ in range(NT)]
        fts = Rot([(sb(f"p2_ft{i}", [128, 4, 128], F32), Res()) for i in range(3)])
        pas = Rot([(ph.enter_context(ups(nc, f"p2_pa{i}", [128, 4, 256], F32)), Res()) for i in range(2)])
        pos = Rot([(ph.enter_context(ups(nc, f"p2_po{i}", [128, 512], F32)), Res()) for i in range(4)])
        for t in range(ntiles):
            ft, r_ft = fts.next()
            S.dma("sp", lambda e, ft=ft, t=t: e.dma_start(out=ft[:], in_=uT[0:512, t * 128:(t + 1) * 128].rearrange("(g p) t -> p g t", p=128)), writes=[r_ft], stream="ld")
            pa, r_pa = pas.next()
            for g in range(4):
                S.op("pe", lambda e, pa=pa, ft=ft, g=g: e.matmul(pa[:, g, :], lhsT=ft[:, g, :], rhs=cs[:], start=True, stop=True), reads=[r_ft, r_cs], writes=[r_pa])
            if t % 2 == 0:
                S.op("act", lambda e, pa=pa, t=t: e.copy(out=FCS[:, t, :, :], in_=pa[:]), reads=[r_pa], writes=[r_fcs[t]])
            else:
                S.op("dve", lambda e, pa=pa, t=t: e.tensor_copy(out=FCS[:, t, :, :], in_=pa[:]), reads=[r_pa], writes=[r_fcs[t]])
        cosb = Rot([(sb(f"p2_cos{i}", [128, NTL, 256], BF16), Res()) for i in range(2)])
        sinb = Rot([(sb(f"p2_sin{i}", [128, NTL, 256], BF16), Res()) for i in range(2)])
        ostg = Rot([(sb(f"p2_os{i}", [128, 256], BF16), Res()) for i in range(3)])
        segs = [(0, NTL, "L")] + ([(NTL, 2, "C")] if l == 0 else [])
        for (t0, ntl, nm) in segs:
            cv = IN["cos" + nm].rearrange("(j p) k -> p j k", p=128)
            sv = IN["nsin" + nm].rearrange("(j p) k -> p j k", p=128)
            for kb in range(ntl * 128 // 256):
                cb, r_cb = cosb.next(); sn, r_sn = sinb.next()
                S.dma("sp", lambda e, cb=cb, kb=kb, cv=cv, ntl=ntl: e.dma_start(out=cb[:, 0:ntl, :], in_=cv[:, :, kb * 256:(kb + 1) * 256]), writes=[r_cb], stream="ld")
                S.dma("act", lambda e, sn=sn, kb=kb, sv=sv, ntl=ntl: e.dma_start(out=sn[:, 0:ntl, :], in_=sv[:, :, kb * 256:(kb + 1) * 256]), writes=[r_sn], stream="ld2")
                for g in range(4):
                    po, r_po = pos.next()
                    for j in range(ntl):
                        S.op("pe", lambda e, po=po, j=j, g=g, cb=cb, t0=t0: e.matmul(po[:, 0:256], lhsT=FCS[:, t0 + j, g, 0:128], rhs=cb[:, j, :], start=(j == 0), stop=False),
                             reads=[r_fcs[t0 + j], r_cb], writes=[r_po])
                        S.op("pe", lambda e, po=po, j=j, g=g, sn=sn, t0=t0, ntl=ntl: e.matmul(po[:, 0:256], lhsT=FCS[:, t0 + j, g, 128:256], rhs=sn[:, j, :], start=False, stop=(j == ntl - 1)),
                             reads=[r_fcs[t0 + j], r_sn], writes=[r_po])
                    og, r_og = ostg.next()
                    if g % 2 == 0:
                        S.op("act", lambda e, og=og, po=po: e.copy(out=og[:], in_=po[:, 0:256]), reads=[r_po], writes=[r_og])
                    else:
                        S.op("dve", lambda e, og=og, po=po: e.tensor_copy(out=og[:], in_=po[:, 0:256]), reads=[r_po], writes=[r_og])
                    S.dma("sp", lambda e, og=og, g=g, t0=t0, kb=kb: e.dma_start(out=mixT[g * 128:(g + 1) * 128, t0 * 128 + kb * 256:t0 * 128 + (kb + 1) * 256], in_=og[:]), reads=[r_og], stream="scr")


WC = T + 4


def tcol(t):
    return t * 128 + (4 if t >= NTL else 0)


def phase3(nc, S, IN, l, uT, abd, mixT, ident, ones, r_const, dbg_oT, bg=()):
    S.barrier()
    with ExitStack() as ph:
        sb = lambda n, s, d: ph.enter_context(usb(nc, n, s, d))
        pst = lambda n, s, d: ph.enter_context(ups(nc, n, s, d))
        r_c3 = Res()
        cm = {}
        for nm in ("m_ls", "m_li", "m_us", "m_ui", "tri_f", "tri_b", "blk"):
            cm[nm] = sb("p3_" + nm, [128, 128], F32)
            S.dma("sp", lambda e, nm=nm: e.dma_start(out=cm[nm][:], in_=IN[nm]), writes=[r_c3], stream="ld")
        cwr = sb("p3_cwr", [5, 1536], F32)
        gor = sb("p3_gor", [1, 128], F32)
        S.dma("sp", lambda e: e.dma_start(out=cwr[:], in_=IN["conv_w"][l]), writes=[r_c3], stream="ld")
        S.dma("sp", lambda e: e.dma_start(out=gor[:], in_=IN["g_out_norm"][l:l + 1, :]), writes=[r_c3], stream="ld")
        alb = sb("p3_alb", [128, 8], F32); dtb = sb("p3_dtb", [128, 8], F32)
        S.dma("sp", lambda e: e.dma_start(out=alb[:], in_=IN["a_log"][l:l + 1, :].to_broadcast([128, 8])), writes=[r_c3], stream="ld")
        S.dma("sp", lambda e: e.dma_start(out=dtb[:], in_=IN["dt_bias"][l:l + 1, :].to_broadcast([128, 8])), writes=[r_c3], stream="ld")
        banks = [pst(f"p3_bank{i}", [128, 512], F32) for i in range(8)]
        qtile = lambda b, q: banks[b][:, q * 128:(q + 1) * 128]
        rbank = [Res(excl=True) for _ in range(8)]
        pcw = banks[0][:, 0:104].rearrange("p (m k) -> p m k", k=8); r_pcw = rbank[0]
        cw = sb("p3_cw", [128, 13, 8], F32)
        for m in range(12):
            S.op("pe", lambda e, m=m: e.transpose(out=pcw[:, m, 0:5], in_=cwr[:, m * 128:(m + 1) * 128], identity=ident[0:5, 0:5]), reads=[r_c3, r_const], writes=[r_pcw])
        S.op("pe", lambda e: e.transpose(out=pcw[:, 12, 0:1], in_=gor[:, :], identity=ident[0:1, 0:1]), reads=[r_c3, r_const], writes=[r_pcw])
        r_cw = Res()
        S.op("dve", lambda e: e.memset(cw[:], 0.0), writes=[r_cw])
        for m in range(12):
            S.op("dve", lambda e, m=m: e.tensor_copy(out=cw[:, m, 0:5], in_=pcw[:, m, 0:5]), reads=[r_pcw], writes=[r_cw])
        S.op("dve", lambda e: e.tensor_copy(out=cw[:, 12, 0:1], in_=pcw[:, 12, 0:1]), reads=[r_pcw], writes=[r_cw])
        nea = sb("p3_nea", [128, 8], F32)
        S.op("act", lambda e: e.activation(out=nea[:], in_=alb[:], func=AF.Exp), reads=[r_c3], writes=[r_c3])
        S.op("dve", lambda e: e.tensor_scalar(out=nea[:], in0=nea[:], scalar1=-1.0, scalar2=None, op0=ALU.mult), reads=[r_c3], writes=[r_c3])
        names = ("BT", "NBT", "GAM", "EG", "BEG", "EK0", "EK1")
        GA = {nm: sb("p3_" + nm, [128, NT, 8], F32) for nm in names}
        r_ga = [Res() for _ in range(NT)]
        abt = Rot([(sb(f"p3_abt{i}", [128, 16], F32), Res()) for i in range(2)])
        tmp = Rot([(sb(f"p3_gt{i}", [128, 4, 8], F32), Res()) for i in range(2)])
        pgs = Rot([(banks[0][:, 128:144], r_pcw)])
        rowm = sb("p3_rowm", [128, 2], F32)
        S.op("dve", lambda e: e.tensor_copy(out=rowm[:, 0:1], in_=cm["blk"][:, 0:1]), reads=[r_c3], writes=[r_c3])
        S.op("dve", lambda e: e.tensor_copy(out=rowm[:, 1:2], in_=cm["blk"][:, 127:128]), reads=[r_c3], writes=[r_c3])
        for t in range(NT):
            ab, r_ab = abt.next()
            S.dma("sp", lambda e, ab=ab, t=t: e.dma_start(out=ab[:], in_=abd[t * 128:(t + 1) * 128, :]), writes=[r_ab], stream="ld")
            tm, r_tm = tmp.next()
            abv = ab[:].rearrange("p (d k h) -> p d k h", d=2, k=2)
            X = tm[:, 0, :].rearrange("p (d h) -> p d h", d=2)
            S.op("dve", lambda e, X=X, abv=abv: e.tensor_tensor(out=X, in0=abv[:, :, 0, :], in1=dtb[:].rearrange("p (d h) -> p d h", d=2), op=ALU.add), reads=[r_ab, r_c3], writes=[r_tm])
            S.op("act", lambda e, tm=tm: e.activation(out=tm[:, 0, :], in_=tm[:, 0, :], func=AF.Exp), reads=[r_tm], writes=[r_tm])
            S.op("act", lambda e, tm=tm: e.activation(out=tm[:, 0, :], in_=tm[:, 0, :], func=AF.Ln, bias=1.0), reads=[r_tm], writes=[r_tm])
            S.op("dve", lambda e, tm=tm: e.tensor_tensor(out=tm[:, 1, :], in0=tm[:, 0, :], in1=nea[:], op=ALU.mult), reads=[r_tm, r_c3], writes=[r_tm])
            B = tm[:, 2, :].rearrange("p (d h) -> p d h", d=2)
            S.op("act", lambda e, B=B, abv=abv: e.activation(out=B, in_=abv[:, :, 1, :], func=AF.Exp, scale=-1.0), reads=[r_ab], writes=[r_tm])
            S.op("dve", lambda e, tm=tm: e.tensor_scalar(out=tm[:, 2, :], in0=tm[:, 2, :], scalar1=1.0, scalar2=None, op0=ALU.add), reads=[r_tm], writes=[r_tm])
            S.op("dve", lambda e, tm=tm, t=t: e.reciprocal(out=GA["BT"][:, t, :], in_=tm[:, 2, :]), reads=[r_tm], writes=[r_ga[t]])
            S.op("dve", lambda e, t=t: e.tensor_scalar(out=GA["NBT"][:, t, :], in0=GA["BT"][:, t, :], scalar1=-1.0, scalar2=None, op0=ALU.mult), reads=[r_ga[t]], writes=[r_ga[t]])
            pg, r_pg = pgs.next()
            S.op("pe", lambda e, pg=pg, tm=tm: e.matmul(pg[:, 0:4], lhsT=cm["tri_f"][:], rhs=tm[:, 1, 0:4], start=True, stop=True), reads=[r_tm, r_c3], writes=[r_pg])
            S.op("pe", lambda e, pg=pg, tm=tm: e.matmul(pg[:, 4:8], lhsT=cm["tri_b"][:], rhs=tm[:, 1, 4:8], start=True, stop=True), reads=[r_tm, r_c3], writes=[r_pg])
            S.op("pe", lambda e, pg=pg, tm=tm: e.matmul(pg[:, 8:16], lhsT=cm["blk"][:], rhs=tm[:, 1, :], start=True, stop=True), reads=[r_tm, r_c3], writes=[r_pg])
            S.op("dve", lambda e, pg=pg, t=t: e.tensor_copy(out=GA["GAM"][:, t, :], in_=pg[:, 0:8]), reads=[r_pg], writes=[r_ga[t]])
            S.op("act", lambda e, pg=pg, t=t: e.activation(out=GA["EG"][:, t, :], in_=pg[:, 0:8], func=AF.Exp), reads=[r_pg], writes=[r_ga[t]])
            S.op("dve", lambda e, t=t: e.tensor_tensor(out=GA["BEG"][:, t, :], in0=GA["EG"][:, t, :], in1=GA["BT"][:, t, :], op=ALU.mult), reads=[r_ga[t]], writes=[r_ga[t]])
            S.op("dve", lambda e, pg=pg, tm=tm, t=t: e.tensor_tensor(out=tm[:, 3, :], in0=pg[:, 8:16], in1=GA["GAM"][:, t, :], op=ALU.subtract), reads=[r_pg, r_ga[t]], writes=[r_tm])
            S.op("act", lambda e, tm=tm: e.activation(out=tm[:, 3, :], in_=tm[:, 3, :], func=AF.Exp), reads=[r_tm], writes=[r_tm])
            S.op("dve", lambda e, tm=tm, t=t: e.tensor_scalar(out=GA["EK0"][:, t, :], in0=tm[:, 3, :], scalar1=rowm[:, 0:1], scalar2=None, op0=ALU.mult), reads=[r_tm, r_c3], writes=[r_ga[t]])
            S.op("dve", lambda e, tm=tm, t=t: e.tensor_scalar(out=GA["EK1"][:, t, :], in0=tm[:, 3, :], scalar1=rowm[:, 1:2], scalar2=None, op0=ALU.mult), reads=[r_tm, r_c3], writes=[r_ga[t]])
        raws = Rot([(sb(f"p3_raw{i}", [128, WC + 4], F32), Res()) for i in range(2)])
        QKV = [(sb(f"p3_qkv{i}", [128, WC], F32), Res()) for i in range(3)]
        oT = sb("p3_oT", [128, WC], F32); r_oT = [Res() for _ in range(NT)]
        r_oTall = Res()
        pn = banks[1]; r_pn = rbank[1]
        rns = Rot([(sb(f"p3_rn{i}", [128, 512], F32), Res()) for i in range(2)])
        zts = Rot([(sb(f"p3_z{i}", [128, 512], F32), Res()) for i in range(2)])
        obs = Rot([(sb(f"p3_ob{i}", [128, 512], BF16), Res()) for i in range(2)])

        KSLOT = 4; DEPTH = 3
        INTER = ("dg", "Dm", "E1", "E2", "N", "NTs", "TT", "Pa", "PTa", "Pb", "PTb", "Rv", "Rw")
        OUTS = ("EGr", "at", "u", "wT", "qg", "ke0", "ke1")
        BF_NAMES = ("Nb", "NTs", "TT", "Pa", "PTa", "Pb", "PTb", "Rv", "Rw")
        BI = [{n: (sb(f"p3_{n}_s{s}", [128, 128], BF16 if n in BF_NAMES else F32), Res()) for n in INTER + ("Nb",)} for s in range(KSLOT)]
        BO = [{n: Rot([(sb(f"p3_{n}_d{d}_{i}", [128, 128], F32), Res()) for i in range(DEPTH)]) for n in OUTS} for d in range(2)]
        VN = [Rot([(sb(f"p3_vn{d}_{i}", [128, 128], F32), Res()) for i in range(2)]) for d in range(2)]
        rbank_ = rbank
        slot_bank = (2, 3, 4, 7)
        PQ = [Rot([(qtile(slot_bank[s], qi), rbank_[slot_bank[s]]) for qi in range(4)]) for s in range(KSLOT)]
        PSC = {d: {n: (qtile(5 + d, qi), rbank_[5 + d]) for qi, n in enumerate(("ps1", "po", "pS"))} for d in range(2)}
        Sst = [(sb(f"p3_S{d}", [128, 128], F32), Res()) for d in range(2)]
        bg = list(bg)
        chunks9 = [(i * 512, 512) for i in range(8)] + [(4100, 256)]

        LVL = 9; NTI = NT
        for h in range(4 if LVL >= 9 else (1 if LVL >= 1 else 0)):
            for which in range(3):
                raw, r_raw = raws.next()
                row0 = 512 + which * 512 + h * 128
                S.op("pool", lambda e, raw=raw: e.memset(raw[:], 0.0), writes=[r_raw])
                S.dma("sp", lambda e, raw=raw, row0=row0: e.dma_start(out=raw[:, 2:2 + SEQ], in_=uT[row0:row0 + 128, 0:SEQ]), writes=[r_raw], stream="ld")
                S.dma("sp", lambda e, raw=raw, row0=row0: e.dma_start(out=raw[:, SEQ + 6:SEQ + 6 + CTX], in_=uT[row0:row0 + 128, SEQ:T]), writes=[r_raw], stream="ld")
                dst, r_dst = QKV[which]
                m = which * 4 + h
                S.op("dve", lambda e, dst=dst, raw=raw, m=m: e.tensor_scalar(out=dst[:], in0=raw[:, 0:WC], scalar1=cw[:, m, 0:1], scalar2=None, op0=ALU.mult), reads=[r_raw, r_cw], writes=[r_dst])
                for k in range(1, 5):
                    S.op("dve", lambda e, dst=dst, raw=raw, m=m, k=k: e.scalar_tensor_tensor(out=dst[:], in0=raw[:, k:k + WC], scalar=cw[:, m, k:k + 1], in1=dst[:], op0=ALU.mult, op1=ALU.add),
                         reads=[r_raw, r_cw, r_dst], writes=[r_dst])
                S.op("act", lambda e, dst=dst: e.activation(out=dst[:], in_=dst[:], func=AF.Silu), reads=[r_dst], writes=[r_dst])
                if which < 2:
                    sq, r_sq = raws.items[(raws.i) % 2]
                    S.op("pool", lambda e, sq=sq, dst=dst: e.tensor_tensor(out=sq[:, 0:WC], in0=dst[:], in1=dst[:], op=ALU.mult), reads=[r_dst], writes=[r_sq])
                    for (c0, cn) in chunks9:
                        S.op("pe", lambda e, sq=sq, c0=c0, cn=cn: e.matmul(pn[:, 0:cn], lhsT=ones[:], rhs=sq[:, c0:c0 + cn], start=True, stop=True), reads=[r_sq, r_const], writes=[r_pn])
                        rn, r_rn = rns.next()
                        S.op("dve", lambda e, rn=rn, cn=cn: e.tensor_scalar(out=rn[:, 0:cn], in0=pn[:, 0:cn], scalar1=EPS, scalar2=None, op0=ALU.add), reads=[r_pn], writes=[r_rn])
                        S.op("act", lambda e, rn=rn, cn=cn: e.sqrt(out=rn[:, 0:cn], in_=rn[:, 0:cn]), reads=[r_rn], writes=[r_rn])
                        S.op("dve", lambda e, rn=rn, cn=cn: e.reciprocal(out=rn[:, 0:cn], in_=rn[:, 0:cn]), reads=[r_rn], writes=[r_rn])
                        sc = (128.0 ** -0.5) if which == 0 else 1.0
                        S.op("dve", lambda e, rn=rn, dst=dst, c0=c0, cn=cn, sc=sc: e.scalar_tensor_tensor(out=dst[:, c0:c0 + cn], in0=dst[:, c0:c0 + cn], scalar=sc, in1=rn[:, 0:cn], op0=ALU.mult, op1=ALU.mult),
                             reads=[r_rn, r_dst], writes=[r_dst])
            qT, r_q = QKV[0]; kT, r_k = QKV[1]; vT, r_v = QKV[2]
            for d in range(2):
                S.op("dve", lambda e, d=d: e.memset(Sst[d][0][:], 0.0), writes=[Sst[d][1]])
            seqs = [[32, 33] + list(range(32)), [33, 32] + list(range(31, -1, -1))]
            if LVL < 2:
                seqs = [[], []]
            else:
                seqs = [s_[:NTI] for s_ in seqs]
            written = set()
            PREP = {}
            scanned = [0, 0]

            def prep_gen(t, d, s):
                c0 = tcol(t); col = d * 4 + h
                bi = BI[s]; pq = PQ[s]
                ksl = kT[:, c0:c0 + 128]; qsl = qT[:, c0:c0 + 128]; vsl = vT[:, c0:c0 + 128]
                gam = GA["GAM"][:, t, col:col + 1]
                dg, r_dg = bi["dg"]; Dm, r_Dm = bi["Dm"]; E1, r_E1 = bi["E1"]; E2, r_E2 = bi["E2"]
                EGr, r_EGr = BO[d]["EGr"].next()
                gr, r_gr = pq.next()
                S.op("dve", lambda e: e.tensor_scalar(out=dg[:], in0=ident[:], scalar1=gam, scalar2=None, op0=ALU.mult), reads=[r_const, r_ga[t]], writes=[r_dg])
                S.op("pe", lambda e: e.matmul(gr[:], lhsT=ones[:], rhs=dg[:], start=True, stop=True), reads=[r_dg, r_const], writes=[r_gr])
                S.op("dve", lambda e: e.tensor_scalar(out=Dm[:], in0=gr[:], scalar1=-1.0, scalar2=gam, op0=ALU.mult, op1=ALU.add), reads=[r_gr, r_ga[t]], writes=[r_Dm])
                S.op("act", lambda e: e.activation(out=EGr[:], in_=gr[:], func=AF.Exp), reads=[r_gr], writes=[r_EGr])
                yield
                m1 = cm["m_ls"] if d == 0 else cm["m_us"]
                m2 = cm["m_ui"] if d == 0 else cm["m_li"]
                S.op("pool", lambda e: e.tensor_tensor(out=E1[:], in0=Dm[:], in1=m1[:], op=ALU.add), reads=[r_Dm, r_c3], writes=[r_E1])
                S.op("pool", lambda e: e.tensor_tensor(out=E2[:], in0=m2[:], in1=Dm[:], op=ALU.subtract), reads=[r_Dm, r_c3], writes=[r_E2])
                S.op("act", lambda e: e.activation(out=E1[:], in_=E1[:], func=AF.Exp), reads=[r_E1], writes=[r_E1])
                S.op("act", lambda e: e.activation(out=E2[:], in_=E2[:], func=AF.Exp), reads=[r_E2], writes=[r_E2])
                yield
                N, r_N = bi["N"]; at, r_at = BO[d]["at"].next()
                nbt = GA["NBT"][:, t, col:col + 1]
                kk, r_kk = pq.next()
                S.op("pe", lambda e: e.matmul(kk[:], lhsT=ksl, rhs=ksl, start=True, stop=True), reads=[r_k], writes=[r_kk])
                S.op("dve", lambda e: e.scalar_tensor_tensor(out=N[:], in0=kk[:], scalar=nbt, in1=E1[:], op0=ALU.mult, op1=ALU.mult), reads=[r_kk, r_ga[t], r_E1], writes=[r_N])
                Nb, r_Nb = bi["Nb"]
                S.op("act", lambda e: e.copy(out=Nb[:], in_=N[:]), reads=[r_N], writes=[r_Nb])
                kq, r_kq = pq.next()
                S.op("pe", lambda e: e.matmul(kq[:], lhsT=ksl, rhs=qsl, start=True, stop=True), reads=[r_k, r_q], writes=[r_kq])
                S.op("dve", lambda e: e.tensor_tensor(out=at[:], in0=kq[:], in1=E2[:], op=ALU.mult), reads=[r_kq, r_E2], writes=[r_at])
                yield
                Rv, r_Rv = bi["Rv"]; Rw, r_Rw = bi["Rw"]
                ke0, r_ke0 = BO[d]["ke0"].next(); ke1, r_ke1 = BO[d]["ke1"].next(); qg, r_qg = BO[d]["qg"].next()
                bt = GA["BT"][:, t, col:col + 1]; beg = GA["BEG"][:, t, col:col + 1]
                kt, r_kt = pq.next()
                S.op("pe", lambda e: e.transpose(out=kt[:], in_=ksl, identity=ident[:]), reads=[r_k, r_const], writes=[r_kt])
                S.op("act", lambda e: e.activation(out=Rw[:], in_=kt[:], func=AF.Copy, scale=beg), reads=[r_kt, r_ga[t]], writes=[r_Rw])
                S.op("dve", lambda e: e.tensor_scalar(out=ke0[:], in0=kt[:], scalar1=GA["EK0"][:, t, col:col + 1], scalar2=None, op0=ALU.mult), reads=[r_kt, r_ga[t]], writes=[r_ke0])
                S.op("dve", lambda e: e.tensor_scalar(out=ke1[:], in0=kt[:], scalar1=GA["EK1"][:, t, col:col + 1], scalar2=None, op0=ALU.mult), reads=[r_kt, r_ga[t]], writes=[r_ke1])
                vt, r_vt = pq.next()
                S.op("pe", lambda e: e.transpose(out=vt[:], in_=vsl, identity=ident[:]), reads=[r_v, r_const], writes=[r_vt])
                S.op("act", lambda e: e.activation(out=Rv[:], in_=vt[:], func=AF.Copy, scale=bt), reads=[r_vt, r_ga[t]], writes=[r_Rv])
                S.op("pool", lambda e: e.tensor_tensor(out=qg[:], in0=qsl, in1=EGr[:], op=ALU.mult), reads=[r_q, r_EGr], writes=[r_qg])
                yield
                NTs, r_NTs = bi["NTs"]; TT, r_TT = bi["TT"]
                ntp, r_ntp = pq.next()
                S.op("pe", lambda e: e.transpose(out=ntp[:], in_=N[:], identity=ident[:]), reads=[r_N, r_const], writes=[r_ntp])
                S.op("act", lambda e: e.copy(out=NTs[:], in_=ntp[:]), reads=[r_ntp], writes=[r_NTs])
                S.op("dve", lambda e: e.tensor_tensor(out=TT[:], in0=ntp[:], in1=ident[:], op=ALU.add), reads=[r_ntp, r_const], writes=[r_TT])
                yield
                P_, r_P = Nb, r_Nb
                PT_, r_PT = NTs, r_NTs
                for lev in range(1, 6):
                    p2, r_p2 = pq.next()
                    S.op("pe", lambda e, p2=p2, PT_=PT_, P_=P_: e.matmul(p2[:], lhsT=PT_[:], rhs=P_[:], start=True, stop=True), reads=[r_P, r_PT], writes=[r_p2])
                    nP, r_nP = bi["Pa" if lev % 2 else "Pb"]
                    S.op("act", lambda e, nP=nP, p2=p2: e.copy(out=nP[:], in_=p2[:]), reads=[r_p2], writes=[r_nP])
                    if lev < 5:
                        pt2, r_pt2 = pq.next()
                        S.op("pe", lambda e, pt2=pt2, PT_=PT_, P_=P_: e.matmul(pt2[:], lhsT=P_[:], rhs=PT_[:], start=True, stop=True), reads=[r_P, r_PT], writes=[r_pt2])
                        nPT, r_nPT = bi["PTa" if lev % 2 else "PTb"]
                        S.op("dve", lambda e, nPT=nPT, pt2=pt2: e.tensor_copy(out=nPT[:], in_=pt2[:]), reads=[r_pt2], writes=[r_nPT])
                    yield
                    up, r_up = pq.next()
                    S.op("pe", lambda e, up=up, nP=nP: e.matmul(up[:], lhsT=nP[:], rhs=TT[:], start=True, stop=True), reads=[r_nP, r_TT], writes=[r_up])
                    S.op("dve", lambda e, up=up: e.tensor_tensor(out=TT[:], in0=up[:], in1=TT[:], op=ALU.add), reads=[r_up, r_TT], writes=[r_TT])
                    P_, r_P = nP, r_nP
                    if lev < 5:
                        PT_, r_PT = nPT, r_nPT
                    yield
                u, r_u = BO[d]["u"].next(); wT, r_wT = BO[d]["wT"].next()
                pu, r_pu = pq.next()
                S.op("pe", lambda e: e.matmul(pu[:], lhsT=TT[:], rhs=Rv[:], start=True, stop=True), reads=[r_TT, r_Rv], writes=[r_pu])
                S.op("act", lambda e: e.copy(out=u[:], in_=pu[:]), reads=[r_pu], writes=[r_u])
                pw, r_pw = pq.next()
                S.op("pe", lambda e: e.matmul(pw[:], lhsT=Rw[:], rhs=TT[:], start=True, stop=True), reads=[r_TT, r_Rw], writes=[r_pw])
                S.op("dve", lambda e: e.tensor_copy(out=wT[:], in_=pw[:]), reads=[r_pw], writes=[r_wT])
                PREP[(t, d)] = dict(EGr=(EGr, r_EGr), at=(at, r_at), u=(u, r_u), wT=(wT, r_wT), qg=(qg, r_qg), ke0=(ke0, r_ke0), ke1=(ke1, r_ke1))

            def scan_gen(d):
                Sd, r_S = Sst[d]
                for t in seqs[d]:
                    while (t, d) not in PREP:
                        yield "wait"
                    pr = PREP[(t, d)]
                    c0 = tcol(t)
                    EGr, r_EGr = pr["EGr"]; at, r_at = pr["at"]; u, r_u = pr["u"]; wT, r_wT = pr["wT"]; qg, r_qg = pr["qg"]
                    for c in ((0, 1) if d == 0 else (1, 0)):
                        cs_ = slice(c * 64, (c + 1) * 64)
                        gcol = c * 64 + (63 if d == 0 else 0)
                        ps1, r_ps1 = PSC[d]["ps1"]; po, r_po = PSC[d]["po"]; pS, r_pS = PSC[d]["pS"]
                        vn, r_vn = VN[d].next()
                        ke, r_ke = pr["ke0"] if c == 0 else pr["ke1"]
                        S.op("pe", lambda e, wT=wT: e.matmul(ps1[:], lhsT=wT[:], rhs=Sd[:], start=True, stop=True), reads=[r_wT, r_S], writes=[r_ps1])
                        yield
                        S.op("dve", lambda e, vn=vn, u=u: e.tensor_tensor(out=vn[:], in0=u[:], in1=ps1[:], op=ALU.subtract), reads=[r_u, r_ps1], writes=[r_vn])
                        yield
                        S.op("pe", lambda e, qg=qg, cs_=cs_: e.matmul(po[:, 0:64], lhsT=Sd[:], rhs=qg[:, cs_], start=True, stop=False), reads=[r_S, r_qg], writes=[r_po])
                        S.op("pe", lambda e, vn=vn, at=at, cs_=cs_: e.matmul(po[:, 0:64], lhsT=vn[:], rhs=at[:, cs_], start=False, stop=True), reads=[r_vn, r_at], writes=[r_po])
                        S.op("pe", lambda e, ke=ke, vn=vn: e.matmul(pS[:], lhsT=ke[:], rhs=vn[:], start=True, stop=True), reads=[r_ke, r_vn], writes=[r_pS])
                        yield
                        osl = oT[:, c0 + c * 64:c0 + (c + 1) * 64]
                        if (t, c) not in written:
                            written.add((t, c))
                            S.op("act", lambda e, osl=osl: e.copy(out=osl, in_=po[:, 0:64]), reads=[r_po], writes=[r_oT[t]])
                        else:
                            S.op("dve", lambda e, osl=osl: e.tensor_tensor(out=osl, in0=po[:, 0:64], in1=osl, op=ALU.add), reads=[r_po, r_oT[t]], writes=[r_oT[t]])
                        S.op("dve", lambda e, EGr=EGr, gcol=gcol: e.scalar_tensor_tensor(out=Sd[:], in0=Sd[:], scalar=EGr[:, gcol:gcol + 1], in1=pS[:], op0=ALU.mult, op1=ALU.add),
                             reads=[r_S, r_EGr, r_pS], writes=[r_S])
                        yield
                    scanned[d] += 1

            queue = []
            for i in range(len(seqs[0])):
                for d in range(2):
                    queue.append((seqs[d][i], d, i))
            active = {}
            scans = [scan_gen(0), scan_gen(1)]
            scan_done = [len(seqs[0]) == 0, len(seqs[1]) == 0]
            nbg = 0
            SCANFIRST = 1; SCANSTEPS = 2

            def adv_scans():
                pr_ = False
                for d in range(2):
                    for _ in range(SCANSTEPS):
                        if not scan_done[d]:
                            try:
                                r_ = next(scans[d])
                                if r_ != "wait":
                                    pr_ = True
                                else:
                                    break
                            except StopIteration:
                                scan_done[d] = True; pr_ = True
                return pr_
            while not all(scan_done):
                progressed = False
                if SCANFIRST:
                    progressed = adv_scans() or progressed
                while queue and len(active) < KSLOT and (queue[0][2] - scanned[queue[0][1]] < DEPTH):
                    t_, d_, i_ = queue.pop(0)
                    s_ = [x for x in range(KSLOT) if x not in active][0]
                    active[s_] = prep_gen(t_, d_, s_)
                    progressed = True
                    nbg += 1
                    if bg and nbg % 2 == 0:
                        bg.pop(0)()
                for s_ in list(active.keys()):
                    try:
                        next(active[s_]); progressed = True
                    except StopIteration:
                        del active[s_]; progressed = True
                if not SCANFIRST:
                    progressed = adv_scans() or progressed
                assert progressed, "phase3 scheduler stuck"
            if dbg_oT is not None:
                S.dma("sp", lambda e, h=h: e.dma_start(out=dbg_oT[h, :, 0:SEQ], in_=oT[:, 0:SEQ]), reads=r_oT, stream="scr")
                S.dma("sp", lambda e, h=h: e.dma_start(out=dbg_oT[h, :, SEQ:T], in_=oT[:, SEQ + 4:SEQ + 4 + CTX]), reads=r_oT, stream="scr")
            sq, r_sq = raws.next()
            for ci, (c0, cn) in enumerate(chunks9):
                tiles = list(range(ci * 4, ci * 4 + 4)) if ci < 8 else [32, 33]
                rds = [r_oT[t] for t in tiles]
                S.op("pool", lambda e, sq=sq, c0=c0, cn=cn: e.tensor_tensor(out=sq[:, c0:c0 + cn], in0=oT[:, c0:c0 + cn], in1=oT[:, c0:c0 + cn], op=ALU.mult), reads=rds, writes=[r_sq])
                S.op("pe", lambda e, sq=sq, c0=c0, cn=cn: e.matmul(pn[:, 0:cn], lhsT=ones[:], rhs=sq[:, c0:c0 + cn], start=True, stop=True), reads=[r_sq, r_const], writes=[r_pn])
                rn, r_rn = rns.next()
                S.op("dve", lambda e, rn=rn, cn=cn: e.tensor_scalar(out=rn[:, 0:cn], in0=pn[:, 0:cn], scalar1=1.0 / 128, scalar2=EPS, op0=ALU.mult, op1=ALU.add), reads=[r_pn], writes=[r_rn])
                S.op("act", lambda e, rn=rn, cn=cn: e.sqrt(out=rn[:, 0:cn], in_=rn[:, 0:cn]), reads=[r_rn], writes=[r_rn])
                S.op("dve", lambda e, rn=rn, cn=cn: e.reciprocal(out=rn[:, 0:cn], in_=rn[:, 0:cn]), reads=[r_rn], writes=[r_rn])
                S.op("dve", lambda e, rn=rn, c0=c0, cn=cn: e.scalar_tensor_tensor(out=rn[:, 0:cn], in0=oT[:, c0:c0 + cn], scalar=cw[:, 12, 0:1], in1=rn[:, 0:cn], op0=ALU.mult, op1=ALU.mult),
                     reads=rds + [r_rn, r_cw], writes=[r_rn])
                zt, r_zt = zts.next()
                tok0 = ci * 512 if ci < 8 else SEQ
                zrow = 2048 + h * 128
                S.dma("sp", lambda e, zt=zt, tok0=tok0, cn=cn, zrow=zrow: e.dma_start(out=zt[:, 0:cn], in_=uT[zrow:zrow + 128, tok0:tok0 + cn]), writes=[r_zt], stream="ld")
                S.op("act", lambda e, zt=zt, cn=cn: e.activation(out=zt[:, 0:cn], in_=zt[:, 0:cn], func=AF.Silu), reads=[r_zt], writes=[r_zt])
                ob, r_ob = obs.next()
                S.op("pool", lambda e, ob=ob, rn=rn, zt=zt, cn=cn: e.tensor_tensor(out=ob[:, 0:cn], in0=rn[:, 0:cn], in1=zt[:, 0:cn], op=ALU.mult), reads=[r_rn, r_zt], writes=[r_ob])
                S.dma("sp", lambda e, ob=ob, tok0=tok0, cn=cn, h=h: e.dma_start(out=mixT[512 + h * 128:512 + (h + 1) * 128, tok0:tok0 + cn], in_=ob[:, 0:cn]), reads=[r_ob], stream="scr")
        while bg:
            bg.pop(0)()


def phase4(nc, S, IN, l, xsrc, xres, modv, mixT, h2rows, gates, r_gates, ident, ones, r_const, nt_act, dbg_g):
    S.barrier()
    with ExitStack() as ph:
        sb = lambda n, s, d: ph.enter_context(usb(nc, n, s, d))
        pst = lambda n, s, d: ph.enter_context(ups(nc, n, s, d))
        nr = 2 if nt_act > NTL else 1
        GT1 = [load_mod_bc(nc, S, ph, modv, l, r, 2, f"p4_GT{r}") for r in range(nr)]
        G2 = [load_mod_bc(nc, S, ph, modv, l, r, 4, f"p4_G{r}", extra_g=IN["g_norm2"][l:l + 1, :], plus1=True) for r in range(nr)]
        SH2 = [load_mod_bc(nc, S, ph, modv, l, r, 3, f"p4_SH{r}") for r in range(nr)]
        woutb = sb("p4_wout", [128, 8, D], BF16); r_wo = Res()
        wv = IN["w_out"][l].rearrange("(j p) n -> p j n", p=128)
        for j in range(8):
            S.dma("pool", lambda e, j=j: e.dma_start(out=woutb[:, j, :], in_=wv[:, j, :]), writes=[r_wo], stream="wc")
        wrf = sb("p4_wr", [128, 8, NE], F32); r_wr = Res()
        brr = sb("p4_br", [1, NE], F32)
        S.dma("sp", lambda e: e.dma_start(out=wrf[:], in_=IN["w_router"][l].rearrange("(j p) n -> p j n", p=128)), writes=[r_wr], stream="ld")
        S.dma("sp", lambda e: e.dma_start(out=brr[:], in_=IN["b_router"][l:l + 1, :]), writes=[r_wr], stream="ld")
        mixs = Rot([(sb(f"p4_mx{i}", [128, 8, 128], BF16), Res()) for i in range(2)])
        xts = Rot([(sb(f"p4_x{i}", [128, D], F32), Res()) for i in range(2)])
        tmps = Rot([(sb(f"p4_t{i}", [128, D], F32), Res()) for i in range(2)])
        xns = Rot([(sb(f"p4_xn{i}", [128, D], F32), Res()) for i in range(2)])
        h2s = Rot([(sb(f"p4_h2{i}", [128, D], F32), Res()) for i in range(2)])
        junk = sb("p4_junk", [128, D], BF16); r_junk = Res()
        sts = Rot([(sb(f"p4_st{i}", [128, 4], F32), Res()) for i in range(2)])
        h2bs = Rot([(sb(f"p4_hb{i}", [128, D], BF16), Res()) for i in range(2)])
        h2fs = Rot([(sb(f"p4_hf{i}", [128, 8, 128], F32), Res()) for i in range(2)])
        lgs = Rot([(sb(f"p4_lg{i}", [128, 4, NE], F32), Res()) for i in range(2)])
        t8s = Rot([(sb(f"p4_t8{i}", [128, 16], F32), Res()) for i in range(2)])
        pys = Rot([(pst(f"p4_py{i}", [128, D], F32), Res()) for i in range(2)])
        ptr = Rot([(pst("p4_ptr", [128, 8, 128], F32), Res(excl=True))])
        pls = Rot([(pst(f"p4_pl{i}", [128, NE], F32), Res()) for i in range(2)])
        for t in range(nt_act):
            r = 0 if t < NTL else 1
            mx, r_mx = mixs.next()
            S.dma("sp", lambda e, mx=mx, t=t: e.dma_start(out=mx[:], in_=mixT[:, t * 128:(t + 1) * 128].rearrange("(j p) t -> p j t", p=128)), writes=[r_mx], stream="ld")
            xt, r_x = xts.next()
            S.dma("act", lambda e, xt=xt, t=t: e.dma_start(out=xt[:], in_=xsrc[t * 128:(t + 1) * 128, :]), writes=[r_x], stream="ld2")
            py, r_py = pys.next()
            for half in range(2):
                for j in range(8):
                    S.op("pe", lambda e, py=py, mx=mx, half=half, j=j: e.matmul(py[:, half * 512:(half + 1) * 512], lhsT=mx[:, j, :], rhs=woutb[:, j, half * 512:(half + 1) * 512], start=(j == 0), stop=(j == 7)),
                         reads=[r_mx, r_wo], writes=[r_py])
            tp, r_tp = tmps.next()
            S.op("dve", lambda e, tp=tp, py=py, r=r: e.tensor_tensor(out=tp[:], in0=py[:], in1=GT1[r][0][:], op=ALU.mult), reads=[r_py, GT1[r][1]], writes=[r_tp])
            xn, r_xn = xns.next()
            S.op("pool", lambda e, xn=xn, tp=tp, xt=xt: e.tensor_tensor(out=xn[:], in0=tp[:], in1=xt[:], op=ALU.add), reads=[r_tp, r_x], writes=[r_xn])
            S.dma("sp", lambda e, xn=xn, t=t: e.dma_start(out=xres[t * 128:(t + 1) * 128, :], in_=xn[:]), reads=[r_xn], stream="scr")
            st, r_st = sts.next()
            rstd_ops(S, xn, r_xn, junk, r_junk, st, r_st)
            h2, r_h2 = h2s.next()
            S.op("dve", lambda e, h2=h2, xn=xn, st=st, r=r: e.scalar_tensor_tensor(out=h2[:], in0=xn[:], scalar=st[:, 3:4], in1=G2[r][0][:], op0=ALU.mult, op1=ALU.mult), reads=[r_xn, r_st, G2[r][1]], writes=[r_h2])
            S.op("pool", lambda e, h2=h2, r=r: e.tensor_tensor(out=h2[:], in0=h2[:], in1=SH2[r][0][:], op=ALU.add), reads=[r_h2, SH2[r][1]], writes=[r_h2])
            pt, r_pt = ptr.next()
            for j in range(8):
                S.op("pe", lambda e, pt=pt, h2=h2, j=j: e.transpose(out=pt[:, j, :], in_=h2[:, j * 128:(j + 1) * 128], identity=ident[:]), reads=[r_h2, r_const], writes=[r_pt])
            hb, r_hb = h2bs.next(); hf, r_hf = h2fs.next()
            S.op("act", lambda e, hb=hb, h2=h2: e.copy(out=hb[:], in_=h2[:]), reads=[r_h2], writes=[r_hb])
            S.op("dve", lambda e, hf=hf, pt=pt: e.tensor_copy(out=hf[:], in_=pt[:]), reads=[r_pt], writes=[r_hf])
            S.dma("sp", lambda e, hb=hb, t=t: e.dma_start(out=h2rows[t * 128:(t + 1) * 128, :], in_=hb[:]), reads=[r_hb], stream="scr")
            pl, r_pl = pls.next()
            for j in range(8):
                S.op("pe", lambda e, pl=pl, hf=hf, j=j: e.matmul(pl[:], lhsT=hf[:, j, :], rhs=wrf[:, j, :], start=(j == 0), stop=False), reads=[r_hf, r_wr], writes=[r_pl])
            S.op("pe", lambda e, pl=pl: e.matmul(pl[:], lhsT=ones[0:1, :], rhs=brr[0:1, :], start=False, stop=True), reads=[r_wr, r_const], writes=[r_pl])
            lg, r_lg = lgs.next(); t8, r_t8 = t8s.next()
            S.op("dve", lambda e, lg=lg, pl=pl: e.tensor_copy(out=lg[:, 0, :], in_=pl[:]), reads=[r_pl], writes=[r_lg])
            S.op("dve", lambda e, lg=lg, t8=t8: e.max(out=t8[:, 0:8], in_=lg[:, 0, :]), reads=[r_lg], writes=[r_t8])
            S.op("dve", lambda e, lg=lg, t8=t8: e.tensor_scalar(out=lg[:, 1, :], in0=lg[:, 0, :], scalar1=t8[:, 3:4], scalar2=None, op0=ALU.is_ge), reads=[r_lg, r_t8], writes=[r_lg])
            S.op("dve", lambda e, t8=t8: e.tensor_scalar(out=t8[:, 8:9], in0=t8[:, 0:1], scalar1=-1.0, scalar2=None, op0=ALU.mult), reads=[r_t8], writes=[r_t8])
            S.op("act", lambda e, lg=lg, t8=t8: e.activation(out=lg[:, 2, :], in_=lg[:, 0, :], func=AF.Exp, bias=t8[:, 8:9], scale=1.0), reads=[r_lg, r_t8], writes=[r_lg])
            S.op("dve", lambda e, lg=lg: e.tensor_tensor(out=lg[:, 3, :], in0=lg[:, 2, :], in1=lg[:, 1, :], op=ALU.mult), reads=[r_lg], writes=[r_lg])
            S.op("dve", lambda e, lg=lg, t8=t8: e.reduce_sum(out=t8[:, 9:10], in_=lg[:, 3, :], axis=AX.X), reads=[r_lg], writes=[r_t8])
            S.op("dve", lambda e, t8=t8: e.reciprocal(out=t8[:, 10:11], in_=t8[:, 9:10]), reads=[r_t8], writes=[r_t8])
            S.op("dve", lambda e, lg=lg, t8=t8, t=t: e.tensor_scalar(out=gates[:, t, :], in0=lg[:, 3, :], scalar1=t8[:, 10:11], scalar2=None, op0=ALU.mult), reads=[r_lg, r_t8], writes=[r_gates[t]])
            if dbg_g is not None:
                S.dma("sp", lambda e, t=t: e.dma_start(out=dbg_g[t * 128:(t + 1) * 128, :], in_=gates[:, t, :]), reads=[r_gates[t]], stream="scr")


def phase5(nc, S, IN, l, xres, modv, h2rows, yacc, slotrec, gates, r_gates, ident, ones, r_const, nt_act, out, wbf, r_wbfl):
    S.barrier()
    last = (l == 1)
    NB = (4 * nt_act * 128) // 128 + NE
    with ExitStack() as ph:
        sb = lambda n, s, d: ph.enter_context(usb(nc, n, s, d))
        pst = lambda n, s, d: ph.enter_context(ups(nc, n, s, d))
        nr = 2 if nt_act > NTL else 1
        GT2 = [load_mod_bc(nc, S, ph, modv, l, r, 5, f"p5_GT{r}") for r in range(nr)]
        if last:
            gfb = sb("p5_gf", [128, D], F32); r_gf = Res()
            S.dma("sp", lambda e: e.dma_start(out=gfb[:], in_=IN["g_final"].to_broadcast([128, D])), writes=[r_gf], stream="ld")
        r_k = Res()
        cst = {}
        for nm, shp in (("tri_s", [128, 128]), ("tokidf", [128, NT]), ("widxbase", [128, 8]), ("blockval", [128, 2]), ("eidx", [128, 1])):
            cst[nm] = sb("p5_" + nm, shp, F32)
            S.dma("sp", lambda e, nm=nm: e.dma_start(out=cst[nm][:], in_=IN[nm]), writes=[r_k], stream="ld")
        bnat = sb("p5_bnat", [NE, 3, D], F32); bb = sb("p5_bb", [NE, 3, D], BF16); r_bb = Res()
        for k, nm in enumerate(("b_gate", "b_up", "b_down")):
            S.dma("sp", lambda e, k=k, nm=nm: e.dma_start(out=bnat[:, k, :], in_=IN[nm][l]), writes=[r_bb], stream="ld")
        S.op("dve", lambda e: e.tensor_copy(out=bb[:], in_=bnat[:]), reads=[r_bb], writes=[r_bb])
        zt = sb("p5_zt", [128, D], F32); r_zt = Res(); r_yacc = Res(); r_h2r = Res(); r_slot = Res()
        zb = sb("p5_zb", [1, D], BF16)
        S.op("dve", lambda e: e.memset(zt[:], 0.0), writes=[r_zt])
        S.op("dve", lambda e: e.memset(zb[:], 0.0), writes=[r_zt])
        for t in range(NT):
            S.dma("sp", lambda e, t=t: e.dma_start(out=yacc[t * 128:(t + 1) * 128, :], in_=zt[:]), reads=[r_zt], writes=[r_yacc], stream="scr")
        S.dma("sp", lambda e: e.dma_start(out=yacc[T:T + 1, :], in_=zt[0:1, :]), reads=[r_zt], writes=[r_yacc], stream="scr")
        S.dma("sp", lambda e: e.dma_start(out=h2rows[T:T + 1, :], in_=zb[:]), reads=[r_zt], writes=[r_h2r], stream="scr")
        prt = sb("p5_prt", [128, NBMAX, 2], F32)
        S.dma("sp", lambda e: e.dma_start(out=prt[:], in_=IN["padrec"]), writes=[r_zt], stream="ld")
        r_slots = [Res() for _ in range(nt_act * 4)]
        S.dma("sp", lambda e: e.dma_start(out=slotrec.rearrange("(p a) b -> p a b", a=NBMAX), in_=prt[:]), reads=[r_zt], writes=[r_slot] + r_slots, stream="scr")
        pm = pst("p5_pm", [128, 512], F32); r_pm = Res(excl=True)
        M = sb("p5_M", [128, NT, NE], F32); r_M = Res()
        POS = sb("p5_POS", [128, NT, NE], F32); r_POS = Res()
        cum = sb("p5_cum", [128, NE], F32); r_cum = Res()
        rg = list(r_gates[:nt_act])
        S.op("dve", lambda e: e.tensor_single_scalar(out=M[:, 0:nt_act, :], in_=gates[:, 0:nt_act, :], scalar=0.0, op=ALU.is_gt), reads=rg, writes=[r_M])
        S.op("dve", lambda e: e.memset(cum[:], 0.0), writes=[r_cum])
        for t in range(nt_act):
            S.op("pe", lambda e, t=t: e.matmul(pm[:, 0:NE], lhsT=cst["tri_s"][:], rhs=M[:, t, :], start=True, stop=False), reads=[r_M, r_k], writes=[r_pm])
            S.op("pe", lambda e, t=t: e.matmul(pm[:, 0:NE], lhsT=ones[:], rhs=cum[:], start=False, stop=True), reads=[r_cum, r_const], writes=[r_pm])
            S.op("act", lambda e, t=t: e.copy(out=POS[:, t, :], in_=pm[:, 0:NE]), reads=[r_pm], writes=[r_POS])
            S.op("dve", lambda e, t=t: e.tensor_tensor(out=cum[:], in0=cum[:], in1=M[:, t, :], op=ALU.add), reads=[r_M, r_cum], writes=[r_cum])
        mt = sb("p5_mt", [128, 8, NE], F32); r_mt = Res()
        mti = sb("p5_mti", [128, 2, NE], I32)
        S.op("pe", lambda e: e.matmul(pm[:, 0:NE], lhsT=ones[:], rhs=cum[:], start=True, stop=True), reads=[r_cum, r_const], writes=[r_pm])
        S.op("dve", lambda e: e.tensor_scalar(out=mti[:, 0, :], in0=pm[:, 0:NE], scalar1=127.0, scalar2=None, op0=ALU.add), reads=[r_pm], writes=[r_mt])
        S.op("dve", lambda e: e.tensor_single_scalar(out=mti[:, 1, :], in_=mti[:, 0, :], scalar=7, op=ALU.arith_shift_right), reads=[r_mt], writes=[r_mt])
        S.op("dve", lambda e: e.tensor_single_scalar(out=mti[:, 0, :], in_=mti[:, 1, :], scalar=7, op=ALU.logical_shift_left), reads=[r_mt], writes=[r_mt])
        S.op("dve", lambda e: e.tensor_copy(out=mt[:, 0, :], in_=mti[:, 0, :]), reads=[r_mt], writes=[r_mt])
        S.op("dve", lambda e: e.memset(mt[:, 7, :], 1.0), writes=[r_mt])
        S.op("dve", lambda e: e.tensor_tensor_scan(out=mt[:, 1, :], data0=mt[:, 7, :], data1=mt[:, 0, :], initial=0.0, op0=ALU.mult, op1=ALU.add), reads=[r_mt], writes=[r_mt])
        S.op("dve", lambda e: e.tensor_tensor(out=mt[:, 2, :], in0=mt[:, 1, :], in1=mt[:, 0, :], op=ALU.subtract), reads=[r_mt], writes=[r_mt])
        S.op("dve", lambda e: e.tensor_single_scalar(out=mt[:, 3, :], in_=mt[:, 0, :], scalar=0.0, op=ALU.is_gt), reads=[r_mt], writes=[r_mt])
        for t in range(nt_act):
            S.op("dve", lambda e, t=t: e.tensor_tensor(out=POS[:, t, :], in0=POS[:, t, :], in1=mt[:, 2, :], op=ALU.add), reads=[r_POS, r_mt], writes=[r_POS])
        recs = sb("p5_recs", [128, NT * 4, 2], F32); r_recs = Res()
        idxf = sb("p5_idxf", [128, NT * 4], F32); idxi = sb("p5_idxi", [128, NT * 4], I32); r_idx = Res()
        v8s = Rot([(sb(f"p5_v8{i}", [128, 8], F32), Res()) for i in range(2)])
        ohs = Rot([(sb(f"p5_oh{i}", [128, NE], F32), Res()) for i in range(2)])
        for t in range(nt_act):
            v8, r_v8 = v8s.next()
            S.op("dve", lambda e, v8=v8, t=t: e.max(out=v8[:], in_=gates[:, t, :]), reads=[r_gates[t]], writes=[r_v8])
            for k in range(4):
                q = t * 4 + k
                oh, r_oh = ohs.next()
                S.op("dve", lambda e, oh=oh, v8=v8, t=t, k=k: e.tensor_scalar(out=oh[:], in0=gates[:, t, :], scalar1=v8[:, k:k + 1], scalar2=None, op0=ALU.is_equal), reads=[r_gates[t], r_v8], writes=[r_oh])
                S.op("dve", lambda e, oh=oh, t=t: e.tensor_tensor(out=oh[:], in0=oh[:], in1=POS[:, t, :], op=ALU.mult), reads=[r_oh, r_POS], writes=[r_oh])
                S.op("dve", lambda e, oh=oh, q=q: e.reduce_sum(out=idxf[:, q:q + 1], in_=oh[:], axis=AX.X), reads=[r_oh], writes=[r_idx])
                S.op("act", lambda e, q=q, t=t: e.copy(out=recs[:, q, 0:1], in_=cst["tokidf"][:, t:t + 1]), reads=[r_k], writes=[r_recs])
                S.op("act", lambda e, q=q, v8=v8, k=k: e.copy(out=recs[:, q, 1:2], in_=v8[:, k:k + 1]), reads=[r_v8], writes=[r_recs])
        S.op("dve", lambda e: e.tensor_copy(out=idxi[:, 0:nt_act * 4], in_=idxf[:, 0:nt_act * 4]), reads=[r_idx], writes=[r_idx])
        for q in range(nt_act * 4):
            S.dma("pool", lambda e, q=q: e.indirect_dma_start(out=slotrec, out_offset=bass.IndirectOffsetOnAxis(ap=idxi[:, q:q + 1], axis=0), in_=recs[:, q, :], in_offset=None),
                  reads=[r_idx, r_recs, r_slot], writes=[r_slots[q]], stream="igs")
        EO = sb("p5_EO", [128, 256], F32); OH = sb("p5_OH", [NE, 256], F32); r_bm = Res()
        dgt = sb("p5_dgt", [128, 128], F32); r_dgt = Res()
        colv = sb("p5_colv", [128, 8], F32); r_colv = Res()
        cmpt = sb("p5_cmp", [128, NE], F32); r_cmp = Res()
        EBt = sb("p5_EB", [128, 256], F32); CHt = sb("p5_CH", [128, 256], F32)
        for c in range(2):
            bv = cst["blockval"][:, c:c + 1]
            S.op("dve", lambda e, bv=bv: e.tensor_scalar(out=cmpt[:], in0=mt[:, 1, :], scalar1=bv, scalar2=None, op0=ALU.is_le), reads=[r_mt, r_k], writes=[r_cmp])
            S.op("dve", lambda e, c=c: e.reduce_sum(out=colv[:, c:c + 1], in_=cmpt[:], axis=AX.X), reads=[r_cmp], writes=[r_colv])
            S.op("dve", lambda e, c=c: e.tensor_scalar(out=colv[:, c:c + 1], in0=colv[:, c:c + 1], scalar1=float(NE - 1), scalar2=None, op0=ALU.min), reads=[r_colv], writes=[r_colv])
            S.op("dve", lambda e, bv=bv: e.tensor_scalar(out=cmpt[:], in0=mt[:, 2, :], scalar1=bv, scalar2=None, op0=ALU.is_equal), reads=[r_mt, r_k], writes=[r_cmp])
            S.op("dve", lambda e: e.tensor_tensor(out=cmpt[:], in0=cmpt[:], in1=mt[:, 3, :], op=ALU.mult), reads=[r_cmp, r_mt], writes=[r_cmp])
            S.op("dve", lambda e, c=c: e.tensor_reduce(out=colv[:, 2 + c:3 + c], in_=cmpt[:], axis=AX.X, op=ALU.max), reads=[r_cmp], writes=[r_colv])
            for kk, dst in ((c, EBt), (2 + c, CHt)):
                S.op("dve", lambda e, kk=kk: e.tensor_scalar(out=dgt[:], in0=ident[:], scalar1=colv[:, kk:kk + 1], scalar2=None, op0=ALU.mult), reads=[r_colv, r_const], writes=[r_dgt])
                S.op("pe", lambda e: e.matmul(pm[:, 0:128], lhsT=ones[:], rhs=dgt[:], start=True, stop=True), reads=[r_dgt, r_const], writes=[r_pm])
                S.op("act", lambda e, dst=dst, c=c: e.copy(out=dst[:, c * 128:(c + 1) * 128], in_=pm[:, 0:128]), reads=[r_pm], writes=[r_bm])
        S.op("dve", lambda e: e.tensor_scalar(out=CHt[:], in0=CHt[:], scalar1=-1.0e7, scalar2=1.0e7, op0=ALU.mult, op1=ALU.add), reads=[r_bm], writes=[r_bm])
        S.op("dve", lambda e: e.scalar_tensor_tensor(out=EO[:], in0=EBt[:], scalar=128.0, in1=CHt[:], op0=ALU.mult, op1=ALU.add), reads=[r_bm], writes=[r_bm])
        S.op("dve", lambda e: e.tensor_scalar(out=OH[:], in0=EBt[0:NE, :], scalar1=cst["eidx"][0:NE, 0:1], scalar2=None, op0=ALU.is_equal), reads=[r_bm, r_k], writes=[r_bm])
        wg = sb("p5_wg", [128, 8, D], BF16); wu = sb("p5_wu", [128, 8, D], BF16); wd = sb("p5_wd", [128, 8, D], BF16)
        r_wg = Res(); r_wu = Res(); r_wd = Res()
        recb = Rot([(sb(f"p5_rb{i}", [128, 2], F32), Res()) for i in range(3)])
        xgs = Rot([(sb(f"p5_xg{i}", [128, D], BF16), Res()) for i in range(2)])
        xTs = Rot([(sb(f"p5_xT{i}", [128, 8, 128], BF16), Res()) for i in range(2)])
        wix = Rot([(sb(f"p5_wi{i}", [128, 1], I32), Res()) for i in range(2)])
        ohb = Rot([(sb(f"p5_ohb{i}", [NE, 128], BF16), Res()) for i in range(2)])
        a_s = Rot([(sb(f"p5_a{i}", [128, 512], F32), Res()) for i in range(2)])
        sg_s = Rot([(sb(f"p5_sg{i}", [128, 512], F32), Res()) for i in range(2)])
        u_s = Rot([(sb(f"p5_u{i}", [128, 512], F32), Res()) for i in range(2)])
        acts = Rot([(sb(f"p5_act{i}", [128, 8, 128], BF16), Res()) for i in range(2)])
        atms = Rot([(sb(f"p5_atm{i}", [128, D], BF16), Res()) for i in range(2)])
        ygs = Rot([(sb(f"p5_yg{i}", [128, D], F32), Res()) for i in range(2)])
        ptr = Rot([(pst("p5_ptr", [128, 8, 128], BF16), Res(excl=True))])
        pAs = Rot([(pst(f"p5_pA{i}", [128, 512], F32), Res()) for i in range(2)])
        pUs = Rot([(pst(f"p5_pU{i}", [128, 512], F32), Res()) for i in range(2)])
        pYs = Rot([(pst(f"p5_pY{i}", [128, 512], F32), Res()) for i in range(2)])
        identb = sb("p5_idb", [128, 128], BF16)
        S.op("dve", lambda e: e.tensor_copy(out=identb[:], in_=ident[:]), reads=[r_const], writes=[r_k])

        BC = {}

        def bcreg(e):
            if "r" not in BC:
                BC["r"] = e.alloc_register(f"bc{l}")
                e.reg_mov(BC["r"], NE * 128 - 1)
            return BC["r"]

        def stage_in(b):
            rb, r_rb = recb.next()
            S.dma("sp", lambda e, rb=rb, b=b: e.dma_start(out=rb[:], in_=slotrec[b * 128:(b + 1) * 128, :]), reads=[r_slot] + r_slots, writes=[r_rb], stream="ld")
            xg, r_xg = xgs.next()
            S.dma("pool", lambda e, xg=xg, rb=rb: e.indirect_dma_start(out=xg[:], out_offset=None, in_=h2rows, in_offset=bass.IndirectOffsetOnAxis(ap=rb[:, 0:1].bitcast(I32), axis=0)),
                  reads=[r_rb, r_h2r], writes=[r_xg], stream="ig")
            wi, r_wi = wix.next()
            S.op("dve", lambda e, wi=wi, b=b: e.tensor_scalar(out=wi[:], in0=cst["eidx"][:], scalar1=EO[:, b:b + 1], scalar2=None, op0=ALU.add), reads=[r_bm, r_k], writes=[r_wi])
            for m, (wt, r_w) in enumerate(((wg, r_wg), (wu, r_wu), (wd, r_wd))):
                S.dma("pool", lambda e, wi=wi, m=m, wt=wt: e.indirect_dma_start(out=wt[:].rearrange("p j f -> p (j f)"), out_offset=None, in_=wbf[m], in_offset=bass.IndirectOffsetOnAxis(ap=wi[:, 0:1], axis=0),
                                                                             bounds_check=bcreg(e), oob_is_err=False), reads=[r_wi, r_wbfl], writes=[r_w], stream="wc")
            ob, r_ob = ohb.next()
            S.op("act", lambda e, ob=ob, b=b: e.activation(out=ob[:], in_=ones[0:NE, :], func=AF.Copy, scale=OH[0:NE, b:b + 1]), reads=[r_bm, r_const], writes=[r_ob])
            return (rb, r_rb, xg, r_xg, ob, r_ob)

        def compute(b, st):
            rb, r_rb, xg, r_xg, ob, r_ob = st
            pt, r_pt = ptr.next()
            for j in range(8):
                S.op("pe", lambda e, pt=pt, xg=xg, j=j: e.transpose(out=pt[:, j, :], in_=xg[:, j:D:8], identity=identb[:]), reads=[r_xg, r_k], writes=[r_pt])
            xT, r_xT = xTs.next()
            S.op("act", lambda e, xT=xT, pt=pt: e.copy(out=xT[:], in_=pt[:]), reads=[r_pt], writes=[r_xT])
            atm, r_atm = atms.next()
            for hf in range(2):
                pA, r_pA = pAs.next(); pU, r_pU = pUs.next()
                for (pp, r_pp, wt, r_w, bk) in ((pA, r_pA, wg, r_wg, 0), (pU, r_pU, wu, r_wu, 1)):
                    for j in range(8):
                        S.op("pe", lambda e, pp=pp, wt=wt, j=j, xT=xT, hf=hf: e.matmul(pp[:], lhsT=xT[:, j, :], rhs=wt[:, j, hf * 512:(hf + 1) * 512], start=(j == 0), stop=False),
                             reads=[r_w, r_xT], writes=[r_pp])
                    S.op("pe", lambda e, pp=pp, bk=bk, ob=ob, hf=hf: e.matmul(pp[:], lhsT=ob[:], rhs=bb[:, bk, hf * 512:(hf + 1) * 512], start=False, stop=True),
                         reads=[r_bb, r_ob], writes=[r_pp])
                a, r_a = a_s.next(); sg, r_sg = sg_s.next(); u1, r_u1 = u_s.next()
                S.op("dve", lambda e, a=a, pA=pA: e.tensor_scalar(out=a[:], in0=pA[:], scalar1=7.0, scalar2=None, op0=ALU.min), reads=[r_pA], writes=[r_a])
                S.op("act", lambda e, sg=sg, a=a: e.activation(out=sg[:], in_=a[:], func=AF.Sigmoid, scale=1.702), reads=[r_a], writes=[r_sg])
                S.op("dve", lambda e, u1=u1, pU=pU: e.tensor_scalar(out=u1[:], in0=pU[:], scalar1=7.0, scalar2=-7.0, op0=ALU.min, op1=ALU.max), reads=[r_pU], writes=[r_u1])
                S.op("dve", lambda e, sg=sg, a=a: e.tensor_tensor(out=sg[:], in0=sg[:], in1=a[:], op=ALU.mult), reads=[r_a, r_sg], writes=[r_sg])
                S.op("dve", lambda e, atm=atm, sg=sg, u1=u1, hf=hf: e.scalar_tensor_tensor(out=atm[:, hf * 512:(hf + 1) * 512], in0=u1[:], scalar=1.0, in1=sg[:], op0=ALU.add, op1=ALU.mult),
                     reads=[r_sg, r_u1], writes=[r_atm])
            pt2, r_pt2 = ptr.next()
            for j in range(8):
                S.op("pe", lambda e, pt2=pt2, atm=atm, j=j: e.transpose(out=pt2[:, j, :], in_=atm[:, j:D:8], identity=identb[:]), reads=[r_atm, r_k], writes=[r_pt2])
            actT, r_act = acts.next()
            S.op("act", lambda e, actT=actT, pt2=pt2: e.copy(out=actT[:], in_=pt2[:]), reads=[r_pt2], writes=[r_act])
            yg, r_yg = ygs.next()
            for half in range(2):
                pY, r_pY = pYs.next()
                for f in range(8):
                    S.op("pe", lambda e, pY=pY, actT=actT, f=f, half=half: e.matmul(pY[:], lhsT=actT[:, f, :], rhs=wd[:, f, half * 512:(half + 1) * 512], start=(f == 0), stop=False),
                         reads=[r_act, r_wd], writes=[r_pY])
                S.op("pe", lambda e, pY=pY, ob=ob, half=half: e.matmul(pY[:], lhsT=ob[:], rhs=bb[:, 2, half * 512:(half + 1) * 512], start=False, stop=True), reads=[r_ob, r_bb], writes=[r_pY])
                S.op("act", lambda e, yg=yg, pY=pY, rb=rb, half=half: e.activation(out=yg[:, half * 512:(half + 1) * 512], in_=pY[:], func=AF.Copy, scale=rb[:, 1:2]), reads=[r_pY, r_rb], writes=[r_yg])
            return (yg, r_yg, rb, r_rb)

        def stage_out(res):
            yg, r_yg, rb, r_rb = res
            S.dma("pool", lambda e, yg=yg, rb=rb: e.indirect_dma_start(out=yacc, out_offset=bass.IndirectOffsetOnAxis(ap=rb[:, 0:1].bitcast(I32), axis=0), in_=yg[:], in_offset=None, compute_op=ALU.add),
                  reads=[r_yg, r_rb], writes=[r_yacc], stream="igs")

        st = stage_in(0)
        for b in range(NB):
            res = compute(b, st)
            if b + 1 < NB:
                st = stage_in(b + 1)
            stage_out(res)
        xts = Rot([(sb(f"p5_xt{i}", [128, D], F32), Res()) for i in range(2)])
        yts = Rot([(sb(f"p5_yt{i}", [128, D], F32), Res()) for i in range(2)])
        junk = sb("p5_junk", [128, D], BF16); r_junk = Res()
        sts = Rot([(sb(f"p5_st{i}", [128, 4], F32), Res()) for i in range(2)])
        for t in range(nt_act):
            r = 0 if t < NTL else 1
            xt, r_xt = xts.next(); yt, r_yt = yts.next()
            S.dma("sp", lambda e, xt=xt, t=t: e.dma_start(out=xt[:], in_=xres[t * 128:(t + 1) * 128, :]), writes=[r_xt], stream="ld")
            S.dma("act", lambda e, yt=yt, t=t: e.dma_start(out=yt[:], in_=yacc[t * 128:(t + 1) * 128, :]), reads=[r_yacc], writes=[r_yt], stream="ld2")
            S.op("dve", lambda e, yt=yt, r=r: e.tensor_tensor(out=yt[:], in0=yt[:], in1=GT2[r][0][:], op=ALU.mult), reads=[r_yt, GT2[r][1]], writes=[r_yt])
            S.op("pool", lambda e, yt=yt, xt=xt: e.tensor_tensor(out=yt[:], in0=yt[:], in1=xt[:], op=ALU.add), reads=[r_yt, r_xt], writes=[r_yt])
            if not last:
                S.dma("sp", lambda e, yt=yt, t=t: e.dma_start(out=xres[t * 128:(t + 1) * 128, :], in_=yt[:]), reads=[r_yt], stream="scr")
            else:
                st_, r_st = sts.next()
                rstd_ops(S, yt, r_yt, junk, r_junk, st_, r_st)
                S.op("dve", lambda e, yt=yt, st_=st_: e.scalar_tensor_tensor(out=yt[:], in0=yt[:], scalar=st_[:, 3:4], in1=gfb[:], op0=ALU.mult, op1=ALU.mult), reads=[r_yt, r_st, r_gf], writes=[r_yt])
                S.dma("sp", lambda e, yt=yt, t=t: e.dma_start(out=out[t * 128:(t + 1) * 128, :], in_=yt[:]), reads=[r_yt], stream="st")


_CACHE = {}


def make_in_maps(inputs):
    consts = host_consts()
    shared = {}
    for nm, shp in W_SPECS:
        a = np.ascontiguousarray(np.asarray(inputs[nm], dtype=np.float32)).reshape(shp)
        shared[nm] = a
    shared.update(consts)
    x = np.asarray(inputs["x"], dtype=np.float32)
    ctx = np.asarray(inputs["ctx"], dtype=np.float32)
    c = np.asarray(inputs["c"], dtype=np.float32)
    c_ctx = np.asarray(inputs["c_ctx"], dtype=np.float32)
    maps = []
    for b in range(8):
        m = dict(shared)
        m["xin"] = np.ascontiguousarray(np.concatenate([x[b], ctx[b]], axis=0))
        m["cc"] = np.ascontiguousarray(np.stack([c[b], c_ctx], axis=0))
        maps.append(m)
    return maps


def kernel(**inputs):
    if "nc" not in _CACHE:
        _CACHE["nc"] = build()[0]
    nc = _CACHE["nc"]
    maps = make_in_maps(inputs)
    res = run_bass_kernel_spmd(nc, maps, core_ids=list(range(8)))
    return np.stack([np.asarray(r["out"], dtype=np.float32) for r in res.results], axis=0)
```

```python
import numpy as np
import concourse.bass as bass
import concourse.mybir as mybir
from concourse.alu_op_type import AluOpType as ALU
from contextlib import ExitStack
from concourse.bass_utils import run_bass_kernel_spmd

F32 = mybir.dt.float32
BF16 = mybir.dt.bfloat16
I32 = mybir.dt.int32
U32 = mybir.dt.uint32
AF = mybir.ActivationFunctionType
AX = mybir.AxisListType


class Res:
    __slots__ = ("name", "w", "rs", "excl")

    def __init__(self, name="", excl=False):
        self.name = name
        self.w = None
        self.rs = {}
        self.excl = excl


class Op:
    __slots__ = ("eng", "fn", "reads", "writes", "stream", "deps", "sig", "sigidx", "waits")

    def __init__(self, eng, fn, reads, writes, stream):
        self.eng = eng
        self.fn = fn
        self.reads = reads
        self.writes = writes
        self.stream = stream
        self.deps = None
        self.sig = False
        self.sigidx = -1
        self.waits = None


class Sched:
    CH = 16000
    CHD = 1000
    COMPUTE = ("pe", "act", "dve", "pool")

    def __init__(self, nc):
        self.nc = nc
        self.ops = []
        self._dcnt = {}

    def op(self, eng, fn, reads=(), writes=()):
        self.ops.append(Op(eng, fn, tuple(reads), tuple(writes), None))

    NSLOT = {"ld": 12, "scr": 12, "wc": 6, "ld2": 6, "st": 4, "ig": 8, "igs": 4, "cv": 8}

    def dma(self, queue, fn, reads=(), writes=(), stream="ld"):
        n = self._dcnt.get(stream, 0)
        self._dcnt[stream] = n + 1
        self.ops.append(Op(queue, fn, tuple(reads), tuple(writes), f"{stream}#{n % self.NSLOT.get(stream, 8)}"))

    def barrier(self):
        self.ops.append(None)

    def finalize(self, es, final_streams=()):
        nc = self.nc
        raw = self.ops
        ops = []
        bar_after = {}
        lastkey = {}
        pend = None
        for o in raw:
            if o is None:
                pend = dict(lastkey)
                continue
            i = len(ops)
            ops.append(o)
            key = o.stream if o.stream is not None else o.eng
            if pend is not None:
                bar_after[i] = pend
                pend = None
            if not key.startswith("cv#"):
                lastkey[key] = i
        self.ops = ops
        cur_bar = set()
        prev_dma = {}
        for i, o in enumerate(ops):
            if i in bar_after:
                cur_bar = set(bar_after[i].values())
                pend = None
            deps = set(cur_bar)
            if any(r.excl for r in o.reads):
                o.writes = tuple(o.writes) + tuple(r for r in o.reads if r.excl)
                o.reads = tuple(r for r in o.reads if not r.excl)
            for r in o.reads:
                if r.w is not None:
                    deps.add(r.w)
            for w in o.writes:
                if w.w is not None:
                    deps.add(w.w)
                for k, j in w.rs.items():
                    deps.add(j)
            key = o.stream if o.stream is not None else o.eng
            for r in o.reads:
                r.rs[key] = i
            for w in o.writes:
                w.w = i
                w.rs = {}
            if o.stream is not None:
                if o.stream in prev_dma:
                    deps.add(prev_dma[o.stream])
                prev_dma[o.stream] = i
            deps.discard(i)
            o.deps = deps
            for j in deps:
                ops[j].sig = True
        cnt = {}
        for o in ops:
            key = o.stream if o.stream is not None else o.eng
            if o.stream is not None:
                o.sig = True
            if o.sig:
                o.sigidx = cnt.get(key, 0)
                cnt[key] = o.sigidx + 1
        self.cnt = cnt
        known = {e: {} for e in ("pe", "act", "dve", "pool", "sp")}
        clocks = [None] * len(ops)
        for i, o in enumerate(ops):
            kn = known[o.eng]
            waits = {}
            for j in sorted(o.deps):
                p = ops[j]
                pkey = p.stream if p.stream is not None else p.eng
                if p.stream is None and p.eng == o.eng:
                    if o.eng == "pe":
                        continue
                if kn.get(pkey, -1) >= p.sigidx:
                    continue
                if waits.get(pkey, -1) < p.sigidx:
                    waits[pkey] = p.sigidx
                pc = clocks[j]
                for k, v in pc.items():
                    if kn.get(k, -1) < v:
                        kn[k] = v
            for k, v in waits.items():
                if kn.get(k, -1) < v:
                    kn[k] = v
            o.waits = waits
            ck = dict(kn)
            if o.sig:
                key = o.stream if o.stream is not None else o.eng
                ck[key] = max(ck.get(key, -1), o.sigidx)
            clocks[i] = ck
        self.sems = {}
        for key, n in cnt.items():
            ch = self.CH if key in self.COMPUTE else self.CHD
            nch = (n + ch - 1) // ch
            self.sems[key] = [es.enter_context(nc.semaphore(f"s_{key}_{c}")) for c in range(nch)]
        per_eng = {e: [] for e in ("pe", "act", "dve", "pool", "sp")}
        for o in ops:
            per_eng[o.eng].append(o)
        block = es.enter_context(nc.Block())
        sems = self.sems
        CH = self.CH
        CHD = self.CHD

        def wait(eng, k, v):
            if k in self.COMPUTE:
                eng.wait_ge(sems[k][v // CH], v % CH + 1)
            else:
                c = v // CHD
                if c > 0:
                    eng.wait_ge(sems[k][c - 1], CHD * 16)
                eng.wait_ge(sems[k][c], (v % CHD + 1) * 16)

        def emit(eng, lst, finals):
            for o in lst:
                for k, v in o.waits.items():
                    wait(eng, k, v)
                inst = o.fn(eng)
                if o.sig:
                    key = o.stream if o.stream is not None else o.eng
                    if o.stream is not None:
                        inst.then_inc(sems[key][o.sigidx // CHD], 16)
                    else:
                        inst.then_inc(sems[key][o.sigidx // CH], 1)
            for k in cnt:
                if k not in self.COMPUTE and k.split("#")[0] in finals:
                    wait(eng, k, cnt[k] - 1)

        @block.sync
        def _(e):
            emit(e, per_eng["sp"], final_streams)

        @block.tensor
        def _(e):
            emit(e, per_eng["pe"], ())

        @block.scalar
        def _(e):
            emit(e, per_eng["act"], ())

        @block.vector
        def _(e):
            emit(e, per_eng["dve"], ())

        @block.gpsimd
        def _(e):
            emit(e, per_eng["pool"], ())
        return {k: len(v) for k, v in per_eng.items()}
D = 1024
SEQ = 4096
CTX = 256
T = SEQ + CTX
NT = T // 128
NTL = SEQ // 128
INW = 2576
NE = 32
EPS = 1e-6
NEG = -30000.0
NBMAX = (4 * T) // 128 + NE


def host_consts():
    import ml_dtypes
    c = {}
    p = np.arange(128)
    c["ident"] = np.eye(128, dtype=np.float32)
    c["ones"] = np.ones((128, 128), np.float32)
    same = (p[:, None] // 64) == (p[None, :] // 64)
    c["m_ls"] = np.where(same & (p[:, None] > p[None, :]), 0.0, NEG).astype(np.float32)
    c["m_li"] = np.where(same & (p[:, None] >= p[None, :]), 0.0, NEG).astype(np.float32)
    c["m_us"] = np.where(same & (p[:, None] < p[None, :]), 0.0, NEG).astype(np.float32)
    c["m_ui"] = np.where(same & (p[:, None] <= p[None, :]), 0.0, NEG).astype(np.float32)
    c["tri_f"] = (same & (p[:, None] <= p[None, :])).astype(np.float32)
    c["tri_b"] = (same & (p[:, None] >= p[None, :])).astype(np.float32)
    c["blk"] = same.astype(np.float32)
    c["tri_s"] = (p[:, None] < p[None, :]).astype(np.float32)
    tok = (np.arange(NT)[None, :] * 128 + p[:, None]).astype(np.int32)
    c["tokidf"] = tok.view(np.float32)
    c["widxbase"] = (np.arange(8)[None, :] * 128 + p[:, None]).astype(np.float32)
    c["blockval"] = (128.0 * (p[:, None] + 128 * np.arange(2)[None, :])).astype(np.float32)
    c["eidx"] = p[:, None].astype(np.float32)
    pr = np.zeros((128, NBMAX, 2), np.int32); pr[:, :, 0] = T
    c["padrec"] = pr.view(np.float32)
    ang = 2 * np.pi * np.outer(p, p) / 128.0
    c["cs128"] = np.concatenate([np.cos(ang), np.sin(ang)], axis=1).astype(np.float32)
    for nm, L in (("L", SEQ), ("C", CTX)):
        t = np.arange(L, dtype=np.int64)
        a = 2 * np.pi * ((np.outer(t, t) % L).astype(np.float64)) / L
        nrm = 1.0 / np.sqrt(L * 128.0)
        c["cos" + nm] = (np.cos(a) * nrm).astype(ml_dtypes.bfloat16)
        c["nsin" + nm] = (-np.sin(a) * nrm).astype(ml_dtypes.bfloat16)
    return c


CONST_SPECS = [("ident", [128, 128], "f"), ("ones", [128, 128], "f"), ("m_ls", [128, 128], "f"),
               ("m_li", [128, 128], "f"), ("m_us", [128, 128], "f"), ("m_ui", [128, 128], "f"),
               ("tri_f", [128, 128], "f"), ("tri_b", [128, 128], "f"), ("blk", [128, 128], "f"),
               ("cs128", [128, 256], "f"), ("tri_s", [128, 128], "f"), ("tokidf", [128, NT], "f"), ("widxbase", [128, 8], "f"),
               ("blockval", [128, 2], "f"), ("eidx", [128, 1], "f"), ("padrec", [128, NBMAX, 2], "f"), ("cosL", [SEQ, SEQ], "b"), ("nsinL", [SEQ, SEQ], "b"),
               ("cosC", [CTX, CTX], "b"), ("nsinC", [CTX, CTX], "b")]

W_SPECS = [("w_mod", [2, D, 6 * D]), ("b_mod", [2, 6 * D]), ("g_norm1", [2, D]), ("w_in", [2, D, INW]),
           ("conv_w", [2, 5, 1536]), ("a_log", [2, 8]), ("dt_bias", [2, 8]), ("g_out_norm", [2, 128]),
           ("w_out", [2, D, D]), ("g_norm2", [2, D]), ("w_router", [2, D, NE]), ("b_router", [2, NE]),
           ("w_gate", [2, NE, D, D]), ("b_gate", [2, NE, D]), ("w_up", [2, NE, D, D]), ("b_up", [2, NE, D]),
           ("w_down", [2, NE, D, D]), ("b_down", [2, NE, D]), ("g_final", [1, D])]


_UC = [0]


def usb(nc, name, shape, dt):
    _UC[0] += 1
    return nc.sbuf_tensor(f"{name}_{_UC[0]}", shape, dt)


def ups(nc, name, shape, dt):
    _UC[0] += 1
    return nc.psum_tensor(f"{name}_{_UC[0]}", shape, dt)


class Rot:
    def __init__(self, items):
        self.items = items
        self.i = 0

    def next(self):
        it = self.items[self.i % len(self.items)]
        self.i += 1
        return it


def build(stage=99, dbg=()):
    nc = bass.Bass("TRN2", target_bir_lowering=False)
    IN = {}

    def din(name, shape, dt=F32):
        IN[name] = nc.dram_tensor(name, shape, dt, kind="ExternalInput").ap()
        return IN[name]

    xin = din("xin", [T, D])
    cc = din("cc", [2, D])
    for nm, shp in W_SPECS:
        din(nm, shp)
    for nm, shp, k in CONST_SPECS:
        din(nm, shp, F32 if k == "f" else BF16)
    out = nc.dram_tensor("out", [SEQ, D], F32, kind="ExternalOutput").ap()

    def dscr(name, shape, dt=F32):
        kind = "ExternalOutput" if name in dbg else "Internal"
        return nc.dram_tensor(name, shape, dt, kind=kind).ap()

    xres = dscr("xres", [T, D])
    modv = dscr("modv", [2, 2, 6 * D])
    uT = dscr("uT", [INW, T])
    abd = dscr("abd", [T, 16])
    mixT = dscr("mixT", [D, T], BF16)
    h2rows = dscr("h2rows", [T + 1, D], BF16)
    yacc = dscr("yacc", [T + 1, D])
    slotrec = dscr("slotrec", [NBMAX * 128, 2])
    wbf = [[dscr(f"wbf{i}_{m}", [NE * 128, 8 * D], BF16) for m in range(3)] for i in range(2)]
    r_wbf = [Res(), Res()]

    def conv_thunks(l):
        th = []
        for ex in range(NE):
            for m, nm in enumerate(("w_gate", "w_up", "w_down")):
                def fn(l=l, ex=ex, m=m, nm=nm):
                    r0 = ex * 128
                    S.dma("pool", lambda e: e.dma_start(out=wbf[l][m][r0:r0 + 128, :], in_=IN[nm][l, ex].rearrange("(p j) f -> p (j f)", j=8), max_dma_last_dim=4096),
                          writes=[r_wbf[l]], stream="cv")
                th.append(fn)
        return th
    dbg_g = dscr("dbg_g", [T, NE]) if "dbg_g" in dbg else None
    dbg_oT = dscr("dbg_oT", [4, 128, T]) if "dbg_oT" in dbg else None

    es = ExitStack()
    with es:
        S = Sched(nc)
        gsb = lambda n, s, d: es.enter_context(usb(nc, n, s, d))
        ident = gsb("identS", [128, 128], F32); r_const = Res("const")
        ones = gsb("onesS", [128, 128], F32)
        identb = gsb("identb", [128, 128], BF16)
        S.dma("sp", lambda e: e.dma_start(out=ident[:], in_=IN["ident"]), writes=[r_const], stream="ld")
        S.dma("sp", lambda e: e.dma_start(out=ones[:], in_=IN["ones"]), writes=[r_const], stream="ld")
        S.op("dve", lambda e: e.tensor_copy(out=identb[:], in_=ident[:]), reads=[r_const], writes=[r_const])
        gates = gsb("gates", [128, NT, NE], F32); r_gates = [Res() for _ in range(NT)]

        phase0(nc, S, IN, modv, ident, ones, r_const)
        for l in range(2):
            if stage < 1:
                break
            nt_act = NT if l == 0 else NTL
            phase1(nc, S, IN, l, xin if l == 0 else xres, modv, uT, abd, ident, identb, ones, r_const)
            if stage < 2:
                break
            phase2(nc, S, IN, l, uT, mixT, r_const)
            if stage < 3:
                break
            phase3(nc, S, IN, l, uT, abd, mixT, ident, ones, r_const, dbg_oT if l == 0 else None, conv_thunks(l))
            if stage < 4:
                break
            phase4(nc, S, IN, l, xin if l == 0 else xres, xres, modv, mixT, h2rows, gates, r_gates, ident, ones, r_const, nt_act, dbg_g if l == 0 else None)
            if stage < 5:
                break
            phase5(nc, S, IN, l, xres, modv, h2rows, yacc, slotrec, gates, r_gates, ident, ones, r_const, nt_act, out, wbf[l], r_wbf[l])
            if stage < 6:
                break
        if stage < 6:
            with ExitStack() as ph:
                z = ph.enter_context(usb(nc, "zz", [128, D], F32)); rz = Res()
                S.barrier()
                S.op("dve", lambda e: e.memset(z[:], 0.0), writes=[rz])
                S.dma("sp", lambda e: e.dma_start(out=out[0:128, :], in_=z[:]), reads=[rz], stream="st")
        S.barrier()
        fin = ("st", "ld", "wc", "scr", "ig", "igs", "cv")
        stats = S.finalize(es, final_streams=fin)
    return nc, stats
def phase0(nc, S, IN, modv, ident, ones, r_const):
    S.barrier()
    with ExitStack() as ph:
        sb = lambda n, s, d: ph.enter_context(usb(nc, n, s, d))
        ccr = sb("p0_ccr", [2, D], F32); r_ccr = Res()
        scT = sb("p0_scT", [128, 8, 2], F32); r_scT = Res()
        bm = sb("p0_bm", [1, 2, 6 * D], F32); r_bm = Res()
        modsb = sb("p0_mod", [2, 2, 6 * D], F32); r_mod = Res()
        wbufs = Rot([(sb(f"p0_w{i}", [128, 8, 512], F32), Res()) for i in range(2)])
        pT = ph.enter_context(ups(nc, "p0_pT", [128, 8, 2], F32)); r_pT = Res()
        pss = Rot([(ph.enter_context(ups(nc, f"p0_ps{i}", [2, 512], F32)), Res()) for i in range(2)])
        S.dma("sp", lambda e: e.dma_start(out=ccr[:], in_=IN["cc"]), writes=[r_ccr], stream="ld")
        S.dma("sp", lambda e: e.dma_start(out=bm[:], in_=IN["b_mod"].rearrange("(o l) n -> o l n", o=1)), writes=[r_bm], stream="ld")
        S.op("act", lambda e: e.activation(out=ccr[:], in_=ccr[:], func=AF.Silu), reads=[r_ccr], writes=[r_ccr])
        for j in range(8):
            S.op("pe", lambda e, j=j: e.transpose(out=pT[:, j, :], in_=ccr[:, j * 128:(j + 1) * 128], identity=ident[0:2, 0:2]),
                 reads=[r_ccr, r_const], writes=[r_pT])
        S.op("dve", lambda e: e.tensor_copy(out=scT[:], in_=pT[:]), reads=[r_pT], writes=[r_scT])
        for l in range(2):
            wv = IN["w_mod"][l].rearrange("(j p) n -> p j n", p=128)
            for n in range(12):
                wt, r_w = wbufs.next()
                S.dma("sp", lambda e, wt=wt, n=n, wv=wv: e.dma_start(out=wt[:], in_=wv[:, :, n * 512:(n + 1) * 512]), writes=[r_w], stream="ld")
                pst, r_ps = pss.next()
                for j in range(8):
                    S.op("pe", lambda e, j=j, wt=wt, pst=pst: e.matmul(pst[:], lhsT=scT[:, j, :], rhs=wt[:, j, :], start=(j == 0), stop=False),
                         reads=[r_scT, r_w], writes=[r_ps])
                S.op("pe", lambda e, pst=pst, l=l, n=n: e.matmul(pst[:], lhsT=ones[0:1, 0:2], rhs=bm[0:1, l, n * 512:(n + 1) * 512], start=False, stop=True),
                     reads=[r_bm, r_const], writes=[r_ps])
                S.op("dve", lambda e, pst=pst, l=l, n=n: e.tensor_copy(out=modsb[:, l, n * 512:(n + 1) * 512], in_=pst[:]), reads=[r_ps], writes=[r_mod])
        S.dma("sp", lambda e: e.dma_start(out=modv.rearrange("l r n -> r l n"), in_=modsb[:]), reads=[r_mod], stream="scr")


def load_mod_bc(nc, S, ph, modv, l, r, k, name, extra_g=None, plus1=False, stream="ld"):
    t = ph.enter_context(usb(nc, name, [128, D], F32)); res = Res()
    S.dma("sp", lambda e: e.dma_start(out=t[:], in_=modv[l, r:r + 1, k * D:(k + 1) * D].to_broadcast([128, D])), writes=[res], stream=stream)
    if plus1:
        g = ph.enter_context(usb(nc, name + "_g", [128, D], F32)); rg = Res()
        S.dma("sp", lambda e: e.dma_start(out=g[:], in_=extra_g.to_broadcast([128, D])), writes=[rg], stream=stream)
        S.op("dve", lambda e: e.scalar_tensor_tensor(out=t[:], in0=t[:], scalar=1.0, in1=g[:], op0=ALU.add, op1=ALU.mult), reads=[res, rg], writes=[res])
    return t, res


def rstd_ops(S, xt, r_x, junk, r_junk, st, r_st):
    S.op("act", lambda e: e.activation(out=junk[:], in_=xt[:], func=AF.Square, accum_out=st[:, 0:1]), reads=[r_x], writes=[r_junk, r_st])
    S.op("dve", lambda e: e.tensor_scalar(out=st[:, 1:2], in0=st[:, 0:1], scalar1=1.0 / D, scalar2=EPS, op0=ALU.mult, op1=ALU.add), reads=[r_st], writes=[r_st])
    S.op("act", lambda e: e.sqrt(out=st[:, 2:3], in_=st[:, 1:2]), reads=[r_st], writes=[r_st])
    S.op("dve", lambda e: e.reciprocal(out=st[:, 3:4], in_=st[:, 2:3]), reads=[r_st], writes=[r_st])


def phase1(nc, S, IN, l, xsrc, modv, uT, abd, ident, identb, ones, r_const):
    S.barrier()
    with ExitStack() as ph:
        sb = lambda n, s, d: ph.enter_context(usb(nc, n, s, d))
        G1 = [None, None]; SH1 = [None, None]
        for r in range(2):
            G1[r] = load_mod_bc(nc, S, ph, modv, l, r, 1, f"p1_G{r}", extra_g=IN["g_norm1"][l:l + 1, :], plus1=True)
            SH1[r] = load_mod_bc(nc, S, ph, modv, l, r, 0, f"p1_SH{r}")
        winb = sb("p1_winb", [128, 8, INW], BF16); r_win = Res()
        wv = IN["w_in"][l].rearrange("(j p) n -> p j n", p=128)
        for j in range(8):
            for h in range(2):
                S.dma("pool", lambda e, j=j, h=h: e.dma_start(out=winb[:, j, h * 1288:(h + 1) * 1288], in_=wv[:, j, h * 1288:(h + 1) * 1288]),
                      writes=[r_win], stream="wc")
        xts = Rot([(sb(f"p1_x{i}", [128, D], F32), Res()) for i in range(3)])
        junk = sb("p1_junk", [128, D], BF16); r_junk = Res()
        sts = Rot([(sb(f"p1_st{i}", [128, 4], F32), Res()) for i in range(3)])
        t1s = Rot([(sb(f"p1_t1{i}", [128, D], F32), Res()) for i in range(2)])
        hxbs = Rot([(sb(f"p1_hxb{i}", [128, D], BF16), Res()) for i in range(2)])
        hxTs = Rot([(sb(f"p1_hxT{i}", [128, 8, 512], BF16), Res()) for i in range(2)])
        stg = Rot([(sb(f"p1_stg{i}", [128, 512], F32), Res()) for i in range(3)])
        abs_ = Rot([(sb(f"p1_ab{i}", [128, 16], F32), Res()) for i in range(2)])
        ptr = Rot([(ph.enter_context(ups(nc, f"p1_ptr{i}", [128, 8, 128], BF16)), Res()) for i in range(2)])
        pmm = Rot([(ph.enter_context(ups(nc, f"p1_pmm{i}", [128, 512], F32)), Res()) for i in range(4)])
        pab = Rot([(ph.enter_context(ups(nc, f"p1_pab{i}", [128, 16], F32)), Res()) for i in range(2)])
        blocks = [(b * 4, 4) for b in range(8)] + [(32, 2)]
        for (t0, ntl) in blocks:
            hxT, r_hxT = hxTs.next()
            ntok = ntl * 128
            for ti in range(ntl):
                t = t0 + ti
                r = 0 if t < NTL else 1
                xt, r_x = xts.next()
                S.dma("sp", lambda e, xt=xt, t=t: e.dma_start(out=xt[:], in_=xsrc[t * 128:(t + 1) * 128, :]), writes=[r_x], stream="ld")
                st, r_st = sts.next()
                rstd_ops(S, xt, r_x, junk, r_junk, st, r_st)
                t1, r_t1 = t1s.next()
                S.op("dve", lambda e, t1=t1, xt=xt, st=st, r=r: e.scalar_tensor_tensor(out=t1[:], in0=xt[:], scalar=st[:, 3:4], in1=G1[r][0][:], op0=ALU.mult, op1=ALU.mult),
                     reads=[r_x, r_st, G1[r][1]], writes=[r_t1])
                hxb, r_hxb = hxbs.next()
                S.op("pool", lambda e, hxb=hxb, t1=t1, r=r: e.tensor_tensor(out=hxb[:], in0=t1[:], in1=SH1[r][0][:], op=ALU.add),
                     reads=[r_t1, SH1[r][1]], writes=[r_hxb])
                pt, r_pt = ptr.next()
                for j in range(8):
                    S.op("pe", lambda e, pt=pt, hxb=hxb, j=j: e.transpose(out=pt[:, j, :], in_=hxb[:, j * 128:(j + 1) * 128], identity=identb[:]),
                         reads=[r_hxb, r_const], writes=[r_pt])
                S.op("act", lambda e, pt=pt, hxT=hxT, ti=ti: e.copy(out=hxT[:, :, ti * 128:(ti + 1) * 128], in_=pt[:]), reads=[r_pt], writes=[r_hxT])
                pa, r_pa = pab.next()
                for j in range(8):
                    S.op("pe", lambda e, pa=pa, hxT=hxT, ti=ti, j=j: e.matmul(pa[:], lhsT=hxT[:, j, ti * 128:(ti + 1) * 128], rhs=winb[:, j, 2560:2576], start=(j == 0), stop=(j == 7)),
                         reads=[r_hxT, r_win], writes=[r_pa])
                ab, r_ab = abs_.next()
                S.op("dve", lambda e, ab=ab, pa=pa: e.tensor_copy(out=ab[:], in_=pa[:]), reads=[r_pa], writes=[r_ab])
                S.dma("sp", lambda e, ab=ab, t=t: e.dma_start(out=abd[t * 128:(t + 1) * 128, :], in_=ab[:]), reads=[r_ab], stream="scr")
            for c in range(20):
                pm, r_pm = pmm.next()
                for j in range(8):
                    S.op("pe", lambda e, pm=pm, hxT=hxT, c=c, j=j, ntok=ntok: e.matmul(pm[:, 0:ntok], lhsT=winb[:, j, c * 128:(c + 1) * 128], rhs=hxT[:, j, 0:ntok], start=(j == 0), stop=(j == 7)),
                         reads=[r_hxT, r_win], writes=[r_pm])
                sg, r_sg = stg.next()
                eng = "act" if c % 2 == 0 else "dve"
                if eng == "act":
                    S.op("act", lambda e, sg=sg, pm=pm, ntok=ntok: e.copy(out=sg[:, 0:ntok], in_=pm[:, 0:ntok]), reads=[r_pm], writes=[r_sg])
                else:
                    S.op("dve", lambda e, sg=sg, pm=pm, ntok=ntok: e.tensor_copy(out=sg[:, 0:ntok], in_=pm[:, 0:ntok]), reads=[r_pm], writes=[r_sg])
                S.dma("sp", lambda e, sg=sg, c=c, t0=t0, ntok=ntok: e.dma_start(out=uT[c * 128:(c + 1) * 128, t0 * 128:t0 * 128 + ntok], in_=sg[:, 0:ntok]), reads=[r_sg], stream="scr")


def phase2(nc, S, IN, l, uT, mixT, r_const):
    S.barrier()
    with ExitStack() as ph:
        sb = lambda n, s, d: ph.enter_context(usb(nc, n, s, d))
        cs = sb("p2_cs", [128, 256], F32); r_cs = Res()
        S.dma("sp", lambda e: e.dma_start(out=cs[:], in_=IN["cs128"]), writes=[r_cs], stream="ld")
        ntiles = NT if l == 0 else NTL
        FCS = sb("p2_fcs", [128, NT, 4, 256], BF16); r_fcs = [Res() for _ in range(NT)]
        fts = Rot([(sb(f"p2_ft{i}", [128, 4, 128], F32), Res()) for i in range(3)])
        pas = Rot([(ph.enter_context(ups(nc, f"p2_pa{i}", [128, 4, 256], F32)), Res()) for i in range(2)])
        pos = Rot([(ph.enter_context(ups(nc, f"p2_po{i}", [128, 512], F32)), Res()) for i in range(4)])
        for t in range(ntiles):
            ft, r_ft = fts.next()
            S.dma("sp", lambda e, ft=ft, t=t: e.dma_start(out=ft[:], in_=uT[0:512, t * 128:(t + 1) * 128].rearrange("(g p) t -> p g t", p=128)), writes=[r_ft], stream="ld")
            pa, r_pa = pas.next()
            for g in range(4):
                S.op("pe", lambda e, pa=pa, ft=ft, g=g: e.matmul(pa[:, g, :], lhsT=ft[:, g, :], rhs=cs[:], start=True, stop=True), reads=[r_ft, r_cs], writes=[r_pa])
            if t % 2 == 0:
                S.op("act", lambda e, pa=pa, t=t: e.copy(out=FCS[:, t, :, :], in_=pa[:]), reads=[r_pa], writes=[r_fcs[t]])
            else:
                S.op("dve", lambda e, pa=pa, t=t: e.tensor_copy(out=FCS[:, t, :, :], in_=pa[:]), reads=[r_pa], writes=[r_fcs[t]])
        cosb = Rot([(sb(f"p2_cos{i}", [128, NTL, 256], BF16), Res()) for i in range(2)])
        sinb = Rot([(sb(f"p2_sin{i}", [128, NTL, 256], BF16), Res()) for i in range(2)])
        ostg = Rot([(sb(f"p2_os{i}", [128, 256], BF16), Res()) for i in range(4)])
        segs = [(0, NTL, "L")] + ([(NTL, 2, "C")] if l == 0 else [])
        cvL = IN["cosL"].rearrange("(j p) k -> p j k", p=128)
        svL = IN["nsinL"].rearrange("(j p) k -> p j k", p=128)
        qss = Rot([(sb(f"p2_qs{i}", [128, 256], F32), Res()) for i in range(2)])
        c0t = sb("p2_c0", [128, NTL, 2], BF16); r_c0 = Res()
        S.dma("sp", lambda e: e.dma_start(out=c0t[:], in_=cvL[:, :, 0:2]), writes=[r_c0], stream="ld")
        for g in range(4):
            po, r_po = pos.next()
            for j in range(NTL):
                S.op("pe", lambda e, po=po, j=j, g=g: e.matmul(po[:, 0:2], lhsT=FCS[:, j, g, 0:128], rhs=c0t[:, j, :], start=(j == 0), stop=(j == NTL - 1)), reads=[r_fcs[j], r_c0], writes=[r_po])
            og, r_og = ostg.next()
            S.op("act", lambda e, og=og, po=po: e.copy(out=og[:, 0:2], in_=po[:, 0:2]), reads=[r_po], writes=[r_og])
            S.dma("sp", lambda e, og=og, g=g: e.dma_start(out=mixT[g * 128:(g + 1) * 128, 0:1], in_=og[:, 0:1], allow_slow_non_contiguous=True), reads=[r_og], stream="scr")
        for b in range(8):
            cb, r_cb = cosb.next(); sn, r_sn = sinb.next()
            S.dma("sp", lambda e, cb=cb, b=b: e.dma_start(out=cb[:], in_=cvL[:, :, b * 256 + 1:b * 256 + 257]), writes=[r_cb], stream="ld")
            S.dma("act", lambda e, sn=sn, b=b: e.dma_start(out=sn[:], in_=svL[:, :, b * 256 + 1:b * 256 + 257]), writes=[r_sn], stream="ld2")
            for g in range(4):
                pP, r_pP = pos.next(); pQ, r_pQ = pos.next()
                for j in range(NTL):
                    S.op("pe", lambda e, pP=pP, j=j, g=g, cb=cb: e.matmul(pP[:, 0:256], lhsT=FCS[:, j, g, 0:128], rhs=cb[:, j, :], start=(j == 0), stop=(j == NTL - 1)), reads=[r_fcs[j], r_cb], writes=[r_pP])
                for j in range(NTL):
                    S.op("pe", lambda e, pQ=pQ, j=j, g=g, sn=sn: e.matmul(pQ[:, 0:256], lhsT=FCS[:, j, g, 128:256], rhs=sn[:, j, :], start=(j == 0), stop=(j == NTL - 1)), reads=[r_fcs[j], r_sn], writes=[r_pQ])
                qs, r_qs = qss.next()
                S.op("act", lambda e, qs=qs, pQ=pQ: e.copy(out=qs[:], in_=pQ[:, 0:256]), reads=[r_pQ], writes=[r_qs])
                og1, r_og1 = ostg.next(); og2, r_og2 = ostg.next()
                S.op("dve", lambda e, og1=og1, pP=pP, qs=qs: e.tensor_tensor(out=og1[:], in0=pP[:, 0:256], in1=qs[:], op=ALU.add), reads=[r_pP, r_qs], writes=[r_og1])
                S.op("dve", lambda e, og2=og2, pP=pP, qs=qs: e.tensor_tensor(out=og2[:, ::-1], in0=pP[:, 0:256], in1=qs[:], op=ALU.subtract), reads=[r_pP, r_qs], writes=[r_og2])
                S.dma("sp", lambda e, og1=og1, g=g, b=b: e.dma_start(out=mixT[g * 128:(g + 1) * 128, b * 256 + 1:b * 256 + 257], in_=og1[:]), reads=[r_og1], stream="scr")
                S.dma("sp", lambda e, og2=og2, g=g, b=b: e.dma_start(out=mixT[g * 128:(g + 1) * 128, (15 - b) * 256:(16 - b) * 256], in_=og2[:]), reads=[r_og2], stream="scr")
        segs = [s_ for s_ in segs if s_[2] != "L"]
        for (t0, ntl, nm) in segs:
            cv = IN["cos" + nm].rearrange("(j p) k -> p j k", p=128)
            sv = IN["nsin" + nm].rearrange("(j p) k -> p j k", p=128)
            for kb in range(ntl * 128 // 256):
                cb, r_cb = cosb.next(); sn, r_sn = sinb.next()
                S.dma("sp", lambda e, cb=cb, kb=kb, cv=cv, ntl=ntl: e.dma_start(out=cb[:, 0:ntl, :], in_=cv[:, :, kb * 256:(kb + 1) * 256]), writes=[r_cb], stream="ld")
                S.dma("act", lambda e, sn=sn, kb=kb, sv=sv, ntl=ntl: e.dma_start(out=sn[:, 0:ntl, :], in_=sv[:, :, kb * 256:(kb + 1) * 256]), writes=[r_sn], stream="ld2")
                for g in range(4):
                    po, r_po = pos.next()
                    for j in range(ntl):
                        S.op("pe", lambda e, po=po, j=j, g=g, cb=cb, t0=t0: e.matmul(po[:, 0:256], lhsT=FCS[:, t0 + j, g, 0:128], rhs=cb[:, j, :], start=(j == 0), stop=False),
                             reads=[r_fcs[t0 + j], r_cb], writes=[r_po])
                        S.op("pe", lambda e, po=po, j=j, g=g, sn=sn, t0=t0, ntl=ntl: e.matmul(po[:, 0:256], lhsT=FCS[:, t0 + j, g, 128:256], rhs=sn[:, j, :], start=False, stop=(j == ntl - 1)),
                             reads=[r_fcs[t0 + j], r_sn], writes=[r_po])
                    og, r_og = ostg.next()
                    if g % 2 == 0:
                        S.op("act", lambda e, og=og, po=po: e.copy(out=og[:], in_=po[:, 0:256]), reads=[r_po], writes=[r_og])
                    else:
                        S.op("dve", lambda e, og=og, po=po: e.tensor_copy(out=og[:], in_=po[:, 0:256]), reads=[r_po], writes=[r_og])
                    S.dma("sp", lambda e, og=og, g=g, t0=t0, kb=kb: e.dma_start(out=mixT[g * 128:(g + 1) * 128, t0 * 128 + kb * 256:t0 * 128 + (kb + 1) * 256], in_=og[:]), reads=[r_og], stream="scr")


WC = T + 4


def tcol(t):
    return t * 128 + (4 if t >= NTL else 0)


def phase3(nc, S, IN, l, uT, abd, mixT, ident, ones, r_const, dbg_oT, bg=()):
    S.barrier()
    with ExitStack() as ph:
        sb = lambda n, s, d: ph.enter_context(usb(nc, n, s, d))
        pst = lambda n, s, d: ph.enter_context(ups(nc, n, s, d))
        r_c3 = Res()
        cm = {}
        for nm in ("m_ls", "m_li", "m_us", "m_ui", "tri_f", "tri_b", "blk"):
            cm[nm] = sb("p3_" + nm, [128, 128], F32)
            S.dma("sp", lambda e, nm=nm: e.dma_start(out=cm[nm][:], in_=IN[nm]), writes=[r_c3], stream="ld")
        cwr = sb("p3_cwr", [5, 1536], F32)
        gor = sb("p3_gor", [1, 128], F32)
        S.dma("sp", lambda e: e.dma_start(out=cwr[:], in_=IN["conv_w"][l]), writes=[r_c3], stream="ld")
        S.dma("sp", lambda e: e.dma_start(out=gor[:], in_=IN["g_out_norm"][l:l + 1, :]), writes=[r_c3], stream="ld")
        alb = sb("p3_alb", [128, 8], F32); dtb = sb("p3_dtb", [128, 8], F32)
        S.dma("sp", lambda e: e.dma_start(out=alb[:], in_=IN["a_log"][l:l + 1, :].to_broadcast([128, 8])), writes=[r_c3], stream="ld")
        S.dma("sp", lambda e: e.dma_start(out=dtb[:], in_=IN["dt_bias"][l:l + 1, :].to_broadcast([128, 8])), writes=[r_c3], stream="ld")
        banks = [pst(f"p3_bank{i}", [128, 512], F32) for i in range(8)]
        qtile = lambda b, q: banks[b][:, q * 128:(q + 1) * 128]
        rbank = [Res(excl=True) for _ in range(8)]
        pcw = banks[0][:, 0:104].rearrange("p (m k) -> p m k", k=8); r_pcw = rbank[0]
        cw = sb("p3_cw", [128, 13, 8], F32)
        for m in range(12):
            S.op("pe", lambda e, m=m: e.transpose(out=pcw[:, m, 0:5], in_=cwr[:, m * 128:(m + 1) * 128], identity=ident[0:5, 0:5]), reads=[r_c3, r_const], writes=[r_pcw])
        S.op("pe", lambda e: e.transpose(out=pcw[:, 12, 0:1], in_=gor[:, :], identity=ident[0:1, 0:1]), reads=[r_c3, r_const], writes=[r_pcw])
        r_cw = Res()
        S.op("dve", lambda e: e.memset(cw[:], 0.0), writes=[r_cw])
        for m in range(12):
            S.op("dve", lambda e, m=m: e.tensor_copy(out=cw[:, m, 0:5], in_=pcw[:, m, 0:5]), reads=[r_pcw], writes=[r_cw])
        S.op("dve", lambda e: e.tensor_copy(out=cw[:, 12, 0:1], in_=pcw[:, 12, 0:1]), reads=[r_pcw], writes=[r_cw])
        nea = sb("p3_nea", [128, 8], F32)
        S.op("act", lambda e: e.activation(out=nea[:], in_=alb[:], func=AF.Exp), reads=[r_c3], writes=[r_c3])
        S.op("dve", lambda e: e.tensor_scalar(out=nea[:], in0=nea[:], scalar1=-1.0, scalar2=None, op0=ALU.mult), reads=[r_c3], writes=[r_c3])
        names = ("BT", "NBT", "GAM", "EG", "BEG", "EK0", "EK1")
        GA = {nm: sb("p3_" + nm, [128, NT, 8], F32) for nm in names}
        r_ga = [Res() for _ in range(NT)]
        abt = Rot([(sb(f"p3_abt{i}", [128, 16], F32), Res()) for i in range(2)])
        tmp = Rot([(sb(f"p3_gt{i}", [128, 4, 8], F32), Res()) for i in range(2)])
        pgs = Rot([(banks[0][:, 128:144], r_pcw)])
        rowm = sb("p3_rowm", [128, 2], F32)
        S.op("dve", lambda e: e.tensor_copy(out=rowm[:, 0:1], in_=cm["blk"][:, 0:1]), reads=[r_c3], writes=[r_c3])
        S.op("dve", lambda e: e.tensor_copy(out=rowm[:, 1:2], in_=cm["blk"][:, 127:128]), reads=[r_c3], writes=[r_c3])
        for t in range(NT):
            ab, r_ab = abt.next()
            S.dma("sp", lambda e, ab=ab, t=t: e.dma_start(out=ab[:], in_=abd[t * 128:(t + 1) * 128, :]), writes=[r_ab], stream="ld")
            tm, r_tm = tmp.next()
            abv = ab[:].rearrange("p (d k h) -> p d k h", d=2, k=2)
            X = tm[:, 0, :].rearrange("p (d h) -> p d h", d=2)
            S.op("dve", lambda e, X=X, abv=abv: e.tensor_tensor(out=X, in0=abv[:, :, 0, :], in1=dtb[:].rearrange("p (d h) -> p d h", d=2), op=ALU.add), reads=[r_ab, r_c3], writes=[r_tm])
            S.op("act", lambda e, tm=tm: e.activation(out=tm[:, 0, :], in_=tm[:, 0, :], func=AF.Exp), reads=[r_tm], writes=[r_tm])
            S.op("act", lambda e, tm=tm: e.activation(out=tm[:, 0, :], in_=tm[:, 0, :], func=AF.Ln, bias=1.0), reads=[r_tm], writes=[r_tm])
            S.op("dve", lambda e, tm=tm: e.tensor_tensor(out=tm[:, 1, :], in0=tm[:, 0, :], in1=nea[:], op=ALU.mult), reads=[r_tm, r_c3], writes=[r_tm])
            B = tm[:, 2, :].rearrange("p (d h) -> p d h", d=2)
            S.op("act", lambda e, B=B, abv=abv: e.activation(out=B, in_=abv[:, :, 1, :], func=AF.Exp, scale=-1.0), reads=[r_ab], writes=[r_tm])
            S.op("dve", lambda e, tm=tm: e.tensor_scalar(out=tm[:, 2, :], in0=tm[:, 2, :], scalar1=1.0, scalar2=None, op0=ALU.add), reads=[r_tm], writes=[r_tm])
            S.op("dve", lambda e, tm=tm, t=t: e.reciprocal(out=GA["BT"][:, t, :], in_=tm[:, 2, :]), reads=[r_tm], writes=[r_ga[t]])
            S.op("dve", lambda e, t=t: e.tensor_scalar(out=GA["NBT"][:, t, :], in0=GA["BT"][:, t, :], scalar1=-1.0, scalar2=None, op0=ALU.mult), reads=[r_ga[t]], writes=[r_ga[t]])
            pg, r_pg = pgs.next()
            S.op("pe", lambda e, pg=pg, tm=tm: e.matmul(pg[:, 0:4], lhsT=cm["tri_f"][:], rhs=tm[:, 1, 0:4], start=True, stop=True), reads=[r_tm, r_c3], writes=[r_pg])
            S.op("pe", lambda e, pg=pg, tm=tm: e.matmul(pg[:, 4:8], lhsT=cm["tri_b"][:], rhs=tm[:, 1, 4:8], start=True, stop=True), reads=[r_tm, r_c3], writes=[r_pg])
            S.op("pe", lambda e, pg=pg, tm=tm: e.matmul(pg[:, 8:16], lhsT=cm["blk"][:], rhs=tm[:, 1, :], start=True, stop=True), reads=[r_tm, r_c3], writes=[r_pg])
            S.op("dve", lambda e, pg=pg, t=t: e.tensor_copy(out=GA["GAM"][:, t, :], in_=pg[:, 0:8]), reads=[r_pg], writes=[r_ga[t]])
            S.op("act", lambda e, pg=pg, t=t: e.activation(out=GA["EG"][:, t, :], in_=pg[:, 0:8], func=AF.Exp), reads=[r_pg], writes=[r_ga[t]])
            S.op("dve", lambda e, t=t: e.tensor_tensor(out=GA["BEG"][:, t, :], in0=GA["EG"][:, t, :], in1=GA["BT"][:, t, :], op=ALU.mult), reads=[r_ga[t]], writes=[r_ga[t]])
            S.op("dve", lambda e, pg=pg, tm=tm, t=t: e.tensor_tensor(out=tm[:, 3, :], in0=pg[:, 8:16], in1=GA["GAM"][:, t, :], op=ALU.subtract), reads=[r_pg, r_ga[t]], writes=[r_tm])
            S.op("act", lambda e, tm=tm: e.activation(out=tm[:, 3, :], in_=tm[:, 3, :], func=AF.Exp), reads=[r_tm], writes=[r_tm])
            S.op("dve", lambda e, tm=tm, t=t: e.tensor_scalar(out=GA["EK0"][:, t, :], in0=tm[:, 3, :], scalar1=rowm[:, 0:1], scalar2=None, op0=ALU.mult), reads=[r_tm, r_c3], writes=[r_ga[t]])
            S.op("dve", lambda e, tm=tm, t=t: e.tensor_scalar(out=GA["EK1"][:, t, :], in0=tm[:, 3, :], scalar1=rowm[:, 1:2], scalar2=None, op0=ALU.mult), reads=[r_tm, r_c3], writes=[r_ga[t]])
        raws = Rot([(sb(f"p3_raw{i}", [128, WC + 4], F32), Res()) for i in range(2)])
        QKV = [(sb(f"p3_qkv{i}", [128, WC], F32), Res()) for i in range(3)]
        oT = sb("p3_oT", [128, WC], F32); r_oT = [Res() for _ in range(NT)]
        r_oTall = Res()
        pn = banks[1]; r_pn = rbank[1]
        rns = Rot([(sb(f"p3_rn{i}", [128, 512], F32), Res()) for i in range(2)])
        zts = Rot([(sb(f"p3_z{i}", [128, 512], F32), Res()) for i in range(2)])
        obs = Rot([(sb(f"p3_ob{i}", [128, 512], BF16), Res()) for i in range(2)])

        KSLOT = 4; DEPTH = 3
        INTER = ("dg", "Dm", "E1", "E2", "N", "NTs", "TT", "Pa", "PTa", "Pb", "PTb", "Rv", "Rw")
        OUTS = ("EGr", "at", "u", "wT", "qg", "ke0", "ke1")
        BF_NAMES = ("Nb", "NTs", "TT", "Pa", "PTa", "Pb", "PTb", "Rv", "Rw")
        BI = [{n: (sb(f"p3_{n}_s{s}", [128, 128], BF16 if n in BF_NAMES else F32), Res()) for n in INTER + ("Nb",)} for s in range(KSLOT)]
        BO = [{n: Rot([(sb(f"p3_{n}_d{d}_{i}", [128, 128], F32), Res()) for i in range(DEPTH)]) for n in OUTS} for d in range(2)]
        VN = [Rot([(sb(f"p3_vn{d}_{i}", [128, 128], F32), Res()) for i in range(2)]) for d in range(2)]
        rbank_ = rbank
        slot_bank = (2, 3, 4, 7)
        PQ = [Rot([(qtile(slot_bank[s], qi), rbank_[slot_bank[s]]) for qi in range(4)]) for s in range(KSLOT)]
        PSC = {d: {n: (qtile(5 + d, qi), rbank_[5 + d]) for qi, n in enumerate(("ps1", "po", "pS"))} for d in range(2)}
        Sst = [(sb(f"p3_S{d}", [128, 128], F32), Res()) for d in range(2)]
        bg = list(bg)
        chunks9 = [(i * 512, 512) for i in range(8)] + [(4100, 256)]

        LVL = 9; NTI = NT
        for h in range(4 if LVL >= 9 else (1 if LVL >= 1 else 0)):
            for which in range(3):
                raw, r_raw = raws.next()
                row0 = 512 + which * 512 + h * 128
                S.op("pool", lambda e, raw=raw: e.memset(raw[:], 0.0), writes=[r_raw])
                S.dma("sp", lambda e, raw=raw, row0=row0: e.dma_start(out=raw[:, 2:2 + SEQ], in_=uT[row0:row0 + 128, 0:SEQ]), writes=[r_raw], stream="ld")
                S.dma("sp", lambda e, raw=raw, row0=row0: e.dma_start(out=raw[:, SEQ + 6:SEQ + 6 + CTX], in_=uT[row0:row0 + 128, SEQ:T]), writes=[r_raw], stream="ld")
                dst, r_dst = QKV[which]
                m = which * 4 + h
                S.op("dve", lambda e, dst=dst, raw=raw, m=m: e.tensor_scalar(out=dst[:], in0=raw[:, 0:WC], scalar1=cw[:, m, 0:1], scalar2=None, op0=ALU.mult), reads=[r_raw, r_cw], writes=[r_dst])
                for k in range(1, 5):
                    S.op("dve", lambda e, dst=dst, raw=raw, m=m, k=k: e.scalar_tensor_tensor(out=dst[:], in0=raw[:, k:k + WC], scalar=cw[:, m, k:k + 1], in1=dst[:], op0=ALU.mult, op1=ALU.add),
                         reads=[r_raw, r_cw, r_dst], writes=[r_dst])
                S.op("act", lambda e, dst=dst: e.activation(out=dst[:], in_=dst[:], func=AF.Silu), reads=[r_dst], writes=[r_dst])
                if which < 2:
                    sq, r_sq = raws.items[(raws.i) % 2]
                    S.op("pool", lambda e, sq=sq, dst=dst: e.tensor_tensor(out=sq[:, 0:WC], in0=dst[:], in1=dst[:], op=ALU.mult), reads=[r_dst], writes=[r_sq])
                    for (c0, cn) in chunks9:
                        S.op("pe", lambda e, sq=sq, c0=c0, cn=cn: e.matmul(pn[:, 0:cn], lhsT=ones[:], rhs=sq[:, c0:c0 + cn], start=True, stop=True), reads=[r_sq, r_const], writes=[r_pn])
                        rn, r_rn = rns.next()
                        S.op("dve", lambda e, rn=rn, cn=cn: e.tensor_scalar(out=rn[:, 0:cn], in0=pn[:, 0:cn], scalar1=EPS, scalar2=None, op0=ALU.add), reads=[r_pn], writes=[r_rn])
                        S.op("act", lambda e, rn=rn, cn=cn: e.sqrt(out=rn[:, 0:cn], in_=rn[:, 0:cn]), reads=[r_rn], writes=[r_rn])
                        S.op("dve", lambda e, rn=rn, cn=cn: e.reciprocal(out=rn[:, 0:cn], in_=rn[:, 0:cn]), reads=[r_rn], writes=[r_rn])
                        sc = (128.0 ** -0.5) if which == 0 else 1.0
                        S.op("dve", lambda e, rn=rn, dst=dst, c0=c0, cn=cn, sc=sc: e.scalar_tensor_tensor(out=dst[:, c0:c0 + cn], in0=dst[:, c0:c0 + cn], scalar=sc, in1=rn[:, 0:cn], op0=ALU.mult, op1=ALU.mult),
                             reads=[r_rn, r_dst], writes=[r_dst])
            qT, r_q = QKV[0]; kT, r_k = QKV[1]; vT, r_v = QKV[2]
            for d in range(2):
                S.op("dve", lambda e, d=d: e.memset(Sst[d][0][:], 0.0), writes=[Sst[d][1]])
            seqs = [[32, 33] + list(range(32)), [33, 32] + list(range(31, -1, -1))]
            if LVL < 2:
                seqs = [[], []]
            else:
                seqs = [s_[:NTI] for s_ in seqs]
            written = set()
            PREP = {}
            scanned = [0, 0]

            def prep_gen(t, d, s):
                c0 = tcol(t); col = d * 4 + h
                bi = BI[s]; pq = PQ[s]
                ksl = kT[:, c0:c0 + 128]; qsl = qT[:, c0:c0 + 128]; vsl = vT[:, c0:c0 + 128]
                gam = GA["GAM"][:, t, col:col + 1]
                dg, r_dg = bi["dg"]; Dm, r_Dm = bi["Dm"]; E1, r_E1 = bi["E1"]; E2, r_E2 = bi["E2"]
                EGr, r_EGr = BO[d]["EGr"].next()
                gr, r_gr = pq.next()
                S.op("dve", lambda e: e.tensor_scalar(out=dg[:], in0=ident[:], scalar1=gam, scalar2=None, op0=ALU.mult), reads=[r_const, r_ga[t]], writes=[r_dg])
                S.op("pe", lambda e: e.matmul(gr[:], lhsT=ones[:], rhs=dg[:], start=True, stop=True), reads=[r_dg, r_const], writes=[r_gr])
                S.op("dve", lambda e: e.tensor_scalar(out=Dm[:], in0=gr[:], scalar1=-1.0, scalar2=gam, op0=ALU.mult, op1=ALU.add), reads=[r_gr, r_ga[t]], writes=[r_Dm])
                S.op("act", lambda e: e.activation(out=EGr[:], in_=gr[:], func=AF.Exp), reads=[r_gr], writes=[r_EGr])
                yield
                m1 = cm["m_ls"] if d == 0 else cm["m_us"]
                m2 = cm["m_ui"] if d == 0 else cm["m_li"]
                S.op("pool", lambda e: e.tensor_tensor(out=E1[:], in0=Dm[:], in1=m1[:], op=ALU.add), reads=[r_Dm, r_c3], writes=[r_E1])
                S.op("pool", lambda e: e.tensor_tensor(out=E2[:], in0=m2[:], in1=Dm[:], op=ALU.subtract), reads=[r_Dm, r_c3], writes=[r_E2])
                S.op("act", lambda e: e.activation(out=E1[:], in_=E1[:], func=AF.Exp), reads=[r_E1], writes=[r_E1])
                S.op("act", lambda e: e.activation(out=E2[:], in_=E2[:], func=AF.Exp), reads=[r_E2], writes=[r_E2])
                yield
                N, r_N = bi["N"]; at, r_at = BO[d]["at"].next()
                nbt = GA["NBT"][:, t, col:col + 1]
                kk, r_kk = pq.next()
                S.op("pe", lambda e: e.matmul(kk[:], lhsT=ksl, rhs=ksl, start=True, stop=True), reads=[r_k], writes=[r_kk])
                S.op("dve", lambda e: e.scalar_tensor_tensor(out=N[:], in0=kk[:], scalar=nbt, in1=E1[:], op0=ALU.mult, op1=ALU.mult), reads=[r_kk, r_ga[t], r_E1], writes=[r_N])
                Nb, r_Nb = bi["Nb"]
                S.op("act", lambda e: e.copy(out=Nb[:], in_=N[:]), reads=[r_N], writes=[r_Nb])
                kq, r_kq = pq.next()
                S.op("pe", lambda e: e.matmul(kq[:], lhsT=ksl, rhs=qsl, start=True, stop=True), reads=[r_k, r_q], writes=[r_kq])
                S.op("dve", lambda e: e.tensor_tensor(out=at[:], in0=kq[:], in1=E2[:], op=ALU.mult), reads=[r_kq, r_E2], writes=[r_at])
                yield
                Rv, r_Rv = bi["Rv"]; Rw, r_Rw = bi["Rw"]
                ke0, r_ke0 = BO[d]["ke0"].next(); ke1, r_ke1 = BO[d]["ke1"].next(); qg, r_qg = BO[d]["qg"].next()
                bt = GA["BT"][:, t, col:col + 1]; beg = GA["BEG"][:, t, col:col + 1]
                kt, r_kt = pq.next()
                S.op("pe", lambda e: e.transpose(out=kt[:], in_=ksl, identity=ident[:]), reads=[r_k, r_const], writes=[r_kt])
                S.op("act", lambda e: e.activation(out=Rw[:], in_=kt[:], func=AF.Copy, scale=beg), reads=[r_kt, r_ga[t]], writes=[r_Rw])
                S.op("dve", lambda e: e.tensor_scalar(out=ke0[:], in0=kt[:], scalar1=GA["EK0"][:, t, col:col + 1], scalar2=None, op0=ALU.mult), reads=[r_kt, r_ga[t]], writes=[r_ke0])
                S.op("dve", lambda e: e.tensor_scalar(out=ke1[:], in0=kt[:], scalar1=GA["EK1"][:, t, col:col + 1], scalar2=None, op0=ALU.mult), reads=[r_kt, r_ga[t]], writes=[r_ke1])
                vt, r_vt = pq.next()
                S.op("pe", lambda e: e.transpose(out=vt[:], in_=vsl, identity=ident[:]), reads=[r_v, r_const], writes=[r_vt])
                S.op("act", lambda e: e.activation(out=Rv[:], in_=vt[:], func=AF.Copy, scale=bt), reads=[r_vt, r_ga[t]], writes=[r_Rv])
                S.op("pool", lambda e: e.tensor_tensor(out=qg[:], in0=qsl, in1=EGr[:], op=ALU.mult), reads=[r_q, r_EGr], writes=[r_qg])
                yield
                NTs, r_NTs = bi["NTs"]; TT, r_TT = bi["TT"]
                ntp, r_ntp = pq.next()
                S.op("pe", lambda e: e.transpose(out=ntp[:], in_=N[:], identity=ident[:]), reads=[r_N, r_const], writes=[r_ntp])
                S.op("act", lambda e: e.copy(out=NTs[:], in_=ntp[:]), reads=[r_ntp], writes=[r_NTs])
                S.op("dve", lambda e: e.tensor_tensor(out=TT[:], in0=ntp[:], in1=ident[:], op=ALU.add), reads=[r_ntp, r_const], writes=[r_TT])
                yield
                P_, r_P = Nb, r_Nb
                PT_, r_PT = NTs, r_NTs
                for lev in range(1, 6):
                    p2, r_p2 = pq.next()
                    S.op("pe", lambda e, p2=p2, PT_=PT_, P_=P_: e.matmul(p2[:], lhsT=PT_[:], rhs=P_[:], start=True, stop=True), reads=[r_P, r_PT], writes=[r_p2])
                    nP, r_nP = bi["Pa" if lev % 2 else "Pb"]
                    S.op("act", lambda e, nP=nP, p2=p2: e.copy(out=nP[:], in_=p2[:]), reads=[r_p2], writes=[r_nP])
                    if lev < 5:
                        pt2, r_pt2 = pq.next()
                        S.op("pe", lambda e, pt2=pt2, PT_=PT_, P_=P_: e.matmul(pt2[:], lhsT=P_[:], rhs=PT_[:], start=True, stop=True), reads=[r_P, r_PT], writes=[r_pt2])
                        nPT, r_nPT = bi["PTa" if lev % 2 else "PTb"]
                        S.op("dve", lambda e, nPT=nPT, pt2=pt2: e.tensor_copy(out=nPT[:], in_=pt2[:]), reads=[r_pt2], writes=[r_nPT])
                    yield
                    up, r_up = pq.next()
                    S.op("pe", lambda e, up=up, nP=nP: e.matmul(up[:], lhsT=nP[:], rhs=TT[:], start=True, stop=True), reads=[r_nP, r_TT], writes=[r_up])
                    S.op("dve", lambda e, up=up: e.tensor_tensor(out=TT[:], in0=up[:], in1=TT[:], op=ALU.add), reads=[r_up, r_TT], writes=[r_TT])
                    P_, r_P = nP, r_nP
                    if lev < 5:
                        PT_, r_PT = nPT, r_nPT
                    yield
                u, r_u = BO[d]["u"].next(); wT, r_wT = BO[d]["wT"].next()
                pu, r_pu = pq.next()
                S.op("pe", lambda e: e.matmul(pu[:], lhsT=TT[:], rhs=Rv[:], start=True, stop=True), reads=[r_TT, r_Rv], writes=[r_pu])
                S.op("act", lambda e: e.copy(out=u[:], in_=pu[:]), reads=[r_pu], writes=[r_u])
                pw, r_pw = pq.next()
                S.op("pe", lambda e: e.matmul(pw[:], lhsT=Rw[:], rhs=TT[:], start=True, stop=True), reads=[r_TT, r_Rw], writes=[r_pw])
                S.op("dve", lambda e: e.tensor_copy(out=wT[:], in_=pw[:]), reads=[r_pw], writes=[r_wT])
                PREP[(t, d)] = dict(EGr=(EGr, r_EGr), at=(at, r_at), u=(u, r_u), wT=(wT, r_wT), qg=(qg, r_qg), ke0=(ke0, r_ke0), ke1=(ke1, r_ke1))

            def scan_gen(d):
                Sd, r_S = Sst[d]
                for t in seqs[d]:
                    while (t, d) not in PREP:
                        yield "wait"
                    pr = PREP[(t, d)]
                    c0 = tcol(t)
                    EGr, r_EGr = pr["EGr"]; at, r_at = pr["at"]; u, r_u = pr["u"]; wT, r_wT = pr["wT"]; qg, r_qg = pr["qg"]
                    for c in ((0, 1) if d == 0 else (1, 0)):
                        cs_ = slice(c * 64, (c + 1) * 64)
                        gcol = c * 64 + (63 if d == 0 else 0)
                        ps1, r_ps1 = PSC[d]["ps1"]; po, r_po = PSC[d]["po"]; pS, r_pS = PSC[d]["pS"]
                        vn, r_vn = VN[d].next()
                        ke, r_ke = pr["ke0"] if c == 0 else pr["ke1"]
                        S.op("pe", lambda e, wT=wT: e.matmul(ps1[:], lhsT=wT[:], rhs=Sd[:], start=True, stop=True), reads=[r_wT, r_S], writes=[r_ps1])
                        yield
                        S.op("dve", lambda e, vn=vn, u=u: e.tensor_tensor(out=vn[:], in0=u[:], in1=ps1[:], op=ALU.subtract), reads=[r_u, r_ps1], writes=[r_vn])
                        yield
                        S.op("pe", lambda e, qg=qg, cs_=cs_: e.matmul(po[:, 0:64], lhsT=Sd[:], rhs=qg[:, cs_], start=True, stop=False), reads=[r_S, r_qg], writes=[r_po])
                        S.op("pe", lambda e, vn=vn, at=at, cs_=cs_: e.matmul(po[:, 0:64], lhsT=vn[:], rhs=at[:, cs_], start=False, stop=True), reads=[r_vn, r_at], writes=[r_po])
                        S.op("pe", lambda e, ke=ke, vn=vn: e.matmul(pS[:], lhsT=ke[:], rhs=vn[:], start=True, stop=True), reads=[r_ke, r_vn], writes=[r_pS])
                        yield
                        osl = oT[:, c0 + c * 64:c0 + (c + 1) * 64]
                        if (t, c) not in written:
                            written.add((t, c))
                            S.op("act", lambda e, osl=osl: e.copy(out=osl, in_=po[:, 0:64]), reads=[r_po], writes=[r_oT[t]])
                        else:
                            S.op("dve", lambda e, osl=osl: e.tensor_tensor(out=osl, in0=po[:, 0:64], in1=osl, op=ALU.add), reads=[r_po, r_oT[t]], writes=[r_oT[t]])
                        S.op("dve", lambda e, EGr=EGr, gcol=gcol: e.scalar_tensor_tensor(out=Sd[:], in0=Sd[:], scalar=EGr[:, gcol:gcol + 1], in1=pS[:], op0=ALU.mult, op1=ALU.add),
                             reads=[r_S, r_EGr, r_pS], writes=[r_S])
                        yield
                    scanned[d] += 1

            queue = []
            for i in range(len(seqs[0])):
                for d in range(2):
                    queue.append((seqs[d][i], d, i))
            active = {}
            scans = [scan_gen(0), scan_gen(1)]
            scan_done = [len(seqs[0]) == 0, len(seqs[1]) == 0]
            nbg = 0
            SCANFIRST = 1; SCANSTEPS = 2

            def adv_scans():
                pr_ = False
                for d in range(2):
                    for _ in range(SCANSTEPS):
                        if not scan_done[d]:
                            try:
                                r_ = next(scans[d])
                                if r_ != "wait":
                                    pr_ = True
                                else:
                                    break
                            except StopIteration:
                                scan_done[d] = True; pr_ = True
                return pr_
            while not all(scan_done):
                progressed = False
                if SCANFIRST:
                    progressed = adv_scans() or progressed
                while queue and len(active) < KSLOT and (queue[0][2] - scanned[queue[0][1]] < DEPTH):
                    t_, d_, i_ = queue.pop(0)
                    s_ = [x for x in range(KSLOT) if x not in active][0]
                    active[s_] = prep_gen(t_, d_, s_)
                    progressed = True
                    nbg += 1
                    if bg and nbg % 2 == 0:
                        bg.pop(0)()
                for s_ in list(active.keys()):
                    try:
                        next(active[s_]); progressed = True
                    except StopIteration:
                        del active[s_]; progressed = True
                if not SCANFIRST:
                    progressed = adv_scans() or progressed
                assert progressed, "phase3 scheduler stuck"
            if dbg_oT is not None:
                S.dma("sp", lambda e, h=h: e.dma_start(out=dbg_oT[h, :, 0:SEQ], in_=oT[:, 0:SEQ]), reads=r_oT, stream="scr")
                S.dma("sp", lambda e, h=h: e.dma_start(out=dbg_oT[h, :, SEQ:T], in_=oT[:, SEQ + 4:SEQ + 4 + CTX]), reads=r_oT, stream="scr")
            sq, r_sq = raws.next()
            for ci, (c0, cn) in enumerate(chunks9):
                tiles = list(range(ci * 4, ci * 4 + 4)) if ci < 8 else [32, 33]
                rds = [r_oT[t] for t in tiles]
                S.op("pool", lambda e, sq=sq, c0=c0, cn=cn: e.tensor_tensor(out=sq[:, c0:c0 + cn], in0=oT[:, c0:c0 + cn], in1=oT[:, c0:c0 + cn], op=ALU.mult), reads=rds, writes=[r_sq])
                S.op("pe", lambda e, sq=sq, c0=c0, cn=cn: e.matmul(pn[:, 0:cn], lhsT=ones[:], rhs=sq[:, c0:c0 + cn], start=True, stop=True), reads=[r_sq, r_const], writes=[r_pn])
                rn, r_rn = rns.next()
                S.op("dve", lambda e, rn=rn, cn=cn: e.tensor_scalar(out=rn[:, 0:cn], in0=pn[:, 0:cn], scalar1=1.0 / 128, scalar2=EPS, op0=ALU.mult, op1=ALU.add), reads=[r_pn], writes=[r_rn])
                S.op("act", lambda e, rn=rn, cn=cn: e.sqrt(out=rn[:, 0:cn], in_=rn[:, 0:cn]), reads=[r_rn], writes=[r_rn])
                S.op("dve", lambda e, rn=rn, cn=cn: e.reciprocal(out=rn[:, 0:cn], in_=rn[:, 0:cn]), reads=[r_rn], writes=[r_rn])
                S.op("dve", lambda e, rn=rn, c0=c0, cn=cn: e.scalar_tensor_tensor(out=rn[:, 0:cn], in0=oT[:, c0:c0 + cn], scalar=cw[:, 12, 0:1], in1=rn[:, 0:cn], op0=ALU.mult, op1=ALU.mult),
                     reads=rds + [r_rn, r_cw], writes=[r_rn])
                zt, r_zt = zts.next()
                tok0 = ci * 512 if ci < 8 else SEQ
                zrow = 2048 + h * 128
                S.dma("sp", lambda e, zt=zt, tok0=tok0, cn=cn, zrow=zrow: e.dma_start(out=zt[:, 0:cn], in_=uT[zrow:zrow + 128, tok0:tok0 + cn]), writes=[r_zt], stream="ld")
                S.op("act", lambda e, zt=zt, cn=cn: e.activation(out=zt[:, 0:cn], in_=zt[:, 0:cn], func=AF.Silu), reads=[r_zt], writes=[r_zt])
                ob, r_ob = obs.next()
                S.op("pool", lambda e, ob=ob, rn=rn, zt=zt, cn=cn: e.tensor_tensor(out=ob[:, 0:cn], in0=rn[:, 0:cn], in1=zt[:, 0:cn], op=ALU.mult), reads=[r_rn, r_zt], writes=[r_ob])
                S.dma("sp", lambda e, ob=ob, tok0=tok0, cn=cn, h=h: e.dma_start(out=mixT[512 + h * 128:512 + (h + 1) * 128, tok0:tok0 + cn], in_=ob[:, 0:cn]), reads=[r_ob], stream="scr")
        while bg:
            bg.pop(0)()


def phase4(nc, S, IN, l, xsrc, xres, modv, mixT, h2rows, gates, r_gates, ident, ones, r_const, nt_act, dbg_g):
    S.barrier()
    with ExitStack() as ph:
        sb = lambda n, s, d: ph.enter_context(usb(nc, n, s, d))
        pst = lambda n, s, d: ph.enter_context(ups(nc, n, s, d))
        nr = 2 if nt_act > NTL else 1
        GT1 = [load_mod_bc(nc, S, ph, modv, l, r, 2, f"p4_GT{r}") for r in range(nr)]
        G2 = [load_mod_bc(nc, S, ph, modv, l, r, 4, f"p4_G{r}", extra_g=IN["g_norm2"][l:l + 1, :], plus1=True) for r in range(nr)]
        SH2 = [load_mod_bc(nc, S, ph, modv, l, r, 3, f"p4_SH{r}") for r in range(nr)]
        woutb = sb("p4_wout", [128, 8, D], BF16); r_wo = Res()
        wv = IN["w_out"][l].rearrange("(j p) n -> p j n", p=128)
        for j in range(8):
            S.dma("pool", lambda e, j=j: e.dma_start(out=woutb[:, j, :], in_=wv[:, j, :]), writes=[r_wo], stream="wc")
        wrf = sb("p4_wr", [128, 8, NE], F32); r_wr = Res()
        brr = sb("p4_br", [1, NE], F32)
        S.dma("sp", lambda e: e.dma_start(out=wrf[:], in_=IN["w_router"][l].rearrange("(j p) n -> p j n", p=128)), writes=[r_wr], stream="ld")
        S.dma("sp", lambda e: e.dma_start(out=brr[:], in_=IN["b_router"][l:l + 1, :]), writes=[r_wr], stream="ld")
        mixs = Rot([(sb(f"p4_mx{i}", [128, 8, 128], BF16), Res()) for i in range(2)])
        xts = Rot([(sb(f"p4_x{i}", [128, D], F32), Res()) for i in range(2)])
        tmps = Rot([(sb(f"p4_t{i}", [128, D], F32), Res()) for i in range(2)])
        xns = Rot([(sb(f"p4_xn{i}", [128, D], F32), Res()) for i in range(2)])
        h2s = Rot([(sb(f"p4_h2{i}", [128, D], F32), Res()) for i in range(2)])
        junk = sb("p4_junk", [128, D], BF16); r_junk = Res()
        sts = Rot([(sb(f"p4_st{i}", [128, 4], F32), Res()) for i in range(2)])
        h2bs = Rot([(sb(f"p4_hb{i}", [128, D], BF16), Res()) for i in range(2)])
        h2fs = Rot([(sb(f"p4_hf{i}", [128, 8, 128], F32), Res()) for i in range(2)])
        lgs = Rot([(sb(f"p4_lg{i}", [128, 4, NE], F32), Res()) for i in range(2)])
        t8s = Rot([(sb(f"p4_t8{i}", [128, 16], F32), Res()) for i in range(2)])
        pys = Rot([(pst(f"p4_py{i}", [128, D], F32), Res()) for i in range(2)])
        ptr = Rot([(pst("p4_ptr", [128, 8, 128], F32), Res(excl=True))])
        pls = Rot([(pst(f"p4_pl{i}", [128, NE], F32), Res()) for i in range(2)])
        for t in range(nt_act):
            r = 0 if t < NTL else 1
            mx, r_mx = mixs.next()
            S.dma("sp", lambda e, mx=mx, t=t: e.dma_start(out=mx[:], in_=mixT[:, t * 128:(t + 1) * 128].rearrange("(j p) t -> p j t", p=128)), writes=[r_mx], stream="ld")
            xt, r_x = xts.next()
            S.dma("act", lambda e, xt=xt, t=t: e.dma_start(out=xt[:], in_=xsrc[t * 128:(t + 1) * 128, :]), writes=[r_x], stream="ld2")
            py, r_py = pys.next()
            for half in range(2):
                for j in range(8):
                    S.op("pe", lambda e, py=py, mx=mx, half=half, j=j: e.matmul(py[:, half * 512:(half + 1) * 512], lhsT=mx[:, j, :], rhs=woutb[:, j, half * 512:(half + 1) * 512], start=(j == 0), stop=(j == 7)),
                         reads=[r_mx, r_wo], writes=[r_py])
            tp, r_tp = tmps.next()
            S.op("dve", lambda e, tp=tp, py=py, r=r: e.tensor_tensor(out=tp[:], in0=py[:], in1=GT1[r][0][:], op=ALU.mult), reads=[r_py, GT1[r][1]], writes=[r_tp])
            xn, r_xn = xns.next()
            S.op("pool", lambda e, xn=xn, tp=tp, xt=xt: e.tensor_tensor(out=xn[:], in0=tp[:], in1=xt[:], op=ALU.add), reads=[r_tp, r_x], writes=[r_xn])
            S.dma("sp", lambda e, xn=xn, t=t: e.dma_start(out=xres[t * 128:(t + 1) * 128, :], in_=xn[:]), reads=[r_xn], stream="scr")
            st, r_st = sts.next()
            rstd_ops(S, xn, r_xn, junk, r_junk, st, r_st)
            h2, r_h2 = h2s.next()
            S.op("dve", lambda e, h2=h2, xn=xn, st=st, r=r: e.scalar_tensor_tensor(out=h2[:], in0=xn[:], scalar=st[:, 3:4], in1=G2[r][0][:], op0=ALU.mult, op1=ALU.mult), reads=[r_xn, r_st, G2[r][1]], writes=[r_h2])
            S.op("pool", lambda e, h2=h2, r=r: e.tensor_tensor(out=h2[:], in0=h2[:], in1=SH2[r][0][:], op=ALU.add), reads=[r_h2, SH2[r][1]], writes=[r_h2])
            pt, r_pt = ptr.next()
            for j in range(8):
                S.op("pe", lambda e, pt=pt, h2=h2, j=j: e.transpose(out=pt[:, j, :], in_=h2[:, j * 128:(j + 1) * 128], identity=ident[:]), reads=[r_h2, r_const], writes=[r_pt])
            hb, r_hb = h2bs.next(); hf, r_hf = h2fs.next()
            S.op("act", lambda e, hb=hb, h2=h2: e.copy(out=hb[:], in_=h2[:]), reads=[r_h2], writes=[r_hb])
            S.op("dve", lambda e, hf=hf, pt=pt: e.tensor_copy(out=hf[:], in_=pt[:]), reads=[r_pt], writes=[r_hf])
            S.dma("sp", lambda e, hb=hb, t=t: e.dma_start(out=h2rows[t * 128:(t + 1) * 128, :], in_=hb[:]), reads=[r_hb], stream="scr")
            pl, r_pl = pls.next()
            for j in range(8):
                S.op("pe", lambda e, pl=pl, hf=hf, j=j: e.matmul(pl[:], lhsT=hf[:, j, :], rhs=wrf[:, j, :], start=(j == 0), stop=False), reads=[r_hf, r_wr], writes=[r_pl])
            S.op("pe", lambda e, pl=pl: e.matmul(pl[:], lhsT=ones[0:1, :], rhs=brr[0:1, :], start=False, stop=True), reads=[r_wr, r_const], writes=[r_pl])
            lg, r_lg = lgs.next(); t8, r_t8 = t8s.next()
            S.op("dve", lambda e, lg=lg, pl=pl: e.tensor_copy(out=lg[:, 0, :], in_=pl[:]), reads=[r_pl], writes=[r_lg])
            S.op("dve", lambda e, lg=lg, t8=t8: e.max(out=t8[:, 0:8], in_=lg[:, 0, :]), reads=[r_lg], writes=[r_t8])
            S.op("dve", lambda e, lg=lg, t8=t8: e.tensor_scalar(out=lg[:, 1, :], in0=lg[:, 0, :], scalar1=t8[:, 3:4], scalar2=None, op0=ALU.is_ge), reads=[r_lg, r_t8], writes=[r_lg])
            S.op("dve", lambda e, t8=t8: e.tensor_scalar(out=t8[:, 8:9], in0=t8[:, 0:1], scalar1=-1.0, scalar2=None, op0=ALU.mult), reads=[r_t8], writes=[r_t8])
            S.op("act", lambda e, lg=lg, t8=t8: e.activation(out=lg[:, 2, :], in_=lg[:, 0, :], func=AF.Exp, bias=t8[:, 8:9], scale=1.0), reads=[r_lg, r_t8], writes=[r_lg])
            S.op("dve", lambda e, lg=lg: e.tensor_tensor(out=lg[:, 3, :], in0=lg[:, 2, :], in1=lg[:, 1, :], op=ALU.mult), reads=[r_lg], writes=[r_lg])
            S.op("dve", lambda e, lg=lg, t8=t8: e.reduce_sum(out=t8[:, 9:10], in_=lg[:, 3, :], axis=AX.X), reads=[r_lg], writes=[r_t8])
            S.op("dve", lambda e, t8=t8: e.reciprocal(out=t8[:, 10:11], in_=t8[:, 9:10]), reads=[r_t8], writes=[r_t8])
            S.op("dve", lambda e, lg=lg, t8=t8, t=t: e.tensor_scalar(out=gates[:, t, :], in0=lg[:, 3, :], scalar1=t8[:, 10:11], scalar2=None, op0=ALU.mult), reads=[r_lg, r_t8], writes=[r_gates[t]])
            if dbg_g is not None:
                S.dma("sp", lambda e, t=t: e.dma_start(out=dbg_g[t * 128:(t + 1) * 128, :], in_=gates[:, t, :]), reads=[r_gates[t]], stream="scr")


def phase5(nc, S, IN, l, xres, modv, h2rows, yacc, slotrec, gates, r_gates, ident, ones, r_const, nt_act, out, wbf, r_wbfl):
    S.barrier()
    last = (l == 1)
    NB = (4 * nt_act * 128) // 128 + NE
    with ExitStack() as ph:
        sb = lambda n, s, d: ph.enter_context(usb(nc, n, s, d))
        pst = lambda n, s, d: ph.enter_context(ups(nc, n, s, d))
        nr = 2 if nt_act > NTL else 1
        GT2 = [load_mod_bc(nc, S, ph, modv, l, r, 5, f"p5_GT{r}") for r in range(nr)]
        if last:
            gfb = sb("p5_gf", [128, D], F32); r_gf = Res()
            S.dma("sp", lambda e: e.dma_start(out=gfb[:], in_=IN["g_final"].to_broadcast([128, D])), writes=[r_gf], stream="ld")
        r_k = Res()
        cst = {}
        for nm, shp in (("tri_s", [128, 128]), ("tokidf", [128, NT]), ("widxbase", [128, 8]), ("blockval", [128, 2]), ("eidx", [128, 1])):
            cst[nm] = sb("p5_" + nm, shp, F32)
            S.dma("sp", lambda e, nm=nm: e.dma_start(out=cst[nm][:], in_=IN[nm]), writes=[r_k], stream="ld")
        bnat = sb("p5_bnat", [NE, 3, D], F32); bb = sb("p5_bb", [NE, 3, D], BF16); r_bb = Res()
        for k, nm in enumerate(("b_gate", "b_up", "b_down")):
            S.dma("sp", lambda e, k=k, nm=nm: e.dma_start(out=bnat[:, k, :], in_=IN[nm][l]), writes=[r_bb], stream="ld")
        S.op("dve", lambda e: e.tensor_copy(out=bb[:], in_=bnat[:]), reads=[r_bb], writes=[r_bb])
        zt = sb("p5_zt", [128, D], F32); r_zt = Res(); r_yacc = Res(); r_h2r = Res(); r_slot = Res()
        zb = sb("p5_zb", [1, D], BF16)
        S.op("dve", lambda e: e.memset(zt[:], 0.0), writes=[r_zt])
        S.op("dve", lambda e: e.memset(zb[:], 0.0), writes=[r_zt])
        for t in range(NT):
            S.dma("sp", lambda e, t=t: e.dma_start(out=yacc[t * 128:(t + 1) * 128, :], in_=zt[:]), reads=[r_zt], writes=[r_yacc], stream="scr")
        S.dma("sp", lambda e: e.dma_start(out=yacc[T:T + 1, :], in_=zt[0:1, :]), reads=[r_zt], writes=[r_yacc], stream="scr")
        S.dma("sp", lambda e: e.dma_start(out=h2rows[T:T + 1, :], in_=zb[:]), reads=[r_zt], writes=[r_h2r], stream="scr")
        prt = sb("p5_prt", [128, NBMAX, 2], F32)
        S.dma("sp", lambda e: e.dma_start(out=prt[:], in_=IN["padrec"]), writes=[r_zt], stream="ld")
        r_slots = [Res() for _ in range(nt_act * 4)]
        S.dma("sp", lambda e: e.dma_start(out=slotrec.rearrange("(p a) b -> p a b", a=NBMAX), in_=prt[:]), reads=[r_zt], writes=[r_slot] + r_slots, stream="scr")
        pm = pst("p5_pm", [128, 512], F32); r_pm = Res(excl=True)
        M = sb("p5_M", [128, NT, NE], F32); r_M = Res()
        POS = sb("p5_POS", [128, NT, NE], F32); r_POS = Res()
        cum = sb("p5_cum", [128, NE], F32); r_cum = Res()
        rg = list(r_gates[:nt_act])
        S.op("dve", lambda e: e.tensor_single_scalar(out=M[:, 0:nt_act, :], in_=gates[:, 0:nt_act, :], scalar=0.0, op=ALU.is_gt), reads=rg, writes=[r_M])
        S.op("dve", lambda e: e.memset(cum[:], 0.0), writes=[r_cum])
        for t in range(nt_act):
            S.op("pe", lambda e, t=t: e.matmul(pm[:, 0:NE], lhsT=cst["tri_s"][:], rhs=M[:, t, :], start=True, stop=False), reads=[r_M, r_k], writes=[r_pm])
            S.op("pe", lambda e, t=t: e.matmul(pm[:, 0:NE], lhsT=ones[:], rhs=cum[:], start=False, stop=True), reads=[r_cum, r_const], writes=[r_pm])
            S.op("act", lambda e, t=t: e.copy(out=POS[:, t, :], in_=pm[:, 0:NE]), reads=[r_pm], writes=[r_POS])
            S.op("dve", lambda e, t=t: e.tensor_tensor(out=cum[:], in0=cum[:], in1=M[:, t, :], op=ALU.add), reads=[r_M, r_cum], writes=[r_cum])
        mt = sb("p5_mt", [128, 8, NE], F32); r_mt = Res()
        mti = sb("p5_mti", [128, 2, NE], I32)
        S.op("pe", lambda e: e.matmul(pm[:, 0:NE], lhsT=ones[:], rhs=cum[:], start=True, stop=True), reads=[r_cum, r_const], writes=[r_pm])
        S.op("dve", lambda e: e.tensor_scalar(out=mti[:, 0, :], in0=pm[:, 0:NE], scalar1=127.0, scalar2=None, op0=ALU.add), reads=[r_pm], writes=[r_mt])
        S.op("dve", lambda e: e.tensor_single_scalar(out=mti[:, 1, :], in_=mti[:, 0, :], scalar=7, op=ALU.arith_shift_right), reads=[r_mt], writes=[r_mt])
        S.op("dve", lambda e: e.tensor_single_scalar(out=mti[:, 0, :], in_=mti[:, 1, :], scalar=7, op=ALU.logical_shift_left), reads=[r_mt], writes=[r_mt])
        S.op("dve", lambda e: e.tensor_copy(out=mt[:, 0, :], in_=mti[:, 0, :]), reads=[r_mt], writes=[r_mt])
        S.op("dve", lambda e: e.memset(mt[:, 7, :], 1.0), writes=[r_mt])
        S.op("dve", lambda e: e.tensor_tensor_scan(out=mt[:, 1, :], data0=mt[:, 7, :], data1=mt[:, 0, :], initial=0.0, op0=ALU.mult, op1=ALU.add), reads=[r_mt], writes=[r_mt])
        S.op("dve", lambda e: e.tensor_tensor(out=mt[:, 2, :], in0=mt[:, 1, :], in1=mt[:, 0, :], op=ALU.subtract), reads=[r_mt], writes=[r_mt])
        S.op("dve", lambda e: e.tensor_single_scalar(out=mt[:, 3, :], in_=mt[:, 0, :], scalar=0.0, op=ALU.is_gt), reads=[r_mt], writes=[r_mt])
        for t in range(nt_act):
            S.op("dve", lambda e, t=t: e.tensor_tensor(out=POS[:, t, :], in0=POS[:, t, :], in1=mt[:, 2, :], op=ALU.add), reads=[r_POS, r_mt], writes=[r_POS])
        recs = sb("p5_recs", [128, NT * 4, 2], F32); r_recs = Res()
        idxf = sb("p5_idxf", [128, NT * 4], F32); idxi = sb("p5_idxi", [128, NT * 4], I32); r_idx = Res()
        v8s = Rot([(sb(f"p5_v8{i}", [128, 8], F32), Res()) for i in range(2)])
        ohs = Rot([(sb(f"p5_oh{i}", [128, NE], F32), Res()) for i in range(2)])
        for t in range(nt_act):
            v8, r_v8 = v8s.next()
            S.op("dve", lambda e, v8=v8, t=t: e.max(out=v8[:], in_=gates[:, t, :]), reads=[r_gates[t]], writes=[r_v8])
            for k in range(4):
                q = t * 4 + k
                oh, r_oh = ohs.next()
                S.op("dve", lambda e, oh=oh, v8=v8, t=t, k=k: e.tensor_scalar(out=oh[:], in0=gates[:, t, :], scalar1=v8[:, k:k + 1], scalar2=None, op0=ALU.is_equal), reads=[r_gates[t], r_v8], writes=[r_oh])
                S.op("dve", lambda e, oh=oh, t=t: e.tensor_tensor(out=oh[:], in0=oh[:], in1=POS[:, t, :], op=ALU.mult), reads=[r_oh, r_POS], writes=[r_oh])
                S.op("dve", lambda e, oh=oh, q=q: e.reduce_sum(out=idxf[:, q:q + 1], in_=oh[:], axis=AX.X), reads=[r_oh], writes=[r_idx])
                S.op("act", lambda e, q=q, t=t: e.copy(out=recs[:, q, 0:1], in_=cst["tokidf"][:, t:t + 1]), reads=[r_k], writes=[r_recs])
                S.op("act", lambda e, q=q, v8=v8, k=k: e.copy(out=recs[:, q, 1:2], in_=v8[:, k:k + 1]), reads=[r_v8], writes=[r_recs])
        S.op("dve", lambda e: e.tensor_copy(out=idxi[:, 0:nt_act * 4], in_=idxf[:, 0:nt_act * 4]), reads=[r_idx], writes=[r_idx])
        for q in range(nt_act * 4):
            S.dma("pool", lambda e, q=q: e.indirect_dma_start(out=slotrec, out_offset=bass.IndirectOffsetOnAxis(ap=idxi[:, q:q + 1], axis=0), in_=recs[:, q, :], in_offset=None),
                  reads=[r_idx, r_recs, r_slot], writes=[r_slots[q]], stream="igs")
        EO = sb("p5_EO", [128, 256], F32); OH = sb("p5_OH", [NE, 256], F32); r_bm = Res()
        dgt = sb("p5_dgt", [128, 128], F32); r_dgt = Res()
        colv = sb("p5_colv", [128, 8], F32); r_colv = Res()
        cmpt = sb("p5_cmp", [128, NE], F32); r_cmp = Res()
        EBt = sb("p5_EB", [128, 256], F32); CHt = sb("p5_CH", [128, 256], F32)
        for c in range(2):
            bv = cst["blockval"][:, c:c + 1]
            S.op("dve", lambda e, bv=bv: e.tensor_scalar(out=cmpt[:], in0=mt[:, 1, :], scalar1=bv, scalar2=None, op0=ALU.is_le), reads=[r_mt, r_k], writes=[r_cmp])
            S.op("dve", lambda e, c=c: e.reduce_sum(out=colv[:, c:c + 1], in_=cmpt[:], axis=AX.X), reads=[r_cmp], writes=[r_colv])
            S.op("dve", lambda e, c=c: e.tensor_scalar(out=colv[:, c:c + 1], in0=colv[:, c:c + 1], scalar1=float(NE - 1), scalar2=None, op0=ALU.min), reads=[r_colv], writes=[r_colv])
            S.op("dve", lambda e, bv=bv: e.tensor_scalar(out=cmpt[:], in0=mt[:, 2, :], scalar1=bv, scalar2=None, op0=ALU.is_equal), reads=[r_mt, r_k], writes=[r_cmp])
            S.op("dve", lambda e: e.tensor_tensor(out=cmpt[:], in0=cmpt[:], in1=mt[:, 3, :], op=ALU.mult), reads=[r_cmp, r_mt], writes=[r_cmp])
            S.op("dve", lambda e, c=c: e.tensor_reduce(out=colv[:, 2 + c:3 + c], in_=cmpt[:], axis=AX.X, op=ALU.max), reads=[r_cmp], writes=[r_colv])
            for kk, dst in ((c, EBt), (2 + c, CHt)):
                S.op("dve", lambda e, kk=kk: e.tensor_scalar(out=dgt[:], in0=ident[:], scalar1=colv[:, kk:kk + 1], scalar2=None, op0=ALU.mult), reads=[r_colv, r_const], writes=[r_dgt])
                S.op("pe", lambda e: e.matmul(pm[:, 0:128], lhsT=ones[:], rhs=dgt[:], start=True, stop=True), reads=[r_dgt, r_const], writes=[r_pm])
                S.op("act", lambda e, dst=dst, c=c: e.copy(out=dst[:, c * 128:(c + 1) * 128], in_=pm[:, 0:128]), reads=[r_pm], writes=[r_bm])
        S.op("dve", lambda e: e.tensor_scalar(out=CHt[:], in0=CHt[:], scalar1=-1.0e7, scalar2=1.0e7, op0=ALU.mult, op1=ALU.add), reads=[r_bm], writes=[r_bm])
        S.op("dve", lambda e: e.scalar_tensor_tensor(out=EO[:], in0=EBt[:], scalar=128.0, in1=CHt[:], op0=ALU.mult, op1=ALU.add), reads=[r_bm], writes=[r_bm])
        S.op("dve", lambda e: e.tensor_scalar(out=OH[:], in0=EBt[0:NE, :], scalar1=cst["eidx"][0:NE, 0:1], scalar2=None, op0=ALU.is_equal), reads=[r_bm, r_k], writes=[r_bm])
        wg = sb("p5_wg", [128, 8, D], BF16); wu = sb("p5_wu", [128, 8, D], BF16); wd = sb("p5_wd", [128, 8, D], BF16)
        r_wg = Res(); r_wu = Res(); r_wd = Res()
        recb = Rot([(sb(f"p5_rb{i}", [128, 2], F32), Res()) for i in range(3)])
        xgs = Rot([(sb(f"p5_xg{i}", [128, D], BF16), Res()) for i in range(2)])
        xTs = Rot([(sb(f"p5_xT{i}", [128, 8, 128], BF16), Res()) for i in range(2)])
        wix = Rot([(sb(f"p5_wi{i}", [128, 1], I32), Res()) for i in range(2)])
        ohb = Rot([(sb(f"p5_ohb{i}", [NE, 128], BF16), Res()) for i in range(2)])
        a_s = Rot([(sb(f"p5_a{i}", [128, 512], F32), Res()) for i in range(2)])
        sg_s = Rot([(sb(f"p5_sg{i}", [128, 512], F32), Res()) for i in range(2)])
        u_s = Rot([(sb(f"p5_u{i}", [128, 512], F32), Res()) for i in range(2)])
        acts = Rot([(sb(f"p5_act{i}", [128, 8, 128], BF16), Res()) for i in range(2)])
        atms = Rot([(sb(f"p5_atm{i}", [128, D], BF16), Res()) for i in range(2)])
        ygs = Rot([(sb(f"p5_yg{i}", [128, D], F32), Res()) for i in range(2)])
        ptr = Rot([(pst("p5_ptr", [128, 8, 128], BF16), Res(excl=True))])
        pAs = Rot([(pst(f"p5_pA{i}", [128, 512], F32), Res()) for i in range(2)])
        pUs = Rot([(pst(f"p5_pU{i}", [128, 512], F32), Res()) for i in range(2)])
        pYs = Rot([(pst(f"p5_pY{i}", [128, 512], F32), Res()) for i in range(2)])
        identb = sb("p5_idb", [128, 128], BF16)
        S.op("dve", lambda e: e.tensor_copy(out=identb[:], in_=ident[:]), reads=[r_const], writes=[r_k])

        BC = {}

        def bcreg(e):
            if "r" not in BC:
                BC["r"] = e.alloc_register(f"bc{l}")
                e.reg_mov(BC["r"], NE * 128 - 1)
            return BC["r"]

        def stage_in(b):
            rb, r_rb = recb.next()
            S.dma("sp", lambda e, rb=rb, b=b: e.dma_start(out=rb[:], in_=slotrec[b * 128:(b + 1) * 128, :]), reads=[r_slot] + r_slots, writes=[r_rb], stream="ld")
            xg, r_xg = xgs.next()
            S.dma("pool", lambda e, xg=xg, rb=rb: e.indirect_dma_start(out=xg[:], out_offset=None, in_=h2rows, in_offset=bass.IndirectOffsetOnAxis(ap=rb[:, 0:1].bitcast(I32), axis=0)),
                  reads=[r_rb, r_h2r], writes=[r_xg], stream="ig")
            wi, r_wi = wix.next()
            S.op("dve", lambda e, wi=wi, b=b: e.tensor_scalar(out=wi[:], in0=cst["eidx"][:], scalar1=EO[:, b:b + 1], scalar2=None, op0=ALU.add), reads=[r_bm, r_k], writes=[r_wi])
            for m, (wt, r_w) in enumerate(((wg, r_wg), (wu, r_wu), (wd, r_wd))):
                S.dma("pool", lambda e, wi=wi, m=m, wt=wt: e.indirect_dma_start(out=wt[:].rearrange("p j f -> p (j f)"), out_offset=None, in_=wbf[m], in_offset=bass.IndirectOffsetOnAxis(ap=wi[:, 0:1], axis=0),
                                                                             bounds_check=bcreg(e), oob_is_err=False), reads=[r_wi, r_wbfl], writes=[r_w], stream="wc")
            ob, r_ob = ohb.next()
            S.op("act", lambda e, ob=ob, b=b: e.activation(out=ob[:], in_=ones[0:NE, :], func=AF.Copy, scale=OH[0:NE, b:b + 1]), reads=[r_bm, r_const], writes=[r_ob])
            return (rb, r_rb, xg, r_xg, ob, r_ob)

        def compute(b, st):
            rb, r_rb, xg, r_xg, ob, r_ob = st
            pt, r_pt = ptr.next()
            for j in range(8):
                S.op("pe", lambda e, pt=pt, xg=xg, j=j: e.transpose(out=pt[:, j, :], in_=xg[:, j:D:8], identity=identb[:]), reads=[r_xg, r_k], writes=[r_pt])
            xT, r_xT = xTs.next()
            S.op("act", lambda e, xT=xT, pt=pt: e.copy(out=xT[:], in_=pt[:]), reads=[r_pt], writes=[r_xT])
            atm, r_atm = atms.next()
            for hf in range(2):
                pA, r_pA = pAs.next(); pU, r_pU = pUs.next()
                for (pp, r_pp, wt, r_w, bk) in ((pA, r_pA, wg, r_wg, 0), (pU, r_pU, wu, r_wu, 1)):
                    for j in range(8):
                        S.op("pe", lambda e, pp=pp, wt=wt, j=j, xT=xT, hf=hf: e.matmul(pp[:], lhsT=xT[:, j, :], rhs=wt[:, j, hf * 512:(hf + 1) * 512], start=(j == 0), stop=False),
                             reads=[r_w, r_xT], writes=[r_pp])
                    S.op("pe", lambda e, pp=pp, bk=bk, ob=ob, hf=hf: e.matmul(pp[:], lhsT=ob[:], rhs=bb[:, bk, hf * 512:(hf + 1) * 512], start=False, stop=True),
                         reads=[r_bb, r_ob], writes=[r_pp])
                a, r_a = a_s.next(); sg, r_sg = sg_s.next(); u1, r_u1 = u_s.next()
                S.op("dve", lambda e, a=a, pA=pA: e.tensor_scalar(out=a[:], in0=pA[:], scalar1=7.0, scalar2=None, op0=ALU.min), reads=[r_pA], writes=[r_a])
                S.op("act", lambda e, sg=sg, a=a: e.activation(out=sg[:], in_=a[:], func=AF.Sigmoid, scale=1.702), reads=[r_a], writes=[r_sg])
                S.op("dve", lambda e, u1=u1, pU=pU: e.tensor_scalar(out=u1[:], in0=pU[:], scalar1=7.0, scalar2=-7.0, op0=ALU.min, op1=ALU.max), reads=[r_pU], writes=[r_u1])
                S.op("dve", lambda e, sg=sg, a=a: e.tensor_tensor(out=sg[:], in0=sg[:], in1=a[:], op=ALU.mult), reads=[r_a, r_sg], writes=[r_sg])
                S.op("dve", lambda e, atm=atm, sg=sg, u1=u1, hf=hf: e.scalar_tensor_tensor(out=atm[:, hf * 512:(hf + 1) * 512], in0=u1[:], scalar=1.0, in1=sg[:], op0=ALU.add, op1=ALU.mult),
                     reads=[r_sg, r_u1], writes=[r_atm])
            pt2, r_pt2 = ptr.next()
            for j in range(8):
                S.op("pe", lambda e, pt2=pt2, atm=atm, j=j: e.transpose(out=pt2[:, j, :], in_=atm[:, j:D:8], identity=identb[:]), reads=[r_atm, r_k], writes=[r_pt2])
            actT, r_act = acts.next()
            S.op("act", lambda e, actT=actT, pt2=pt2: e.copy(out=actT[:], in_=pt2[:]), reads=[r_pt2], writes=[r_act])
            yg, r_yg = ygs.next()
            for half in range(2):
                pY, r_pY = pYs.next()
                for f in range(8):
                    S.op("pe", lambda e, pY=pY, actT=actT, f=f, half=half: e.matmul(pY[:], lhsT=actT[:, f, :], rhs=wd[:, f, half * 512:(half + 1) * 512], start=(f == 0), stop=False),
                         reads=[r_act, r_wd], writes=[r_pY])
                S.op("pe", lambda e, pY=pY, ob=ob, half=half: e.matmul(pY[:], lhsT=ob[:], rhs=bb[:, 2, half * 512:(half + 1) * 512], start=False, stop=True), reads=[r_ob, r_bb], writes=[r_pY])
                S.op("act", lambda e, yg=yg, pY=pY, rb=rb, half=half: e.activation(out=yg[:, half * 512:(half + 1) * 512], in_=pY[:], func=AF.Copy, scale=rb[:, 1:2]), reads=[r_pY, r_rb], writes=[r_yg])
            return (yg, r_yg, rb, r_rb)

        def stage_out(res):
            yg, r_yg, rb, r_rb = res
            S.dma("pool", lambda e, yg=yg, rb=rb: e.indirect_dma_start(out=yacc, out_offset=bass.IndirectOffsetOnAxis(ap=rb[:, 0:1].bitcast(I32), axis=0), in_=yg[:], in_offset=None, compute_op=ALU.add),
                  reads=[r_yg, r_rb], writes=[r_yacc], stream="igs")

        st = stage_in(0)
        for b in range(NB):
            res = compute(b, st)
            if b + 1 < NB:
                st = stage_in(b + 1)
            stage_out(res)
        xts = Rot([(sb(f"p5_xt{i}", [128, D], F32), Res()) for i in range(2)])
        yts = Rot([(sb(f"p5_yt{i}", [128, D], F32), Res()) for i in range(2)])
        junk = sb("p5_junk", [128, D], BF16); r_junk = Res()
        sts = Rot([(sb(f"p5_st{i}", [128, 4], F32), Res()) for i in range(2)])
        for t in range(nt_act):
            r = 0 if t < NTL else 1
            xt, r_xt = xts.next(); yt, r_yt = yts.next()
            S.dma("sp", lambda e, xt=xt, t=t: e.dma_start(out=xt[:], in_=xres[t * 128:(t + 1) * 128, :]), writes=[r_xt], stream="ld")
            S.dma("act", lambda e, yt=yt, t=t: e.dma_start(out=yt[:], in_=yacc[t * 128:(t + 1) * 128, :]), reads=[r_yacc], writes=[r_yt], stream="ld2")
            S.op("dve", lambda e, yt=yt, r=r: e.tensor_tensor(out=yt[:], in0=yt[:], in1=GT2[r][0][:], op=ALU.mult), reads=[r_yt, GT2[r][1]], writes=[r_yt])
            S.op("pool", lambda e, yt=yt, xt=xt: e.tensor_tensor(out=yt[:], in0=yt[:], in1=xt[:], op=ALU.add), reads=[r_yt, r_xt], writes=[r_yt])
            if not last:
                S.dma("sp", lambda e, yt=yt, t=t: e.dma_start(out=xres[t * 128:(t + 1) * 128, :], in_=yt[:]), reads=[r_yt], stream="scr")
            else:
                st_, r_st = sts.next()
                rstd_ops(S, yt, r_yt, junk, r_junk, st_, r_st)
                S.op("dve", lambda e, yt=yt, st_=st_: e.scalar_tensor_tensor(out=yt[:], in0=yt[:], scalar=st_[:, 3:4], in1=gfb[:], op0=ALU.mult, op1=ALU.mult), reads=[r_yt, r_st, r_gf], writes=[r_yt])
                S.dma("sp", lambda e, yt=yt, t=t: e.dma_start(out=out[t * 128:(t + 1) * 128, :], in_=yt[:]), reads=[r_yt], stream="st")


_CACHE = {}


def make_in_maps(inputs):
    consts = host_consts()
    shared = {}
    for nm, shp in W_SPECS:
        a = np.ascontiguousarray(np.asarray(inputs[nm], dtype=np.float32)).reshape(shp)
        shared[nm] = a
    shared.update(consts)
    x = np.asarray(inputs["x"], dtype=np.float32)
    ctx = np.asarray(inputs["ctx"], dtype=np.float32)
    c = np.asarray(inputs["c"], dtype=np.float32)
    c_ctx = np.asarray(inputs["c_ctx"], dtype=np.float32)
    maps = []
    for b in range(8):
        m = dict(shared)
        m["xin"] = np.ascontiguousarray(np.concatenate([x[b], ctx[b]], axis=0))
        m["cc"] = np.ascontiguousarray(np.stack([c[b], c_ctx], axis=0))
        maps.append(m)
    return maps


def kernel(**inputs):
    if "nc" not in _CACHE:
        _CACHE["nc"] = build()[0]
    nc = _CACHE["nc"]
    maps = make_in_maps(inputs)
    res = run_bass_kernel_spmd(nc, maps, core_ids=list(range(8)))
    return np.stack([np.asarray(r["out"], dtype=np.float32) for r in res.results], axis=0)
```

```python
import numpy as np
import concourse.bass as bass
import concourse.mybir as mybir
from concourse.alu_op_type import AluOpType as ALU
from contextlib import ExitStack
from concourse.bass_utils import run_bass_kernel_spmd

F32 = mybir.dt.float32
BF16 = mybir.dt.bfloat16
I32 = mybir.dt.int32
U32 = mybir.dt.uint32
AF = mybir.ActivationFunctionType
AX = mybir.AxisListType


class Res:
    __slots__ = ("name", "w", "rs", "excl")

    def __init__(self, name="", excl=False):
        self.name = name
        self.w = None
        self.rs = {}
        self.excl = excl


class Op:
    __slots__ = ("eng", "fn", "reads", "writes", "stream", "deps", "sig", "sigidx", "waits")

    def __init__(self, eng, fn, reads, writes, stream):
        self.eng = eng
        self.fn = fn
        self.reads = reads
        self.writes = writes
        self.stream = stream
        self.deps = None
        self.sig = False
        self.sigidx = -1
        self.waits = None


class Sched:
    CH = 16000
    CHD = 1000
    COMPUTE = ("pe", "act", "dve", "pool")

    def __init__(self, nc):
        self.nc = nc
        self.ops = []
        self._dcnt = {}

    def op(self, eng, fn, reads=(), writes=()):
        self.ops.append(Op(eng, fn, tuple(reads), tuple(writes), None))

    NSLOT = {"ld": 12, "scr": 12, "wc": 6, "ld2": 6, "st": 4, "ig": 8, "igs": 4, "cv": 8}

    def dma(self, queue, fn, reads=(), writes=(), stream="ld"):
        n = self._dcnt.get(stream, 0)
        self._dcnt[stream] = n + 1
        self.ops.append(Op(queue, fn, tuple(reads), tuple(writes), f"{stream}#{n % self.NSLOT.get(stream, 8)}"))

    def barrier(self):
        self.ops.append(None)

    def finalize(self, es, final_streams=()):
        nc = self.nc
        raw = self.ops
        ops = []
        bar_after = {}
        lastkey = {}
        pend = None
        for o in raw:
            if o is None:
                pend = dict(lastkey)
                continue
            i = len(ops)
            ops.append(o)
            key = o.stream if o.stream is not None else o.eng
            if pend is not None:
                bar_after[i] = pend
                pend = None
            if not key.startswith("cv#"):
                lastkey[key] = i
        self.ops = ops
        cur_bar = set()
        prev_dma = {}
        for i, o in enumerate(ops):
            if i in bar_after:
                cur_bar = set(bar_after[i].values())
                pend = None
            deps = set(cur_bar)
            if any(r.excl for r in o.reads):
                o.writes = tuple(o.writes) + tuple(r for r in o.reads if r.excl)
                o.reads = tuple(r for r in o.reads if not r.excl)
            for r in o.reads:
                if r.w is not None:
                    deps.add(r.w)
            for w in o.writes:
                if w.w is not None:
                    deps.add(w.w)
                for k, j in w.rs.items():
                    deps.add(j)
            key = o.stream if o.stream is not None else o.eng
            for r in o.reads:
                r.rs[key] = i
            for w in o.writes:
                w.w = i
                w.rs = {}
            if o.stream is not None:
                if o.stream in prev_dma:
                    deps.add(prev_dma[o.stream])
                prev_dma[o.stream] = i
            deps.discard(i)
            o.deps = deps
            for j in deps:
                ops[j].sig = True
        cnt = {}
        for o in ops:
            key = o.stream if o.stream is not None else o.eng
            if o.stream is not None:
                o.sig = True
            if o.sig:
                o.sigidx = cnt.get(key, 0)
                cnt[key] = o.sigidx + 1
        self.cnt = cnt
        known = {e: {} for e in ("pe", "act", "dve", "pool", "sp")}
        clocks = [None] * len(ops)
        for i, o in enumerate(ops):
            kn = known[o.eng]
            waits = {}
            for j in sorted(o.deps):
                p = ops[j]
                pkey = p.stream if p.stream is not None else p.eng
                if p.stream is None and p.eng == o.eng:
                    if o.eng == "pe":
                        continue
                if kn.get(pkey, -1) >= p.sigidx:
                    continue
                if waits.get(pkey, -1) < p.sigidx:
                    waits[pkey] = p.sigidx
                pc = clocks[j]
                for k, v in pc.items():
                    if kn.get(k, -1) < v:
                        kn[k] = v
            for k, v in waits.items():
                if kn.get(k, -1) < v:
                    kn[k] = v
            o.waits = waits
            ck = dict(kn)
            if o.sig:
                key = o.stream if o.stream is not None else o.eng
                ck[key] = max(ck.get(key, -1), o.sigidx)
            clocks[i] = ck
        self.sems = {}
        for key, n in cnt.items():
            ch = self.CH if key in self.COMPUTE else self.CHD
            nch = (n + ch - 1) // ch
            self.sems[key] = [es.enter_context(nc.semaphore(f"s_{key}_{c}")) for c in range(nch)]
        per_eng = {e: [] for e in ("pe", "act", "dve", "pool", "sp")}
        for o in ops:
            per_eng[o.eng].append(o)
        block = es.enter_context(nc.Block())
        sems = self.sems
        CH = self.CH
        CHD = self.CHD

        def wait(eng, k, v):
            if k in self.COMPUTE:
                eng.wait_ge(sems[k][v // CH], v % CH + 1)
            else:
                c = v // CHD
                if c > 0:
                    eng.wait_ge(sems[k][c - 1], CHD * 16)
                eng.wait_ge(sems[k][c], (v % CHD + 1) * 16)

        def emit(eng, lst, finals):
            for o in lst:
                for k, v in o.waits.items():
                    wait(eng, k, v)
                inst = o.fn(eng)
                if o.sig:
                    key = o.stream if o.stream is not None else o.eng
                    if o.stream is not None:
                        inst.then_inc(sems[key][o.sigidx // CHD], 16)
                    else:
                        inst.then_inc(sems[key][o.sigidx // CH], 1)
            for k in cnt:
                if k not in self.COMPUTE and k.split("#")[0] in finals:
                    wait(eng, k, cnt[k] - 1)

        @block.sync
        def _(e):
            emit(e, per_eng["sp"], final_streams)

        @block.tensor
        def _(e):
            emit(e, per_eng["pe"], ())

        @block.scalar
        def _(e):
            emit(e, per_eng["act"], ())

        @block.vector
        def _(e):
            emit(e, per_eng["dve"], ())

        @block.gpsimd
        def _(e):
            emit(e, per_eng["pool"], ())
        return {k: len(v) for k, v in per_eng.items()}
D = 1024
SEQ = 4096
CTX = 256
T = SEQ + CTX
NT = T // 128
NTL = SEQ // 128
INW = 2576
NE = 32
EPS = 1e-6
NEG = -30000.0
NBMAX = (4 * T) // 128 + NE


def host_consts():
    import ml_dtypes
    c = {}
    p = np.arange(128)
    c["ident"] = np.eye(128, dtype=np.float32)
    c["ones"] = np.ones((128, 128), np.float32)
    same = (p[:, None] // 64) == (p[None, :] // 64)
    c["m_ls"] = np.where(same & (p[:, None] > p[None, :]), 0.0, NEG).astype(np.float32)
    c["m_li"] = np.where(same & (p[:, None] >= p[None, :]), 0.0, NEG).astype(np.float32)
    c["m_us"] = np.where(same & (p[:, None] < p[None, :]), 0.0, NEG).astype(np.float32)
    c["m_ui"] = np.where(same & (p[:, None] <= p[None, :]), 0.0, NEG).astype(np.float32)
    c["tri_f"] = (same & (p[:, None] <= p[None, :])).astype(np.float32)
    c["tri_b"] = (same & (p[:, None] >= p[None, :])).astype(np.float32)
    c["blk"] = same.astype(np.float32)
    c["tri_s"] = (p[:, None] < p[None, :]).astype(np.float32)
    tok = (np.arange(NT)[None, :] * 128 + p[:, None]).astype(np.int32)
    c["tokidf"] = tok.view(np.float32)
    c["widxbase"] = (np.arange(8)[None, :] * 128 + p[:, None]).astype(np.float32)
    c["blockval"] = (128.0 * (p[:, None] + 128 * np.arange(2)[None, :])).astype(np.float32)
    c["eidx"] = p[:, None].astype(np.float32)
    pr = np.zeros((128, NBMAX, 2), np.int32); pr[:, :, 0] = T
    c["padrec"] = pr.view(np.float32)
    ang = 2 * np.pi * np.outer(p, p) / 128.0
    c["cs128"] = np.concatenate([np.cos(ang), np.sin(ang)], axis=1).astype(np.float32)
    for nm, L in (("L", SEQ), ("C", CTX)):
        t = np.arange(L, dtype=np.int64)
        a = 2 * np.pi * ((np.outer(t, t) % L).astype(np.float64)) / L
        nrm = 1.0 / np.sqrt(L * 128.0)
        c["cos" + nm] = (np.cos(a) * nrm).astype(ml_dtypes.bfloat16)
        c["nsin" + nm] = (-np.sin(a) * nrm).astype(ml_dtypes.bfloat16)
    return c


CONST_SPECS = [("ident", [128, 128], "f"), ("ones", [128, 128], "f"), ("m_ls", [128, 128], "f"),
               ("m_li", [128, 128], "f"), ("m_us", [128, 128], "f"), ("m_ui", [128, 128], "f"),
               ("tri_f", [128, 128], "f"), ("tri_b", [128, 128], "f"), ("blk", [128, 128], "f"),
               ("cs128", [128, 256], "f"), ("tri_s", [128, 128], "f"), ("tokidf", [128, NT], "f"), ("widxbase", [128, 8], "f"),
               ("blockval", [128, 2], "f"), ("eidx", [128, 1], "f"), ("padrec", [128, NBMAX, 2], "f"), ("cosL", [SEQ, SEQ], "b"), ("nsinL", [SEQ, SEQ], "b"),
               ("cosC", [CTX, CTX], "b"), ("nsinC", [CTX, CTX], "b")]

W_SPECS = [("w_mod", [2, D, 6 * D]), ("b_mod", [2, 6 * D]), ("g_norm1", [2, D]), ("w_in", [2, D, INW]),
           ("conv_w", [2, 5, 1536]), ("a_log", [2, 8]), ("dt_bias", [2, 8]), ("g_out_norm", [2, 128]),
           ("w_out", [2, D, D]), ("g_norm2", [2, D]), ("w_router", [2, D, NE]), ("b_router", [2, NE]),
           ("w_gate", [2, NE, D, D]), ("b_gate", [2, NE, D]), ("w_up", [2, NE, D, D]), ("b_up", [2, NE, D]),
           ("w_down", [2, NE, D, D]), ("b_down", [2, NE, D]), ("g_final", [1, D])]


_UC = [0]


def usb(nc, name, shape, dt):
    _UC[0] += 1
    return nc.sbuf_tensor(f"{name}_{_UC[0]}", shape, dt)


def ups(nc, name, shape, dt):
    _UC[0] += 1
    return nc.psum_tensor(f"{name}_{_UC[0]}", shape, dt)


class Rot:
    def __init__(self, items):
        self.items = items
        self.i = 0

    def next(self):
        it = self.items[self.i % len(self.items)]
        self.i += 1
        return it


def build(stage=99, dbg=()):
    nc = bass.Bass("TRN2", target_bir_lowering=False)
    IN = {}

    def din(name, shape, dt=F32):
        IN[name] = nc.dram_tensor(name, shape, dt, kind="ExternalInput").ap()
        return IN[name]

    xin = din("xin", [T, D])
    cc = din("cc", [2, D])
    for nm, shp in W_SPECS:
        din(nm, shp)
    for nm, shp, k in CONST_SPECS:
        din(nm, shp, F32 if k == "f" else BF16)
    out = nc.dram_tensor("out", [SEQ, D], F32, kind="ExternalOutput").ap()

    def dscr(name, shape, dt=F32):
        kind = "ExternalOutput" if name in dbg else "Internal"
        return nc.dram_tensor(name, shape, dt, kind=kind).ap()

    xres = dscr("xres", [T, D])
    modv = dscr("modv", [2, 2, 6 * D])
    uT = dscr("uT", [INW, T])
    abd = dscr("abd", [T, 16])
    mixT = dscr("mixT", [D, T], BF16)
    h2rows = dscr("h2rows", [T + 1, D], BF16)
    yacc = dscr("yacc", [T + 1, D])
    slotrec = dscr("slotrec", [NBMAX * 128, 2])
    wbf = [[dscr(f"wbf{i}_{m}", [NE * 128, 8 * D], BF16) for m in range(3)] for i in range(2)]
    r_wbf = [Res(), Res()]

    def conv_thunks(l):
        th = []
        for ex in range(NE):
            for m, nm in enumerate(("w_gate", "w_up", "w_down")):
                def fn(l=l, ex=ex, m=m, nm=nm):
                    r0 = ex * 128
                    S.dma("pool", lambda e: e.dma_start(out=wbf[l][m][r0:r0 + 128, :], in_=IN[nm][l, ex].rearrange("(p j) f -> p (j f)", j=8), max_dma_last_dim=4096),
                          writes=[r_wbf[l]], stream="cv")
                th.append(fn)
        return th
    dbg_g = dscr("dbg_g", [T, NE]) if "dbg_g" in dbg else None
    dbg_oT = dscr("dbg_oT", [4, 128, T]) if "dbg_oT" in dbg else None

    es = ExitStack()
    with es:
        S = Sched(nc)
        gsb = lambda n, s, d: es.enter_context(usb(nc, n, s, d))
        ident = gsb("identS", [128, 128], F32); r_const = Res("const")
        ones = gsb("onesS", [128, 128], F32)
        identb = gsb("identb", [128, 128], BF16)
        S.dma("sp", lambda e: e.dma_start(out=ident[:], in_=IN["ident"]), writes=[r_const], stream="ld")
        S.dma("sp", lambda e: e.dma_start(out=ones[:], in_=IN["ones"]), writes=[r_const], stream="ld")
        S.op("dve", lambda e: e.tensor_copy(out=identb[:], in_=ident[:]), reads=[r_const], writes=[r_const])
        gates = gsb("gates", [128, NT, NE], F32); r_gates = [Res() for _ in range(NT)]

        phase0(nc, S, IN, modv, ident, ones, r_const)
        for l in range(2):
            if stage < 1:
                break
            nt_act = NT if l == 0 else NTL
            phase1(nc, S, IN, l, xin if l == 0 else xres, modv, uT, abd, ident, identb, ones, r_const)
            if stage < 2:
                break
            phase2(nc, S, IN, l, uT, mixT, r_const)
            if stage < 3:
                break
            phase3(nc, S, IN, l, uT, abd, mixT, ident, ones, r_const, dbg_oT if l == 0 else None, conv_thunks(l))
            if stage < 4:
                break
            phase4(nc, S, IN, l, xin if l == 0 else xres, xres, modv, mixT, h2rows, gates, r_gates, ident, ones, r_const, nt_act, dbg_g if l == 0 else None)
            if stage < 5:
                break
            phase5(nc, S, IN, l, xres, modv, h2rows, yacc, slotrec, gates, r_gates, ident, ones, r_const, nt_act, out, wbf[l], r_wbf[l])
            if stage < 6:
                break
        if stage < 6:
            with ExitStack() as ph:
                z = ph.enter_context(usb(nc, "zz", [128, D], F32)); rz = Res()
                S.barrier()
                S.op("dve", lambda e: e.memset(z[:], 0.0), writes=[rz])
                S.dma("sp", lambda e: e.dma_start(out=out[0:128, :], in_=z[:]), reads=[rz], stream="st")
        S.barrier()
        fin = ("st", "ld", "wc", "scr", "ig", "igs", "cv")
        stats = S.finalize(es, final_streams=fin)
    return nc, stats
def phase0(nc, S, IN, modv, ident, ones, r_const):
    S.barrier()
    with ExitStack() as ph:
        sb = lambda n, s, d: ph.enter_context(usb(nc, n, s, d))
        ccr = sb("p0_ccr", [2, D], F32); r_ccr = Res()
        scT = sb("p0_scT", [128, 8, 2], F32); r_scT = Res()
        bm = sb("p0_bm", [1, 2, 6 * D], F32); r_bm = Res()
        modsb = sb("p0_mod", [2, 2, 6 * D], F32); r_mod = Res()
        wbufs = Rot([(sb(f"p0_w{i}", [128, 8, 512], F32), Res()) for i in range(2)])
        pT = ph.enter_context(ups(nc, "p0_pT", [128, 8, 2], F32)); r_pT = Res()
        pss = Rot([(ph.enter_context(ups(nc, f"p0_ps{i}", [2, 512], F32)), Res()) for i in range(2)])
        S.dma("sp", lambda e: e.dma_start(out=ccr[:], in_=IN["cc"]), writes=[r_ccr], stream="ld")
        S.dma("sp", lambda e: e.dma_start(out=bm[:], in_=IN["b_mod"].rearrange("(o l) n -> o l n", o=1)), writes=[r_bm], stream="ld")
        S.op("act", lambda e: e.activation(out=ccr[:], in_=ccr[:], func=AF.Silu), reads=[r_ccr], writes=[r_ccr])
        for j in range(8):
            S.op("pe", lambda e, j=j: e.transpose(out=pT[:, j, :], in_=ccr[:, j * 128:(j + 1) * 128], identity=ident[0:2, 0:2]),
                 reads=[r_ccr, r_const], writes=[r_pT])
        S.op("dve", lambda e: e.tensor_copy(out=scT[:], in_=pT[:]), reads=[r_pT], writes=[r_scT])
        for l in range(2):
            wv = IN["w_mod"][l].rearrange("(j p) n -> p j n", p=128)
            for n in range(12):
                wt, r_w = wbufs.next()
                S.dma("sp", lambda e, wt=wt, n=n, wv=wv: e.dma_start(out=wt[:], in_=wv[:, :, n * 512:(n + 1) * 512]), writes=[r_w], stream="ld")
                pst, r_ps = pss.next()
                for j in range(8):
                    S.op("pe", lambda e, j=j, wt=wt, pst=pst: e.matmul(pst[:], lhsT=scT[:, j, :], rhs=wt[:, j, :], start=(j == 0), stop=False),
                         reads=[r_scT, r_w], writes=[r_ps])
                S.op("pe", lambda e, pst=pst, l=l, n=n: e.matmul(pst[:], lhsT=ones[0:1, 0:2], rhs=bm[0:1, l, n * 512:(n + 1) * 512], start=False, stop=True),
                     reads=[r_bm, r_const], writes=[r_ps])
                S.op("dve", lambda e, pst=pst, l=l, n=n: e.tensor_copy(out=modsb[:, l, n * 512:(n + 1) * 512], in_=pst[:]), reads=[r_ps], writes=[r_mod])
        S.dma("sp", lambda e: e.dma_start(out=modv.rearrange("l r n -> r l n"), in_=modsb[:]), reads=[r_mod], stream="scr")


def load_mod_bc(nc, S, ph, modv, l, r, k, name, extra_g=None, plus1=False, stream="ld"):
    t = ph.enter_context(usb(nc, name, [128, D], F32)); res = Res()
    S.dma("sp", lambda e: e.dma_start(out=t[:], in_=modv[l, r:r + 1, k * D:(k + 1) * D].to_broadcast([128, D])), writes=[res], stream=stream)
    if plus1:
        g = ph.enter_context(usb(nc, name + "_g", [128, D], F32)); rg = Res()
        S.dma("sp", lambda e: e.dma_start(out=g[:], in_=extra_g.to_broadcast([128, D])), writes=[rg], stream=stream)
        S.op("dve", lambda e: e.scalar_tensor_tensor(out=t[:], in0=t[:], scalar=1.0, in1=g[:], op0=ALU.add, op1=ALU.mult), reads=[res, rg], writes=[res])
    return t, res


def rstd_ops(S, xt, r_x, junk, r_junk, st, r_st):
    S.op("act", lambda e: e.activation(out=junk[:], in_=xt[:], func=AF.Square, accum_out=st[:, 0:1]), reads=[r_x], writes=[r_junk, r_st])
    S.op("dve", lambda e: e.tensor_scalar(out=st[:, 1:2], in0=st[:, 0:1], scalar1=1.0 / D, scalar2=EPS, op0=ALU.mult, op1=ALU.add), reads=[r_st], writes=[r_st])
    S.op("act", lambda e: e.sqrt(out=st[:, 2:3], in_=st[:, 1:2]), reads=[r_st], writes=[r_st])
    S.op("dve", lambda e: e.reciprocal(out=st[:, 3:4], in_=st[:, 2:3]), reads=[r_st], writes=[r_st])


def phase1(nc, S, IN, l, xsrc, modv, uT, abd, ident, identb, ones, r_const):
    S.barrier()
    with ExitStack() as ph:
        sb = lambda n, s, d: ph.enter_context(usb(nc, n, s, d))
        G1 = [None, None]; SH1 = [None, None]
        for r in range(2):
            G1[r] = load_mod_bc(nc, S, ph, modv, l, r, 1, f"p1_G{r}", extra_g=IN["g_norm1"][l:l + 1, :], plus1=True)
            SH1[r] = load_mod_bc(nc, S, ph, modv, l, r, 0, f"p1_SH{r}")
        winb = sb("p1_winb", [128, 8, INW], BF16); r_win = Res()
        wv = IN["w_in"][l].rearrange("(j p) n -> p j n", p=128)
        for j in range(8):
            for h in range(2):
                S.dma("pool", lambda e, j=j, h=h: e.dma_start(out=winb[:, j, h * 1288:(h + 1) * 1288], in_=wv[:, j, h * 1288:(h + 1) * 1288]),
                      writes=[r_win], stream="wc")
        xts = Rot([(sb(f"p1_x{i}", [128, D], F32), Res()) for i in range(3)])
        junk = sb("p1_junk", [128, D], BF16); r_junk = Res()
        sts = Rot([(sb(f"p1_st{i}", [128, 4], F32), Res()) for i in range(3)])
        t1s = Rot([(sb(f"p1_t1{i}", [128, D], F32), Res()) for i in range(2)])
        hxbs = Rot([(sb(f"p1_hxb{i}", [128, D], BF16), Res()) for i in range(2)])
        hxTs = Rot([(sb(f"p1_hxT{i}", [128, 8, 512], BF16), Res()) for i in range(2)])
        stg = Rot([(sb(f"p1_stg{i}", [128, 512], F32), Res()) for i in range(3)])
        abs_ = Rot([(sb(f"p1_ab{i}", [128, 16], F32), Res()) for i in range(2)])
        ptr = Rot([(ph.enter_context(ups(nc, f"p1_ptr{i}", [128, 8, 128], BF16)), Res()) for i in range(2)])
        pmm = Rot([(ph.enter_context(ups(nc, f"p1_pmm{i}", [128, 512], F32)), Res()) for i in range(4)])
        pab = Rot([(ph.enter_context(ups(nc, f"p1_pab{i}", [128, 16], F32)), Res()) for i in range(2)])
        blocks = [(b * 4, 4) for b in range(8)] + [(32, 2)]
        for (t0, ntl) in blocks:
            hxT, r_hxT = hxTs.next()
            ntok = ntl * 128
            for ti in range(ntl):
                t = t0 + ti
                r = 0 if t < NTL else 1
                xt, r_x = xts.next()
                S.dma("sp", lambda e, xt=xt, t=t: e.dma_start(out=xt[:], in_=xsrc[t * 128:(t + 1) * 128, :]), writes=[r_x], stream="ld")
                st, r_st = sts.next()
                rstd_ops(S, xt, r_x, junk, r_junk, st, r_st)
                t1, r_t1 = t1s.next()
                S.op("dve", lambda e, t1=t1, xt=xt, st=st, r=r: e.scalar_tensor_tensor(out=t1[:], in0=xt[:], scalar=st[:, 3:4], in1=G1[r][0][:], op0=ALU.mult, op1=ALU.mult),
                     reads=[r_x, r_st, G1[r][1]], writes=[r_t1])
                hxb, r_hxb = hxbs.next()
                S.op("pool", lambda e, hxb=hxb, t1=t1, r=r: e.tensor_tensor(out=hxb[:], in0=t1[:], in1=SH1[r][0][:], op=ALU.add),
                     reads=[r_t1, SH1[r][1]], writes=[r_hxb])
                pt, r_pt = ptr.next()
                for j in range(8):
                    S.op("pe", lambda e, pt=pt, hxb=hxb, j=j: e.transpose(out=pt[:, j, :], in_=hxb[:, j * 128:(j + 1) * 128], identity=identb[:]),
                         reads=[r_hxb, r_const], writes=[r_pt])
                S.op("act", lambda e, pt=pt, hxT=hxT, ti=ti: e.copy(out=hxT[:, :, ti * 128:(ti + 1) * 128], in_=pt[:]), reads=[r_pt], writes=[r_hxT])
                pa, r_pa = pab.next()
                for j in range(8):
                    S.op("pe", lambda e, pa=pa, hxT=hxT, ti=ti, j=j: e.matmul(pa[:], lhsT=hxT[:, j, ti * 128:(ti + 1) * 128], rhs=winb[:, j, 2560:2576], start=(j == 0), stop=(j == 7)),
                         reads=[r_hxT, r_win], writes=[r_pa])
                ab, r_ab = abs_.next()
                S.op("dve", lambda e, ab=ab, pa=pa: e.tensor_copy(out=ab[:], in_=pa[:]), reads=[r_pa], writes=[r_ab])
                S.dma("sp", lambda e, ab=ab, t=t: e.dma_start(out=abd[t * 128:(t + 1) * 128, :], in_=ab[:]), reads=[r_ab], stream="scr")
            for c in range(20):
                pm, r_pm = pmm.next()
                for j in range(8):
                    S.op("pe", lambda e, pm=pm, hxT=hxT, c=c, j=j, ntok=ntok: e.matmul(pm[:, 0:ntok], lhsT=winb[:, j, c * 128:(c + 1) * 128], rhs=hxT[:, j, 0:ntok], start=(j == 0), stop=(j == 7)),
                         reads=[r_hxT, r_win], writes=[r_pm])
                sg, r_sg = stg.next()
                eng = "act" if c % 2 == 0 else "dve"
                if eng == "act":
                    S.op("act", lambda e, sg=sg, pm=pm, ntok=ntok: e.copy(out=sg[:, 0:ntok], in_=pm[:, 0:ntok]), reads=[r_pm], writes=[r_sg])
                else:
                    S.op("dve", lambda e, sg=sg, pm=pm, ntok=ntok: e.tensor_copy(out=sg[:, 0:ntok], in_=pm[:, 0:ntok]), reads=[r_pm], writes=[r_sg])
                S.dma("sp", lambda e, sg=sg, c=c, t0=t0, ntok=ntok: e.dma_start(out=uT[c * 128:(c + 1) * 128, t0 * 128:t0 * 128 + ntok], in_=sg[:, 0:ntok]), reads=[r_sg], stream="scr")


def phase2(nc, S, IN, l, uT, mixT, r_const):
    S.barrier()
    with ExitStack() as ph:
        sb = lambda n, s, d: ph.enter_context(usb(nc, n, s, d))
        cs = sb("p2_cs", [128, 256], F32); r_cs = Res()
        S.dma("sp", lambda e: e.dma_start(out=cs[:], in_=IN["cs128"]), writes=[r_cs], stream="ld")
        ntiles = NT if l == 0 else NTL
        FCS = sb("p2_fcs", [128, NT, 4, 256], BF16); r_fcs = [Res() for _ in range(NT)]
        fts = Rot([(sb(f"p2_ft{i}", [128, 4, 128], F32), Res()) for i in range(3)])
        pas = Rot([(ph.enter_context(ups(nc, f"p2_pa{i}", [128, 4, 256], F32)), Res()) for i in range(2)])
        pos = Rot([(ph.enter_context(ups(nc, f"p2_po{i}", [128, 512], F32)), Res()) for i in range(4)])
        for t in range(ntiles):
            ft, r_ft = fts.next()
            S.dma("sp", lambda e, ft=ft, t=t: e.dma_start(out=ft[:], in_=uT[0:512, t * 128:(t + 1) * 128].rearrange("(g p) t -> p g t", p=128)), writes=[r_ft], stream="ld")
            pa, r_pa = pas.next()
            for g in range(4):
                S.op("pe", lambda e, pa=pa, ft=ft, g=g: e.matmul(pa[:, g, :], lhsT=ft[:, g, :], rhs=cs[:], start=True, stop=True), reads=[r_ft, r_cs], writes=[r_pa])
            if t % 2 == 0:
                S.op("act", lambda e, pa=pa, t=t: e.copy(out=FCS[:, t, :, :], in_=pa[:]), reads=[r_pa], writes=[r_fcs[t]])
            else:
                S.op("dve", lambda e, pa=pa, t=t: e.tensor_copy(out=FCS[:, t, :, :], in_=pa[:]), reads=[r_pa], writes=[r_fcs[t]])
        cosb = Rot([(sb(f"p2_cos{i}", [128, NTL, 256], BF16), Res()) for i in range(2)])
        sinb = Rot([(sb(f"p2_sin{i}", [128, NTL, 256], BF16), Res()) for i in range(2)])
        ostg = Rot([(sb(f"p2_os{i}", [128, 256], BF16), Res()) for i in range(4)])
        segs = [(0, NTL, "L")] + ([(NTL, 2, "C")] if l == 0 else [])
        cvL = IN["cosL"].rearrange("(j p) k -> p j k", p=128)
        svL = IN["nsinL"].rearrange("(j p) k -> p j k", p=128)
        qss = Rot([(sb(f"p2_qs{i}", [128, 256], F32), Res()) for i in range(2)])
        c0t = sb("p2_c0", [128, NTL, 2], BF16); r_c0 = Res()
        S.dma("sp", lambda e: e.dma_start(out=c0t[:], in_=cvL[:, :, 0:2]), writes=[r_c0], stream="ld")
        for g in range(4):
            po, r_po = pos.next()
            for j in range(NTL):
                S.op("pe", lambda e, po=po, j=j, g=g: e.matmul(po[:, 0:2], lhsT=FCS[:, j, g, 0:128], rhs=c0t[:, j, :], start=(j == 0), stop=(j == NTL - 1)), reads=[r_fcs[j], r_c0], writes=[r_po])
            og, r_og = ostg.next()
            S.op("act", lambda e, og=og, po=po: e.copy(out=og[:, 0:2], in_=po[:, 0:2]), reads=[r_po], writes=[r_og])
            S.dma("sp", lambda e, og=og, g=g: e.dma_start(out=mixT[g * 128:(g + 1) * 128, 0:1], in_=og[:, 0:1], allow_slow_non_contiguous=True), reads=[r_og], stream="scr")
        for b in range(8):
            cb, r_cb = cosb.next(); sn, r_sn = sinb.next()
            S.dma("sp", lambda e, cb=cb, b=b: e.dma_start(out=cb[:], in_=cvL[:, :, b * 256 + 1:b * 256 + 257]), writes=[r_cb], stream="ld")
            S.dma("act", lambda e, sn=sn, b=b: e.dma_start(out=sn[:], in_=svL[:, :, b * 256 + 1:b * 256 + 257]), writes=[r_sn], stream="ld2")
            for g in range(4):
                pP, r_pP = pos.next(); pQ, r_pQ = pos.next()
                for j in range(NTL):
                    S.op("pe", lambda e, pP=pP, j=j, g=g, cb=cb: e.matmul(pP[:, 0:256], lhsT=FCS[:, j, g, 0:128], rhs=cb[:, j, :], start=(j == 0), stop=(j == NTL - 1)), reads=[r_fcs[j], r_cb], writes=[r_pP])
                for j in range(NTL):
                    S.op("pe", lambda e, pQ=pQ, j=j, g=g, sn=sn: e.matmul(pQ[:, 0:256], lhsT=FCS[:, j, g, 128:256], rhs=sn[:, j, :], start=(j == 0), stop=(j == NTL - 1)), reads=[r_fcs[j], r_sn], writes=[r_pQ])
                qs, r_qs = qss.next()
                S.op("act", lambda e, qs=qs, pQ=pQ: e.copy(out=qs[:], in_=pQ[:, 0:256]), reads=[r_pQ], writes=[r_qs])
                og1, r_og1 = ostg.next(); og2, r_og2 = ostg.next()
                S.op("dve", lambda e, og1=og1, pP=pP, qs=qs: e.tensor_tensor(out=og1[:], in0=pP[:, 0:256], in1=qs[:], op=ALU.add), reads=[r_pP, r_qs], writes=[r_og1])
                S.op("dve", lambda e, og2=og2, pP=pP, qs=qs: e.tensor_tensor(out=og2[:, ::-1], in0=pP[:, 0:256], in1=qs[:], op=ALU.subtract), reads=[r_pP, r_qs], writes=[r_og2])
                S.dma("sp", lambda e, og1=og1, g=g, b=b: e.dma_start(out=mixT[g * 128:(g + 1) * 128, b * 256 + 1:b * 256 + 257], in_=og1[:]), reads=[r_og1], stream="scr")
                S.dma("sp", lambda e, og2=og2, g=g, b=b: e.dma_start(out=mixT[g * 128:(g + 1) * 128, (15 - b) * 256:(16 - b) * 256], in_=og2[:]), reads=[r_og2], stream="scr")
        segs = [s_ for s_ in segs if s_[2] != "L"]
        for (t0, ntl, nm) in segs:
            cv = IN["cos" + nm].rearrange("(j p) k -> p j k", p=128)
            sv = IN["nsin" + nm].rearrange("(j p) k -> p j k", p=128)
            for kb in range(ntl * 128 // 256):
                cb, r_cb = cosb.next(); sn, r_sn = sinb.next()
                S.dma("sp", lambda e, cb=cb, kb=kb, cv=cv, ntl=ntl: e.dma_start(out=cb[:, 0:ntl, :], in_=cv[:, :, kb * 256:(kb + 1) * 256]), writes=[r_cb], stream="ld")
                S.dma("act", lambda e, sn=sn, kb=kb, sv=sv, ntl=ntl: e.dma_start(out=sn[:, 0:ntl, :], in_=sv[:, :, kb * 256:(kb + 1) * 256]), writes=[r_sn], stream="ld2")
                for g in range(4):
                    po, r_po = pos.next()
                    for j in range(ntl):
                        S.op("pe", lambda e, po=po, j=j, g=g, cb=cb, t0=t0: e.matmul(po[:, 0:256], lhsT=FCS[:, t0 + j, g, 0:128], rhs=cb[:, j, :], start=(j == 0), stop=False),
                             reads=[r_fcs[t0 + j], r_cb], writes=[r_po])
                        S.op("pe", lambda e, po=po, j=j, g=g, sn=sn, t0=t0, ntl=ntl: e.matmul(po[:, 0:256], lhsT=FCS[:, t0 + j, g, 128:256], rhs=sn[:, j, :], start=False, stop=(j == ntl - 1)),
                             reads=[r_fcs[t0 + j], r_sn], writes=[r_po])
                    og, r_og = ostg.next()
                    if g % 2 == 0:
                        S.op("act", lambda e, og=og, po=po: e.copy(out=og[:], in_=po[:, 0:256]), reads=[r_po], writes=[r_og])
                    else:
                        S.op("dve", lambda e, og=og, po=po: e.tensor_copy(out=og[:], in_=po[:, 0:256]), reads=[r_po], writes=[r_og])
                    S.dma("sp", lambda e, og=og, g=g, t0=t0, kb=kb: e.dma_start(out=mixT[g * 128:(g + 1) * 128, t0 * 128 + kb * 256:t0 * 128 + (kb + 1) * 256], in_=og[:]), reads=[r_og], stream="scr")


WC = T + 4


def tcol(t):
    return t * 128 + (4 if t >= NTL else 0)


def phase3(nc, S, IN, l, uT, abd, mixT, ident, ones, r_const, dbg_oT, bg=()):
    S.barrier()
    with ExitStack() as ph:
        sb = lambda n, s, d: ph.enter_context(usb(nc, n, s, d))
        pst = lambda n, s, d: ph.enter_context(ups(nc, n, s, d))
        r_c3 = Res()
        cm = {}
        for nm in ("m_ls", "m_li", "m_us", "m_ui", "tri_f", "tri_b", "blk"):
            cm[nm] = sb("p3_" + nm, [128, 128], F32)
            S.dma("sp", lambda e, nm=nm: e.dma_start(out=cm[nm][:], in_=IN[nm]), writes=[r_c3], stream="ld")
        cwr = sb("p3_cwr", [5, 1536], F32)
        gor = sb("p3_gor", [1, 128], F32)
        S.dma("sp", lambda e: e.dma_start(out=cwr[:], in_=IN["conv_w"][l]), writes=[r_c3], stream="ld")
        S.dma("sp", lambda e: e.dma_start(out=gor[:], in_=IN["g_out_norm"][l:l + 1, :]), writes=[r_c3], stream="ld")
        alb = sb("p3_alb", [128, 8], F32); dtb = sb("p3_dtb", [128, 8], F32)
        S.dma("sp", lambda e: e.dma_start(out=alb[:], in_=IN["a_log"][l:l + 1, :].to_broadcast([128, 8])), writes=[r_c3], stream="ld")
        S.dma("sp", lambda e: e.dma_start(out=dtb[:], in_=IN["dt_bias"][l:l + 1, :].to_broadcast([128, 8])), writes=[r_c3], stream="ld")
        banks = [pst(f"p3_bank{i}", [128, 512], F32) for i in range(8)]
        qtile = lambda b, q: banks[b][:, q * 128:(q + 1) * 128]
        rbank = [Res(excl=True) for _ in range(8)]
        pcw = banks[0][:, 0:104].rearrange("p (m k) -> p m k", k=8); r_pcw = rbank[0]
        cw = sb("p3_cw", [128, 13, 8], F32)
        for m in range(12):
            S.op("pe", lambda e, m=m: e.transpose(out=pcw[:, m, 0:5], in_=cwr[:, m * 128:(m + 1) * 128], identity=ident[0:5, 0:5]), reads=[r_c3, r_const], writes=[r_pcw])
        S.op("pe", lambda e: e.transpose(out=pcw[:, 12, 0:1], in_=gor[:, :], identity=ident[0:1, 0:1]), reads=[r_c3, r_const], writes=[r_pcw])
        r_cw = Res()
        S.op("dve", lambda e: e.memset(cw[:], 0.0), writes=[r_cw])
        for m in range(12):
            S.op("dve", lambda e, m=m: e.tensor_copy(out=cw[:, m, 0:5], in_=pcw[:, m, 0:5]), reads=[r_pcw], writes=[r_cw])
        S.op("dve", lambda e: e.tensor_copy(out=cw[:, 12, 0:1], in_=pcw[:, 12, 0:1]), reads=[r_pcw], writes=[r_cw])
        nea = sb("p3_nea", [128, 8], F32)
        S.op("act", lambda e: e.activation(out=nea[:], in_=alb[:], func=AF.Exp), reads=[r_c3], writes=[r_c3])
        S.op("dve", lambda e: e.tensor_scalar(out=nea[:], in0=nea[:], scalar1=-1.0, scalar2=None, op0=ALU.mult), reads=[r_c3], writes=[r_c3])
        names = ("BT", "NBT", "GAM", "EG", "BEG", "EK0", "EK1")
        GA = {nm: sb("p3_" + nm, [128, NT, 8], F32) for nm in names}
        r_ga = [Res() for _ in range(NT)]
        abt = Rot([(sb(f"p3_abt{i}", [128, 16], F32), Res()) for i in range(2)])
        tmp = Rot([(sb(f"p3_gt{i}", [128, 4, 8], F32), Res()) for i in range(2)])
        pgs = Rot([(banks[0][:, 128:144], r_pcw)])
        rowm = sb("p3_rowm", [128, 2], F32)
        S.op("dve", lambda e: e.tensor_copy(out=rowm[:, 0:1], in_=cm["blk"][:, 0:1]), reads=[r_c3], writes=[r_c3])
        S.op("dve", lambda e: e.tensor_copy(out=rowm[:, 1:2], in_=cm["blk"][:, 127:128]), reads=[r_c3], writes=[r_c3])
        for t in range(NT):
            ab, r_ab = abt.next()
            S.dma("sp", lambda e, ab=ab, t=t: e.dma_start(out=ab[:], in_=abd[t * 128:(t + 1) * 128, :]), writes=[r_ab], stream="ld")
            tm, r_tm = tmp.next()
            abv = ab[:].rearrange("p (d k h) -> p d k h", d=2, k=2)
            X = tm[:, 0, :].rearrange("p (d h) -> p d h", d=2)
            S.op("dve", lambda e, X=X, abv=abv: e.tensor_tensor(out=X, in0=abv[:, :, 0, :], in1=dtb[:].rearrange("p (d h) -> p d h", d=2), op=ALU.add), reads=[r_ab, r_c3], writes=[r_tm])
            S.op("act", lambda e, tm=tm: e.activation(out=tm[:, 0, :], in_=tm[:, 0, :], func=AF.Exp), reads=[r_tm], writes=[r_tm])
            S.op("act", lambda e, tm=tm: e.activation(out=tm[:, 0, :], in_=tm[:, 0, :], func=AF.Ln, bias=1.0), reads=[r_tm], writes=[r_tm])
            S.op("dve", lambda e, tm=tm: e.tensor_tensor(out=tm[:, 1, :], in0=tm[:, 0, :], in1=nea[:], op=ALU.mult), reads=[r_tm, r_c3], writes=[r_tm])
            B = tm[:, 2, :].rearrange("p (d h) -> p d h", d=2)
            S.op("act", lambda e, B=B, abv=abv: e.activation(out=B, in_=abv[:, :, 1, :], func=AF.Exp, scale=-1.0), reads=[r_ab], writes=[r_tm])
            S.op("dve", lambda e, tm=tm: e.tensor_scalar(out=tm[:, 2, :], in0=tm[:, 2, :], scalar1=1.0, scalar2=None, op0=ALU.add), reads=[r_tm], writes=[r_tm])
            S.op("dve", lambda e, tm=tm, t=t: e.reciprocal(out=GA["BT"][:, t, :], in_=tm[:, 2, :]), reads=[r_tm], writes=[r_ga[t]])
            S.op("dve", lambda e, t=t: e.tensor_scalar(out=GA["NBT"][:, t, :], in0=GA["BT"][:, t, :], scalar1=-1.0, scalar2=None, op0=ALU.mult), reads=[r_ga[t]], writes=[r_ga[t]])
            pg, r_pg = pgs.next()
            S.op("pe", lambda e, pg=pg, tm=tm: e.matmul(pg[:, 0:4], lhsT=cm["tri_f"][:], rhs=tm[:, 1, 0:4], start=True, stop=True), reads=[r_tm, r_c3], writes=[r_pg])
            S.op("pe", lambda e, pg=pg, tm=tm: e.matmul(pg[:, 4:8], lhsT=cm["tri_b"][:], rhs=tm[:, 1, 4:8], start=True, stop=True), reads=[r_tm, r_c3], writes=[r_pg])
            S.op("pe", lambda e, pg=pg, tm=tm: e.matmul(pg[:, 8:16], lhsT=cm["blk"][:], rhs=tm[:, 1, :], start=True, stop=True), reads=[r_tm, r_c3], writes=[r_pg])
            S.op("dve", lambda e, pg=pg, t=t: e.tensor_copy(out=GA["GAM"][:, t, :], in_=pg[:, 0:8]), reads=[r_pg], writes=[r_ga[t]])
            S.op("act", lambda e, pg=pg, t=t: e.activation(out=GA["EG"][:, t, :], in_=pg[:, 0:8], func=AF.Exp), reads=[r_pg], writes=[r_ga[t]])
            S.op("dve", lambda e, t=t: e.tensor_tensor(out=GA["BEG"][:, t, :], in0=GA["EG"][:, t, :], in1=GA["BT"][:, t, :], op=ALU.mult), reads=[r_ga[t]], writes=[r_ga[t]])
            S.op("dve", lambda e, pg=pg, tm=tm, t=t: e.tensor_tensor(out=tm[:, 3, :], in0=pg[:, 8:16], in1=GA["GAM"][:, t, :], op=ALU.subtract), reads=[r_pg, r_ga[t]], writes=[r_tm])
            S.op("act", lambda e, tm=tm: e.activation(out=tm[:, 3, :], in_=tm[:, 3, :], func=AF.Exp), reads=[r_tm], writes=[r_tm])
            S.op("dve", lambda e, tm=tm, t=t: e.tensor_scalar(out=GA["EK0"][:, t, :], in0=tm[:, 3, :], scalar1=rowm[:, 0:1], scalar2=None, op0=ALU.mult), reads=[r_tm, r_c3], writes=[r_ga[t]])
            S.op("dve", lambda e, tm=tm, t=t: e.tensor_scalar(out=GA["EK1"][:, t, :], in0=tm[:, 3, :], scalar1=rowm[:, 1:2], scalar2=None, op0=ALU.mult), reads=[r_tm, r_c3], writes=[r_ga[t]])
        raws = Rot([(sb(f"p3_raw{i}", [128, WC + 4], F32), Res()) for i in range(2)])
        QKV = [(sb(f"p3_qkv{i}", [128, WC], F32), Res()) for i in range(3)]
        oT = sb("p3_oT", [128, WC], F32); r_oT = [Res() for _ in range(NT)]
        r_oTall = Res()
        pn = banks[1]; r_pn = rbank[1]
        rns = Rot([(sb(f"p3_rn{i}", [128, 512], F32), Res()) for i in range(2)])
        zts = Rot([(sb(f"p3_z{i}", [128, 512], F32), Res()) for i in range(2)])
        obs = Rot([(sb(f"p3_ob{i}", [128, 512], BF16), Res()) for i in range(2)])

        KSLOT = 4; DEPTH = 3
        INTER = ("dg", "Dm", "E1", "E2", "N", "NTs", "TT", "Pa", "PTa", "Pb", "PTb", "Rv", "Rw")
        OUTS = ("EGr", "at", "u", "wT", "qg", "ke0", "ke1")
        BF_NAMES = ("Nb", "NTs", "TT", "Pa", "PTa", "Pb", "PTb", "Rv", "Rw")
        BI = [{n: (sb(f"p3_{n}_s{s}", [128, 128], BF16 if n in BF_NAMES else F32), Res()) for n in INTER + ("Nb",)} for s in range(KSLOT)]
        BO = [{n: Rot([(sb(f"p3_{n}_d{d}_{i}", [128, 128], F32), Res()) for i in range(DEPTH)]) for n in OUTS} for d in range(2)]
        VN = [Rot([(sb(f"p3_vn{d}_{i}", [128, 128], F32), Res()) for i in range(2)]) for d in range(2)]
        rbank_ = rbank
        slot_bank = (2, 3, 4, 7)
        PQ = [Rot([(qtile(slot_bank[s], qi), rbank_[slot_bank[s]]) for qi in range(4)]) for s in range(KSLOT)]
        PSC = {d: {n: (qtile(5 + d, qi), rbank_[5 + d]) for qi, n in enumerate(("ps1", "po", "pS"))} for d in range(2)}
        Sst = [(sb(f"p3_S{d}", [128, 128], F32), Res()) for d in range(2)]
        bg = list(bg)
        chunks9 = [(i * 512, 512) for i in range(8)] + [(4100, 256)]

        LVL = 9; NTI = NT
        for h in range(4 if LVL >= 9 else (1 if LVL >= 1 else 0)):
            for which in range(3):
                raw, r_raw = raws.next()
                row0 = 512 + which * 512 + h * 128
                S.op("pool", lambda e, raw=raw: e.memset(raw[:], 0.0), writes=[r_raw])
                S.dma("sp", lambda e, raw=raw, row0=row0: e.dma_start(out=raw[:, 2:2 + SEQ], in_=uT[row0:row0 + 128, 0:SEQ]), writes=[r_raw], stream="ld")
                S.dma("sp", lambda e, raw=raw, row0=row0: e.dma_start(out=raw[:, SEQ + 6:SEQ + 6 + CTX], in_=uT[row0:row0 + 128, SEQ:T]), writes=[r_raw], stream="ld")
                dst, r_dst = QKV[which]
                m = which * 4 + h
                S.op("dve", lambda e, dst=dst, raw=raw, m=m: e.tensor_scalar(out=dst[:], in0=raw[:, 0:WC], scalar1=cw[:, m, 0:1], scalar2=None, op0=ALU.mult), reads=[r_raw, r_cw], writes=[r_dst])
                for k in range(1, 5):
                    S.op("dve", lambda e, dst=dst, raw=raw, m=m, k=k: e.scalar_tensor_tensor(out=dst[:], in0=raw[:, k:k + WC], scalar=cw[:, m, k:k + 1], in1=dst[:], op0=ALU.mult, op1=ALU.add),
                         reads=[r_raw, r_cw, r_dst], writes=[r_dst])
                S.op("act", lambda e, dst=dst: e.activation(out=dst[:], in_=dst[:], func=AF.Silu), reads=[r_dst], writes=[r_dst])
                if which < 2:
                    sq, r_sq = raws.items[(raws.i) % 2]
                    S.op("pool", lambda e, sq=sq, dst=dst: e.tensor_tensor(out=sq[:, 0:WC], in0=dst[:], in1=dst[:], op=ALU.mult), reads=[r_dst], writes=[r_sq])
                    for (c0, cn) in chunks9:
                        S.op("pe", lambda e, sq=sq, c0=c0, cn=cn: e.matmul(pn[:, 0:cn], lhsT=ones[:], rhs=sq[:, c0:c0 + cn], start=True, stop=True), reads=[r_sq, r_const], writes=[r_pn])
                        rn, r_rn = rns.next()
                        S.op("dve", lambda e, rn=rn, cn=cn: e.tensor_scalar(out=rn[:, 0:cn], in0=pn[:, 0:cn], scalar1=EPS, scalar2=None, op0=ALU.add), reads=[r_pn], writes=[r_rn])
                        S.op("act", lambda e, rn=rn, cn=cn: e.sqrt(out=rn[:, 0:cn], in_=rn[:, 0:cn]), reads=[r_rn], writes=[r_rn])
                        S.op("dve", lambda e, rn=rn, cn=cn: e.reciprocal(out=rn[:, 0:cn], in_=rn[:, 0:cn]), reads=[r_rn], writes=[r_rn])
                        sc = (128.0 ** -0.5) if which == 0 else 1.0
                        S.op("dve", lambda e, rn=rn, dst=dst, c0=c0, cn=cn, sc=sc: e.scalar_tensor_tensor(out=dst[:, c0:c0 + cn], in0=dst[:, c0:c0 + cn], scalar=sc, in1=rn[:, 0:cn], op0=ALU.mult, op1=ALU.mult),
                             reads=[r_rn, r_dst], writes=[r_dst])
            qT, r_q = QKV[0]; kT, r_k = QKV[1]; vT, r_v = QKV[2]
            for d in range(2):
                S.op("dve", lambda e, d=d: e.memset(Sst[d][0][:], 0.0), writes=[Sst[d][1]])
            seqs = [[32, 33] + list(range(32)), [33, 32] + list(range(31, -1, -1))]
            if LVL < 2:
                seqs = [[], []]
            else:
                seqs = [s_[:NTI] for s_ in seqs]
            written = set()
            PREP = {}
            scanned = [0, 0]

            def prep_gen(t, d, s):
                c0 = tcol(t); col = d * 4 + h
                bi = BI[s]; pq = PQ[s]
                ksl = kT[:, c0:c0 + 128]; qsl = qT[:, c0:c0 + 128]; vsl = vT[:, c0:c0 + 128]
                gam = GA["GAM"][:, t, col:col + 1]
                dg, r_dg = bi["dg"]; Dm, r_Dm = bi["Dm"]; E1, r_E1 = bi["E1"]; E2, r_E2 = bi["E2"]
                EGr, r_EGr = BO[d]["EGr"].next()
                gr, r_gr = pq.next()
                S.op("dve", lambda e: e.tensor_scalar(out=dg[:], in0=ident[:], scalar1=gam, scalar2=None, op0=ALU.mult), reads=[r_const, r_ga[t]], writes=[r_dg])
                S.op("pe", lambda e: e.matmul(gr[:], lhsT=ones[:], rhs=dg[:], start=True, stop=True), reads=[r_dg, r_const], writes=[r_gr])
                S.op("dve", lambda e: e.tensor_scalar(out=Dm[:], in0=gr[:], scalar1=-1.0, scalar2=gam, op0=ALU.mult, op1=ALU.add), reads=[r_gr, r_ga[t]], writes=[r_Dm])
                S.op("act", lambda e: e.activation(out=EGr[:], in_=gr[:], func=AF.Exp), reads=[r_gr], writes=[r_EGr])
                yield
                m1 = cm["m_ls"] if d == 0 else cm["m_us"]
                m2 = cm["m_ui"] if d == 0 else cm["m_li"]
                S.op("pool", lambda e: e.tensor_tensor(out=E1[:], in0=Dm[:], in1=m1[:], op=ALU.add), reads=[r_Dm, r_c3], writes=[r_E1])
                S.op("pool", lambda e: e.tensor_tensor(out=E2[:], in0=m2[:], in1=Dm[:], op=ALU.subtract), reads=[r_Dm, r_c3], writes=[r_E2])
                S.op("act", lambda e: e.activation(out=E1[:], in_=E1[:], func=AF.Exp), reads=[r_E1], writes=[r_E1])
                S.op("act", lambda e: e.activation(out=E2[:], in_=E2[:], func=AF.Exp), reads=[r_E2], writes=[r_E2])
                yield
                N, r_N = bi["N"]; at, r_at = BO[d]["at"].next()
                nbt = GA["NBT"][:, t, col:col + 1]
                kk, r_kk = pq.next()
                S.op("pe", lambda e: e.matmul(kk[:], lhsT=ksl, rhs=ksl, start=True, stop=True), reads=[r_k], writes=[r_kk])
                S.op("dve", lambda e: e.scalar_tensor_tensor(out=N[:], in0=kk[:], scalar=nbt, in1=E1[:], op0=ALU.mult, op1=ALU.mult), reads=[r_kk, r_ga[t], r_E1], writes=[r_N])
                Nb, r_Nb = bi["Nb"]
                S.op("act", lambda e: e.copy(out=Nb[:], in_=N[:]), reads=[r_N], writes=[r_Nb])
                kq, r_kq = pq.next()
                S.op("pe", lambda e: e.matmul(kq[:], lhsT=ksl, rhs=qsl, start=True, stop=True), reads=[r_k, r_q], writes=[r_kq])
                S.op("dve", lambda e: e.tensor_tensor(out=at[:], in0=kq[:], in1=E2[:], op=ALU.mult), reads=[r_kq, r_E2], writes=[r_at])
                yield
                Rv, r_Rv = bi["Rv"]; Rw, r_Rw = bi["Rw"]
                ke0, r_ke0 = BO[d]["ke0"].next(); ke1, r_ke1 = BO[d]["ke1"].next(); qg, r_qg = BO[d]["qg"].next()
                bt = GA["BT"][:, t, col:col + 1]; beg = GA["BEG"][:, t, col:col + 1]
                kt, r_kt = pq.next()
                S.op("pe", lambda e: e.transpose(out=kt[:], in_=ksl, identity=ident[:]), reads=[r_k, r_const], writes=[r_kt])
                S.op("act", lambda e: e.activation(out=Rw[:], in_=kt[:], func=AF.Copy, scale=beg), reads=[r_kt, r_ga[t]], writes=[r_Rw])
                S.op("dve", lambda e: e.tensor_scalar(out=ke0[:], in0=kt[:], scalar1=GA["EK0"][:, t, col:col + 1], scalar2=None, op0=ALU.mult), reads=[r_kt, r_ga[t]], writes=[r_ke0])
                S.op("dve", lambda e: e.tensor_scalar(out=ke1[:], in0=kt[:], scalar1=GA["EK1"][:, t, col:col + 1], scalar2=None, op0=ALU.mult), reads=[r_kt, r_ga[t]], writes=[r_ke1])
                vt, r_vt = pq.next()
                S.op("pe", lambda e: e.transpose(out=vt[:], in_=vsl, identity=ident[:]), reads=[r_v, r_const], writes=[r_vt])
                S.op("act", lambda e: e.activation(out=Rv[:], in_=vt[:], func=AF.Copy, scale=bt), reads=[r_vt, r_ga[t]], writes=[r_Rv])
                S.op("pool", lambda e: e.tensor_tensor(out=qg[:], in0=qsl, in1=EGr[:], op=ALU.mult), reads=[r_q, r_EGr], writes=[r_qg])
                yield
                NTs, r_NTs = bi["NTs"]; TT, r_TT = bi["TT"]
                ntp, r_ntp = pq.next()
                S.op("pe", lambda e: e.transpose(out=ntp[:], in_=N[:], identity=ident[:]), reads=[r_N, r_const], writes=[r_ntp])
                S.op("act", lambda e: e.copy(out=NTs[:], in_=ntp[:]), reads=[r_ntp], writes=[r_NTs])
                S.op("dve", lambda e: e.tensor_tensor(out=TT[:], in0=ntp[:], in1=ident[:], op=ALU.add), reads=[r_ntp, r_const], writes=[r_TT])
                yield
                P_, r_P = Nb, r_Nb
                PT_, r_PT = NTs, r_NTs
                for lev in range(1, 6):
                    p2, r_p2 = pq.next()
                    S.op("pe", lambda e, p2=p2, PT_=PT_, P_=P_: e.matmul(p2[:], lhsT=PT_[:], rhs=P_[:], start=True, stop=True), reads=[r_P, r_PT], writes=[r_p2])
                    nP, r_nP = bi["Pa" if lev % 2 else "Pb"]
                    S.op("act", lambda e, nP=nP, p2=p2: e.copy(out=nP[:], in_=p2[:]), reads=[r_p2], writes=[r_nP])
                    if lev < 5:
                        pt2, r_pt2 = pq.next()
                        S.op("pe", lambda e, pt2=pt2, PT_=PT_, P_=P_: e.matmul(pt2[:], lhsT=P_[:], rhs=PT_[:], start=True, stop=True), reads=[r_P, r_PT], writes=[r_pt2])
                        nPT, r_nPT = bi["PTa" if lev % 2 else "PTb"]
                        S.op("dve", lambda e, nPT=nPT, pt2=pt2: e.tensor_copy(out=nPT[:], in_=pt2[:]), reads=[r_pt2], writes=[r_nPT])
                    yield
                    up, r_up = pq.next()
                    S.op("pe", lambda e, up=up, nP=nP: e.matmul(up[:], lhsT=nP[:], rhs=TT[:], start=True, stop=True), reads=[r_nP, r_TT], writes=[r_up])
                    S.op("dve", lambda e, up=up: e.tensor_tensor(out=TT[:], in0=up[:], in1=TT[:], op=ALU.add), reads=[r_up, r_TT], writes=[r_TT])
                    P_, r_P = nP, r_nP
                    if lev < 5:
                        PT_, r_PT = nPT, r_nPT
                    yield
                u, r_u = BO[d]["u"].next(); wT, r_wT = BO[d]["wT"].next()
                pu, r_pu = pq.next()
                S.op("pe", lambda e: e.matmul(pu[:], lhsT=TT[:], rhs=Rv[:], start=True, stop=True), reads=[r_TT, r_Rv], writes=[r_pu])
                S.op("act", lambda e: e.copy(out=u[:], in_=pu[:]), reads=[r_pu], writes=[r_u])
                pw, r_pw = pq.next()
                S.op("pe", lambda e: e.matmul(pw[:], lhsT=Rw[:], rhs=TT[:], start=True, stop=True), reads=[r_TT, r_Rw], writes=[r_pw])
                S.op("dve", lambda e: e.tensor_copy(out=wT[:], in_=pw[:]), reads=[r_pw], writes=[r_wT])
                PREP[(t, d)] = dict(EGr=(EGr, r_EGr), at=(at, r_at), u=(u, r_u), wT=(wT, r_wT), qg=(qg, r_qg), ke0=(ke0, r_ke0), ke1=(ke1, r_ke1))

            def scan_gen(d):
                Sd, r_S = Sst[d]
                for t in seqs[d]:
                    while (t, d) not in PREP:
                        yield "wait"
                    pr = PREP[(t, d)]
                    c0 = tcol(t)
                    EGr, r_EGr = pr["EGr"]; at, r_at = pr["at"]; u, r_u = pr["u"]; wT, r_wT = pr["wT"]; qg, r_qg = pr["qg"]
                    for c in ((0, 1) if d == 0 else (1, 0)):
                        cs_ = slice(c * 64, (c + 1) * 64)
                        gcol = c * 64 + (63 if d == 0 else 0)
                        ps1, r_ps1 = PSC[d]["ps1"]; po, r_po = PSC[d]["po"]; pS, r_pS = PSC[d]["pS"]
                        vn, r_vn = VN[d].next()
                        ke, r_ke = pr["ke0"] if c == 0 else pr["ke1"]
                        S.op("pe", lambda e, wT=wT: e.matmul(ps1[:], lhsT=wT[:], rhs=Sd[:], start=True, stop=True), reads=[r_wT, r_S], writes=[r_ps1])
                        yield
                        S.op("dve", lambda e, vn=vn, u=u: e.tensor_tensor(out=vn[:], in0=u[:], in1=ps1[:], op=ALU.subtract), reads=[r_u, r_ps1], writes=[r_vn])
                        yield
                        S.op("pe", lambda e, qg=qg, cs_=cs_: e.matmul(po[:, 0:64], lhsT=Sd[:], rhs=qg[:, cs_], start=True, stop=False), reads=[r_S, r_qg], writes=[r_po])
                        S.op("pe", lambda e, vn=vn, at=at, cs_=cs_: e.matmul(po[:, 0:64], lhsT=vn[:], rhs=at[:, cs_], start=False, stop=True), reads=[r_vn, r_at], writes=[r_po])
                        S.op("pe", lambda e, ke=ke, vn=vn: e.matmul(pS[:], lhsT=ke[:], rhs=vn[:], start=True, stop=True), reads=[r_ke, r_vn], writes=[r_pS])
                        yield
                        osl = oT[:, c0 + c * 64:c0 + (c + 1) * 64]
                        if (t, c) not in written:
                            written.add((t, c))
                            S.op("act", lambda e, osl=osl: e.copy(out=osl, in_=po[:, 0:64]), reads=[r_po], writes=[r_oT[t]])
                        else:
                            S.op("dve", lambda e, osl=osl: e.tensor_tensor(out=osl, in0=po[:, 0:64], in1=osl, op=ALU.add), reads=[r_po, r_oT[t]], writes=[r_oT[t]])
                        S.op("dve", lambda e, EGr=EGr, gcol=gcol: e.scalar_tensor_tensor(out=Sd[:], in0=Sd[:], scalar=EGr[:, gcol:gcol + 1], in1=pS[:], op0=ALU.mult, op1=ALU.add),
                             reads=[r_S, r_EGr, r_pS], writes=[r_S])
                        yield
                    scanned[d] += 1

            queue = []
            for i in range(len(seqs[0])):
                for d in range(2):
                    queue.append((seqs[d][i], d, i))
            active = {}
            scans = [scan_gen(0), scan_gen(1)]
            scan_done = [len(seqs[0]) == 0, len(seqs[1]) == 0]
            nbg = 0
            SCANFIRST = 1; SCANSTEPS = 2

            def adv_scans():
                pr_ = False
                for d in range(2):
                    for _ in range(SCANSTEPS):
                        if not scan_done[d]:
                            try:
                                r_ = next(scans[d])
                                if r_ != "wait":
                                    pr_ = True
                                else:
                                    break
                            except StopIteration:
                                scan_done[d] = True; pr_ = True
                return pr_
            while not all(scan_done):
                progressed = False
                if SCANFIRST:
                    progressed = adv_scans() or progressed
                while queue and len(active) < KSLOT and (queue[0][2] - scanned[queue[0][1]] < DEPTH):
                    t_, d_, i_ = queue.pop(0)
                    s_ = [x for x in range(KSLOT) if x not in active][0]
                    active[s_] = prep_gen(t_, d_, s_)
                    progressed = True
                    nbg += 1
                    if bg and nbg % 2 == 0:
                        bg.pop(0)()
                for s_ in list(active.keys()):
                    try:
                        next(active[s_]); progressed = True
                    except StopIteration:
                        del active[s_]; progressed = True
                if not SCANFIRST:
                    progressed = adv_scans() or progressed
                assert progressed, "phase3 scheduler stuck"
            if dbg_oT is not None:
                S.dma("sp", lambda e, h=h: e.dma_start(out=dbg_oT[h, :, 0:SEQ], in_=oT[:, 0:SEQ]), reads=r_oT, stream="scr")
                S.dma("sp", lambda e, h=h: e.dma_start(out=dbg_oT[h, :, SEQ:T], in_=oT[:, SEQ + 4:SEQ + 4 + CTX]), reads=r_oT, stream="scr")
            sq, r_sq = raws.next()
            for ci, (c0, cn) in enumerate(chunks9):
                tiles = list(range(ci * 4, ci * 4 + 4)) if ci < 8 else [32, 33]
                rds = [r_oT[t] for t in tiles]
                S.op("pool", lambda e, sq=sq, c0=c0, cn=cn: e.tensor_tensor(out=sq[:, c0:c0 + cn], in0=oT[:, c0:c0 + cn], in1=oT[:, c0:c0 + cn], op=ALU.mult), reads=rds, writes=[r_sq])
                S.op("pe", lambda e, sq=sq, c0=c0, cn=cn: e.matmul(pn[:, 0:cn], lhsT=ones[:], rhs=sq[:, c0:c0 + cn], start=True, stop=True), reads=[r_sq, r_const], writes=[r_pn])
                rn, r_rn = rns.next()
                S.op("dve", lambda e, rn=rn, cn=cn: e.tensor_scalar(out=rn[:, 0:cn], in0=pn[:, 0:cn], scalar1=1.0 / 128, scalar2=EPS, op0=ALU.mult, op1=ALU.add), reads=[r_pn], writes=[r_rn])
                S.op("act", lambda e, rn=rn, cn=cn: e.sqrt(out=rn[:, 0:cn], in_=rn[:, 0:cn]), reads=[r_rn], writes=[r_rn])
                S.op("dve", lambda e, rn=rn, cn=cn: e.reciprocal(out=rn[:, 0:cn], in_=rn[:, 0:cn]), reads=[r_rn], writes=[r_rn])
                S.op("dve", lambda e, rn=rn, c0=c0, cn=cn: e.scalar_tensor_tensor(out=rn[:, 0:cn], in0=oT[:, c0:c0 + cn], scalar=cw[:, 12, 0:1], in1=rn[:, 0:cn], op0=ALU.mult, op1=ALU.mult),
                     reads=rds + [r_rn, r_cw], writes=[r_rn])
                zt, r_zt = zts.next()
                tok0 = ci * 512 if ci < 8 else SEQ
                zrow = 2048 + h * 128
                S.dma("sp", lambda e, zt=zt, tok0=tok0, cn=cn, zrow=zrow: e.dma_start(out=zt[:, 0:cn], in_=uT[zrow:zrow + 128, tok0:tok0 + cn]), writes=[r_zt], stream="ld")
                S.op("act", lambda e, zt=zt, cn=cn: e.activation(out=zt[:, 0:cn], in_=zt[:, 0:cn], func=AF.Silu), reads=[r_zt], writes=[r_zt])
                ob, r_ob = obs.next()
                S.op("pool", lambda e, ob=ob, rn=rn, zt=zt, cn=cn: e.tensor_tensor(out=ob[:, 0:cn], in0=rn[:, 0:cn], in1=zt[:, 0:cn], op=ALU.mult), reads=[r_rn, r_zt], writes=[r_ob])
                S.dma("sp", lambda e, ob=ob, tok0=tok0, cn=cn, h=h: e.dma_start(out=mixT[512 + h * 128:512 + (h + 1) * 128, tok0:tok0 + cn], in_=ob[:, 0:cn]), reads=[r_ob], stream="scr")
        while bg:
            bg.pop(0)()


def phase4(nc, S, IN, l, xsrc, xres, modv, mixT, h2rows, gates, r_gates, ident, ones, r_const, nt_act, dbg_g):
    S.barrier()
    with ExitStack() as ph:
        sb = lambda n, s, d: ph.enter_context(usb(nc, n, s, d))
        pst = lambda n, s, d: ph.enter_context(ups(nc, n, s, d))
        nr = 2 if nt_act > NTL else 1
        GT1 = [load_mod_bc(nc, S, ph, modv, l, r, 2, f"p4_GT{r}") for r in range(nr)]
        G2 = [load_mod_bc(nc, S, ph, modv, l, r, 4, f"p4_G{r}", extra_g=IN["g_norm2"][l:l + 1, :], plus1=True) for r in range(nr)]
        SH2 = [load_mod_bc(nc, S, ph, modv, l, r, 3, f"p4_SH{r}") for r in range(nr)]
        woutb = sb("p4_wout", [128, 8, D], BF16); r_wo = Res()
        wv = IN["w_out"][l].rearrange("(j p) n -> p j n", p=128)
        for j in range(8):
            S.dma("pool", lambda e, j=j: e.dma_start(out=woutb[:, j, :], in_=wv[:, j, :]), writes=[r_wo], stream="wc")
        wrf = sb("p4_wr", [128, 8, NE], F32); r_wr = Res()
        brr = sb("p4_br", [1, NE], F32)
        S.dma("sp", lambda e: e.dma_start(out=wrf[:], in_=IN["w_router"][l].rearrange("(j p) n -> p j n", p=128)), writes=[r_wr], stream="ld")
        S.dma("sp", lambda e: e.dma_start(out=brr[:], in_=IN["b_router"][l:l + 1, :]), writes=[r_wr], stream="ld")
        mixs = Rot([(sb(f"p4_mx{i}", [128, 8, 128], BF16), Res()) for i in range(2)])
        xts = Rot([(sb(f"p4_x{i}", [128, D], F32), Res()) for i in range(2)])
        tmps = Rot([(sb(f"p4_t{i}", [128, D], F32), Res()) for i in range(2)])
        xns = Rot([(sb(f"p4_xn{i}", [128, D], F32), Res()) for i in range(2)])
        h2s = Rot([(sb(f"p4_h2{i}", [128, D], F32), Res()) for i in range(2)])
        junk = sb("p4_junk", [128, D], BF16); r_junk = Res()
        sts = Rot([(sb(f"p4_st{i}", [128, 4], F32), Res()) for i in range(2)])
        h2bs = Rot([(sb(f"p4_hb{i}", [128, D], BF16), Res()) for i in range(2)])
        h2fs = Rot([(sb(f"p4_hf{i}", [128, 8, 128], F32), Res()) for i in range(2)])
        lgs = Rot([(sb(f"p4_lg{i}", [128, 4, NE], F32), Res()) for i in range(2)])
        t8s = Rot([(sb(f"p4_t8{i}", [128, 16], F32), Res()) for i in range(2)])
        pys = Rot([(pst(f"p4_py{i}", [128, D], F32), Res()) for i in range(2)])
        ptr = Rot([(pst("p4_ptr", [128, 8, 128], F32), Res(excl=True))])
        pls = Rot([(pst(f"p4_pl{i}", [128, NE], F32), Res()) for i in range(2)])
        for t in range(nt_act):
            r = 0 if t < NTL else 1
            mx, r_mx = mixs.next()
            S.dma("sp", lambda e, mx=mx, t=t: e.dma_start(out=mx[:], in_=mixT[:, t * 128:(t + 1) * 128].rearrange("(j p) t -> p j t", p=128)), writes=[r_mx], stream="ld")
            xt, r_x = xts.next()
            S.dma("act", lambda e, xt=xt, t=t: e.dma_start(out=xt[:], in_=xsrc[t * 128:(t + 1) * 128, :]), writes=[r_x], stream="ld2")
            py, r_py = pys.next()
            for half in range(2):
                for j in range(8):
                    S.op("pe", lambda e, py=py, mx=mx, half=half, j=j: e.matmul(py[:, half * 512:(half + 1) * 512], lhsT=mx[:, j, :], rhs=woutb[:, j, half * 512:(half + 1) * 512], start=(j == 0), stop=(j == 7)),
                         reads=[r_mx, r_wo], writes=[r_py])
            tp, r_tp = tmps.next()
            S.op("dve", lambda e, tp=tp, py=py, r=r: e.tensor_tensor(out=tp[:], in0=py[:], in1=GT1[r][0][:], op=ALU.mult), reads=[r_py, GT1[r][1]], writes=[r_tp])
            xn, r_xn = xns.next()
            S.op("pool", lambda e, xn=xn, tp=tp, xt=xt: e.tensor_tensor(out=xn[:], in0=tp[:], in1=xt[:], op=ALU.add), reads=[r_tp, r_x], writes=[r_xn])
            S.dma("sp", lambda e, xn=xn, t=t: e.dma_start(out=xres[t * 128:(t + 1) * 128, :], in_=xn[:]), reads=[r_xn], stream="scr")
            st, r_st = sts.next()
            rstd_ops(S, xn, r_xn, junk, r_junk, st, r_st)
            h2, r_h2 = h2s.next()
            S.op("dve", lambda e, h2=h2, xn=xn, st=st, r=r: e.scalar_tensor_tensor(out=h2[:], in0=xn[:], scalar=st[:, 3:4], in1=G2[r][0][:], op0=ALU.mult, op1=ALU.mult), reads=[r_xn, r_st, G2[r][1]], writes=[r_h2])
            S.op("pool", lambda e, h2=h2, r=r: e.tensor_tensor(out=h2[:], in0=h2[:], in1=SH2[r][0][:], op=ALU.add), reads=[r_h2, SH2[r][1]], writes=[r_h2])
            pt, r_pt = ptr.next()
            for j in range(8):
                S.op("pe", lambda e, pt=pt, h2=h2, j=j: e.transpose(out=pt[:, j, :], in_=h2[:, j * 128:(j + 1) * 128], identity=ident[:]), reads=[r_h2, r_const], writes=[r_pt])
            hb, r_hb = h2bs.next(); hf, r_hf = h2fs.next()
            S.op("act", lambda e, hb=hb, h2=h2: e.copy(out=hb[:], in_=h2[:]), reads=[r_h2], writes=[r_hb])
            S.op("dve", lambda e, hf=hf, pt=pt: e.tensor_copy(out=hf[:], in_=pt[:]), reads=[r_pt], writes=[r_hf])
            S.dma("sp", lambda e, hb=hb, t=t: e.dma_start(out=h2rows[t * 128:(t + 1) * 128, :], in_=hb[:]), reads=[r_hb], stream="scr")
            pl, r_pl = pls.next()
            for j in range(8):
                S.op("pe", lambda e, pl=pl, hf=hf, j=j: e.matmul(pl[:], lhsT=hf[:, j, :], rhs=wrf[:, j, :], start=(j == 0), stop=False), reads=[r_hf, r_wr], writes=[r_pl])
            S.op("pe", lambda e, pl=pl: e.matmul(pl[:], lhsT=ones[0:1, :], rhs=brr[0:1, :], start=False, stop=True), reads=[r_wr, r_const], writes=[r_pl])
            lg, r_lg = lgs.next(); t8, r_t8 = t8s.next()
            S.op("dve", lambda e, lg=lg, pl=pl: e.tensor_copy(out=lg[:, 0, :], in_=pl[:]), reads=[r_pl], writes=[r_lg])
            S.op("dve", lambda e, lg=lg, t8=t8: e.max(out=t8[:, 0:8], in_=lg[:, 0, :]), reads=[r_lg], writes=[r_t8])
            S.op("dve", lambda e, lg=lg, t8=t8: e.tensor_scalar(out=lg[:, 1, :], in0=lg[:, 0, :], scalar1=t8[:, 3:4], scalar2=None, op0=ALU.is_ge), reads=[r_lg, r_t8], writes=[r_lg])
            S.op("dve", lambda e, t8=t8: e.tensor_scalar(out=t8[:, 8:9], in0=t8[:, 0:1], scalar1=-1.0, scalar2=None, op0=ALU.mult), reads=[r_t8], writes=[r_t8])
            S.op("act", lambda e, lg=lg, t8=t8: e.activation(out=lg[:, 2, :], in_=lg[:, 0, :], func=AF.Exp, bias=t8[:, 8:9], scale=1.0), reads=[r_lg, r_t8], writes=[r_lg])
            S.op("dve", lambda e, lg=lg: e.tensor_tensor(out=lg[:, 3, :], in0=lg[:, 2, :], in1=lg[:, 1, :], op=ALU.mult), reads=[r_lg], writes=[r_lg])
            S.op("dve", lambda e, lg=lg, t8=t8: e.reduce_sum(out=t8[:, 9:10], in_=lg[:, 3, :], axis=AX.X), reads=[r_lg], writes=[r_t8])
            S.op("dve", lambda e, t8=t8: e.reciprocal(out=t8[:, 10:11], in_=t8[:, 9:10]), reads=[r_t8], writes=[r_t8])
            S.op("dve", lambda e, lg=lg, t8=t8, t=t: e.tensor_scalar(out=gates[:, t, :], in0=lg[:, 3, :], scalar1=t8[:, 10:11], scalar2=None, op0=ALU.mult), reads=[r_lg, r_t8], writes=[r_gates[t]])
            if dbg_g is not None:
                S.dma("sp", lambda e, t=t: e.dma_start(out=dbg_g[t * 128:(t + 1) * 128, :], in_=gates[:, t, :]), reads=[r_gates[t]], stream="scr")


def phase5(nc, S, IN, l, xres, modv, h2rows, yacc, slotrec, gates, r_gates, ident, ones, r_const, nt_act, out, wbf, r_wbfl):
    S.barrier()
    last = (l == 1)
    NB = (4 * nt_act * 128) // 128 + NE
    with ExitStack() as ph:
        sb = lambda n, s, d: ph.enter_context(usb(nc, n, s, d))
        pst = lambda n, s, d: ph.enter_context(ups(nc, n, s, d))
        nr = 2 if nt_act > NTL else 1
        GT2 = [load_mod_bc(nc, S, ph, modv, l, r, 5, f"p5_GT{r}") for r in range(nr)]
        if last:
            gfb = sb("p5_gf", [128, D], F32); r_gf = Res()
            S.dma("sp", lambda e: e.dma_start(out=gfb[:], in_=IN["g_final"].to_broadcast([128, D])), writes=[r_gf], stream="ld")
        r_k = Res()
        cst = {}
        for nm, shp in (("tri_s", [128, 128]), ("tokidf", [128, NT]), ("widxbase", [128, 8]), ("blockval", [128, 2]), ("eidx", [128, 1])):
            cst[nm] = sb("p5_" + nm, shp, F32)
            S.dma("sp", lambda e, nm=nm: e.dma_start(out=cst[nm][:], in_=IN[nm]), writes=[r_k], stream="ld")
        bnat = sb("p5_bnat", [NE, 3, D], F32); bb = sb("p5_bb", [NE, 3, D], BF16); r_bb = Res()
        for k, nm in enumerate(("b_gate", "b_up", "b_down")):
            S.dma("sp", lambda e, k=k, nm=nm: e.dma_start(out=bnat[:, k, :], in_=IN[nm][l]), writes=[r_bb], stream="ld")
        S.op("dve", lambda e: e.tensor_copy(out=bb[:], in_=bnat[:]), reads=[r_bb], writes=[r_bb])
        zt = sb("p5_zt", [128, D], F32); r_zt = Res(); r_yacc = Res(); r_h2r = Res(); r_slot = Res()
        zb = sb("p5_zb", [1, D], BF16)
        S.op("dve", lambda e: e.memset(zt[:], 0.0), writes=[r_zt])
        S.op("dve", lambda e: e.memset(zb[:], 0.0), writes=[r_zt])
        for t in range(NT):
            S.dma("sp", lambda e, t=t: e.dma_start(out=yacc[t * 128:(t + 1) * 128, :], in_=zt[:]), reads=[r_zt], writes=[r_yacc], stream="scr")
        S.dma("sp", lambda e: e.dma_start(out=yacc[T:T + 1, :], in_=zt[0:1, :]), reads=[r_zt], writes=[r_yacc], stream="scr")
        S.dma("sp", lambda e: e.dma_start(out=h2rows[T:T + 1, :], in_=zb[:]), reads=[r_zt], writes=[r_h2r], stream="scr")
        prt = sb("p5_prt", [128, NBMAX, 2], F32)
        S.dma("sp", lambda e: e.dma_start(out=prt[:], in_=IN["padrec"]), writes=[r_zt], stream="ld")
        r_slots = [Res() for _ in range(nt_act * 4)]
        S.dma("sp", lambda e: e.dma_start(out=slotrec.rearrange("(p a) b -> p a b", a=NBMAX), in_=prt[:]), reads=[r_zt], writes=[r_slot] + r_slots, stream="scr")
        pm = pst("p5_pm", [128, 512], F32); r_pm = Res(excl=True)
        M = sb("p5_M", [128, NT, NE], F32); r_M = Res()
        POS = sb("p5_POS", [128, NT, NE], F32); r_POS = Res()
        cum = sb("p5_cum", [128, NE], F32); r_cum = Res()
        rg = list(r_gates[:nt_act])
        S.op("dve", lambda e: e.tensor_single_scalar(out=M[:, 0:nt_act, :], in_=gates[:, 0:nt_act, :], scalar=0.0, op=ALU.is_gt), reads=rg, writes=[r_M])
        S.op("dve", lambda e: e.memset(cum[:], 0.0), writes=[r_cum])
        for t in range(nt_act):
            S.op("pe", lambda e, t=t: e.matmul(pm[:, 0:NE], lhsT=cst["tri_s"][:], rhs=M[:, t, :], start=True, stop=False), reads=[r_M, r_k], writes=[r_pm])
            S.op("pe", lambda e, t=t: e.matmul(pm[:, 0:NE], lhsT=ones[:], rhs=cum[:], start=False, stop=True), reads=[r_cum, r_const], writes=[r_pm])
            S.op("act", lambda e, t=t: e.copy(out=POS[:, t, :], in_=pm[:, 0:NE]), reads=[r_pm], writes=[r_POS])
            S.op("dve", lambda e, t=t: e.tensor_tensor(out=cum[:], in0=cum[:], in1=M[:, t, :], op=ALU.add), reads=[r_M, r_cum], writes=[r_cum])
        mt = sb("p5_mt", [128, 8, NE], F32); r_mt = Res()
        mti = sb("p5_mti", [128, 2, NE], I32)
        S.op("pe", lambda e: e.matmul(pm[:, 0:NE], lhsT=ones[:], rhs=cum[:], start=True, stop=True), reads=[r_cum, r_const], writes=[r_pm])
        S.op("dve", lambda e: e.tensor_scalar(out=mti[:, 0, :], in0=pm[:, 0:NE], scalar1=127.0, scalar2=None, op0=ALU.add), reads=[r_pm], writes=[r_mt])
        S.op("dve", lambda e: e.tensor_single_scalar(out=mti[:, 1, :], in_=mti[:, 0, :], scalar=7, op=ALU.arith_shift_right), reads=[r_mt], writes=[r_mt])
        S.op("dve", lambda e: e.tensor_single_scalar(out=mti[:, 0, :], in_=mti[:, 1, :], scalar=7, op=ALU.logical_shift_left), reads=[r_mt], writes=[r_mt])
        S.op("dve", lambda e: e.tensor_copy(out=mt[:, 0, :], in_=mti[:, 0, :]), reads=[r_mt], writes=[r_mt])
        S.op("dve", lambda e: e.memset(mt[:, 7, :], 1.0), writes=[r_mt])
        S.op("dve", lambda e: e.tensor_tensor_scan(out=mt[:, 1, :], data0=mt[:, 7, :], data1=mt[:, 0, :], initial=0.0, op0=ALU.mult, op1=ALU.add), reads=[r_mt], writes=[r_mt])
        S.op("dve", lambda e: e.tensor_tensor(out=mt[:, 2, :], in0=mt[:, 1, :], in1=mt[:, 0, :], op=ALU.subtract), reads=[r_mt], writes=[r_mt])
        S.op("dve", lambda e: e.tensor_single_scalar(out=mt[:, 3, :], in_=mt[:, 0, :], scalar=0.0, op=ALU.is_gt), reads=[r_mt], writes=[r_mt])
        for t in range(nt_act):
            S.op("dve", lambda e, t=t: e.tensor_tensor(out=POS[:, t, :], in0=POS[:, t, :], in1=mt[:, 2, :], op=ALU.add), reads=[r_POS, r_mt], writes=[r_POS])
        recs = sb("p5_recs", [128, NT * 4, 2], F32); r_recs = Res()
        idxf = sb("p5_idxf", [128, NT * 4], F32); idxi = sb("p5_idxi", [128, NT * 4], I32); r_idx = Res()
        v8s = Rot([(sb(f"p5_v8{i}", [128, 8], F32), Res()) for i in range(2)])
        ohs = Rot([(sb(f"p5_oh{i}", [128, NE], F32), Res()) for i in range(2)])
        for t in range(nt_act):
            v8, r_v8 = v8s.next()
            S.op("dve", lambda e, v8=v8, t=t: e.max(out=v8[:], in_=gates[:, t, :]), reads=[r_gates[t]], writes=[r_v8])
            for k in range(4):
                q = t * 4 + k
                oh, r_oh = ohs.next()
                S.op("dve", lambda e, oh=oh, v8=v8, t=t, k=k: e.tensor_scalar(out=oh[:], in0=gates[:, t, :], scalar1=v8[:, k:k + 1], scalar2=None, op0=ALU.is_equal), reads=[r_gates[t], r_v8], writes=[r_oh])
                S.op("dve", lambda e, oh=oh, t=t: e.tensor_tensor(out=oh[:], in0=oh[:], in1=POS[:, t, :], op=ALU.mult), reads=[r_oh, r_POS], writes=[r_oh])
                S.op("dve", lambda e, oh=oh, q=q: e.reduce_sum(out=idxf[:, q:q + 1], in_=oh[:], axis=AX.X), reads=[r_oh], writes=[r_idx])
                S.op("act", lambda e, q=q, t=t: e.copy(out=recs[:, q, 0:1], in_=cst["tokidf"][:, t:t + 1]), reads=[r_k], writes=[r_recs])
                S.op("act", lambda e, q=q, v8=v8, k=k: e.copy(out=recs[:, q, 1:2], in_=v8[:, k:k + 1]), reads=[r_v8], writes=[r_recs])
        S.op("dve", lambda e: e.tensor_copy(out=idxi[:, 0:nt_act * 4], in_=idxf[:, 0:nt_act * 4]), reads=[r_idx], writes=[r_idx])
        for q in range(nt_act * 4):
            S.dma("pool", lambda e, q=q: e.indirect_dma_start(out=slotrec, out_offset=bass.IndirectOffsetOnAxis(ap=idxi[:, q:q + 1], axis=0), in_=recs[:, q, :], in_offset=None),
                  reads=[r_idx, r_recs, r_slot], writes=[r_slots[q]], stream="igs")
        EO = sb("p5_EO", [128, 256], F32); OH = sb("p5_OH", [NE, 256], F32); r_bm = Res()
        dgt = sb("p5_dgt", [128, 128], F32); r_dgt = Res()
        colv = sb("p5_colv", [128, 8], F32); r_colv = Res()
        cmpt = sb("p5_cmp", [128, NE], F32); r_cmp = Res()
        EBt = sb("p5_EB", [128, 256], F32); CHt = sb("p5_CH", [128, 256], F32)
        for c in range(2):
            bv = cst["blockval"][:, c:c + 1]
            S.op("dve", lambda e, bv=bv: e.tensor_scalar(out=cmpt[:], in0=mt[:, 1, :], scalar1=bv, scalar2=None, op0=ALU.is_le), reads=[r_mt, r_k], writes=[r_cmp])
            S.op("dve", lambda e, c=c: e.reduce_sum(out=colv[:, c:c + 1], in_=cmpt[:], axis=AX.X), reads=[r_cmp], writes=[r_colv])
            S.op("dve", lambda e, c=c: e.tensor_scalar(out=colv[:, c:c + 1], in0=colv[:, c:c + 1], scalar1=float(NE - 1), scalar2=None, op0=ALU.min), reads=[r_colv], writes=[r_colv])
            S.op("dve", lambda e, bv=bv: e.tensor_scalar(out=cmpt[:], in0=mt[:, 2, :], scalar1=bv, scalar2=None, op0=ALU.is_equal), reads=[r_mt, r_k], writes=[r_cmp])
            S.op("dve", lambda e: e.tensor_tensor(out=cmpt[:], in0=cmpt[:], in1=mt[:, 3, :], op=ALU.mult), reads=[r_cmp, r_mt], writes=[r_cmp])
            S.op("dve", lambda e, c=c: e.tensor_reduce(out=colv[:, 2 + c:3 + c], in_=cmpt[:], axis=AX.X, op=ALU.max), reads=[r_cmp], writes=[r_colv])
            for kk, dst in ((c, EBt), (2 + c, CHt)):
                S.op("dve", lambda e, kk=kk: e.tensor_scalar(out=dgt[:], in0=ident[:], scalar1=colv[:, kk:kk + 1], scalar2=None, op0=ALU.mult), reads=[r_colv, r_const], writes=[r_dgt])
                S.op("pe", lambda e: e.matmul(pm[:, 0:128], lhsT=ones[:], rhs=dgt[:], start=True, stop=True), reads=[r_dgt, r_const], writes=[r_pm])
                S.op("act", lambda e, dst=dst, c=c: e.copy(out=dst[:, c * 128:(c + 1) * 128], in_=pm[:, 0:128]), reads=[r_pm], writes=[r_bm])
        S.op("dve", lambda e: e.tensor_scalar(out=CHt[:], in0=CHt[:], scalar1=-1.0e7, scalar2=1.0e7, op0=ALU.mult, op1=ALU.add), reads=[r_bm], writes=[r_bm])
        S.op("dve", lambda e: e.scalar_tensor_tensor(out=EO[:], in0=EBt[:], scalar=128.0, in1=CHt[:], op0=ALU.mult, op1=ALU.add), reads=[r_bm], writes=[r_bm])
        S.op("dve", lambda e: e.tensor_scalar(out=OH[:], in0=EBt[0:NE, :], scalar1=cst["eidx"][0:NE, 0:1], scalar2=None, op0=ALU.is_equal), reads=[r_bm, r_k], writes=[r_bm])
        wg = sb("p5_wg", [128, 8, D], BF16); wu = sb("p5_wu", [128, 8, D], BF16); wd = sb("p5_wd", [128, 8, D], BF16)
        r_wg = Res(); r_wu = Res(); r_wd = Res()
        recb = Rot([(sb(f"p5_rb{i}", [128, 2], F32), Res()) for i in range(4)])
        xgs = Rot([(sb(f"p5_xg{i}", [128, D], BF16), Res()) for i in range(3)])
        xTs = Rot([(sb(f"p5_xT{i}", [128, 8, 128], BF16), Res()) for i in range(2)])
        wix = Rot([(sb(f"p5_wi{i}", [128, 1], I32), Res()) for i in range(4)])
        ohb = Rot([(sb(f"p5_ohb{i}", [NE, 128], BF16), Res()) for i in range(4)])
        a_s = Rot([(sb(f"p5_a{i}", [128, 512], F32), Res()) for i in range(2)])
        sg_s = Rot([(sb(f"p5_sg{i}", [128, 512], F32), Res()) for i in range(2)])
        u_s = Rot([(sb(f"p5_u{i}", [128, 512], F32), Res()) for i in range(2)])
        acts = Rot([(sb(f"p5_act{i}", [128, 8, 128], BF16), Res()) for i in range(2)])
        atms = Rot([(sb(f"p5_atm{i}", [128, D], BF16), Res()) for i in range(2)])
        ygs = Rot([(sb(f"p5_yg{i}", [128, D], F32), Res()) for i in range(2)])
        ptr = Rot([(pst("p5_ptr", [128, 8, 128], BF16), Res(excl=True))])
        pAs = Rot([(pst(f"p5_pA{i}", [128, 512], F32), Res()) for i in range(2)])
        pUs = Rot([(pst(f"p5_pU{i}", [128, 512], F32), Res()) for i in range(2)])
        pYs = Rot([(pst(f"p5_pY{i}", [128, 512], F32), Res()) for i in range(2)])
        identb = sb("p5_idb", [128, 128], BF16)
        S.op("dve", lambda e: e.tensor_copy(out=identb[:], in_=ident[:]), reads=[r_const], writes=[r_k])

        BC = {}

        def bcreg(e):
            if "r" not in BC:
                BC["r"] = e.alloc_register(f"bc{l}")
                e.reg_mov(BC["r"], NE * 128 - 1)
            return BC["r"]

        def stage_in(b):
            rb, r_rb = recb.next()
            S.dma("sp", lambda e, rb=rb, b=b: e.dma_start(out=rb[:], in_=slotrec[b * 128:(b + 1) * 128, :]), reads=[r_slot] + r_slots, writes=[r_rb], stream="ld")
            xg, r_xg = xgs.next()
            S.dma("pool", lambda e, xg=xg, rb=rb: e.indirect_dma_start(out=xg[:], out_offset=None, in_=h2rows, in_offset=bass.IndirectOffsetOnAxis(ap=rb[:, 0:1].bitcast(I32), axis=0)),
                  reads=[r_rb, r_h2r], writes=[r_xg], stream="ig")
            wi, r_wi = wix.next()
            S.op("dve", lambda e, wi=wi, b=b: e.tensor_scalar(out=wi[:], in0=cst["eidx"][:], scalar1=EO[:, b:b + 1], scalar2=None, op0=ALU.add), reads=[r_bm, r_k], writes=[r_wi])
            ob, r_ob = ohb.next()
            S.op("act", lambda e, ob=ob, b=b: e.activation(out=ob[:], in_=ones[0:NE, :], func=AF.Copy, scale=OH[0:NE, b:b + 1]), reads=[r_bm, r_const], writes=[r_ob])
            return (rb, r_rb, xg, r_xg, ob, r_ob, wi, r_wi)

        def stage_w(st, which):
            wi, r_wi = st[6], st[7]
            for m, (wt, r_w) in enumerate(((wg, r_wg), (wu, r_wu), (wd, r_wd))):
                if m not in which:
                    continue
                S.dma("pool", lambda e, wi=wi, m=m, wt=wt: e.indirect_dma_start(out=wt[:].rearrange("p j f -> p (j f)"), out_offset=None, in_=wbf[m], in_offset=bass.IndirectOffsetOnAxis(ap=wi[:, 0:1], axis=0),
                                                                             bounds_check=bcreg(e), oob_is_err=False), reads=[r_wi, r_wbfl], writes=[r_w], stream="wc")

        def compute_gu(b, st):
            rb, r_rb, xg, r_xg, ob, r_ob = st[:6]
            pt, r_pt = ptr.next()
            for j in range(8):
                S.op("pe", lambda e, pt=pt, xg=xg, j=j: e.transpose(out=pt[:, j, :], in_=xg[:, j:D:8], identity=identb[:]), reads=[r_xg, r_k], writes=[r_pt])
            xT, r_xT = xTs.next()
            S.op("act", lambda e, xT=xT, pt=pt: e.copy(out=xT[:], in_=pt[:]), reads=[r_pt], writes=[r_xT])
            atm, r_atm = atms.next()
            for hf in range(2):
                pA, r_pA = pAs.next(); pU, r_pU = pUs.next()
                for (pp, r_pp, wt, r_w, bk) in ((pA, r_pA, wg, r_wg, 0), (pU, r_pU, wu, r_wu, 1)):
                    for j in range(8):
                        S.op("pe", lambda e, pp=pp, wt=wt, j=j, xT=xT, hf=hf: e.matmul(pp[:], lhsT=xT[:, j, :], rhs=wt[:, j, hf * 512:(hf + 1) * 512], start=(j == 0), stop=False),
                             reads=[r_w, r_xT], writes=[r_pp])
                    S.op("pe", lambda e, pp=pp, bk=bk, ob=ob, hf=hf: e.matmul(pp[:], lhsT=ob[:], rhs=bb[:, bk, hf * 512:(hf + 1) * 512], start=False, stop=True),
                         reads=[r_bb, r_ob], writes=[r_pp])
                a, r_a = a_s.next(); sg, r_sg = sg_s.next(); u1, r_u1 = u_s.next()
                S.op("dve", lambda e, a=a, pA=pA: e.tensor_scalar(out=a[:], in0=pA[:], scalar1=7.0, scalar2=None, op0=ALU.min), reads=[r_pA], writes=[r_a])
                S.op("act", lambda e, sg=sg, a=a: e.activation(out=sg[:], in_=a[:], func=AF.Sigmoid, scale=1.702), reads=[r_a], writes=[r_sg])
                S.op("dve", lambda e, u1=u1, pU=pU: e.tensor_scalar(out=u1[:], in0=pU[:], scalar1=7.0, scalar2=-7.0, op0=ALU.min, op1=ALU.max), reads=[r_pU], writes=[r_u1])
                S.op("dve", lambda e, sg=sg, a=a: e.tensor_tensor(out=sg[:], in0=sg[:], in1=a[:], op=ALU.mult), reads=[r_a, r_sg], writes=[r_sg])
                S.op("dve", lambda e, atm=atm, sg=sg, u1=u1, hf=hf: e.scalar_tensor_tensor(out=atm[:, hf * 512:(hf + 1) * 512], in0=u1[:], scalar=1.0, in1=sg[:], op0=ALU.add, op1=ALU.mult),
                     reads=[r_sg, r_u1], writes=[r_atm])
            return (atm, r_atm, rb, r_rb, ob, r_ob)

        def compute_y(b, gu):
            atm, r_atm, rb, r_rb, ob, r_ob = gu
            pt2, r_pt2 = ptr.next()
            for j in range(8):
                S.op("pe", lambda e, pt2=pt2, atm=atm, j=j: e.transpose(out=pt2[:, j, :], in_=atm[:, j:D:8], identity=identb[:]), reads=[r_atm, r_k], writes=[r_pt2])
            actT, r_act = acts.next()
            S.op("act", lambda e, actT=actT, pt2=pt2: e.copy(out=actT[:], in_=pt2[:]), reads=[r_pt2], writes=[r_act])
            yg, r_yg = ygs.next()
            for half in range(2):
                pY, r_pY = pYs.next()
                for f in range(8):
                    S.op("pe", lambda e, pY=pY, actT=actT, f=f, half=half: e.matmul(pY[:], lhsT=actT[:, f, :], rhs=wd[:, f, half * 512:(half + 1) * 512], start=(f == 0), stop=False),
                         reads=[r_act, r_wd], writes=[r_pY])
                S.op("pe", lambda e, pY=pY, ob=ob, half=half: e.matmul(pY[:], lhsT=ob[:], rhs=bb[:, 2, half * 512:(half + 1) * 512], start=False, stop=True), reads=[r_ob, r_bb], writes=[r_pY])
                S.op("act", lambda e, yg=yg, pY=pY, rb=rb, half=half: e.activation(out=yg[:, half * 512:(half + 1) * 512], in_=pY[:], func=AF.Copy, scale=rb[:, 1:2]), reads=[r_pY, r_rb], writes=[r_yg])
            return (yg, r_yg, rb, r_rb)

        def stage_out(res):
            yg, r_yg, rb, r_rb = res
            S.dma("pool", lambda e, yg=yg, rb=rb: e.indirect_dma_start(out=yacc, out_offset=bass.IndirectOffsetOnAxis(ap=rb[:, 0:1].bitcast(I32), axis=0), in_=yg[:], in_offset=None, compute_op=ALU.add),
                  reads=[r_yg, r_rb], writes=[r_yacc], stream="igs")

        sts = {0: stage_in(0)}
        stage_w(sts[0], (0, 1, 2))
        gus = {0: compute_gu(0, sts[0])}
        if NB > 1:
            sts[1] = stage_in(1)
            stage_w(sts[1], (0, 1))
        for b in range(NB):
            if b + 1 < NB:
                gus[b + 1] = compute_gu(b + 1, sts[b + 1])
            if b + 2 < NB:
                sts[b + 2] = stage_in(b + 2)
                stage_w(sts[b + 2], (0, 1))
            res = compute_y(b, gus[b])
            if b + 1 < NB:
                stage_w(sts[b + 1], (2,))
            stage_out(res)
            sts.pop(b, None); gus.pop(b, None)
        xts = Rot([(sb(f"p5_xt{i}", [128, D], F32), Res()) for i in range(2)])
        yts = Rot([(sb(f"p5_yt{i}", [128, D], F32), Res()) for i in range(2)])
        junk = sb("p5_junk", [128, D], BF16); r_junk = Res()
        sts = Rot([(sb(f"p5_st{i}", [128, 4], F32), Res()) for i in range(2)])
        for t in range(nt_act):
            r = 0 if t < NTL else 1
            xt, r_xt = xts.next(); yt, r_yt = yts.next()
            S.dma("sp", lambda e, xt=xt, t=t: e.dma_start(out=xt[:], in_=xres[t * 128:(t + 1) * 128, :]), writes=[r_xt], stream="ld")
            S.dma("act", lambda e, yt=yt, t=t: e.dma_start(out=yt[:], in_=yacc[t * 128:(t + 1) * 128, :]), reads=[r_yacc], writes=[r_yt], stream="ld2")
            S.op("dve", lambda e, yt=yt, r=r: e.tensor_tensor(out=yt[:], in0=yt[:], in1=GT2[r][0][:], op=ALU.mult), reads=[r_yt, GT2[r][1]], writes=[r_yt])
            S.op("pool", lambda e, yt=yt, xt=xt: e.tensor_tensor(out=yt[:], in0=yt[:], in1=xt[:], op=ALU.add), reads=[r_yt, r_xt], writes=[r_yt])
            if not last:
                S.dma("sp", lambda e, yt=yt, t=t: e.dma_start(out=xres[t * 128:(t + 1) * 128, :], in_=yt[:]), reads=[r_yt], stream="scr")
            else:
                st_, r_st = sts.next()
                rstd_ops(S, yt, r_yt, junk, r_junk, st_, r_st)
                S.op("dve", lambda e, yt=yt, st_=st_: e.scalar_tensor_tensor(out=yt[:], in0=yt[:], scalar=st_[:, 3:4], in1=gfb[:], op0=ALU.mult, op1=ALU.mult), reads=[r_yt, r_st, r_gf], writes=[r_yt])
                S.dma("sp", lambda e, yt=yt, t=t: e.dma_start(out=out[t * 128:(t + 1) * 128, :], in_=yt[:]), reads=[r_yt], stream="st")


_CACHE = {}


def make_in_maps(inputs):
    consts = host_consts()
    shared = {}
    for nm, shp in W_SPECS:
        a = np.ascontiguousarray(np.asarray(inputs[nm], dtype=np.float32)).reshape(shp)
        shared[nm] = a
    shared.update(consts)
    x = np.asarray(inputs["x"], dtype=np.float32)
    ctx = np.asarray(inputs["ctx"], dtype=np.float32)
    c = np.asarray(inputs["c"], dtype=np.float32)
    c_ctx = np.asarray(inputs["c_ctx"], dtype=np.float32)
    maps = []
    for b in range(8):
        m = dict(shared)
        m["xin"] = np.ascontiguousarray(np.concatenate([x[b], ctx[b]], axis=0))
        m["cc"] = np.ascontiguousarray(np.stack([c[b], c_ctx], axis=0))
        maps.append(m)
    return maps


def kernel(**inputs):
    if "nc" not in _CACHE:
        _CACHE["nc"] = build()[0]
    nc = _CACHE["nc"]
    maps = make_in_maps(inputs)
    res = run_bass_kernel_spmd(nc, maps, core_ids=list(range(8)))
    return np.stack([np.asarray(r["out"], dtype=np.float32) for r in res.results], axis=0)
```

```python
import numpy as np
import concourse.bass as bass
import concourse.mybir as mybir
from concourse.alu_op_type import AluOpType as ALU
from contextlib import ExitStack
from concourse.bass_utils import run_bass_kernel_spmd

F32 = mybir.dt.float32
BF16 = mybir.dt.bfloat16
I32 = mybir.dt.int32
U32 = mybir.dt.uint32
AF = mybir.ActivationFunctionType
AX = mybir.AxisListType


class Res:
    __slots__ = ("name", "w", "rs", "excl")

    def __init__(self, name="", excl=False):
        self.name = name
        self.w = None
        self.rs = {}
        self.excl = excl


class Op:
    __slots__ = ("eng", "fn", "reads", "writes", "stream", "deps", "sig", "sigidx", "waits")

    def __init__(self, eng, fn, reads, writes, stream):
        self.eng = eng
        self.fn = fn
        self.reads = reads
        self.writes = writes
        self.stream = stream
        self.deps = None
        self.sig = False
        self.sigidx = -1
        self.waits = None


class Sched:
    CH = 16000
    CHD = 1000
    COMPUTE = ("pe", "act", "dve", "pool")

    def __init__(self, nc):
        self.nc = nc
        self.ops = []
        self._dcnt = {}

    def op(self, eng, fn, reads=(), writes=()):
        self.ops.append(Op(eng, fn, tuple(reads), tuple(writes), None))

    NSLOT = {"ld": 12, "scr": 12, "wc": 6, "ld2": 6, "st": 4, "ig": 8, "igs": 4, "cv": 8}

    def dma(self, queue, fn, reads=(), writes=(), stream="ld"):
        n = self._dcnt.get(stream, 0)
        self._dcnt[stream] = n + 1
        self.ops.append(Op(queue, fn, tuple(reads), tuple(writes), f"{stream}#{n % self.NSLOT.get(stream, 8)}"))

    def barrier(self):
        self.ops.append(None)

    def finalize(self, es, final_streams=()):
        nc = self.nc
        raw = self.ops
        ops = []
        bar_after = {}
        lastkey = {}
        pend = None
        for o in raw:
            if o is None:
                pend = dict(lastkey)
                continue
            i = len(ops)
            ops.append(o)
            key = o.stream if o.stream is not None else o.eng
            if pend is not None:
                bar_after[i] = pend
                pend = None
            if not key.startswith("cv#"):
                lastkey[key] = i
        self.ops = ops
        cur_bar = set()
        prev_dma = {}
        for i, o in enumerate(ops):
            if i in bar_after:
                cur_bar = set(bar_after[i].values())
                pend = None
            deps = set(cur_bar)
            if any(r.excl for r in o.reads):
                o.writes = tuple(o.writes) + tuple(r for r in o.reads if r.excl)
                o.reads = tuple(r for r in o.reads if not r.excl)
            for r in o.reads:
                if r.w is not None:
                    deps.add(r.w)
            for w in o.writes:
                if w.w is not None:
                    deps.add(w.w)
                for k, j in w.rs.items():
                    deps.add(j)
            key = o.stream if o.stream is not None else o.eng
            for r in o.reads:
                r.rs[key] = i
            for w in o.writes:
                w.w = i
                w.rs = {}
            if o.stream is not None:
                if o.stream in prev_dma:
                    deps.add(prev_dma[o.stream])
                prev_dma[o.stream] = i
            deps.discard(i)
            o.deps = deps
            for j in deps:
                ops[j].sig = True
        cnt = {}
        for o in ops:
            key = o.stream if o.stream is not None else o.eng
            if o.stream is not None:
                o.sig = True
            if o.sig:
                o.sigidx = cnt.get(key, 0)
                cnt[key] = o.sigidx + 1
        self.cnt = cnt
        known = {e: {} for e in ("pe", "act", "dve", "pool", "sp")}
        clocks = [None] * len(ops)
        for i, o in enumerate(ops):
            kn = known[o.eng]
            waits = {}
            for j in sorted(o.deps):
                p = ops[j]
                pkey = p.stream if p.stream is not None else p.eng
                if p.stream is None and p.eng == o.eng:
                    if o.eng == "pe":
                        continue
                if kn.get(pkey, -1) >= p.sigidx:
                    continue
                if waits.get(pkey, -1) < p.sigidx:
                    waits[pkey] = p.sigidx
                pc = clocks[j]
                for k, v in pc.items():
                    if kn.get(k, -1) < v:
                        kn[k] = v
            for k, v in waits.items():
                if kn.get(k, -1) < v:
                    kn[k] = v
            o.waits = waits
            ck = dict(kn)
            if o.sig:
                key = o.stream if o.stream is not None else o.eng
                ck[key] = max(ck.get(key, -1), o.sigidx)
            clocks[i] = ck
        self.sems = {}
        for key, n in cnt.items():
            ch = self.CH if key in self.COMPUTE else self.CHD
            nch = (n + ch - 1) // ch
            self.sems[key] = [es.enter_context(nc.semaphore(f"s_{key}_{c}")) for c in range(nch)]
        per_eng = {e: [] for e in ("pe", "act", "dve", "pool", "sp")}
        for o in ops:
            per_eng[o.eng].append(o)
        block = es.enter_context(nc.Block())
        sems = self.sems
        CH = self.CH
        CHD = self.CHD

        def wait(eng, k, v):
            if k in self.COMPUTE:
                eng.wait_ge(sems[k][v // CH], v % CH + 1)
            else:
                c = v // CHD
                if c > 0:
                    eng.wait_ge(sems[k][c - 1], CHD * 16)
                eng.wait_ge(sems[k][c], (v % CHD + 1) * 16)

        def emit(eng, lst, finals):
            for o in lst:
                for k, v in o.waits.items():
                    wait(eng, k, v)
                inst = o.fn(eng)
                if o.sig:
                    key = o.stream if o.stream is not None else o.eng
                    if o.stream is not None:
                        inst.then_inc(sems[key][o.sigidx // CHD], 16)
                    else:
                        inst.then_inc(sems[key][o.sigidx // CH], 1)
            for k in cnt:
                if k not in self.COMPUTE and k.split("#")[0] in finals:
                    wait(eng, k, cnt[k] - 1)

        @block.sync
        def _(e):
            emit(e, per_eng["sp"], final_streams)

        @block.tensor
        def _(e):
            emit(e, per_eng["pe"], ())

        @block.scalar
        def _(e):
            emit(e, per_eng["act"], ())

        @block.vector
        def _(e):
            emit(e, per_eng["dve"], ())

        @block.gpsimd
        def _(e):
            emit(e, per_eng["pool"], ())
        return {k: len(v) for k, v in per_eng.items()}
D = 1024
SEQ = 4096
CTX = 256
T = SEQ + CTX
NT = T // 128
NTL = SEQ // 128
INW = 2576
NE = 32
EPS = 1e-6
NEG = -30000.0
NBMAX = (4 * T) // 128 + NE


def host_consts():
    import ml_dtypes
    c = {}
    p = np.arange(128)
    c["ident"] = np.eye(128, dtype=np.float32)
    c["ones"] = np.ones((128, 128), np.float32)
    same = (p[:, None] // 64) == (p[None, :] // 64)
    c["m_ls"] = np.where(same & (p[:, None] > p[None, :]), 0.0, NEG).astype(np.float32)
    c["m_li"] = np.where(same & (p[:, None] >= p[None, :]), 0.0, NEG).astype(np.float32)
    c["m_us"] = np.where(same & (p[:, None] < p[None, :]), 0.0, NEG).astype(np.float32)
    c["m_ui"] = np.where(same & (p[:, None] <= p[None, :]), 0.0, NEG).astype(np.float32)
    c["tri_f"] = (same & (p[:, None] <= p[None, :])).astype(np.float32)
    c["tri_b"] = (same & (p[:, None] >= p[None, :])).astype(np.float32)
    c["blk"] = same.astype(np.float32)
    c["tri_s"] = (p[:, None] < p[None, :]).astype(np.float32)
    tok = (np.arange(NT)[None, :] * 128 + p[:, None]).astype(np.int32)
    c["tokidf"] = tok.view(np.float32)
    c["widxbase"] = (np.arange(8)[None, :] * 128 + p[:, None]).astype(np.float32)
    c["blockval"] = (128.0 * (p[:, None] + 128 * np.arange(2)[None, :])).astype(np.float32)
    c["eidx"] = p[:, None].astype(np.float32)
    pr = np.zeros((128, NBMAX, 2), np.int32); pr[:, :, 0] = T
    c["padrec"] = pr.view(np.float32)
    ang = 2 * np.pi * np.outer(p, p) / 128.0
    c["cs128"] = np.concatenate([np.cos(ang), np.sin(ang)], axis=1).astype(np.float32)
    for nm, L in (("L", SEQ), ("C", CTX)):
        t = np.arange(L, dtype=np.int64)
        a = 2 * np.pi * ((np.outer(t, t) % L).astype(np.float64)) / L
        nrm = 1.0 / np.sqrt(L * 128.0)
        c["cos" + nm] = (np.cos(a) * nrm).astype(ml_dtypes.bfloat16)
        c["nsin" + nm] = (-np.sin(a) * nrm).astype(ml_dtypes.bfloat16)
    return c


CONST_SPECS = [("ident", [128, 128], "f"), ("ones", [128, 128], "f"), ("m_ls", [128, 128], "f"),
               ("m_li", [128, 128], "f"), ("m_us", [128, 128], "f"), ("m_ui", [128, 128], "f"),
               ("tri_f", [128, 128], "f"), ("tri_b", [128, 128], "f"), ("blk", [128, 128], "f"),
               ("cs128", [128, 256], "f"), ("tri_s", [128, 128], "f"), ("tokidf", [128, NT], "f"), ("widxbase", [128, 8], "f"),
               ("blockval", [128, 2], "f"), ("eidx", [128, 1], "f"), ("padrec", [128, NBMAX, 2], "f"), ("cosL", [SEQ, SEQ], "b"), ("nsinL", [SEQ, SEQ], "b"),
               ("cosC", [CTX, CTX], "b"), ("nsinC", [CTX, CTX], "b")]

W_SPECS = [("w_mod", [2, D, 6 * D]), ("b_mod", [2, 6 * D]), ("g_norm1", [2, D]), ("w_in", [2, D, INW]),
           ("conv_w", [2, 5, 1536]), ("a_log", [2, 8]), ("dt_bias", [2, 8]), ("g_out_norm", [2, 128]),
           ("w_out", [2, D, D]), ("g_norm2", [2, D]), ("w_router", [2, D, NE]), ("b_router", [2, NE]),
           ("w_gate", [2, NE, D, D]), ("b_gate", [2, NE, D]), ("w_up", [2, NE, D, D]), ("b_up", [2, NE, D]),
           ("w_down", [2, NE, D, D]), ("b_down", [2, NE, D]), ("g_final", [1, D])]


_UC = [0]


def usb(nc, name, shape, dt):
    _UC[0] += 1
    return nc.sbuf_tensor(f"{name}_{_UC[0]}", shape, dt)


def ups(nc, name, shape, dt):
    _UC[0] += 1
    return nc.psum_tensor(f"{name}_{_UC[0]}", shape, dt)


class Rot:
    def __init__(self, items):
        self.items = items
        self.i = 0

    def next(self):
        it = self.items[self.i % len(self.items)]
        self.i += 1
        return it


def build(stage=99, dbg=()):
    nc = bass.Bass("TRN2", target_bir_lowering=False)
    IN = {}

    def din(name, shape, dt=F32):
        IN[name] = nc.dram_tensor(name, shape, dt, kind="ExternalInput").ap()
        return IN[name]

    xin = din("xin", [T, D])
    cc = din("cc", [2, D])
    for nm, shp in W_SPECS:
        din(nm, shp)
    for nm, shp, k in CONST_SPECS:
        din(nm, shp, F32 if k == "f" else BF16)
    out = nc.dram_tensor("out", [SEQ, D], F32, kind="ExternalOutput").ap()

    def dscr(name, shape, dt=F32):
        kind = "ExternalOutput" if name in dbg else "Internal"
        return nc.dram_tensor(name, shape, dt, kind=kind).ap()

    xres = dscr("xres", [T, D])
    modv = dscr("modv", [2, 2, 6 * D])
    uT = dscr("uT", [INW, T])
    abd = dscr("abd", [T, 16])
    mixT = dscr("mixT", [D, T], BF16)
    h2rows = dscr("h2rows", [T + 1, D], BF16)
    yacc = dscr("yacc", [T + 1, D])
    slotrec = dscr("slotrec", [NBMAX * 128, 2])
    wbf = [[dscr(f"wbf{i}_{m}", [NE * 128, 8 * D], BF16) for m in range(3)] for i in range(2)]
    r_wbf = [Res(), Res()]

    def conv_thunks(l):
        th = []
        for ex in range(NE):
            for m, nm in enumerate(("w_gate", "w_up", "w_down")):
                def fn(l=l, ex=ex, m=m, nm=nm):
                    r0 = ex * 128
                    S.dma("pool", lambda e: e.dma_start(out=wbf[l][m][r0:r0 + 128, :], in_=IN[nm][l, ex].rearrange("(p j) f -> p (j f)", j=8), max_dma_last_dim=4096),
                          writes=[r_wbf[l]], stream="cv")
                th.append(fn)
        return th
    dbg_g = dscr("dbg_g", [T, NE]) if "dbg_g" in dbg else None
    dbg_oT = dscr("dbg_oT", [4, 128, T]) if "dbg_oT" in dbg else None

    es = ExitStack()
    with es:
        S = Sched(nc)
        gsb = lambda n, s, d: es.enter_context(usb(nc, n, s, d))
        ident = gsb("identS", [128, 128], F32); r_const = Res("const")
        ones = gsb("onesS", [128, 128], F32)
        identb = gsb("identb", [128, 128], BF16)
        S.dma("sp", lambda e: e.dma_start(out=ident[:], in_=IN["ident"]), writes=[r_const], stream="ld")
        S.dma("sp", lambda e: e.dma_start(out=ones[:], in_=IN["ones"]), writes=[r_const], stream="ld")
        S.op("dve", lambda e: e.tensor_copy(out=identb[:], in_=ident[:]), reads=[r_const], writes=[r_const])
        gates = gsb("gates", [128, NT, NE], F32); r_gates = [Res() for _ in range(NT)]

        phase0(nc, S, IN, modv, ident, ones, r_const)
        for l in range(2):
            if stage < 1:
                break
            nt_act = NT if l == 0 else NTL
            phase1(nc, S, IN, l, xin if l == 0 else xres, modv, uT, abd, ident, identb, ones, r_const)
            if stage < 2:
                break
            phase2(nc, S, IN, l, uT, mixT, r_const)
            if stage < 3:
                break
            phase3(nc, S, IN, l, uT, abd, mixT, ident, ones, r_const, dbg_oT if l == 0 else None, conv_thunks(l))
            if stage < 4:
                break
            phase4(nc, S, IN, l, xin if l == 0 else xres, xres, modv, mixT, h2rows, gates, r_gates, ident, ones, r_const, nt_act, dbg_g if l == 0 else None)
            if stage < 5:
                break
            phase5(nc, S, IN, l, xres, modv, h2rows, yacc, slotrec, gates, r_gates, ident, ones, r_const, nt_act, out, wbf[l], r_wbf[l])
            if stage < 6:
                break
        if stage < 6:
            with ExitStack() as ph:
                z = ph.enter_context(usb(nc, "zz", [128, D], F32)); rz = Res()
                S.barrier()
                S.op("dve", lambda e: e.memset(z[:], 0.0), writes=[rz])
                S.dma("sp", lambda e: e.dma_start(out=out[0:128, :], in_=z[:]), reads=[rz], stream="st")
        S.barrier()
        fin = ("st", "ld", "wc", "scr", "ig", "igs", "cv")
        stats = S.finalize(es, final_streams=fin)
    return nc, stats
def phase0(nc, S, IN, modv, ident, ones, r_const):
    S.barrier()
    with ExitStack() as ph:
        sb = lambda n, s, d: ph.enter_context(usb(nc, n, s, d))
        ccr = sb("p0_ccr", [2, D], F32); r_ccr = Res()
        scT = sb("p0_scT", [128, 8, 2], F32); r_scT = Res()
        bm = sb("p0_bm", [1, 2, 6 * D], F32); r_bm = Res()
        modsb = sb("p0_mod", [2, 2, 6 * D], F32); r_mod = Res()
        wbufs = Rot([(sb(f"p0_w{i}", [128, 8, 512], F32), Res()) for i in range(2)])
        pT = ph.enter_context(ups(nc, "p0_pT", [128, 8, 2], F32)); r_pT = Res()
        pss = Rot([(ph.enter_context(ups(nc, f"p0_ps{i}", [2, 512], F32)), Res()) for i in range(2)])
        S.dma("sp", lambda e: e.dma_start(out=ccr[:], in_=IN["cc"]), writes=[r_ccr], stream="ld")
        S.dma("sp", lambda e: e.dma_start(out=bm[:], in_=IN["b_mod"].rearrange("(o l) n -> o l n", o=1)), writes=[r_bm], stream="ld")
        S.op("act", lambda e: e.activation(out=ccr[:], in_=ccr[:], func=AF.Silu), reads=[r_ccr], writes=[r_ccr])
        for j in range(8):
            S.op("pe", lambda e, j=j: e.transpose(out=pT[:, j, :], in_=ccr[:, j * 128:(j + 1) * 128], identity=ident[0:2, 0:2]),
                 reads=[r_ccr, r_const], writes=[r_pT])
        S.op("dve", lambda e: e.tensor_copy(out=scT[:], in_=pT[:]), reads=[r_pT], writes=[r_scT])
        for l in range(2):
            wv = IN["w_mod"][l].rearrange("(j p) n -> p j n", p=128)
            for n in range(12):
                wt, r_w = wbufs.next()
                S.dma("sp", lambda e, wt=wt, n=n, wv=wv: e.dma_start(out=wt[:], in_=wv[:, :, n * 512:(n + 1) * 512]), writes=[r_w], stream="ld")
                pst, r_ps = pss.next()
                for j in range(8):
                    S.op("pe", lambda e, j=j, wt=wt, pst=pst: e.matmul(pst[:], lhsT=scT[:, j, :], rhs=wt[:, j, :], start=(j == 0), stop=False),
                         reads=[r_scT, r_w], writes=[r_ps])
                S.op("pe", lambda e, pst=pst, l=l, n=n: e.matmul(pst[:], lhsT=ones[0:1, 0:2], rhs=bm[0:1, l, n * 512:(n + 1) * 512], start=False, stop=True),
                     reads=[r_bm, r_const], writes=[r_ps])
                S.op("dve", lambda e, pst=pst, l=l, n=n: e.tensor_copy(out=modsb[:, l, n * 512:(n + 1) * 512], in_=pst[:]), reads=[r_ps], writes=[r_mod])
        S.dma("sp", lambda e: e.dma_start(out=modv.rearrange("l r n -> r l n"), in_=modsb[:]), reads=[r_mod], stream="scr")


def load_mod_bc(nc, S, ph, modv, l, r, k, name, extra_g=None, plus1=False, stream="ld"):
    t = ph.enter_context(usb(nc, name, [128, D], F32)); res = Res()
    S.dma("sp", lambda e: e.dma_start(out=t[:], in_=modv[l, r:r + 1, k * D:(k + 1) * D].to_broadcast([128, D])), writes=[res], stream=stream)
    if plus1:
        g = ph.enter_context(usb(nc, name + "_g", [128, D], F32)); rg = Res()
        S.dma("sp", lambda e: e.dma_start(out=g[:], in_=extra_g.to_broadcast([128, D])), writes=[rg], stream=stream)
        S.op("dve", lambda e: e.scalar_tensor_tensor(out=t[:], in0=t[:], scalar=1.0, in1=g[:], op0=ALU.add, op1=ALU.mult), reads=[res, rg], writes=[res])
    return t, res


def rstd_ops(S, xt, r_x, junk, r_junk, st, r_st):
    S.op("act", lambda e: e.activation(out=junk[:], in_=xt[:], func=AF.Square, accum_out=st[:, 0:1]), reads=[r_x], writes=[r_junk, r_st])
    S.op("dve", lambda e: e.tensor_scalar(out=st[:, 1:2], in0=st[:, 0:1], scalar1=1.0 / D, scalar2=EPS, op0=ALU.mult, op1=ALU.add), reads=[r_st], writes=[r_st])
    S.op("act", lambda e: e.sqrt(out=st[:, 2:3], in_=st[:, 1:2]), reads=[r_st], writes=[r_st])
    S.op("dve", lambda e: e.reciprocal(out=st[:, 3:4], in_=st[:, 2:3]), reads=[r_st], writes=[r_st])


def phase1(nc, S, IN, l, xsrc, modv, uT, abd, ident, identb, ones, r_const):
    S.barrier()
    with ExitStack() as ph:
        sb = lambda n, s, d: ph.enter_context(usb(nc, n, s, d))
        G1 = [None, None]; SH1 = [None, None]
        for r in range(2):
            G1[r] = load_mod_bc(nc, S, ph, modv, l, r, 1, f"p1_G{r}", extra_g=IN["g_norm1"][l:l + 1, :], plus1=True)
            SH1[r] = load_mod_bc(nc, S, ph, modv, l, r, 0, f"p1_SH{r}")
        winb = sb("p1_winb", [128, 8, INW], BF16); r_win = Res()
        wv = IN["w_in"][l].rearrange("(j p) n -> p j n", p=128)
        for j in range(8):
            for h in range(2):
                S.dma("pool", lambda e, j=j, h=h: e.dma_start(out=winb[:, j, h * 1288:(h + 1) * 1288], in_=wv[:, j, h * 1288:(h + 1) * 1288]),
                      writes=[r_win], stream="wc")
        xts = Rot([(sb(f"p1_x{i}", [128, D], F32), Res()) for i in range(3)])
        junk = sb("p1_junk", [128, D], BF16); r_junk = Res()
        sts = Rot([(sb(f"p1_st{i}", [128, 4], F32), Res()) for i in range(3)])
        t1s = Rot([(sb(f"p1_t1{i}", [128, D], F32), Res()) for i in range(2)])
        hxbs = Rot([(sb(f"p1_hxb{i}", [128, D], BF16), Res()) for i in range(2)])
        hxTs = Rot([(sb(f"p1_hxT{i}", [128, 8, 512], BF16), Res()) for i in range(2)])
        stg = Rot([(sb(f"p1_stg{i}", [128, 512], F32), Res()) for i in range(3)])
        abs_ = Rot([(sb(f"p1_ab{i}", [128, 16], F32), Res()) for i in range(2)])
        ptr = Rot([(ph.enter_context(ups(nc, f"p1_ptr{i}", [128, 8, 128], BF16)), Res()) for i in range(2)])
        pmm = Rot([(ph.enter_context(ups(nc, f"p1_pmm{i}", [128, 512], F32)), Res()) for i in range(4)])
        pab = Rot([(ph.enter_context(ups(nc, f"p1_pab{i}", [128, 16], F32)), Res()) for i in range(2)])
        blocks = [(b * 4, 4) for b in range(8)] + [(32, 2)]
        def fnN(t0, ntl):
                hxT, r_hxT = hxTs.next()
                ntok = ntl * 128
                for ti in range(ntl):
                    t = t0 + ti
                    r = 0 if t < NTL else 1
                    xt, r_x = xts.next()
                    S.dma("sp", lambda e, xt=xt, t=t: e.dma_start(out=xt[:], in_=xsrc[t * 128:(t + 1) * 128, :]), writes=[r_x], stream="ld")
                    st, r_st = sts.next()
                    rstd_ops(S, xt, r_x, junk, r_junk, st, r_st)
                    t1, r_t1 = t1s.next()
                    S.op("dve", lambda e, t1=t1, xt=xt, st=st, r=r: e.scalar_tensor_tensor(out=t1[:], in0=xt[:], scalar=st[:, 3:4], in1=G1[r][0][:], op0=ALU.mult, op1=ALU.mult),
                         reads=[r_x, r_st, G1[r][1]], writes=[r_t1])
                    hxb, r_hxb = hxbs.next()
                    S.op("pool", lambda e, hxb=hxb, t1=t1, r=r: e.tensor_tensor(out=hxb[:], in0=t1[:], in1=SH1[r][0][:], op=ALU.add),
                         reads=[r_t1, SH1[r][1]], writes=[r_hxb])
                    pt, r_pt = ptr.next()
                    for j in range(8):
                        S.op("pe", lambda e, pt=pt, hxb=hxb, j=j: e.transpose(out=pt[:, j, :], in_=hxb[:, j * 128:(j + 1) * 128], identity=identb[:]),
                             reads=[r_hxb, r_const], writes=[r_pt])
                    S.op("act", lambda e, pt=pt, hxT=hxT, ti=ti: e.copy(out=hxT[:, :, ti * 128:(ti + 1) * 128], in_=pt[:]), reads=[r_pt], writes=[r_hxT])
                    pa, r_pa = pab.next()
                    for j in range(8):
                        S.op("pe", lambda e, pa=pa, hxT=hxT, ti=ti, j=j: e.matmul(pa[:], lhsT=hxT[:, j, ti * 128:(ti + 1) * 128], rhs=winb[:, j, 2560:2576], start=(j == 0), stop=(j == 7)),
                             reads=[r_hxT, r_win], writes=[r_pa])
                    ab, r_ab = abs_.next()
                    S.op("dve", lambda e, ab=ab, pa=pa: e.tensor_copy(out=ab[:], in_=pa[:]), reads=[r_pa], writes=[r_ab])
                    S.dma("sp", lambda e, ab=ab, t=t: e.dma_start(out=abd[t * 128:(t + 1) * 128, :], in_=ab[:]), reads=[r_ab], stream="scr")
                return hxT, r_hxT, ntok

        def fnM(t0, ntl, hxT, r_hxT, ntok):
                for c in range(20):
                    pm, r_pm = pmm.next()
                    for j in range(8):
                        S.op("pe", lambda e, pm=pm, hxT=hxT, c=c, j=j, ntok=ntok: e.matmul(pm[:, 0:ntok], lhsT=winb[:, j, c * 128:(c + 1) * 128], rhs=hxT[:, j, 0:ntok], start=(j == 0), stop=(j == 7)),
                             reads=[r_hxT, r_win], writes=[r_pm])
                    sg, r_sg = stg.next()
                    eng = "act" if c % 2 == 0 else "dve"
                    if eng == "act":
                        S.op("act", lambda e, sg=sg, pm=pm, ntok=ntok: e.copy(out=sg[:, 0:ntok], in_=pm[:, 0:ntok]), reads=[r_pm], writes=[r_sg])
                    else:
                        S.op("dve", lambda e, sg=sg, pm=pm, ntok=ntok: e.tensor_copy(out=sg[:, 0:ntok], in_=pm[:, 0:ntok]), reads=[r_pm], writes=[r_sg])
                    S.dma("sp", lambda e, sg=sg, c=c, t0=t0, ntok=ntok: e.dma_start(out=uT[c * 128:(c + 1) * 128, t0 * 128:t0 * 128 + ntok], in_=sg[:, 0:ntok]), reads=[r_sg], stream="scr")

        pend = fnN(*blocks[0])
        for bi, (t0, ntl) in enumerate(blocks):
            cur = pend
            if bi + 1 < len(blocks):
                pend = fnN(*blocks[bi + 1])
            fnM(t0, ntl, *cur)


def phase2(nc, S, IN, l, uT, mixT, r_const):
    S.barrier()
    with ExitStack() as ph:
        sb = lambda n, s, d: ph.enter_context(usb(nc, n, s, d))
        cs = sb("p2_cs", [128, 256], F32); r_cs = Res()
        S.dma("sp", lambda e: e.dma_start(out=cs[:], in_=IN["cs128"]), writes=[r_cs], stream="ld")
        ntiles = NT if l == 0 else NTL
        FCS = sb("p2_fcs", [128, NT, 4, 256], BF16); r_fcs = [Res() for _ in range(NT)]
        fts = Rot([(sb(f"p2_ft{i}", [128, 4, 128], F32), Res()) for i in range(3)])
        pas = Rot([(ph.enter_context(ups(nc, f"p2_pa{i}", [128, 4, 256], F32)), Res()) for i in range(2)])
        pos = Rot([(ph.enter_context(ups(nc, f"p2_po{i}", [128, 512], F32)), Res()) for i in range(4)])
        for t in range(ntiles):
            ft, r_ft = fts.next()
            S.dma("sp", lambda e, ft=ft, t=t: e.dma_start(out=ft[:], in_=uT[0:512, t * 128:(t + 1) * 128].rearrange("(g p) t -> p g t", p=128)), writes=[r_ft], stream="ld")
            pa, r_pa = pas.next()
            for g in range(4):
                S.op("pe", lambda e, pa=pa, ft=ft, g=g: e.matmul(pa[:, g, :], lhsT=ft[:, g, :], rhs=cs[:], start=True, stop=True), reads=[r_ft, r_cs], writes=[r_pa])
            if t % 2 == 0:
                S.op("act", lambda e, pa=pa, t=t: e.copy(out=FCS[:, t, :, :], in_=pa[:]), reads=[r_pa], writes=[r_fcs[t]])
            else:
                S.op("dve", lambda e, pa=pa, t=t: e.tensor_copy(out=FCS[:, t, :, :], in_=pa[:]), reads=[r_pa], writes=[r_fcs[t]])
        cosb = Rot([(sb(f"p2_cos{i}", [128, NTL, 256], BF16), Res()) for i in range(2)])
        sinb = Rot([(sb(f"p2_sin{i}", [128, NTL, 256], BF16), Res()) for i in range(2)])
        ostg = Rot([(sb(f"p2_os{i}", [128, 256], BF16), Res()) for i in range(4)])
        segs = [(0, NTL, "L")] + ([(NTL, 2, "C")] if l == 0 else [])
        cvL = IN["cosL"].rearrange("(j p) k -> p j k", p=128)
        svL = IN["nsinL"].rearrange("(j p) k -> p j k", p=128)
        qss = Rot([(sb(f"p2_qs{i}", [128, 256], F32), Res()) for i in range(2)])
        c0t = sb("p2_c0", [128, NTL, 2], BF16); r_c0 = Res()
        S.dma("sp", lambda e: e.dma_start(out=c0t[:], in_=cvL[:, :, 0:2]), writes=[r_c0], stream="ld")
        for g in range(4):
            po, r_po = pos.next()
            for j in range(NTL):
                S.op("pe", lambda e, po=po, j=j, g=g: e.matmul(po[:, 0:2], lhsT=FCS[:, j, g, 0:128], rhs=c0t[:, j, :], start=(j == 0), stop=(j == NTL - 1)), reads=[r_fcs[j], r_c0], writes=[r_po])
            og, r_og = ostg.next()
            S.op("act", lambda e, og=og, po=po: e.copy(out=og[:, 0:2], in_=po[:, 0:2]), reads=[r_po], writes=[r_og])
            S.dma("sp", lambda e, og=og, g=g: e.dma_start(out=mixT[g * 128:(g + 1) * 128, 0:1], in_=og[:, 0:1], allow_slow_non_contiguous=True), reads=[r_og], stream="scr")
        for b in range(8):
            cb, r_cb = cosb.next(); sn, r_sn = sinb.next()
            S.dma("sp", lambda e, cb=cb, b=b: e.dma_start(out=cb[:], in_=cvL[:, :, b * 256 + 1:b * 256 + 257]), writes=[r_cb], stream="ld")
            S.dma("act", lambda e, sn=sn, b=b: e.dma_start(out=sn[:], in_=svL[:, :, b * 256 + 1:b * 256 + 257]), writes=[r_sn], stream="ld2")
            for g in range(4):
                pP, r_pP = pos.next(); pQ, r_pQ = pos.next()
                for j in range(NTL):
                    S.op("pe", lambda e, pP=pP, j=j, g=g, cb=cb: e.matmul(pP[:, 0:256], lhsT=FCS[:, j, g, 0:128], rhs=cb[:, j, :], start=(j == 0), stop=(j == NTL - 1)), reads=[r_fcs[j], r_cb], writes=[r_pP])
                for j in range(NTL):
                    S.op("pe", lambda e, pQ=pQ, j=j, g=g, sn=sn: e.matmul(pQ[:, 0:256], lhsT=FCS[:, j, g, 128:256], rhs=sn[:, j, :], start=(j == 0), stop=(j == NTL - 1)), reads=[r_fcs[j], r_sn], writes=[r_pQ])
                qs, r_qs = qss.next()
                S.op("act", lambda e, qs=qs, pQ=pQ: e.copy(out=qs[:], in_=pQ[:, 0:256]), reads=[r_pQ], writes=[r_qs])
                og1, r_og1 = ostg.next(); og2, r_og2 = ostg.next()
                S.op("dve", lambda e, og1=og1, pP=pP, qs=qs: e.tensor_tensor(out=og1[:], in0=pP[:, 0:256], in1=qs[:], op=ALU.add), reads=[r_pP, r_qs], writes=[r_og1])
                S.op("dve", lambda e, og2=og2, pP=pP, qs=qs: e.tensor_tensor(out=og2[:, ::-1], in0=pP[:, 0:256], in1=qs[:], op=ALU.subtract), reads=[r_pP, r_qs], writes=[r_og2])
                S.dma("sp", lambda e, og1=og1, g=g, b=b: e.dma_start(out=mixT[g * 128:(g + 1) * 128, b * 256 + 1:b * 256 + 257], in_=og1[:]), reads=[r_og1], stream="scr")
                S.dma("sp", lambda e, og2=og2, g=g, b=b: e.dma_start(out=mixT[g * 128:(g + 1) * 128, (15 - b) * 256:(16 - b) * 256], in_=og2[:]), reads=[r_og2], stream="scr")
        segs = [s_ for s_ in segs if s_[2] != "L"]
        for (t0, ntl, nm) in segs:
            cv = IN["cos" + nm].rearrange("(j p) k -> p j k", p=128)
            sv = IN["nsin" + nm].rearrange("(j p) k -> p j k", p=128)
            for kb in range(ntl * 128 // 256):
                cb, r_cb = cosb.next(); sn, r_sn = sinb.next()
                S.dma("sp", lambda e, cb=cb, kb=kb, cv=cv, ntl=ntl: e.dma_start(out=cb[:, 0:ntl, :], in_=cv[:, :, kb * 256:(kb + 1) * 256]), writes=[r_cb], stream="ld")
                S.dma("act", lambda e, sn=sn, kb=kb, sv=sv, ntl=ntl: e.dma_start(out=sn[:, 0:ntl, :], in_=sv[:, :, kb * 256:(kb + 1) * 256]), writes=[r_sn], stream="ld2")
                for g in range(4):
                    po, r_po = pos.next()
                    for j in range(ntl):
                        S.op("pe", lambda e, po=po, j=j, g=g, cb=cb, t0=t0: e.matmul(po[:, 0:256], lhsT=FCS[:, t0 + j, g, 0:128], rhs=cb[:, j, :], start=(j == 0), stop=False),
                             reads=[r_fcs[t0 + j], r_cb], writes=[r_po])
                        S.op("pe", lambda e, po=po, j=j, g=g, sn=sn, t0=t0, ntl=ntl: e.matmul(po[:, 0:256], lhsT=FCS[:, t0 + j, g, 128:256], rhs=sn[:, j, :], start=False, stop=(j == ntl - 1)),
                             reads=[r_fcs[t0 + j], r_sn], writes=[r_po])
                    og, r_og = ostg.next()
                    if g % 2 == 0:
                        S.op("act", lambda e, og=og, po=po: e.copy(out=og[:], in_=po[:, 0:256]), reads=[r_po], writes=[r_og])
                    else:
                        S.op("dve", lambda e, og=og, po=po: e.tensor_copy(out=og[:], in_=po[:, 0:256]), reads=[r_po], writes=[r_og])
                    S.dma("sp", lambda e, og=og, g=g, t0=t0, kb=kb: e.dma_start(out=mixT[g * 128:(g + 1) * 128, t0 * 128 + kb * 256:t0 * 128 + (kb + 1) * 256], in_=og[:]), reads=[r_og], stream="scr")


WC = T + 4


def tcol(t):
    return t * 128 + (4 if t >= NTL else 0)


def phase3(nc, S, IN, l, uT, abd, mixT, ident, ones, r_const, dbg_oT, bg=()):
    S.barrier()
    with ExitStack() as ph:
        sb = lambda n, s, d: ph.enter_context(usb(nc, n, s, d))
        pst = lambda n, s, d: ph.enter_context(ups(nc, n, s, d))
        r_c3 = Res()
        cm = {}
        for nm in ("m_ls", "m_li", "m_us", "m_ui", "tri_f", "tri_b", "blk"):
            cm[nm] = sb("p3_" + nm, [128, 128], F32)
            S.dma("sp", lambda e, nm=nm: e.dma_start(out=cm[nm][:], in_=IN[nm]), writes=[r_c3], stream="ld")
        cwr = sb("p3_cwr", [5, 1536], F32)
        gor = sb("p3_gor", [1, 128], F32)
        S.dma("sp", lambda e: e.dma_start(out=cwr[:], in_=IN["conv_w"][l]), writes=[r_c3], stream="ld")
        S.dma("sp", lambda e: e.dma_start(out=gor[:], in_=IN["g_out_norm"][l:l + 1, :]), writes=[r_c3], stream="ld")
        alb = sb("p3_alb", [128, 8], F32); dtb = sb("p3_dtb", [128, 8], F32)
        S.dma("sp", lambda e: e.dma_start(out=alb[:], in_=IN["a_log"][l:l + 1, :].to_broadcast([128, 8])), writes=[r_c3], stream="ld")
        S.dma("sp", lambda e: e.dma_start(out=dtb[:], in_=IN["dt_bias"][l:l + 1, :].to_broadcast([128, 8])), writes=[r_c3], stream="ld")
        banks = [pst(f"p3_bank{i}", [128, 512], F32) for i in range(8)]
        qtile = lambda b, q: banks[b][:, q * 128:(q + 1) * 128]
        rbank = [Res(excl=True) for _ in range(8)]
        pcw = banks[0][:, 0:104].rearrange("p (m k) -> p m k", k=8); r_pcw = rbank[0]
        cw = sb("p3_cw", [128, 13, 8], F32)
        for m in range(12):
            S.op("pe", lambda e, m=m: e.transpose(out=pcw[:, m, 0:5], in_=cwr[:, m * 128:(m + 1) * 128], identity=ident[0:5, 0:5]), reads=[r_c3, r_const], writes=[r_pcw])
        S.op("pe", lambda e: e.transpose(out=pcw[:, 12, 0:1], in_=gor[:, :], identity=ident[0:1, 0:1]), reads=[r_c3, r_const], writes=[r_pcw])
        r_cw = Res()
        S.op("dve", lambda e: e.memset(cw[:], 0.0), writes=[r_cw])
        for m in range(12):
            S.op("dve", lambda e, m=m: e.tensor_copy(out=cw[:, m, 0:5], in_=pcw[:, m, 0:5]), reads=[r_pcw], writes=[r_cw])
        S.op("dve", lambda e: e.tensor_copy(out=cw[:, 12, 0:1], in_=pcw[:, 12, 0:1]), reads=[r_pcw], writes=[r_cw])
        nea = sb("p3_nea", [128, 8], F32)
        S.op("act", lambda e: e.activation(out=nea[:], in_=alb[:], func=AF.Exp), reads=[r_c3], writes=[r_c3])
        S.op("dve", lambda e: e.tensor_scalar(out=nea[:], in0=nea[:], scalar1=-1.0, scalar2=None, op0=ALU.mult), reads=[r_c3], writes=[r_c3])
        names = ("BT", "NBT", "GAM", "EG", "BEG", "EK0", "EK1")
        GA = {nm: sb("p3_" + nm, [128, NT, 8], F32) for nm in names}
        r_ga = [Res() for _ in range(NT)]
        abt = Rot([(sb(f"p3_abt{i}", [128, 16], F32), Res()) for i in range(2)])
        tmp = Rot([(sb(f"p3_gt{i}", [128, 4, 8], F32), Res()) for i in range(2)])
        pgs = Rot([(banks[0][:, 128:144], r_pcw)])
        rowm = sb("p3_rowm", [128, 2], F32)
        S.op("dve", lambda e: e.tensor_copy(out=rowm[:, 0:1], in_=cm["blk"][:, 0:1]), reads=[r_c3], writes=[r_c3])
        S.op("dve", lambda e: e.tensor_copy(out=rowm[:, 1:2], in_=cm["blk"][:, 127:128]), reads=[r_c3], writes=[r_c3])
        for t in range(NT):
            ab, r_ab = abt.next()
            S.dma("sp", lambda e, ab=ab, t=t: e.dma_start(out=ab[:], in_=abd[t * 128:(t + 1) * 128, :]), writes=[r_ab], stream="ld")
            tm, r_tm = tmp.next()
            abv = ab[:].rearrange("p (d k h) -> p d k h", d=2, k=2)
            X = tm[:, 0, :].rearrange("p (d h) -> p d h", d=2)
            S.op("dve", lambda e, X=X, abv=abv: e.tensor_tensor(out=X, in0=abv[:, :, 0, :], in1=dtb[:].rearrange("p (d h) -> p d h", d=2), op=ALU.add), reads=[r_ab, r_c3], writes=[r_tm])
            S.op("act", lambda e, tm=tm: e.activation(out=tm[:, 0, :], in_=tm[:, 0, :], func=AF.Exp), reads=[r_tm], writes=[r_tm])
            S.op("act", lambda e, tm=tm: e.activation(out=tm[:, 0, :], in_=tm[:, 0, :], func=AF.Ln, bias=1.0), reads=[r_tm], writes=[r_tm])
            S.op("dve", lambda e, tm=tm: e.tensor_tensor(out=tm[:, 1, :], in0=tm[:, 0, :], in1=nea[:], op=ALU.mult), reads=[r_tm, r_c3], writes=[r_tm])
            B = tm[:, 2, :].rearrange("p (d h) -> p d h", d=2)
            S.op("act", lambda e, B=B, abv=abv: e.activation(out=B, in_=abv[:, :, 1, :], func=AF.Exp, scale=-1.0), reads=[r_ab], writes=[r_tm])
            S.op("dve", lambda e, tm=tm: e.tensor_scalar(out=tm[:, 2, :], in0=tm[:, 2, :], scalar1=1.0, scalar2=None, op0=ALU.add), reads=[r_tm], writes=[r_tm])
            S.op("dve", lambda e, tm=tm, t=t: e.reciprocal(out=GA["BT"][:, t, :], in_=tm[:, 2, :]), reads=[r_tm], writes=[r_ga[t]])
            S.op("dve", lambda e, t=t: e.tensor_scalar(out=GA["NBT"][:, t, :], in0=GA["BT"][:, t, :], scalar1=-1.0, scalar2=None, op0=ALU.mult), reads=[r_ga[t]], writes=[r_ga[t]])
            pg, r_pg = pgs.next()
            S.op("pe", lambda e, pg=pg, tm=tm: e.matmul(pg[:, 0:4], lhsT=cm["tri_f"][:], rhs=tm[:, 1, 0:4], start=True, stop=True), reads=[r_tm, r_c3], writes=[r_pg])
            S.op("pe", lambda e, pg=pg, tm=tm: e.matmul(pg[:, 4:8], lhsT=cm["tri_b"][:], rhs=tm[:, 1, 4:8], start=True, stop=True), reads=[r_tm, r_c3], writes=[r_pg])
            S.op("pe", lambda e, pg=pg, tm=tm: e.matmul(pg[:, 8:16], lhsT=cm["blk"][:], rhs=tm[:, 1, :], start=True, stop=True), reads=[r_tm, r_c3], writes=[r_pg])
            S.op("dve", lambda e, pg=pg, t=t: e.tensor_copy(out=GA["GAM"][:, t, :], in_=pg[:, 0:8]), reads=[r_pg], writes=[r_ga[t]])
            S.op("act", lambda e, pg=pg, t=t: e.activation(out=GA["EG"][:, t, :], in_=pg[:, 0:8], func=AF.Exp), reads=[r_pg], writes=[r_ga[t]])
            S.op("dve", lambda e, t=t: e.tensor_tensor(out=GA["BEG"][:, t, :], in0=GA["EG"][:, t, :], in1=GA["BT"][:, t, :], op=ALU.mult), reads=[r_ga[t]], writes=[r_ga[t]])
            S.op("dve", lambda e, pg=pg, tm=tm, t=t: e.tensor_tensor(out=tm[:, 3, :], in0=pg[:, 8:16], in1=GA["GAM"][:, t, :], op=ALU.subtract), reads=[r_pg, r_ga[t]], writes=[r_tm])
            S.op("act", lambda e, tm=tm: e.activation(out=tm[:, 3, :], in_=tm[:, 3, :], func=AF.Exp), reads=[r_tm], writes=[r_tm])
            S.op("dve", lambda e, tm=tm, t=t: e.tensor_scalar(out=GA["EK0"][:, t, :], in0=tm[:, 3, :], scalar1=rowm[:, 0:1], scalar2=None, op0=ALU.mult), reads=[r_tm, r_c3], writes=[r_ga[t]])
            S.op("dve", lambda e, tm=tm, t=t: e.tensor_scalar(out=GA["EK1"][:, t, :], in0=tm[:, 3, :], scalar1=rowm[:, 1:2], scalar2=None, op0=ALU.mult), reads=[r_tm, r_c3], writes=[r_ga[t]])
        raws = Rot([(sb(f"p3_raw{i}", [128, WC + 4], F32), Res()) for i in range(2)])
        QKV = [(sb(f"p3_qkv{i}", [128, WC], F32), Res()) for i in range(3)]
        oT = sb("p3_oT", [128, WC], F32); r_oT = [Res() for _ in range(NT)]
        r_oTall = Res()
        pn = banks[1]; r_pn = rbank[1]
        rns = Rot([(sb(f"p3_rn{i}", [128, 512], F32), Res()) for i in range(2)])
        zts = Rot([(sb(f"p3_z{i}", [128, 512], F32), Res()) for i in range(2)])
        obs = Rot([(sb(f"p3_ob{i}", [128, 512], BF16), Res()) for i in range(2)])

        KSLOT = 4; DEPTH = 3
        INTER = ("dg", "Dm", "E1", "E2", "N", "NTs", "TT", "Pa", "PTa", "Pb", "PTb", "Rv", "Rw")
        OUTS = ("EGr", "at", "u", "wT", "qg", "ke0", "ke1")
        BF_NAMES = ("Nb", "NTs", "TT", "Pa", "PTa", "Pb", "PTb", "Rv", "Rw")
        BI = [{n: (sb(f"p3_{n}_s{s}", [128, 128], BF16 if n in BF_NAMES else F32), Res()) for n in INTER + ("Nb",)} for s in range(KSLOT)]
        BO = [{n: Rot([(sb(f"p3_{n}_d{d}_{i}", [128, 128], F32), Res()) for i in range(DEPTH)]) for n in OUTS} for d in range(2)]
        VN = [Rot([(sb(f"p3_vn{d}_{i}", [128, 128], F32), Res()) for i in range(2)]) for d in range(2)]
        rbank_ = rbank
        slot_bank = (2, 3, 4, 7)
        PQ = [Rot([(qtile(slot_bank[s], qi), rbank_[slot_bank[s]]) for qi in range(4)]) for s in range(KSLOT)]
        PSC = {d: {n: (qtile(5 + d, qi), rbank_[5 + d]) for qi, n in enumerate(("ps1", "po", "pS"))} for d in range(2)}
        Sst = [(sb(f"p3_S{d}", [128, 128], F32), Res()) for d in range(2)]
        bg = list(bg)
        chunks9 = [(i * 512, 512) for i in range(8)] + [(4100, 256)]

        LVL = 9; NTI = NT
        for h in range(4 if LVL >= 9 else (1 if LVL >= 1 else 0)):
            for which in range(3):
                raw, r_raw = raws.next()
                row0 = 512 + which * 512 + h * 128
                S.op("pool", lambda e, raw=raw: e.memset(raw[:], 0.0), writes=[r_raw])
                S.dma("sp", lambda e, raw=raw, row0=row0: e.dma_start(out=raw[:, 2:2 + SEQ], in_=uT[row0:row0 + 128, 0:SEQ]), writes=[r_raw], stream="ld")
                S.dma("sp", lambda e, raw=raw, row0=row0: e.dma_start(out=raw[:, SEQ + 6:SEQ + 6 + CTX], in_=uT[row0:row0 + 128, SEQ:T]), writes=[r_raw], stream="ld")
                dst, r_dst = QKV[which]
                m = which * 4 + h
                S.op("dve", lambda e, dst=dst, raw=raw, m=m: e.tensor_scalar(out=dst[:], in0=raw[:, 0:WC], scalar1=cw[:, m, 0:1], scalar2=None, op0=ALU.mult), reads=[r_raw, r_cw], writes=[r_dst])
                for k in range(1, 5):
                    S.op("dve", lambda e, dst=dst, raw=raw, m=m, k=k: e.scalar_tensor_tensor(out=dst[:], in0=raw[:, k:k + WC], scalar=cw[:, m, k:k + 1], in1=dst[:], op0=ALU.mult, op1=ALU.add),
                         reads=[r_raw, r_cw, r_dst], writes=[r_dst])
                S.op("act", lambda e, dst=dst: e.activation(out=dst[:], in_=dst[:], func=AF.Silu), reads=[r_dst], writes=[r_dst])
                if which < 2:
                    sq, r_sq = raws.items[(raws.i) % 2]
                    S.op("pool", lambda e, sq=sq, dst=dst: e.tensor_tensor(out=sq[:, 0:WC], in0=dst[:], in1=dst[:], op=ALU.mult), reads=[r_dst], writes=[r_sq])
                    for (c0, cn) in chunks9:
                        S.op("pe", lambda e, sq=sq, c0=c0, cn=cn: e.matmul(pn[:, 0:cn], lhsT=ones[:], rhs=sq[:, c0:c0 + cn], start=True, stop=True), reads=[r_sq, r_const], writes=[r_pn])
                        rn, r_rn = rns.next()
                        S.op("dve", lambda e, rn=rn, cn=cn: e.tensor_scalar(out=rn[:, 0:cn], in0=pn[:, 0:cn], scalar1=EPS, scalar2=None, op0=ALU.add), reads=[r_pn], writes=[r_rn])
                        S.op("act", lambda e, rn=rn, cn=cn: e.sqrt(out=rn[:, 0:cn], in_=rn[:, 0:cn]), reads=[r_rn], writes=[r_rn])
                        S.op("dve", lambda e, rn=rn, cn=cn: e.reciprocal(out=rn[:, 0:cn], in_=rn[:, 0:cn]), reads=[r_rn], writes=[r_rn])
                        sc = (128.0 ** -0.5) if which == 0 else 1.0
                        S.op("dve", lambda e, rn=rn, dst=dst, c0=c0, cn=cn, sc=sc: e.scalar_tensor_tensor(out=dst[:, c0:c0 + cn], in0=dst[:, c0:c0 + cn], scalar=sc, in1=rn[:, 0:cn], op0=ALU.mult, op1=ALU.mult),
                             reads=[r_rn, r_dst], writes=[r_dst])
            qT, r_q = QKV[0]; kT, r_k = QKV[1]; vT, r_v = QKV[2]
            for d in range(2):
                S.op("dve", lambda e, d=d: e.memset(Sst[d][0][:], 0.0), writes=[Sst[d][1]])
            seqs = [[32, 33] + list(range(32)), [33, 32] + list(range(31, -1, -1))]
            if LVL < 2:
                seqs = [[], []]
            else:
                seqs = [s_[:NTI] for s_ in seqs]
            written = set()
            PREP = {}
            scanned = [0, 0]

            def prep_gen(t, d, s):
                c0 = tcol(t); col = d * 4 + h
                bi = BI[s]; pq = PQ[s]
                ksl = kT[:, c0:c0 + 128]; qsl = qT[:, c0:c0 + 128]; vsl = vT[:, c0:c0 + 128]
                gam = GA["GAM"][:, t, col:col + 1]
                dg, r_dg = bi["dg"]; Dm, r_Dm = bi["Dm"]; E1, r_E1 = bi["E1"]; E2, r_E2 = bi["E2"]
                EGr, r_EGr = BO[d]["EGr"].next()
                gr, r_gr = pq.next()
                S.op("dve", lambda e: e.tensor_scalar(out=dg[:], in0=ident[:], scalar1=gam, scalar2=None, op0=ALU.mult), reads=[r_const, r_ga[t]], writes=[r_dg])
                S.op("pe", lambda e: e.matmul(gr[:], lhsT=ones[:], rhs=dg[:], start=True, stop=True), reads=[r_dg, r_const], writes=[r_gr])
                S.op("dve", lambda e: e.tensor_scalar(out=Dm[:], in0=gr[:], scalar1=-1.0, scalar2=gam, op0=ALU.mult, op1=ALU.add), reads=[r_gr, r_ga[t]], writes=[r_Dm])
                S.op("act", lambda e: e.activation(out=EGr[:], in_=gr[:], func=AF.Exp), reads=[r_gr], writes=[r_EGr])
                yield
                m1 = cm["m_ls"] if d == 0 else cm["m_us"]
                m2 = cm["m_ui"] if d == 0 else cm["m_li"]
                S.op("pool", lambda e: e.tensor_tensor(out=E1[:], in0=Dm[:], in1=m1[:], op=ALU.add), reads=[r_Dm, r_c3], writes=[r_E1])
                S.op("pool", lambda e: e.tensor_tensor(out=E2[:], in0=m2[:], in1=Dm[:], op=ALU.subtract), reads=[r_Dm, r_c3], writes=[r_E2])
                S.op("act", lambda e: e.activation(out=E1[:], in_=E1[:], func=AF.Exp), reads=[r_E1], writes=[r_E1])
                S.op("act", lambda e: e.activation(out=E2[:], in_=E2[:], func=AF.Exp), reads=[r_E2], writes=[r_E2])
                yield
                N, r_N = bi["N"]; at, r_at = BO[d]["at"].next()
                nbt = GA["NBT"][:, t, col:col + 1]
                kk, r_kk = pq.next()
                S.op("pe", lambda e: e.matmul(kk[:], lhsT=ksl, rhs=ksl, start=True, stop=True), reads=[r_k], writes=[r_kk])
                S.op("dve", lambda e: e.scalar_tensor_tensor(out=N[:], in0=kk[:], scalar=nbt, in1=E1[:], op0=ALU.mult, op1=ALU.mult), reads=[r_kk, r_ga[t], r_E1], writes=[r_N])
                Nb, r_Nb = bi["Nb"]
                S.op("act", lambda e: e.copy(out=Nb[:], in_=N[:]), reads=[r_N], writes=[r_Nb])
                kq, r_kq = pq.next()
                S.op("pe", lambda e: e.matmul(kq[:], lhsT=ksl, rhs=qsl, start=True, stop=True), reads=[r_k, r_q], writes=[r_kq])
                S.op("dve", lambda e: e.tensor_tensor(out=at[:], in0=kq[:], in1=E2[:], op=ALU.mult), reads=[r_kq, r_E2], writes=[r_at])
                yield
                Rv, r_Rv = bi["Rv"]; Rw, r_Rw = bi["Rw"]
                ke0, r_ke0 = BO[d]["ke0"].next(); ke1, r_ke1 = BO[d]["ke1"].next(); qg, r_qg = BO[d]["qg"].next()
                bt = GA["BT"][:, t, col:col + 1]; beg = GA["BEG"][:, t, col:col + 1]
                kt, r_kt = pq.next()
                S.op("pe", lambda e: e.transpose(out=kt[:], in_=ksl, identity=ident[:]), reads=[r_k, r_const], writes=[r_kt])
                S.op("act", lambda e: e.activation(out=Rw[:], in_=kt[:], func=AF.Copy, scale=beg), reads=[r_kt, r_ga[t]], writes=[r_Rw])
                S.op("dve", lambda e: e.tensor_scalar(out=ke0[:], in0=kt[:], scalar1=GA["EK0"][:, t, col:col + 1], scalar2=None, op0=ALU.mult), reads=[r_kt, r_ga[t]], writes=[r_ke0])
                S.op("dve", lambda e: e.tensor_scalar(out=ke1[:], in0=kt[:], scalar1=GA["EK1"][:, t, col:col + 1], scalar2=None, op0=ALU.mult), reads=[r_kt, r_ga[t]], writes=[r_ke1])
                vt, r_vt = pq.next()
                S.op("pe", lambda e: e.transpose(out=vt[:], in_=vsl, identity=ident[:]), reads=[r_v, r_const], writes=[r_vt])
                S.op("act", lambda e: e.activation(out=Rv[:], in_=vt[:], func=AF.Copy, scale=bt), reads=[r_vt, r_ga[t]], writes=[r_Rv])
                S.op("pool", lambda e: e.tensor_tensor(out=qg[:], in0=qsl, in1=EGr[:], op=ALU.mult), reads=[r_q, r_EGr], writes=[r_qg])
                yield
                NTs, r_NTs = bi["NTs"]; TT, r_TT = bi["TT"]
                ntp, r_ntp = pq.next()
                S.op("pe", lambda e: e.transpose(out=ntp[:], in_=N[:], identity=ident[:]), reads=[r_N, r_const], writes=[r_ntp])
                S.op("act", lambda e: e.copy(out=NTs[:], in_=ntp[:]), reads=[r_ntp], writes=[r_NTs])
                S.op("dve", lambda e: e.tensor_tensor(out=TT[:], in0=ntp[:], in1=ident[:], op=ALU.add), reads=[r_ntp, r_const], writes=[r_TT])
                yield
                P_, r_P = Nb, r_Nb
                PT_, r_PT = NTs, r_NTs
                for lev in range(1, 6):
                    p2, r_p2 = pq.next()
                    S.op("pe", lambda e, p2=p2, PT_=PT_, P_=P_: e.matmul(p2[:], lhsT=PT_[:], rhs=P_[:], start=True, stop=True), reads=[r_P, r_PT], writes=[r_p2])
                    nP, r_nP = bi["Pa" if lev % 2 else "Pb"]
                    S.op("act", lambda e, nP=nP, p2=p2: e.copy(out=nP[:], in_=p2[:]), reads=[r_p2], writes=[r_nP])
                    if lev < 5:
                        pt2, r_pt2 = pq.next()
                        S.op("pe", lambda e, pt2=pt2, PT_=PT_, P_=P_: e.matmul(pt2[:], lhsT=P_[:], rhs=PT_[:], start=True, stop=True), reads=[r_P, r_PT], writes=[r_pt2])
                        nPT, r_nPT = bi["PTa" if lev % 2 else "PTb"]
                        S.op("dve", lambda e, nPT=nPT, pt2=pt2: e.tensor_copy(out=nPT[:], in_=pt2[:]), reads=[r_pt2], writes=[r_nPT])
                    yield
                    up, r_up = pq.next()
                    S.op("pe", lambda e, up=up, nP=nP: e.matmul(up[:], lhsT=nP[:], rhs=TT[:], start=True, stop=True), reads=[r_nP, r_TT], writes=[r_up])
                    S.op("dve", lambda e, up=up: e.tensor_tensor(out=TT[:], in0=up[:], in1=TT[:], op=ALU.add), reads=[r_up, r_TT], writes=[r_TT])
                    P_, r_P = nP, r_nP
                    if lev < 5:
                        PT_, r_PT = nPT, r_nPT
                    yield
                u, r_u = BO[d]["u"].next(); wT, r_wT = BO[d]["wT"].next()
                pu, r_pu = pq.next()
                S.op("pe", lambda e: e.matmul(pu[:], lhsT=TT[:], rhs=Rv[:], start=True, stop=True), reads=[r_TT, r_Rv], writes=[r_pu])
                S.op("act", lambda e: e.copy(out=u[:], in_=pu[:]), reads=[r_pu], writes=[r_u])
                pw, r_pw = pq.next()
                S.op("pe", lambda e: e.matmul(pw[:], lhsT=Rw[:], rhs=TT[:], start=True, stop=True), reads=[r_TT, r_Rw], writes=[r_pw])
                S.op("dve", lambda e: e.tensor_copy(out=wT[:], in_=pw[:]), reads=[r_pw], writes=[r_wT])
                PREP[(t, d)] = dict(EGr=(EGr, r_EGr), at=(at, r_at), u=(u, r_u), wT=(wT, r_wT), qg=(qg, r_qg), ke0=(ke0, r_ke0), ke1=(ke1, r_ke1))

            def scan_gen(d):
                Sd, r_S = Sst[d]
                for t in seqs[d]:
                    while (t, d) not in PREP:
                        yield "wait"
                    pr = PREP[(t, d)]
                    c0 = tcol(t)
                    EGr, r_EGr = pr["EGr"]; at, r_at = pr["at"]; u, r_u = pr["u"]; wT, r_wT = pr["wT"]; qg, r_qg = pr["qg"]
                    for c in ((0, 1) if d == 0 else (1, 0)):
                        cs_ = slice(c * 64, (c + 1) * 64)
                        gcol = c * 64 + (63 if d == 0 else 0)
                        ps1, r_ps1 = PSC[d]["ps1"]; po, r_po = PSC[d]["po"]; pS, r_pS = PSC[d]["pS"]
                        vn, r_vn = VN[d].next()
                        ke, r_ke = pr["ke0"] if c == 0 else pr["ke1"]
                        S.op("pe", lambda e, wT=wT: e.matmul(ps1[:], lhsT=wT[:], rhs=Sd[:], start=True, stop=True), reads=[r_wT, r_S], writes=[r_ps1])
                        yield
                        S.op("dve", lambda e, vn=vn, u=u: e.tensor_tensor(out=vn[:], in0=u[:], in1=ps1[:], op=ALU.subtract), reads=[r_u, r_ps1], writes=[r_vn])
                        yield
                        S.op("pe", lambda e, qg=qg, cs_=cs_: e.matmul(po[:, 0:64], lhsT=Sd[:], rhs=qg[:, cs_], start=True, stop=False), reads=[r_S, r_qg], writes=[r_po])
                        S.op("pe", lambda e, vn=vn, at=at, cs_=cs_: e.matmul(po[:, 0:64], lhsT=vn[:], rhs=at[:, cs_], start=False, stop=True), reads=[r_vn, r_at], writes=[r_po])
                        S.op("pe", lambda e, ke=ke, vn=vn: e.matmul(pS[:], lhsT=ke[:], rhs=vn[:], start=True, stop=True), reads=[r_ke, r_vn], writes=[r_pS])
                        yield
                        osl = oT[:, c0 + c * 64:c0 + (c + 1) * 64]
                        if (t, c) not in written:
                            written.add((t, c))
                            S.op("act", lambda e, osl=osl: e.copy(out=osl, in_=po[:, 0:64]), reads=[r_po], writes=[r_oT[t]])
                        else:
                            S.op("dve", lambda e, osl=osl: e.tensor_tensor(out=osl, in0=po[:, 0:64], in1=osl, op=ALU.add), reads=[r_po, r_oT[t]], writes=[r_oT[t]])
                        S.op("dve", lambda e, EGr=EGr, gcol=gcol: e.scalar_tensor_tensor(out=Sd[:], in0=Sd[:], scalar=EGr[:, gcol:gcol + 1], in1=pS[:], op0=ALU.mult, op1=ALU.add),
                             reads=[r_S, r_EGr, r_pS], writes=[r_S])
                        yield
                    scanned[d] += 1

            queue = []
            for i in range(len(seqs[0])):
                for d in range(2):
                    queue.append((seqs[d][i], d, i))
            active = {}
            scans = [scan_gen(0), scan_gen(1)]
            scan_done = [len(seqs[0]) == 0, len(seqs[1]) == 0]
            nbg = 0
            SCANFIRST = 1; SCANSTEPS = 2

            def adv_scans():
                pr_ = False
                for d in range(2):
                    for _ in range(SCANSTEPS):
                        if not scan_done[d]:
                            try:
                                r_ = next(scans[d])
                                if r_ != "wait":
                                    pr_ = True
                                else:
                                    break
                            except StopIteration:
                                scan_done[d] = True; pr_ = True
                return pr_
            while not all(scan_done):
                progressed = False
                if SCANFIRST:
                    progressed = adv_scans() or progressed
                while queue and len(active) < KSLOT and (queue[0][2] - scanned[queue[0][1]] < DEPTH):
                    t_, d_, i_ = queue.pop(0)
                    s_ = [x for x in range(KSLOT) if x not in active][0]
                    active[s_] = prep_gen(t_, d_, s_)
                    progressed = True
                    nbg += 1
                    if bg and nbg % 2 == 0:
                        bg.pop(0)()
                for s_ in list(active.keys()):
                    try:
                        next(active[s_]); progressed = True
                    except StopIteration:
                        del active[s_]; progressed = True
                if not SCANFIRST:
                    progressed = adv_scans() or progressed
                assert progressed, "phase3 scheduler stuck"
            if dbg_oT is not None:
                S.dma("sp", lambda e, h=h: e.dma_start(out=dbg_oT[h, :, 0:SEQ], in_=oT[:, 0:SEQ]), reads=r_oT, stream="scr")
                S.dma("sp", lambda e, h=h: e.dma_start(out=dbg_oT[h, :, SEQ:T], in_=oT[:, SEQ + 4:SEQ + 4 + CTX]), reads=r_oT, stream="scr")
            sq, r_sq = raws.next()
            for ci, (c0, cn) in enumerate(chunks9):
                tiles = list(range(ci * 4, ci * 4 + 4)) if ci < 8 else [32, 33]
                rds = [r_oT[t] for t in tiles]
                S.op("pool", lambda e, sq=sq, c0=c0, cn=cn: e.tensor_tensor(out=sq[:, c0:c0 + cn], in0=oT[:, c0:c0 + cn], in1=oT[:, c0:c0 + cn], op=ALU.mult), reads=rds, writes=[r_sq])
                S.op("pe", lambda e, sq=sq, c0=c0, cn=cn: e.matmul(pn[:, 0:cn], lhsT=ones[:], rhs=sq[:, c0:c0 + cn], start=True, stop=True), reads=[r_sq, r_const], writes=[r_pn])
                rn, r_rn = rns.next()
                S.op("dve", lambda e, rn=rn, cn=cn: e.tensor_scalar(out=rn[:, 0:cn], in0=pn[:, 0:cn], scalar1=1.0 / 128, scalar2=EPS, op0=ALU.mult, op1=ALU.add), reads=[r_pn], writes=[r_rn])
                S.op("act", lambda e, rn=rn, cn=cn: e.sqrt(out=rn[:, 0:cn], in_=rn[:, 0:cn]), reads=[r_rn], writes=[r_rn])
                S.op("dve", lambda e, rn=rn, cn=cn: e.reciprocal(out=rn[:, 0:cn], in_=rn[:, 0:cn]), reads=[r_rn], writes=[r_rn])
                S.op("dve", lambda e, rn=rn, c0=c0, cn=cn: e.scalar_tensor_tensor(out=rn[:, 0:cn], in0=oT[:, c0:c0 + cn], scalar=cw[:, 12, 0:1], in1=rn[:, 0:cn], op0=ALU.mult, op1=ALU.mult),
                     reads=rds + [r_rn, r_cw], writes=[r_rn])
                zt, r_zt = zts.next()
                tok0 = ci * 512 if ci < 8 else SEQ
                zrow = 2048 + h * 128
                S.dma("sp", lambda e, zt=zt, tok0=tok0, cn=cn, zrow=zrow: e.dma_start(out=zt[:, 0:cn], in_=uT[zrow:zrow + 128, tok0:tok0 + cn]), writes=[r_zt], stream="ld")
                S.op("act", lambda e, zt=zt, cn=cn: e.activation(out=zt[:, 0:cn], in_=zt[:, 0:cn], func=AF.Silu), reads=[r_zt], writes=[r_zt])
                ob, r_ob = obs.next()
                S.op("pool", lambda e, ob=ob, rn=rn, zt=zt, cn=cn: e.tensor_tensor(out=ob[:, 0:cn], in0=rn[:, 0:cn], in1=zt[:, 0:cn], op=ALU.mult), reads=[r_rn, r_zt], writes=[r_ob])
                S.dma("sp", lambda e, ob=ob, tok0=tok0, cn=cn, h=h: e.dma_start(out=mixT[512 + h * 128:512 + (h + 1) * 128, tok0:tok0 + cn], in_=ob[:, 0:cn]), reads=[r_ob], stream="scr")
        while bg:
            bg.pop(0)()


def phase4(nc, S, IN, l, xsrc, xres, modv, mixT, h2rows, gates, r_gates, ident, ones, r_const, nt_act, dbg_g):
    S.barrier()
    with ExitStack() as ph:
        sb = lambda n, s, d: ph.enter_context(usb(nc, n, s, d))
        pst = lambda n, s, d: ph.enter_context(ups(nc, n, s, d))
        nr = 2 if nt_act > NTL else 1
        GT1 = [load_mod_bc(nc, S, ph, modv, l, r, 2, f"p4_GT{r}") for r in range(nr)]
        G2 = [load_mod_bc(nc, S, ph, modv, l, r, 4, f"p4_G{r}", extra_g=IN["g_norm2"][l:l + 1, :], plus1=True) for r in range(nr)]
        SH2 = [load_mod_bc(nc, S, ph, modv, l, r, 3, f"p4_SH{r}") for r in range(nr)]
        woutb = sb("p4_wout", [128, 8, D], BF16); r_wo = Res()
        wv = IN["w_out"][l].rearrange("(j p) n -> p j n", p=128)
        for j in range(8):
            S.dma("pool", lambda e, j=j: e.dma_start(out=woutb[:, j, :], in_=wv[:, j, :]), writes=[r_wo], stream="wc")
        wrf = sb("p4_wr", [128, 8, NE], F32); r_wr = Res()
        brr = sb("p4_br", [1, NE], F32)
        S.dma("sp", lambda e: e.dma_start(out=wrf[:], in_=IN["w_router"][l].rearrange("(j p) n -> p j n", p=128)), writes=[r_wr], stream="ld")
        S.dma("sp", lambda e: e.dma_start(out=brr[:], in_=IN["b_router"][l:l + 1, :]), writes=[r_wr], stream="ld")
        mixs = Rot([(sb(f"p4_mx{i}", [128, 8, 128], BF16), Res()) for i in range(2)])
        xts = Rot([(sb(f"p4_x{i}", [128, D], F32), Res()) for i in range(2)])
        tmps = Rot([(sb(f"p4_t{i}", [128, D], F32), Res()) for i in range(2)])
        xns = Rot([(sb(f"p4_xn{i}", [128, D], F32), Res()) for i in range(2)])
        h2s = Rot([(sb(f"p4_h2{i}", [128, D], F32), Res()) for i in range(2)])
        junk = sb("p4_junk", [128, D], BF16); r_junk = Res()
        sts = Rot([(sb(f"p4_st{i}", [128, 4], F32), Res()) for i in range(2)])
        h2bs = Rot([(sb(f"p4_hb{i}", [128, D], BF16), Res()) for i in range(2)])
        h2fs = Rot([(sb(f"p4_hf{i}", [128, 8, 128], F32), Res()) for i in range(2)])
        lgs = Rot([(sb(f"p4_lg{i}", [128, 4, NE], F32), Res()) for i in range(2)])
        t8s = Rot([(sb(f"p4_t8{i}", [128, 16], F32), Res()) for i in range(2)])
        pys = Rot([(pst(f"p4_py{i}", [128, D], F32), Res()) for i in range(2)])
        ptr = Rot([(pst("p4_ptr", [128, 8, 128], F32), Res(excl=True))])
        pls = Rot([(pst(f"p4_pl{i}", [128, NE], F32), Res()) for i in range(2)])
        def tile_gen(t):
            r = 0 if t < NTL else 1
            mx, r_mx = mixs.next()
            S.dma("sp", lambda e, mx=mx, t=t: e.dma_start(out=mx[:], in_=mixT[:, t * 128:(t + 1) * 128].rearrange("(j p) t -> p j t", p=128)), writes=[r_mx], stream="ld")
            xt, r_x = xts.next()
            S.dma("act", lambda e, xt=xt, t=t: e.dma_start(out=xt[:], in_=xsrc[t * 128:(t + 1) * 128, :]), writes=[r_x], stream="ld2")
            py, r_py = pys.next()
            for half in range(2):
                for j in range(8):
                    S.op("pe", lambda e, py=py, mx=mx, half=half, j=j: e.matmul(py[:, half * 512:(half + 1) * 512], lhsT=mx[:, j, :], rhs=woutb[:, j, half * 512:(half + 1) * 512], start=(j == 0), stop=(j == 7)),
                         reads=[r_mx, r_wo], writes=[r_py])
            tp, r_tp = tmps.next()
            S.op("dve", lambda e, tp=tp, py=py, r=r: e.tensor_tensor(out=tp[:], in0=py[:], in1=GT1[r][0][:], op=ALU.mult), reads=[r_py, GT1[r][1]], writes=[r_tp])
            xn, r_xn = xns.next()
            S.op("pool", lambda e, xn=xn, tp=tp, xt=xt: e.tensor_tensor(out=xn[:], in0=tp[:], in1=xt[:], op=ALU.add), reads=[r_tp, r_x], writes=[r_xn])
            S.dma("sp", lambda e, xn=xn, t=t: e.dma_start(out=xres[t * 128:(t + 1) * 128, :], in_=xn[:]), reads=[r_xn], stream="scr")
            yield
            st, r_st = sts.next()
            rstd_ops(S, xn, r_xn, junk, r_junk, st, r_st)
            h2, r_h2 = h2s.next()
            S.op("dve", lambda e, h2=h2, xn=xn, st=st, r=r: e.scalar_tensor_tensor(out=h2[:], in0=xn[:], scalar=st[:, 3:4], in1=G2[r][0][:], op0=ALU.mult, op1=ALU.mult), reads=[r_xn, r_st, G2[r][1]], writes=[r_h2])
            S.op("pool", lambda e, h2=h2, r=r: e.tensor_tensor(out=h2[:], in0=h2[:], in1=SH2[r][0][:], op=ALU.add), reads=[r_h2, SH2[r][1]], writes=[r_h2])
            pt, r_pt = ptr.next()
            for j in range(8):
                S.op("pe", lambda e, pt=pt, h2=h2, j=j: e.transpose(out=pt[:, j, :], in_=h2[:, j * 128:(j + 1) * 128], identity=ident[:]), reads=[r_h2, r_const], writes=[r_pt])
            hb, r_hb = h2bs.next(); hf, r_hf = h2fs.next()
            S.op("act", lambda e, hb=hb, h2=h2: e.copy(out=hb[:], in_=h2[:]), reads=[r_h2], writes=[r_hb])
            S.op("dve", lambda e, hf=hf, pt=pt: e.tensor_copy(out=hf[:], in_=pt[:]), reads=[r_pt], writes=[r_hf])
            S.dma("sp", lambda e, hb=hb, t=t: e.dma_start(out=h2rows[t * 128:(t + 1) * 128, :], in_=hb[:]), reads=[r_hb], stream="scr")
            yield
            pl, r_pl = pls.next()
            for j in range(8):
                S.op("pe", lambda e, pl=pl, hf=hf, j=j: e.matmul(pl[:], lhsT=hf[:, j, :], rhs=wrf[:, j, :], start=(j == 0), stop=False), reads=[r_hf, r_wr], writes=[r_pl])
            S.op("pe", lambda e, pl=pl: e.matmul(pl[:], lhsT=ones[0:1, :], rhs=brr[0:1, :], start=False, stop=True), reads=[r_wr, r_const], writes=[r_pl])
            lg, r_lg = lgs.next(); t8, r_t8 = t8s.next()
            S.op("dve", lambda e, lg=lg, pl=pl: e.tensor_copy(out=lg[:, 0, :], in_=pl[:]), reads=[r_pl], writes=[r_lg])
            S.op("dve", lambda e, lg=lg, t8=t8: e.max(out=t8[:, 0:8], in_=lg[:, 0, :]), reads=[r_lg], writes=[r_t8])
            S.op("dve", lambda e, lg=lg, t8=t8: e.tensor_scalar(out=lg[:, 1, :], in0=lg[:, 0, :], scalar1=t8[:, 3:4], scalar2=None, op0=ALU.is_ge), reads=[r_lg, r_t8], writes=[r_lg])
            S.op("dve", lambda e, t8=t8: e.tensor_scalar(out=t8[:, 8:9], in0=t8[:, 0:1], scalar1=-1.0, scalar2=None, op0=ALU.mult), reads=[r_t8], writes=[r_t8])
            S.op("act", lambda e, lg=lg, t8=t8: e.activation(out=lg[:, 2, :], in_=lg[:, 0, :], func=AF.Exp, bias=t8[:, 8:9], scale=1.0), reads=[r_lg, r_t8], writes=[r_lg])
            S.op("dve", lambda e, lg=lg: e.tensor_tensor(out=lg[:, 3, :], in0=lg[:, 2, :], in1=lg[:, 1, :], op=ALU.mult), reads=[r_lg], writes=[r_lg])
            S.op("dve", lambda e, lg=lg, t8=t8: e.reduce_sum(out=t8[:, 9:10], in_=lg[:, 3, :], axis=AX.X), reads=[r_lg], writes=[r_t8])
            S.op("dve", lambda e, t8=t8: e.reciprocal(out=t8[:, 10:11], in_=t8[:, 9:10]), reads=[r_t8], writes=[r_t8])
            S.op("dve", lambda e, lg=lg, t8=t8, t=t: e.tensor_scalar(out=gates[:, t, :], in0=lg[:, 3, :], scalar1=t8[:, 10:11], scalar2=None, op0=ALU.mult), reads=[r_lg, r_t8], writes=[r_gates[t]])
            if dbg_g is not None:
                S.dma("sp", lambda e, t=t: e.dma_start(out=dbg_g[t * 128:(t + 1) * 128, :], in_=gates[:, t, :]), reads=[r_gates[t]], stream="scr")

        gens = {}
        for s in range(nt_act + 2):
            if s < nt_act:
                gens[s] = tile_gen(s)
                next(gens[s])
            if 0 <= s - 1 < nt_act:
                next(gens[s - 1])
            if 0 <= s - 2 < nt_act:
                for _ in gens.pop(s - 2):
                    pass

def phase5(nc, S, IN, l, xres, modv, h2rows, yacc, slotrec, gates, r_gates, ident, ones, r_const, nt_act, out, wbf, r_wbfl):
    S.barrier()
    last = (l == 1)
    NB = (4 * nt_act * 128) // 128 + NE
    with ExitStack() as ph:
        sb = lambda n, s, d: ph.enter_context(usb(nc, n, s, d))
        pst = lambda n, s, d: ph.enter_context(ups(nc, n, s, d))
        nr = 2 if nt_act > NTL else 1
        GT2 = [load_mod_bc(nc, S, ph, modv, l, r, 5, f"p5_GT{r}") for r in range(nr)]
        if last:
            gfb = sb("p5_gf", [128, D], F32); r_gf = Res()
            S.dma("sp", lambda e: e.dma_start(out=gfb[:], in_=IN["g_final"].to_broadcast([128, D])), writes=[r_gf], stream="ld")
        r_k = Res()
        cst = {}
        for nm, shp in (("tri_s", [128, 128]), ("tokidf", [128, NT]), ("widxbase", [128, 8]), ("blockval", [128, 2]), ("eidx", [128, 1])):
            cst[nm] = sb("p5_" + nm, shp, F32)
            S.dma("sp", lambda e, nm=nm: e.dma_start(out=cst[nm][:], in_=IN[nm]), writes=[r_k], stream="ld")
        bnat = sb("p5_bnat", [NE, 3, D], F32); bb = sb("p5_bb", [NE, 3, D], BF16); r_bb = Res()
        for k, nm in enumerate(("b_gate", "b_up", "b_down")):
            S.dma("sp", lambda e, k=k, nm=nm: e.dma_start(out=bnat[:, k, :], in_=IN[nm][l]), writes=[r_bb], stream="ld")
        S.op("dve", lambda e: e.tensor_copy(out=bb[:], in_=bnat[:]), reads=[r_bb], writes=[r_bb])
        zt = sb("p5_zt", [128, D], F32); r_zt = Res(); r_yacc = Res(); r_h2r = Res(); r_slot = Res()
        zb = sb("p5_zb", [1, D], BF16)
        S.op("dve", lambda e: e.memset(zt[:], 0.0), writes=[r_zt])
        S.op("dve", lambda e: e.memset(zb[:], 0.0), writes=[r_zt])
        for t in range(NT):
            S.dma("sp", lambda e, t=t: e.dma_start(out=yacc[t * 128:(t + 1) * 128, :], in_=zt[:]), reads=[r_zt], writes=[r_yacc], stream="scr")
        S.dma("sp", lambda e: e.dma_start(out=yacc[T:T + 1, :], in_=zt[0:1, :]), reads=[r_zt], writes=[r_yacc], stream="scr")
        S.dma("sp", lambda e: e.dma_start(out=h2rows[T:T + 1, :], in_=zb[:]), reads=[r_zt], writes=[r_h2r], stream="scr")
        prt = sb("p5_prt", [128, NBMAX, 2], F32)
        S.dma("sp", lambda e: e.dma_start(out=prt[:], in_=IN["padrec"]), writes=[r_zt], stream="ld")
        r_slots = [Res() for _ in range(nt_act * 4)]
        S.dma("sp", lambda e: e.dma_start(out=slotrec.rearrange("(p a) b -> p a b", a=NBMAX), in_=prt[:]), reads=[r_zt], writes=[r_slot] + r_slots, stream="scr")
        pm = pst("p5_pm", [128, 512], F32); r_pm = Res(excl=True)
        M = sb("p5_M", [128, NT, NE], F32); r_M = Res()
        POS = sb("p5_POS", [128, NT, NE], F32); r_POS = Res()
        cum = sb("p5_cum", [128, NE], F32); r_cum = Res()
        rg = list(r_gates[:nt_act])
        S.op("dve", lambda e: e.tensor_single_scalar(out=M[:, 0:nt_act, :], in_=gates[:, 0:nt_act, :], scalar=0.0, op=ALU.is_gt), reads=rg, writes=[r_M])
        S.op("dve", lambda e: e.memset(cum[:], 0.0), writes=[r_cum])
        for t in range(nt_act):
            S.op("pe", lambda e, t=t: e.matmul(pm[:, 0:NE], lhsT=cst["tri_s"][:], rhs=M[:, t, :], start=True, stop=False), reads=[r_M, r_k], writes=[r_pm])
            S.op("pe", lambda e, t=t: e.matmul(pm[:, 0:NE], lhsT=ones[:], rhs=cum[:], start=False, stop=True), reads=[r_cum, r_const], writes=[r_pm])
            S.op("act", lambda e, t=t: e.copy(out=POS[:, t, :], in_=pm[:, 0:NE]), reads=[r_pm], writes=[r_POS])
            S.op("dve", lambda e, t=t: e.tensor_tensor(out=cum[:], in0=cum[:], in1=M[:, t, :], op=ALU.add), reads=[r_M, r_cum], writes=[r_cum])
        mt = sb("p5_mt", [128, 8, NE], F32); r_mt = Res()
        mti = sb("p5_mti", [128, 2, NE], I32)
        S.op("pe", lambda e: e.matmul(pm[:, 0:NE], lhsT=ones[:], rhs=cum[:], start=True, stop=True), reads=[r_cum, r_const], writes=[r_pm])
        S.op("dve", lambda e: e.tensor_scalar(out=mti[:, 0, :], in0=pm[:, 0:NE], scalar1=127.0, scalar2=None, op0=ALU.add), reads=[r_pm], writes=[r_mt])
        S.op("dve", lambda e: e.tensor_single_scalar(out=mti[:, 1, :], in_=mti[:, 0, :], scalar=7, op=ALU.arith_shift_right), reads=[r_mt], writes=[r_mt])
        S.op("dve", lambda e: e.tensor_single_scalar(out=mti[:, 0, :], in_=mti[:, 1, :], scalar=7, op=ALU.logical_shift_left), reads=[r_mt], writes=[r_mt])
        S.op("dve", lambda e: e.tensor_copy(out=mt[:, 0, :], in_=mti[:, 0, :]), reads=[r_mt], writes=[r_mt])
        S.op("dve", lambda e: e.memset(mt[:, 7, :], 1.0), writes=[r_mt])
        S.op("dve", lambda e: e.tensor_tensor_scan(out=mt[:, 1, :], data0=mt[:, 7, :], data1=mt[:, 0, :], initial=0.0, op0=ALU.mult, op1=ALU.add), reads=[r_mt], writes=[r_mt])
        S.op("dve", lambda e: e.tensor_tensor(out=mt[:, 2, :], in0=mt[:, 1, :], in1=mt[:, 0, :], op=ALU.subtract), reads=[r_mt], writes=[r_mt])
        S.op("dve", lambda e: e.tensor_single_scalar(out=mt[:, 3, :], in_=mt[:, 0, :], scalar=0.0, op=ALU.is_gt), reads=[r_mt], writes=[r_mt])
        for t in range(nt_act):
            S.op("dve", lambda e, t=t: e.tensor_tensor(out=POS[:, t, :], in0=POS[:, t, :], in1=mt[:, 2, :], op=ALU.add), reads=[r_POS, r_mt], writes=[r_POS])
        recs = sb("p5_recs", [128, NT * 4, 2], F32); r_recs = Res()
        idxf = sb("p5_idxf", [128, NT * 4], F32); idxi = sb("p5_idxi", [128, NT * 4], I32); r_idx = Res()
        v8s = Rot([(sb(f"p5_v8{i}", [128, 8], F32), Res()) for i in range(2)])
        ohs = Rot([(sb(f"p5_oh{i}", [128, NE], F32), Res()) for i in range(2)])
        for t in range(nt_act):
            v8, r_v8 = v8s.next()
            S.op("dve", lambda e, v8=v8, t=t: e.max(out=v8[:], in_=gates[:, t, :]), reads=[r_gates[t]], writes=[r_v8])
            for k in range(4):
                q = t * 4 + k
                oh, r_oh = ohs.next()
                S.op("dve", lambda e, oh=oh, v8=v8, t=t, k=k: e.tensor_scalar(out=oh[:], in0=gates[:, t, :], scalar1=v8[:, k:k + 1], scalar2=None, op0=ALU.is_equal), reads=[r_gates[t], r_v8], writes=[r_oh])
                S.op("dve", lambda e, oh=oh, t=t: e.tensor_tensor(out=oh[:], in0=oh[:], in1=POS[:, t, :], op=ALU.mult), reads=[r_oh, r_POS], writes=[r_oh])
                S.op("dve", lambda e, oh=oh, q=q: e.reduce_sum(out=idxf[:, q:q + 1], in_=oh[:], axis=AX.X), reads=[r_oh], writes=[r_idx])
                S.op("act", lambda e, q=q, t=t: e.copy(out=recs[:, q, 0:1], in_=cst["tokidf"][:, t:t + 1]), reads=[r_k], writes=[r_recs])
                S.op("act", lambda e, q=q, v8=v8, k=k: e.copy(out=recs[:, q, 1:2], in_=v8[:, k:k + 1]), reads=[r_v8], writes=[r_recs])
        S.op("dve", lambda e: e.tensor_copy(out=idxi[:, 0:nt_act * 4], in_=idxf[:, 0:nt_act * 4]), reads=[r_idx], writes=[r_idx])
        for q in range(nt_act * 4):
            S.dma("pool", lambda e, q=q: e.indirect_dma_start(out=slotrec, out_offset=bass.IndirectOffsetOnAxis(ap=idxi[:, q:q + 1], axis=0), in_=recs[:, q, :], in_offset=None),
                  reads=[r_idx, r_recs, r_slot], writes=[r_slots[q]], stream="igs")
        EO = sb("p5_EO", [128, 256], F32); OH = sb("p5_OH", [NE, 256], F32); r_bm = Res()
        dgt = sb("p5_dgt", [128, 128], F32); r_dgt = Res()
        colv = sb("p5_colv", [128, 8], F32); r_colv = Res()
        cmpt = sb("p5_cmp", [128, NE], F32); r_cmp = Res()
        EBt = sb("p5_EB", [128, 256], F32); CHt = sb("p5_CH", [128, 256], F32)
        for c in range(2):
            bv = cst["blockval"][:, c:c + 1]
            S.op("dve", lambda e, bv=bv: e.tensor_scalar(out=cmpt[:], in0=mt[:, 1, :], scalar1=bv, scalar2=None, op0=ALU.is_le), reads=[r_mt, r_k], writes=[r_cmp])
            S.op("dve", lambda e, c=c: e.reduce_sum(out=colv[:, c:c + 1], in_=cmpt[:], axis=AX.X), reads=[r_cmp], writes=[r_colv])
            S.op("dve", lambda e, c=c: e.tensor_scalar(out=colv[:, c:c + 1], in0=colv[:, c:c + 1], scalar1=float(NE - 1), scalar2=None, op0=ALU.min), reads=[r_colv], writes=[r_colv])
            S.op("dve", lambda e, bv=bv: e.tensor_scalar(out=cmpt[:], in0=mt[:, 2, :], scalar1=bv, scalar2=None, op0=ALU.is_equal), reads=[r_mt, r_k], writes=[r_cmp])
            S.op("dve", lambda e: e.tensor_tensor(out=cmpt[:], in0=cmpt[:], in1=mt[:, 3, :], op=ALU.mult), reads=[r_cmp, r_mt], writes=[r_cmp])
            S.op("dve", lambda e, c=c: e.tensor_reduce(out=colv[:, 2 + c:3 + c], in_=cmpt[:], axis=AX.X, op=ALU.max), reads=[r_cmp], writes=[r_colv])
            for kk, dst in ((c, EBt), (2 + c, CHt)):
                S.op("dve", lambda e, kk=kk: e.tensor_scalar(out=dgt[:], in0=ident[:], scalar1=colv[:, kk:kk + 1], scalar2=None, op0=ALU.mult), reads=[r_colv, r_const], writes=[r_dgt])
                S.op("pe", lambda e: e.matmul(pm[:, 0:128], lhsT=ones[:], rhs=dgt[:], start=True, stop=True), reads=[r_dgt, r_const], writes=[r_pm])
                S.op("act", lambda e, dst=dst, c=c: e.copy(out=dst[:, c * 128:(c + 1) * 128], in_=pm[:, 0:128]), reads=[r_pm], writes=[r_bm])
        S.op("dve", lambda e: e.tensor_scalar(out=CHt[:], in0=CHt[:], scalar1=-1.0e7, scalar2=1.0e7, op0=ALU.mult, op1=ALU.add), reads=[r_bm], writes=[r_bm])
        S.op("dve", lambda e: e.scalar_tensor_tensor(out=EO[:], in0=EBt[:], scalar=128.0, in1=CHt[:], op0=ALU.mult, op1=ALU.add), reads=[r_bm], writes=[r_bm])
        S.op("dve", lambda e: e.tensor_scalar(out=OH[:], in0=EBt[0:NE, :], scalar1=cst["eidx"][0:NE, 0:1], scalar2=None, op0=ALU.is_equal), reads=[r_bm, r_k], writes=[r_bm])
        wg = sb("p5_wg", [128, 8, D], BF16); wu = sb("p5_wu", [128, 8, D], BF16); wd = sb("p5_wd", [128, 8, D], BF16)
        r_wg = Res(); r_wu = Res(); r_wd = Res()
        recb = Rot([(sb(f"p5_rb{i}", [128, 2], F32), Res()) for i in range(4)])
        xgs = Rot([(sb(f"p5_xg{i}", [128, D], BF16), Res()) for i in range(3)])
        xTs = Rot([(sb(f"p5_xT{i}", [128, 8, 128], BF16), Res()) for i in range(2)])
        wix = Rot([(sb(f"p5_wi{i}", [128, 1], I32), Res()) for i in range(4)])
        ohb = Rot([(sb(f"p5_ohb{i}", [NE, 128], BF16), Res()) for i in range(4)])
        a_s = Rot([(sb(f"p5_a{i}", [128, 512], F32), Res()) for i in range(2)])
        sg_s = Rot([(sb(f"p5_sg{i}", [128, 512], F32), Res()) for i in range(2)])
        u_s = Rot([(sb(f"p5_u{i}", [128, 512], F32), Res()) for i in range(2)])
        acts = Rot([(sb(f"p5_act{i}", [128, 8, 128], BF16), Res()) for i in range(2)])
        atms = Rot([(sb(f"p5_atm{i}", [128, D], BF16), Res()) for i in range(2)])
        ygs = Rot([(sb(f"p5_yg{i}", [128, D], F32), Res()) for i in range(2)])
        ptr = Rot([(pst("p5_ptr", [128, 8, 128], BF16), Res(excl=True))])
        pAs = Rot([(pst(f"p5_pA{i}", [128, 512], F32), Res()) for i in range(2)])
        pUs = Rot([(pst(f"p5_pU{i}", [128, 512], F32), Res()) for i in range(2)])
        pYs = Rot([(pst(f"p5_pY{i}", [128, 512], F32), Res()) for i in range(2)])
        identb = sb("p5_idb", [128, 128], BF16)
        S.op("dve", lambda e: e.tensor_copy(out=identb[:], in_=ident[:]), reads=[r_const], writes=[r_k])

        BC = {}

        def bcreg(e):
            if "r" not in BC:
                BC["r"] = e.alloc_register(f"bc{l}")
                e.reg_mov(BC["r"], NE * 128 - 1)
            return BC["r"]

        def stage_in(b):
            rb, r_rb = recb.next()
            S.dma("sp", lambda e, rb=rb, b=b: e.dma_start(out=rb[:], in_=slotrec[b * 128:(b + 1) * 128, :]), reads=[r_slot] + r_slots, writes=[r_rb], stream="ld")
            xg, r_xg = xgs.next()
            S.dma("pool", lambda e, xg=xg, rb=rb: e.indirect_dma_start(out=xg[:], out_offset=None, in_=h2rows, in_offset=bass.IndirectOffsetOnAxis(ap=rb[:, 0:1].bitcast(I32), axis=0)),
                  reads=[r_rb, r_h2r], writes=[r_xg], stream="ig")
            wi, r_wi = wix.next()
            S.op("dve", lambda e, wi=wi, b=b: e.tensor_scalar(out=wi[:], in0=cst["eidx"][:], scalar1=EO[:, b:b + 1], scalar2=None, op0=ALU.add), reads=[r_bm, r_k], writes=[r_wi])
            ob, r_ob = ohb.next()
            S.op("act", lambda e, ob=ob, b=b: e.activation(out=ob[:], in_=ones[0:NE, :], func=AF.Copy, scale=OH[0:NE, b:b + 1]), reads=[r_bm, r_const], writes=[r_ob])
            return (rb, r_rb, xg, r_xg, ob, r_ob, wi, r_wi)

        def stage_w(st, which):
            wi, r_wi = st[6], st[7]
            for m, (wt, r_w) in enumerate(((wg, r_wg), (wu, r_wu), (wd, r_wd))):
                if m not in which:
                    continue
                S.dma("pool", lambda e, wi=wi, m=m, wt=wt: e.indirect_dma_start(out=wt[:].rearrange("p j f -> p (j f)"), out_offset=None, in_=wbf[m], in_offset=bass.IndirectOffsetOnAxis(ap=wi[:, 0:1], axis=0),
                                                                             bounds_check=bcreg(e), oob_is_err=False), reads=[r_wi, r_wbfl], writes=[r_w], stream="wc")

        def compute_gu(b, st):
            rb, r_rb, xg, r_xg, ob, r_ob = st[:6]
            pt, r_pt = ptr.next()
            for j in range(8):
                S.op("pe", lambda e, pt=pt, xg=xg, j=j: e.transpose(out=pt[:, j, :], in_=xg[:, j:D:8], identity=identb[:]), reads=[r_xg, r_k], writes=[r_pt])
            xT, r_xT = xTs.next()
            S.op("act", lambda e, xT=xT, pt=pt: e.copy(out=xT[:], in_=pt[:]), reads=[r_pt], writes=[r_xT])
            atm, r_atm = atms.next()
            for hf in range(2):
                pA, r_pA = pAs.next(); pU, r_pU = pUs.next()
                for (pp, r_pp, wt, r_w, bk) in ((pA, r_pA, wg, r_wg, 0), (pU, r_pU, wu, r_wu, 1)):
                    for j in range(8):
                        S.op("pe", lambda e, pp=pp, wt=wt, j=j, xT=xT, hf=hf: e.matmul(pp[:], lhsT=xT[:, j, :], rhs=wt[:, j, hf * 512:(hf + 1) * 512], start=(j == 0), stop=False),
                             reads=[r_w, r_xT], writes=[r_pp])
                    S.op("pe", lambda e, pp=pp, bk=bk, ob=ob, hf=hf: e.matmul(pp[:], lhsT=ob[:], rhs=bb[:, bk, hf * 512:(hf + 1) * 512], start=False, stop=True),
                         reads=[r_bb, r_ob], writes=[r_pp])
                a, r_a = a_s.next(); sg, r_sg = sg_s.next(); u1, r_u1 = u_s.next()
                S.op("dve", lambda e, a=a, pA=pA: e.tensor_scalar(out=a[:], in0=pA[:], scalar1=7.0, scalar2=None, op0=ALU.min), reads=[r_pA], writes=[r_a])
                S.op("act", lambda e, sg=sg, a=a: e.activation(out=sg[:], in_=a[:], func=AF.Sigmoid, scale=1.702), reads=[r_a], writes=[r_sg])
                S.op("dve", lambda e, u1=u1, pU=pU: e.tensor_scalar(out=u1[:], in0=pU[:], scalar1=7.0, scalar2=-7.0, op0=ALU.min, op1=ALU.max), reads=[r_pU], writes=[r_u1])
                S.op("dve", lambda e, sg=sg, a=a: e.tensor_tensor(out=sg[:], in0=sg[:], in1=a[:], op=ALU.mult), reads=[r_a, r_sg], writes=[r_sg])
                S.op("dve", lambda e, atm=atm, sg=sg, u1=u1, hf=hf: e.scalar_tensor_tensor(out=atm[:, hf * 512:(hf + 1) * 512], in0=u1[:], scalar=1.0, in1=sg[:], op0=ALU.add, op1=ALU.mult),
                     reads=[r_sg, r_u1], writes=[r_atm])
            return (atm, r_atm, rb, r_rb, ob, r_ob)

        def compute_y(b, gu):
            atm, r_atm, rb, r_rb, ob, r_ob = gu
            pt2, r_pt2 = ptr.next()
            for j in range(8):
                S.op("pe", lambda e, pt2=pt2, atm=atm, j=j: e.transpose(out=pt2[:, j, :], in_=atm[:, j:D:8], identity=identb[:]), reads=[r_atm, r_k], writes=[r_pt2])
            actT, r_act = acts.next()
            S.op("act", lambda e, actT=actT, pt2=pt2: e.copy(out=actT[:], in_=pt2[:]), reads=[r_pt2], writes=[r_act])
            yg, r_yg = ygs.next()
            for half in range(2):
                pY, r_pY = pYs.next()
                for f in range(8):
                    S.op("pe", lambda e, pY=pY, actT=actT, f=f, half=half: e.matmul(pY[:], lhsT=actT[:, f, :], rhs=wd[:, f, half * 512:(half + 1) * 512], start=(f == 0), stop=False),
                         reads=[r_act, r_wd], writes=[r_pY])
                S.op("pe", lambda e, pY=pY, ob=ob, half=half: e.matmul(pY[:], lhsT=ob[:], rhs=bb[:, 2, half * 512:(half + 1) * 512], start=False, stop=True), reads=[r_ob, r_bb], writes=[r_pY])
                S.op("act", lambda e, yg=yg, pY=pY, rb=rb, half=half: e.activation(out=yg[:, half * 512:(half + 1) * 512], in_=pY[:], func=AF.Copy, scale=rb[:, 1:2]), reads=[r_pY, r_rb], writes=[r_yg])
            return (yg, r_yg, rb, r_rb)

        def stage_out(res):
            yg, r_yg, rb, r_rb = res
            S.dma("pool", lambda e, yg=yg, rb=rb: e.indirect_dma_start(out=yacc, out_offset=bass.IndirectOffsetOnAxis(ap=rb[:, 0:1].bitcast(I32), axis=0), in_=yg[:], in_offset=None, compute_op=ALU.add),
                  reads=[r_yg, r_rb], writes=[r_yacc], stream="igs")

        sts = {0: stage_in(0)}
        stage_w(sts[0], (0, 1, 2))
        gus = {0: compute_gu(0, sts[0])}
        if NB > 1:
            sts[1] = stage_in(1)
            stage_w(sts[1], (0, 1))
        for b in range(NB):
            if b + 1 < NB:
                gus[b + 1] = compute_gu(b + 1, sts[b + 1])
            if b + 2 < NB:
                sts[b + 2] = stage_in(b + 2)
                stage_w(sts[b + 2], (0, 1))
            res = compute_y(b, gus[b])
            if b + 1 < NB:
                stage_w(sts[b + 1], (2,))
            stage_out(res)
            sts.pop(b, None); gus.pop(b, None)
        xts = Rot([(sb(f"p5_xt{i}", [128, D], F32), Res()) for i in range(2)])
        yts = Rot([(sb(f"p5_yt{i}", [128, D], F32), Res()) for i in range(2)])
        junk = sb("p5_junk", [128, D], BF16); r_junk = Res()
        sts = Rot([(sb(f"p5_st{i}", [128, 4], F32), Res()) for i in range(2)])
        for t in range(nt_act):
            r = 0 if t < NTL else 1
            xt, r_xt = xts.next(); yt, r_yt = yts.next()
            S.dma("sp", lambda e, xt=xt, t=t: e.dma_start(out=xt[:], in_=xres[t * 128:(t + 1) * 128, :]), writes=[r_xt], stream="ld")
            S.dma("act", lambda e, yt=yt, t=t: e.dma_start(out=yt[:], in_=yacc[t * 128:(t + 1) * 128, :]), reads=[r_yacc], writes=[r_yt], stream="ld2")
            S.op("dve", lambda e, yt=yt, r=r: e.tensor_tensor(out=yt[:], in0=yt[:], in1=GT2[r][0][:], op=ALU.mult), reads=[r_yt, GT2[r][1]], writes=[r_yt])
            S.op("pool", lambda e, yt=yt, xt=xt: e.tensor_tensor(out=yt[:], in0=yt[:], in1=xt[:], op=ALU.add), reads=[r_yt, r_xt], writes=[r_yt])
            if not last:
                S.dma("sp", lambda e, yt=yt, t=t: e.dma_start(out=xres[t * 128:(t + 1) * 128, :], in_=yt[:]), reads=[r_yt], stream="scr")
            else:
                st_, r_st = sts.next()
                rstd_ops(S, yt, r_yt, junk, r_junk, st_, r_st)
                S.op("dve", lambda e, yt=yt, st_=st_: e.scalar_tensor_tensor(out=yt[:], in0=yt[:], scalar=st_[:, 3:4], in1=gfb[:], op0=ALU.mult, op1=ALU.mult), reads=[r_yt, r_st, r_gf], writes=[r_yt])
                S.dma("sp", lambda e, yt=yt, t=t: e.dma_start(out=out[t * 128:(t + 1) * 128, :], in_=yt[:]), reads=[r_yt], stream="st")


_CACHE = {}


def make_in_maps(inputs):
    consts = host_consts()
    shared = {}
    for nm, shp in W_SPECS:
        a = np.ascontiguousarray(np.asarray(inputs[nm], dtype=np.float32)).reshape(shp)
        shared[nm] = a
    shared.update(consts)
    x = np.asarray(inputs["x"], dtype=np.float32)
    ctx = np.asarray(inputs["ctx"], dtype=np.float32)
    c = np.asarray(inputs["c"], dtype=np.float32)
    c_ctx = np.asarray(inputs["c_ctx"], dtype=np.float32)
    maps = []
    for b in range(8):
        m = dict(shared)
        m["xin"] = np.ascontiguousarray(np.concatenate([x[b], ctx[b]], axis=0))
        m["cc"] = np.ascontiguousarray(np.stack([c[b], c_ctx], axis=0))
        maps.append(m)
    return maps


def kernel(**inputs):
    if "nc" not in _CACHE:
        _CACHE["nc"] = build()[0]
    nc = _CACHE["nc"]
    maps = make_in_maps(inputs)
    res = run_bass_kernel_spmd(nc, maps, core_ids=list(range(8)))
    return np.stack([np.asarray(r["out"], dtype=np.float32) for r in res.results], axis=0)
```
